# Optimizing a Trainium2 kernel written in Bass

```python
import math
import jax, jax.numpy as jnp
from jax import lax
import numpy as np

D_MODEL = 1024
BATCH = 8
SEQ = 4096
DEPTH = 4

GRID_W = 64
CTX_LEN = 256

BRANCH_W = D_MODEL // 2
N_BRANCHES = 3
GLA_HEADS = 4
GLA_DV = BRANCH_W // GLA_HEADS
GLA_DK = GLA_DV // 2
GLA_RANK = 16
GLA_TAU = 16.0
GLA_CHUNK = 64
DIFF_HEADS = 4
DIFF_DV = BRANCH_W // DIFF_HEADS
DIFF_DH = DIFF_DV // 2
DIFF_QBLOCK = 128
NA_HEADS = 8
NA_DH = BRANCH_W // NA_HEADS
NA_WIN_H = 8
NA_WIN_W = 16
NA_QCOLS = 16
NA_KCOLS = NA_QCOLS + NA_WIN_W
N_EXPERTS = 16
N_GROUPS = 4
EXPERTS_PER_GROUP = N_EXPERTS // N_GROUPS
TOP_K = 2
D_EXPERT = D_MODEL // 2
ROPE_BASE = 10000.0
LN_EPS = 1e-5
RMS_EPS = 1e-6
NEG_BIG = -1e30
DEEPNORM_ALPHA = (2 * DEPTH) ** 0.25
DEEPNORM_BETA = (8 * DEPTH) ** -0.25
IN_SPLITS = (
    GLA_HEADS * GLA_DK, GLA_HEADS * GLA_DK, GLA_HEADS * GLA_DV, GLA_HEADS * GLA_DV, 2 * GLA_RANK,
    DIFF_HEADS * 2 * DIFF_DH, DIFF_HEADS * 2 * DIFF_DH, DIFF_HEADS * DIFF_DV,
    NA_HEADS * NA_DH, NA_HEADS * NA_DH, NA_HEADS * NA_DH,
    N_BRANCHES * D_MODEL,
)
D_IN = sum(IN_SPLITS)

kernel_name = "hybrid_gla_diffattn_natten_groupmoe_dit"


def _layernorm(x, g, b):
    xf = x.astype(jnp.float32)
    mu = jnp.mean(xf, -1, keepdims=True)
    var = jnp.mean(jnp.square(xf - mu), -1, keepdims=True)
    return ((xf - mu) * lax.rsqrt(var + LN_EPS) * g + b).astype(x.dtype)


def _rms(x, g):
    xf = x.astype(jnp.float32)
    return (xf * lax.rsqrt(jnp.mean(xf * xf, -1, keepdims=True) + RMS_EPS) * g).astype(x.dtype)


def _heads(t, h):
    b, l, _ = t.shape
    return t.reshape(b, l, h, -1).transpose(0, 2, 1, 3)


def _merge_heads(o):
    b, h, l, d = o.shape
    return o.transpose(0, 2, 1, 3).reshape(b, l, h * d)


def _axial_rope(n_tok, dim):
    t = jnp.arange(n_tok)
    row = (t // GRID_W).astype(jnp.float32)
    col = (t % GRID_W).astype(jnp.float32)
    n_freq = dim // 4
    inv = ROPE_BASE ** (-jnp.arange(n_freq, dtype=jnp.float32) / n_freq)
    ang = jnp.concatenate([row[:, None] * inv, col[:, None] * inv], -1)
    return jnp.cos(ang), jnp.sin(ang)


def _rope(x, cos, sin):
    x1, x2 = jnp.split(x, 2, axis=-1)
    c = cos.astype(x.dtype)
    s = sin.astype(x.dtype)
    return jnp.concatenate([x1 * c - x2 * s, x1 * s + x2 * c], -1)


def _gla_chunk_scan(q, k, v, logd, s0):
    b, h, l, dk = q.shape
    dv = v.shape[-1]
    n = l // GLA_CHUNK

    def chunks(t):
        return t.reshape(b, h, n, GLA_CHUNK, t.shape[-1]).transpose(2, 0, 1, 3, 4)

    qc, kc, vc, gc = chunks(q), chunks(k), chunks(v), chunks(logd)
    cum = jnp.cumsum(gc, axis=-2)
    last = cum[..., -1:, :]
    q_in = qc * jnp.exp(cum)
    k_in = kc * jnp.exp(-cum)
    k_st = kc * jnp.exp(last - cum)
    causal_in_chunk = jnp.tril(jnp.ones((GLA_CHUNK, GLA_CHUNK), dtype=bool))
    att = jnp.where(causal_in_chunk, jnp.einsum('nbhid,nbhjd->nbhij', q_in, k_in), 0.0)
    o_intra = jnp.einsum('nbhij,nbhjv->nbhiv', att, vc)
    kv = jnp.einsum('nbhjd,nbhjv->nbhdv', k_st, vc)
    decay = jnp.exp(last[..., 0, :])

    def step(s, inp):
        dec, kv_n = inp
        return dec[..., None] * s + kv_n, s

    s_final, s_start = lax.scan(step, s0, (decay, kv))
    o_inter = jnp.einsum('nbhid,nbhdv->nbhiv', q_in, s_start)
    o = (o_intra + o_inter).transpose(1, 2, 0, 3, 4).reshape(b, h, l, dv)
    return o, s_final


def _gla_mixer(parts_l, parts_c, w_decay, b_decay, norm_g, need_ctx):
    f32 = jnp.float32

    def prep(parts):
        q, k, v, _, r = parts
        b, l, _ = q.shape
        qh = _heads(q.astype(f32), GLA_HEADS) * (GLA_DK ** -0.5)
        kh = _heads(k.astype(f32), GLA_HEADS)
        vh = _heads(v.astype(f32), GLA_HEADS)
        r2 = r.astype(f32).reshape(b, l, 2, GLA_RANK)
        z = jnp.einsum('bldr,drk->dblk', r2, w_decay.astype(f32)) + b_decay.astype(f32)[:, None, None, :]
        logd = jax.nn.log_sigmoid(z) / GLA_TAU
        return qh, kh, vh, _heads(logd[0], GLA_HEADS), _heads(logd[1], GLA_HEADS)

    qL, kL, vL, dLf, dLb = prep(parts_l)
    qC, kC, vC, dCf, dCb = prep(parts_c)
    b = qL.shape[0]
    s_zero = jnp.zeros((b, GLA_HEADS, GLA_DK, GLA_DV), f32)
    flip = lambda t: t[:, :, ::-1]
    oCf, sCf = _gla_chunk_scan(qC, kC, vC, dCf, s_zero)
    oLf, _ = _gla_chunk_scan(qL, kL, vL, dLf, sCf)
    oCb, sCb = _gla_chunk_scan(flip(qC), flip(kC), flip(vC), flip(dCb), s_zero)
    oLb, _ = _gla_chunk_scan(flip(qL), flip(kL), flip(vL), flip(dLb), sCb)

    def finish(o, g):
        o = _merge_heads(_rms(o, norm_g.astype(f32)))
        return (o * jax.nn.silu(g.astype(f32))).astype(g.dtype)

    out_l = finish(oLf + flip(oLb), parts_l[3])
    out_c = finish(oCf + flip(oCb), parts_c[3]) if need_ctx else None
    return out_l, out_c


def _diff_mixer(qL, kL, vL, qC, kC, vC, lam_q, lam_k, norm_g, lam_init, need_ctx):
    b, l, _ = qL.shape
    scale = DIFF_DH ** -0.5

    def qk_heads(t):
        return t.reshape(b, t.shape[1], DIFF_HEADS, 2, DIFF_DH).transpose(0, 2, 3, 1, 4)

    cos, sin = _axial_rope(l, DIFF_DH)
    qLh = _rope(qk_heads(qL), cos, sin) * scale
    kLh = _rope(qk_heads(kL), cos, sin)
    qCh = qk_heads(qC) * scale
    kCh = qk_heads(kC)
    vLh = _heads(vL, DIFF_HEADS)
    vCh = _heads(vC, DIFF_HEADS)
    lq = lam_q.astype(jnp.float32)
    lk = lam_k.astype(jnp.float32)
    lam = jnp.exp(jnp.sum(lq[0] * lk[0])) - jnp.exp(jnp.sum(lq[1] * lk[1])) + lam_init

    def attend(q, k, v):
        s = jnp.einsum('bhcqd,bhckd->bhcqk', q, k).astype(jnp.float32)
        p = jax.nn.softmax(s, axis=-1)
        w = (p[:, :, 0] - lam * p[:, :, 1]).astype(v.dtype)
        return jnp.einsum('bhqk,bhkv->bhqv', w, v)

    k_all = jnp.concatenate([kCh, kLh], axis=3)
    v_all = jnp.concatenate([vCh, vLh], axis=2)
    nb = l // DIFF_QBLOCK
    q_blocks = qLh.reshape(b, DIFF_HEADS, 2, nb, DIFF_QBLOCK, DIFF_DH).transpose(3, 0, 1, 2, 4, 5)
    oL = lax.map(lambda qb: attend(qb, k_all, v_all), q_blocks)
    oL = oL.transpose(1, 2, 0, 3, 4).reshape(b, DIFF_HEADS, l, DIFF_DV)

    def finish(o):
        return _merge_heads(_rms(o, norm_g) * (1.0 - lam_init))

    out_c = finish(attend(qCh, kCh, vCh)) if need_ctx else None
    return finish(oL), out_c


def _na_mixer(qL, kL, vL, qC, kC, vC, rpb, need_ctx):
    b, l, _ = qL.shape
    rows = l // GRID_W
    wh = min(NA_WIN_H, rows)
    ncb = GRID_W // NA_QCOLS
    n_loc = wh * NA_KCOLS
    q = _heads(qL, NA_HEADS) * (NA_DH ** -0.5)
    k_grid = _heads(kL, NA_HEADS).reshape(b, NA_HEADS, rows, GRID_W, NA_DH)
    v_grid = _heads(vL, NA_HEADS).reshape(b, NA_HEADS, rows, GRID_W, NA_DH)
    kc = _heads(kC, NA_HEADS)
    vc = _heads(vC, NA_HEADS)
    qcol = np.arange(GRID_W).reshape(ncb, NA_QCOLS)
    kstart = np.clip(qcol[:, 0] - NA_WIN_W // 2, 0, GRID_W - NA_KCOLS)
    kcol = kstart[:, None] + np.arange(NA_KCOLS)
    cstart = np.clip(qcol - NA_WIN_W // 2, 0, GRID_W - NA_WIN_W)
    kc_b = kcol[:, None, :]
    col_in = (kc_b >= cstart[..., None]) & (kc_b < cstart[..., None] + NA_WIN_W)
    col_idx = np.clip(kc_b - qcol[:, :, None] + NA_WIN_W - 1, 0, 2 * NA_WIN_W - 2)
    loc_mask = np.broadcast_to(col_in[:, :, None, :], (ncb, NA_QCOLS, wh, NA_KCOLS)).reshape(
        ncb, NA_QCOLS, n_loc)
    q_rows = q.reshape(b, NA_HEADS, rows, GRID_W, NA_DH).transpose(2, 0, 1, 3, 4)

    def row_block(inp):
        r, q_r = inp
        rs = jnp.clip(r - NA_WIN_H // 2, 0, rows - wh)
        k_rows = lax.dynamic_slice_in_dim(k_grid, rs, wh, axis=2)
        v_rows = lax.dynamic_slice_in_dim(v_grid, rs, wh, axis=2)
        k_blk = k_rows[:, :, :, kcol].transpose(0, 1, 3, 2, 4, 5).reshape(b, NA_HEADS, ncb, n_loc, NA_DH)
        v_blk = v_rows[:, :, :, kcol].transpose(0, 1, 3, 2, 4, 5).reshape(b, NA_HEADS, ncb, n_loc, NA_DH)
        q_blk = q_r.reshape(b, NA_HEADS, ncb, NA_QCOLS, NA_DH)
        row_idx = rs + jnp.arange(wh) - r + (NA_WIN_H - 1)
        bias = rpb[:, row_idx][:, :, col_idx]
        bias = bias.transpose(0, 2, 3, 1, 4).reshape(NA_HEADS, ncb, NA_QCOLS, n_loc).astype(jnp.float32)
        s_loc = jnp.einsum('bhnqd,bhnkd->bhnqk', q_blk, k_blk).astype(jnp.float32) + bias
        s_loc = jnp.where(loc_mask, s_loc, NEG_BIG)
        s_ctx = jnp.einsum('bhnqd,bhkd->bhnqk', q_blk, kc).astype(jnp.float32)
        p = jax.nn.softmax(jnp.concatenate([s_loc, s_ctx], -1), axis=-1).astype(v_blk.dtype)
        o = (jnp.einsum('bhnqk,bhnkd->bhnqd', p[..., :n_loc], v_blk)
             + jnp.einsum('bhnqk,bhkd->bhnqd', p[..., n_loc:], vc))
        return o.reshape(b, NA_HEADS, GRID_W, NA_DH)

    o = lax.map(row_block, (jnp.arange(rows, dtype=jnp.int32), q_rows))
    out_l = _merge_heads(o.transpose(1, 2, 0, 3, 4).reshape(b, NA_HEADS, l, NA_DH))
    out_c = None
    if need_ctx:
        qch = _heads(qC, NA_HEADS) * (NA_DH ** -0.5)
        p = jax.nn.softmax(jnp.einsum('bhqd,bhkd->bhqk', qch, kc).astype(jnp.float32), axis=-1)
        out_c = _merge_heads(jnp.einsum('bhqk,bhkd->bhqd', p.astype(vc.dtype), vc))
    return out_l, out_c


def _merge(branches, gate_logits, w_branch, w_o):
    b, l = gate_logits.shape[:2]
    proj = jnp.einsum('blne,ned->blnd', branches, w_branch)
    gates = jax.nn.sigmoid(gate_logits.reshape(b, l, N_BRANCHES, -1))
    return jnp.sum(gates * proj, axis=2) @ w_o


def _mixer_sublayer(hL, hC, w_in, w_decay, b_decay, gla_g, lam_q, lam_k, diff_g, lam_init, rpb,
                    w_branch, w_o, need_ctx):
    splits = [int(i) for i in np.cumsum(IN_SPLITS)[:-1]]
    pL = jnp.split(hL @ w_in, splits, axis=-1)
    pC = jnp.split(hC @ w_in, splits, axis=-1)
    gla_l, gla_c = _gla_mixer(pL[0:5], pC[0:5], w_decay, b_decay, gla_g, need_ctx)
    diff_l, diff_c = _diff_mixer(pL[5], pL[6], pL[7], pC[5], pC[6], pC[7], lam_q, lam_k, diff_g,
                                 lam_init, need_ctx)
    na_l, na_c = _na_mixer(pL[8], pL[9], pL[10], pC[8], pC[9], pC[10], rpb, need_ctx)
    y_l = _merge(jnp.stack([gla_l, diff_l, na_l], axis=2), pL[11], w_branch, w_o)
    y_c = _merge(jnp.stack([gla_c, diff_c, na_c], axis=2), pC[11], w_branch, w_o) if need_ctx else None
    return y_l, y_c


def _moe(h, w_router, b_router, w_gate, w_up, w_down):
    b, t, _ = h.shape
    aff = jax.nn.sigmoid(jnp.einsum('btd,de->bte', h, w_router).astype(jnp.float32))
    sel = aff + b_router.astype(jnp.float32)
    group_score = jnp.sum(lax.top_k(sel.reshape(b, t, N_GROUPS, EXPERTS_PER_GROUP), TOP_K)[0], -1)
    best_group = jnp.argmax(group_score, axis=-1)
    in_group = (jnp.arange(N_EXPERTS) // EXPERTS_PER_GROUP) == best_group[..., None]
    _, top_idx = lax.top_k(jnp.where(in_group, sel, -jnp.inf), TOP_K)
    top_aff = jnp.take_along_axis(aff, top_idx, axis=-1)
    top_w = top_aff / jnp.sum(top_aff, -1, keepdims=True)
    combine = jnp.sum(jax.nn.one_hot(top_idx, N_EXPERTS, dtype=jnp.float32) * top_w[..., None], -2)
    combine = combine.astype(h.dtype)
    out = jnp.zeros_like(h)
    for e in range(N_EXPERTS):
        a = jax.nn.silu(h @ w_gate[e]) * (h @ w_up[e])
        out = out + combine[..., e:e + 1] * (a @ w_down[e])
    return out


def setup_inputs(seed: int = 0) -> dict:
    key = jax.random.key(seed)
    ks = jax.random.split(key, 24)
    f32 = jnp.float32
    d = D_MODEL

    def nrm(k, shape, s):
        return jax.random.normal(k, shape, f32) * s

    return {
        "x": nrm(ks[0], (BATCH, SEQ, d), 1.0),
        "c": nrm(ks[1], (BATCH, d), 1.0),
        "ctx": nrm(ks[2], (BATCH, CTX_LEN, d), 1.0),
        "c_ctx": nrm(ks[3], (d,), 1.0),
        "w_ada": nrm(ks[4], (DEPTH, d, 6 * d), 0.5 * d ** -0.5),
        "b_ada": nrm(ks[5], (DEPTH, 6 * d), 0.02),
        "w_in": nrm(ks[6], (DEPTH, d, D_IN), d ** -0.5),
        "gla_w_decay": nrm(ks[7], (DEPTH, 2, GLA_RANK, GLA_HEADS * GLA_DK), GLA_RANK ** -0.5),
        "gla_b_decay": nrm(ks[8], (DEPTH, 2, GLA_HEADS * GLA_DK), 0.1),
        "gla_norm_g": 1.0 + nrm(ks[9], (DEPTH, GLA_DV), 0.02),
        "diff_lam_q": nrm(ks[10], (DEPTH, 2, DIFF_DH), 0.1),
        "diff_lam_k": nrm(ks[11], (DEPTH, 2, DIFF_DH), 0.1),
        "diff_norm_g": 1.0 + nrm(ks[12], (DEPTH, DIFF_DV), 0.02),
        "na_rpb": nrm(ks[13], (DEPTH, NA_HEADS, 2 * NA_WIN_H - 1, 2 * NA_WIN_W - 1), 0.1),
        "w_branch": nrm(ks[14], (DEPTH, N_BRANCHES, BRANCH_W, d), BRANCH_W ** -0.5),
        "w_o": nrm(ks[15], (DEPTH, d, d), DEEPNORM_BETA * d ** -0.5),
        "ln_g": 1.0 + nrm(ks[16], (DEPTH, 2, d), 0.02),
        "ln_b": nrm(ks[17], (DEPTH, 2, d), 0.02),
        "w_router": nrm(ks[18], (d, N_EXPERTS), d ** -0.5),
        "b_router": nrm(ks[19], (N_EXPERTS,), 0.01),
        "w_exp_gate": nrm(ks[20], (DEPTH, N_EXPERTS, d, D_EXPERT), d ** -0.5),
        "w_exp_up": nrm(ks[21], (DEPTH, N_EXPERTS, d, D_EXPERT), d ** -0.5),
        "w_exp_down": nrm(ks[22], (DEPTH, N_EXPERTS, D_EXPERT, d), DEEPNORM_BETA * D_EXPERT ** -0.5),
    }


def reference(x, c, ctx, c_ctx, w_ada, b_ada, w_in, gla_w_decay, gla_b_decay, gla_norm_g,
              diff_lam_q, diff_lam_k, diff_norm_g, na_rpb, w_branch, w_o, ln_g, ln_b,
              w_router, b_router, w_exp_gate, w_exp_up, w_exp_down):
    b, l, d = x.shape
    lc = ctx.shape[1]
    xL, xC = x, ctx
    for layer in range(DEPTH):
        need_ctx = layer < DEPTH - 1
        lam_init = 0.8 - 0.6 * math.exp(-0.3 * layer)
        mL = (jax.nn.silu(c) @ w_ada[layer] + b_ada[layer]).reshape(b, 6, 1, d)
        mC = (jax.nn.silu(c_ctx) @ w_ada[layer] + b_ada[layer]).reshape(6, d)
        hL = xL * (1.0 + mL[:, 1]) + mL[:, 0]
        hC = xC * (1.0 + mC[1]) + mC[0]
        yL, yC = _mixer_sublayer(hL, hC, w_in[layer], gla_w_decay[layer], gla_b_decay[layer],
                                 gla_norm_g[layer], diff_lam_q[layer], diff_lam_k[layer],
                                 diff_norm_g[layer], lam_init, na_rpb[layer], w_branch[layer],
                                 w_o[layer], need_ctx)
        xL = _layernorm(DEEPNORM_ALPHA * xL + mL[:, 2] * yL, ln_g[layer, 0], ln_b[layer, 0])
        hL = xL * (1.0 + mL[:, 4]) + mL[:, 3]
        if need_ctx:
            xC = _layernorm(DEEPNORM_ALPHA * xC + mC[2] * yC, ln_g[layer, 0], ln_b[layer, 0])
            hC = xC * (1.0 + mC[4]) + mC[3]
            y = _moe(jnp.concatenate([hC, hL], axis=1), w_router, b_router,
                     w_exp_gate[layer], w_exp_up[layer], w_exp_down[layer])
            yC, yL = y[:, :lc], y[:, lc:]
            xC = _layernorm(DEEPNORM_ALPHA * xC + mC[5] * yC, ln_g[layer, 1], ln_b[layer, 1])
        else:
            yL = _moe(hL, w_router, b_router, w_exp_gate[layer], w_exp_up[layer], w_exp_down[layer])
        xL = _layernorm(DEEPNORM_ALPHA * xL + mL[:, 5] * yL, ln_g[layer, 1], ln_b[layer, 1])
    return xL
```

```python
import contextlib
import math
import numpy as np
import ml_dtypes
import concourse.bass as bass
import concourse.mybir as mybir
from concourse.bass_utils import run_bass_kernel_spmd

F32 = mybir.dt.float32
BF16 = mybir.dt.bfloat16
AF = mybir.ActivationFunctionType
ALU = mybir.AluOpType
AX = mybir.AxisListType

D = 1024
LC = 256
LL = 4096
T = LC + LL
NT = T // 128
DEPTH = 4
D_IN = 7712
ALPHA = (2 * DEPTH) ** 0.25
LN_EPS = 1e-5
RMS_EPS = 1e-6
O_GQ, O_GK, O_GV, O_GG, O_GR = 0, 256, 512, 1024, 1536
O_DQ, O_DK, O_DV = 1568, 2080, 2592
O_NQ, O_NK, O_NV = 3104, 3616, 4128
O_GT = 4640


class Buf:
    __slots__ = ("name", "t", "last_write", "reads", "excl")

    def __init__(self, name, t=None, excl=False):
        self.name = name
        self.t = t
        self.last_write = None
        self.reads = {}
        self.excl = excl

    def __getitem__(self, key):
        return self.t[key]


class _Op:
    __slots__ = ("fn", "waits", "tok", "signal", "val")

    def __init__(self, fn):
        self.fn = fn
        self.waits = []
        self.tok = None
        self.signal = False
        self.val = None


class _Rec:
    call = None

    def __getattr__(self, name):
        def f(*a, **k):
            self.call = (name, a, k)
            return self
        return f


class _Eng:
    def __init__(self, name):
        self.name = name
        self.ops = []
        self.seen = {}
        self.is_pe = name == "pe"
        self.sem = None
        self.dsems = []
        self.dcount = []
        self.drr = 0
        self.emitted = 0
        self.sigcount = 0


class FW:
    ENGS = ("pe", "act", "dve", "pool", "sp")

    def __init__(self, nc, st, n_dma_sems=16):
        self.nc = nc
        self.e = {n: _Eng(n) for n in self.ENGS}
        self.n_dma_sems = n_dma_sems
        for en in self.ENGS:
            E = self.e[en]
            E.sem = st.enter_context(nc.semaphore("s_" + en))
            if en in ("sp", "pool", "act"):
                E.dcount = [0] * n_dma_sems
                E.dsems = [st.enter_context(nc.semaphore("d_%s_%d" % (en, i))) for i in range(n_dma_sems)]

    def _deps(self, E, reads, writes):
        deps = {}

        def add(tok):
            if tok is None:
                return
            if tok[0] == "c":
                if E.is_pe and tok[1] == "pe":
                    return
                key = ("c", tok[1])
            else:
                key = ("d", tok[1])
            if key not in deps or deps[key][2] < tok[2]:
                deps[key] = tok

        for b in reads:
            add(b.last_write)
            if b.excl:
                for t in b.reads.values():
                    add(t)
        for b in writes:
            add(b.last_write)
            for t in b.reads.values():
                add(t)
        out = []
        for key, tok in deps.items():
            if E.seen.get(key, -1) >= tok[2]:
                continue
            E.seen[key] = tok[2]
            out.append(tok)
        return out

    def _commit(self, tok, reads, writes):
        for b in writes:
            b.last_write = tok
            b.reads = {}
        for b in reads:
            if b.excl:
                b.last_write = tok
                b.reads = {}
            else:
                b.reads[(tok[0], tok[1])] = tok

    def op(self, eng, fn, R=(), W=()):
        E = self.e[eng]
        rec = _Rec()
        fn(rec)
        call = rec.call
        o = _Op(lambda e, c=call: getattr(e, c[0])(*c[1], **c[2]))
        R = [b for b in R if b is not None]
        W = [b for b in W if b is not None]
        o.waits = self._deps(E, R, W)
        o.tok = ("c", eng, len(E.ops))
        E.ops.append(o)
        self._commit(o.tok, R, W)
        return o

    def dma(self, eng, out, in_, R=(), W=(), **kw):
        E = self.e[eng]
        o = _Op(lambda e: e.dma_start(out=out, in_=in_, **kw))
        R = [b for b in R if b is not None]
        W = [b for b in W if b is not None]
        o.waits = self._deps(E, R, W)
        i = E.drr
        E.drr = (E.drr + 1) % self.n_dma_sems
        key = ("d", (eng, i))
        prev = E.dcount[i]
        if prev > 0 and E.seen.get(key, -1) < prev:
            E.seen[key] = prev
            o.waits.append(("d", (eng, i), prev))
        E.dcount[i] = prev + 16
        o.tok = ("d", (eng, i), prev + 16)
        E.ops.append(o)
        self._commit(o.tok, R, W)
        return o

    def phase_end(self):
        nc = self.nc
        E = self.e["sp"]
        o = _Op(None)
        for en in self.ENGS:
            Q = self.e[en]
            for i, c in enumerate(Q.dcount):
                if c > 0 and E.seen.get(("d", (en, i)), -1) < c:
                    o.waits.append(("d", (en, i), c))
            if en != "sp":
                for q in reversed(Q.ops[Q.emitted:]):
                    if q.tok[0] == "c" and q.fn is not None:
                        o.waits.append(q.tok)
                        break
        o.tok = ("c", "sp", len(E.ops))
        E.ops.append(o)
        for en in self.ENGS:
            Q = self.e[en]
            for q in Q.ops[Q.emitted:]:
                for w in q.waits:
                    if w[0] == "c":
                        P = self.e[w[1]]
                        assert w[2] >= P.emitted, "wait on op of an earlier phase"
                        P.ops[w[2]].signal = True
        for en in self.ENGS:
            Q = self.e[en]
            for q in Q.ops[Q.emitted:]:
                if q.tok[0] == "c" and q.signal:
                    assert q.fn is not None
                    Q.sigcount += 1
                    q.val = Q.sigcount

        with nc.Block() as block:
            def run(en, eng):
                Q = self.e[en]
                for q in Q.ops[Q.emitted:]:
                    for w in q.waits:
                        if w[0] == "c":
                            P = self.e[w[1]]
                            eng.wait_ge(P.sem, P.ops[w[2]].val)
                        else:
                            qn, i = w[1]
                            eng.wait_ge(self.e[qn].dsems[i], w[2])
                    if q.fn is None:
                        continue
                    ins = q.fn(eng)
                    if q.tok[0] == "d":
                        qn, i = q.tok[1]
                        ins.then_inc(self.e[qn].dsems[i], 16)
                    elif q.signal:
                        ins.then_inc(Q.sem, 1)

            @block.tensor
            def _(eng):
                run("pe", eng)

            @block.scalar
            def _(eng):
                run("act", eng)

            @block.vector
            def _(eng):
                run("dve", eng)

            @block.gpsimd
            def _(eng):
                run("pool", eng)

            @block.sync
            def _(eng):
                run("sp", eng)

        nc.all_engine_barrier()
        nops = {en: len(self.e[en].ops) - self.e[en].emitted for en in self.ENGS}
        for en in self.ENGS:
            Q = self.e[en]
            Q.emitted = len(Q.ops)
        for en in self.ENGS:
            Q = self.e[en]
            for pn in self.ENGS:
                P = self.e[pn]
                Q.seen[("c", pn)] = len(P.ops) - 1
                for i, c in enumerate(P.dcount):
                    Q.seen[("d", (pn, i))] = c
        return nops


def _consts():
    c = {}
    c["ident"] = np.eye(128, dtype=np.float32)
    n = np.arange(LL)
    row = (n // 64).astype(np.float32)
    col = (n % 64).astype(np.float32)
    nf = 16
    inv = (10000.0 ** (-np.arange(nf, dtype=np.float32) / nf)).astype(np.float32)
    ang = np.concatenate([row[:, None] * inv, col[:, None] * inv], -1).astype(np.float32)
    cos = np.cos(ang).astype(np.float32)
    sin = np.sin(ang).astype(np.float32)
    ct = np.ones((128, T), np.float32)
    stb = np.zeros((128, T), np.float32)
    for p in range(128):
        j = p % 64
        f = j % 32
        ct[p, LC:] = cos[:, f]
        stb[p, LC:] = (-sin[:, f]) if j < 32 else sin[:, f]
    c["rope_c"] = ct
    c["rope_s"] = stb
    s = np.arange(128)[:, None]
    t = np.arange(128)[None, :]
    le = (s <= t).astype(np.float32)
    gt = (s > t).astype(np.float32)
    tri = np.stack([le, le.T, gt, gt.T]).astype(np.float32)
    c["tri"] = tri
    c["mask4"] = np.stack([np.tile(le, (1, 4)), np.tile(le.T, (1, 4))]).astype(np.float32)
    J = np.zeros((64, 128), np.float32)
    for m in range(64):
        J[m, 63 - m] = 1.0
        J[m, 64 + 63 - m] = 1.0
    c["j64"] = J
    cc = np.arange(64)
    cs = np.clip(cc - 8, 0, 48)
    W = ((cc[:, None] >= cs[None, :]) & (cc[:, None] < cs[None, :] + 16)).astype(np.float32)
    W2 = np.concatenate([W, W], 0)
    c["nawin"] = np.tile(W2[:, None, :], (1, 8, 1)).reshape(128, 512).astype(np.float32)
    sel = np.zeros((16, 16, 128), np.float32)
    for e in range(16):
        sel[e, e, :] = 1.0
    c["sel"] = sel
    return c


CONST_SHAPES = {
    "ident": [128, 128], "rope_c": [128, T], "rope_s": [128, T], "tri": [4, 128, 128],
    "mask4": [2, 128, 512], "j64": [64, 128], "nawin": [128, 512], "sel": [16, 16, 128],
}

INPUT_SHAPES = {
    "x": [LL, D], "c": [D], "ctx": [LC, D], "c_ctx": [D],
    "w_ada": [DEPTH, D, 6 * D], "b_ada": [DEPTH, 6 * D], "w_in": [DEPTH, D, D_IN],
    "gla_w_decay": [DEPTH, 2, 16, 256], "gla_b_decay": [DEPTH, 2, 256], "gla_norm_g": [DEPTH, 128],
    "diff_lam_q": [DEPTH, 2, 64], "diff_lam_k": [DEPTH, 2, 64], "diff_norm_g": [DEPTH, 128],
    "na_rpb": [DEPTH, 8, 15, 31], "w_branch": [DEPTH, 3, 512, D], "w_o": [DEPTH, D, D],
    "ln_g": [DEPTH, 2, D], "ln_b": [DEPTH, 2, D], "w_router": [D, 16], "b_router": [16],
    "w_exp_gate": [DEPTH, 16, D, 512], "w_exp_up": [DEPTH, 16, D, 512], "w_exp_down": [DEPTH, 16, 512, D],
}


class KB:
    def __init__(self, nlayers=DEPTH, debug=(), stop_after=None):
        self.nlayers = nlayers
        self.debug = set(debug)
        self.stop_after = stop_after
        self.nc = bass.Bass("TRN2", target_bir_lowering=False)
        nc = self.nc
        self.I = {k: nc.dram_tensor(k, v, F32, kind="ExternalInput").ap() for k, v in INPUT_SHAPES.items()}
        self.C = {k: nc.dram_tensor("k_" + k, v, F32, kind="ExternalInput").ap() for k, v in CONST_SHAPES.items()}
        self.out = nc.dram_tensor("out", [LL, D], F32, kind="ExternalOutput").ap()
        self.S = {}
        self.uid = 0

    def scratch(self, name, shape, dt):
        kind = "ExternalOutput" if (name in self.debug or name == "TAB") else "Internal"
        self.S[name] = self.nc.dram_tensor("s_" + name, shape, dt, kind=kind).ap()
        return self.S[name]

    def begin(self):
        self.pst = contextlib.ExitStack()
        self.pst.__enter__()
        self._cc = {}

    def warm(self, funcs):
        return
        fw = self.fw
        w = self.sb("warm", [128, 2048], F32)
        fw.op("dve", lambda e: e.memset(w[:], 1.0), W=[w])
        for f in funcs:
            for _ in range(2):
                fw.op("act", lambda e: e.activation(out=w[:], in_=w[:], func=f), R=[w], W=[w])
            fw.op("dve", lambda e: e.memset(w[:], 1.0), W=[w])

    def const_col(self, val):
        if val not in self._cc:
            t = self.sb("cc", [128, 1], F32)
            self.fw.op("dve", lambda e: e.memset(t[:], float(val)), W=[t])
            self._cc[val] = t
        return self._cc[val]

    def end(self, name=""):
        nops = self.fw.phase_end()
        self.pst.__exit__(None, None, None)
        print("phase", name, nops, flush=True)

    def sb(self, name, shape, dt):
        self.uid += 1
        return Buf(name, self.pst.enter_context(self.nc.sbuf_tensor("%s_%d" % (name, self.uid), shape, dt)))

    def ps(self, name, shape, dt=F32):
        self.uid += 1
        return Buf(name, self.pst.enter_context(self.nc.psum_tensor("%s_%d" % (name, self.uid), shape, dt)),
                   excl=True)

    def pool(self, name, shape, dt, n, psum=False):
        bufs = [(self.ps if psum else self.sb)(name + str(i), shape, dt) for i in range(n)]
        state = {"i": 0}

        def nxt():
            b = bufs[state["i"] % n]
            state["i"] += 1
            return b
        return nxt

    def build(self):
        nc = self.nc
        with contextlib.ExitStack() as gst:
            self.fw = FW(nc, gst)
            self.alloc_scratch()
            self.ph_mod()
            if self.stop_after == "mod":
                return nc
            for l in range(self.nlayers):
                self.ph_proj(l)
                if self.stop_after in ("proj", "ht"):
                    break
                self.ph_gla(l)
                self.ph_glac(l)
                if self.stop_after == "gla":
                    break
                self.ph_diff(l)
                if self.stop_after == "diff":
                    break
                self.ph_na(l)
                if self.stop_after in ("na", "natab"):
                    break
                self.ph_merge(l)
                if self.stop_after == "merge":
                    break
                self.ph_moe(l, last=(l == self.nlayers - 1))
        return nc

    def alloc_scratch(self):
        s = self.scratch
        s("XS", [T, D], F32)
        s("MODF", [DEPTH, 128, 48, 2], F32)
        s("MODR", [DEPTH, 2, 2, D], F32)
        s("GQT", [4, 64, T], BF16)
        s("GKT", [4, 64, T], BF16)
        s("GK", [T, 256], BF16)
        s("GV", [T, 512], BF16)
        s("GG", [512, T], BF16)
        s("GR", [64, T], BF16)
        s("DQT", [4, 128, T], BF16)
        s("DKT", [4, 128, T], BF16)
        s("DV", [T, 512], BF16)
        s("NQT", [4, 128, T], BF16)
        s("NKT", [4, 128, T], BF16)
        s("NV", [T, 512], BF16)
        s("GT", [3, D, T], BF16)
        s("OG", [2, 4, 128, T], F32)
        s("BR", [3, 512, T], BF16)
        s("NAG", [120, 128], F32)

    def ph_mod(self):
        fw, I = self.fw, self.I
        self.begin()
        sc = self.sb("sc", [128, 8, 2], F32)
        ones = self.sb("ones", [1, 128], F32)
        idf, idb = self.load_ident()
        sc0 = self.sb("sc0", [128, 8], F32)
        sc1 = self.sb("sc1", [128, 8], F32)
        self.diag_cols(sc0, sc0[:], I["c"].rearrange("(o n) -> o n", o=1), 8, idf)
        self.diag_cols(sc1, sc1[:], I["c_ctx"].rearrange("(o n) -> o n", o=1), 8, idf)
        fw.op("dve", lambda e: e.tensor_copy(out=sc[:, :, 0], in_=sc0[:]), R=[sc0], W=[sc])
        fw.op("dve", lambda e: e.tensor_copy(out=sc[:, :, 1], in_=sc1[:]), R=[sc1], W=[sc])
        fw.op("act", lambda e: e.activation(out=sc[:], in_=sc[:], func=AF.Silu), R=[sc], W=[sc])
        fw.op("dve", lambda e: e.memset(ones[:], 1.0), W=[ones])
        if "DBG1" in self.debug:
            self.scratch("DBG1", [128, 16], F32)
            self.scratch("DBG2", [1, 6 * D], F32)
            fw.dma("pool", self.S["DBG1"], sc[:].rearrange("p k s -> p (k s)"), R=[sc])
        wpool = self.pool("wada", [128, 8, 512], F32, 2)
        psf = self.pool("psf", [128, 4, 2], F32, 2, psum=True)
        psr = self.pool("psr", [2, 512], F32, 2, psum=True)
        for l in range(DEPTH):
            bfm = self.sb("bfm", [128, 48], F32)
            brow = self.sb("brow", [2, 2, D], F32)
            modf = self.sb("modf", [128, 48, 2], F32)
            modr = self.sb("modr", [2, 2, D], F32)
            fw.op("dve", lambda e: e.memset(modf[:], 0.0), W=[modf])
            self.diag_cols(bfm, bfm[:], I["b_ada"][l:l + 1, :], 48, idf)
            for jj, j in enumerate((2, 5)):
                fw.dma("sp", brow[:, jj, :], I["b_ada"][l:l + 1, j * D:(j + 1) * D].partition_broadcast(2), W=[brow])
            for j in (1, 4):
                fw.op("dve", lambda e, j=j: e.tensor_scalar_add(out=bfm[:, j * 8:(j + 1) * 8], in0=bfm[:, j * 8:(j + 1) * 8], scalar1=1.0), R=[bfm], W=[bfm])
            for jn in range(12):
                j = jn // 2
                w = wpool()
                fw.dma("sp", w[:], I["w_ada"][l, :, jn * 512:(jn + 1) * 512].rearrange("(kc p) n -> p kc n", p=128), W=[w])
                if j in (2, 5):
                    p = psr()
                    for kc in range(8):
                        fw.op("pe", lambda e: e.matmul(p[:], lhsT=sc[:, kc, :], rhs=w[:, kc, :], start=(kc == 0), stop=(kc == 7)),
                              R=[sc, w], W=[p])
                    jj = 0 if j == 2 else 1
                    half = jn % 2
                    fw.op("dve", lambda e: e.tensor_tensor(out=modr[:, jj, half * 512:(half + 1) * 512], in0=p[:], in1=brow[:, jj, half * 512:(half + 1) * 512], op=ALU.add),
                          R=[p, brow], W=[modr])
                else:
                    p = psf()
                    for sub in range(4):
                        for kc in range(8):
                            fw.op("pe", lambda e: e.matmul(p[:, sub, :], lhsT=w[:, kc, sub * 128:(sub + 1) * 128], rhs=sc[:, kc, :], start=(kc == 0), stop=(kc == 7)),
                                  R=[sc, w], W=[p])
                    for s_ in range(2):
                        fw.op("dve", lambda e: e.tensor_tensor(out=modf[:, jn * 4:(jn + 1) * 4, s_], in0=p[:, :, s_], in1=bfm[:, jn * 4:(jn + 1) * 4], op=ALU.add),
                              R=[p, bfm], W=[modf])
            fw.dma("pool", self.S["MODF"][l], modf[:], R=[modf])
            fw.dma("pool", self.S["MODR"][l].rearrange("s j d -> s (j d)"), modr[:].rearrange("s j d -> s (j d)"), R=[modr])
        self.end("mod")

    def diag_cols(self, dstbuf, dst, src_row_ap, nch, idf):
        fw = self.fw
        bc = self.sb("dgbc", [128, nch, 128], F32)
        fw.dma("sp", bc[:].rearrange("p k j -> p (k j)"), src_row_ap.partition_broadcast(128), W=[bc])
        for k in range(nch):
            fw.op("dve", lambda e: e.tensor_tensor(out=bc[:, k, :], in0=bc[:, k, :], in1=idf[:], op=ALU.mult), R=[bc, idf], W=[bc])
        fw.op("dve", lambda e: e.reduce_sum(out=dst, in_=bc[:], axis=AX.X), R=[bc], W=[dstbuf])

    def x_src(self, l, i):
        if l == 0:
            if i < 2:
                return self.I["ctx"][i * 128:(i + 1) * 128, :]
            return self.I["x"][(i - 2) * 128:(i - 1) * 128, :]
        return self.S["XS"][i * 128:(i + 1) * 128, :]

    def load_ident(self):
        fw = self.fw
        idf = self.sb("idf", [128, 128], F32)
        idb = self.sb("idb", [128, 128], BF16)
        fw.dma("sp", idf[:], self.C["ident"][:, :], W=[idf])
        fw.op("dve", lambda e: e.tensor_copy(out=idb[:], in_=idf[:]), R=[idf], W=[idb])
        return idf, idb

    def ph_proj(self, l):
        fw, I, S, C = self.fw, self.I, self.S, self.C
        self.begin()
        idf, idb = self.load_ident()
        modf = self.sb("modf", [128, 48, 2], F32)
        fw.dma("sp", modf[:], S["MODF"][l], W=[modf])
        hT = self.sb("hT", [128, 8, T], BF16)
        xp = self.pool("xt", [128, D], F32, 2)
        xbp = self.pool("xb", [128, D], BF16, 2)
        ptr = self.pool("ptr", [128, 8, 128], BF16, 2, psum=True)
        for i in range(NT):
            xt = xp()
            xb = xbp()
            fw.dma("sp", xt[:], self.x_src(l, i), W=[xt])
            fw.op("dve", lambda e, xt=xt, xb=xb: e.tensor_copy(out=xb[:], in_=xt[:]), R=[xt], W=[xb])
            p = ptr()
            for kc in range(8):
                fw.op("pe", lambda e, p=p, xb=xb, kc=kc: e.transpose(out=p[:, kc, :], in_=xb[:, kc * 128:(kc + 1) * 128], identity=idb[:]),
                      R=[xb, idb], W=[p])
            st = 1 if i < 2 else 0
            for kc in range(8):
                fw.op("act", lambda e, p=p, kc=kc, i=i, st=st: e.activation(
                    out=hT[:, kc, i * 128:(i + 1) * 128], in_=p[:, kc, :], func=AF.Identity,
                    bias=modf[:, kc, st:st + 1], scale=modf[:, 8 + kc, st:st + 1]), R=[p, modf], W=[hT])
        if "HT" in self.debug:
            self.scratch("HT", [128, 8, T], BF16)
            fw.dma("pool", S["HT"], hT[:], R=[hT])
        if self.stop_after == "ht":
            self.end("ht")
            return
        rc = self.sb("rc", [128, T], F32)
        rs = self.sb("rs", [128, T], F32)
        fw.dma("sp", rc[:], C["rope_c"][:, :], W=[rc])
        fw.dma("sp", rs[:], C["rope_s"][:, :], W=[rs])

        wfp = self.pool("wf", [128, 8, 256], F32, 2)
        wbp = self.pool("wb", [128, 8, 256], BF16, 3)
        pmm = self.pool("pmm", [128, 512], F32, 4, psum=True)
        stg = self.pool("stg", [128, T], BF16, 2)
        tmp = self.pool("tmp", [128, 512], F32, 3)
        tstg = self.pool("tstg", [128, 256], BF16, 3)
        win = I["w_in"][l]
        chunks = [(0, 256)] + [(256 + 512 * j, 512) for j in range(8)]
        self._cast_i = 0

        def load_w(pieces, ncols, zero=False):
            wf = wfp()
            wb = wbp()
            if zero:
                fw.op("dve", lambda e, wf=wf: e.memset(wf[:], 0.0), W=[wf])
            for (dc, scol, n) in pieces:
                fw.dma("sp", wf[:, :, dc:dc + n], win[:, scol:scol + n].rearrange("(kc p) n -> p kc n", p=128), W=[wf])
            eng = "dve" if (self._cast_i % 2 == 0) else "act"
            self._cast_i += 1
            if eng == "act":
                fw.op("act", lambda e: e.activation(out=wb[:, :, 0:ncols], in_=wf[:, :, 0:ncols], func=AF.Copy), R=[wf], W=[wb])
            else:
                fw.op("dve", lambda e: e.tensor_copy(out=wb[:, :, 0:ncols], in_=wf[:, :, 0:ncols]), R=[wf], W=[wb])
            return wb

        def fm_mm(wb, m, c0, cn, col0=0):
            p = pmm()
            for kc in range(8):
                fw.op("pe", lambda e, p=p, wb=wb, kc=kc: e.matmul(p[0:m, 0:cn], lhsT=wb[:, kc, col0:col0 + m], rhs=hT[:, kc, c0:c0 + cn], start=(kc == 0), stop=(kc == 7)),
                      R=[wb, hT], W=[p])
            return p

        def fm_group(pieces, m, evac, dsts, zero=False):
            wb = load_w(pieces, m, zero)
            sg = stg()
            for (c0, cn) in chunks:
                p = fm_mm(wb, m, c0, cn)
                evac(p, sg, c0, cn, m)
            for (r0, nr, dst) in dsts:
                fw.dma("pool", dst, sg[r0:r0 + nr, :], R=[sg])

        def ev_copy(p, sg, c0, cn, m):
            fw.op("act", lambda e: e.activation(out=sg[0:m, c0:c0 + cn], in_=p[0:m, 0:cn], func=AF.Copy), R=[p], W=[sg])

        def ev_silu(p, sg, c0, cn, m):
            fw.op("act", lambda e: e.activation(out=sg[0:m, c0:c0 + cn], in_=p[0:m, 0:cn], func=AF.Silu), R=[p], W=[sg])

        def ev_sigm(p, sg, c0, cn, m):
            fw.op("act", lambda e: e.activation(out=sg[0:m, c0:c0 + cn], in_=p[0:m, 0:cn], func=AF.Sigmoid), R=[p], W=[sg])

        for (o0, dst) in ((O_GQ, S["GQT"]), (O_GK, S["GKT"])):
            for g in range(2):
                fm_group([(0, o0 + g * 128, 128)], 128, ev_copy,
                         [(0, 64, dst[2 * g]), (64, 64, dst[2 * g + 1])])
        for g in range(4):
            fm_group([(0, O_GG + g * 128, 128)], 128, ev_silu, [(0, 128, S["GG"][g * 128:(g + 1) * 128, :])])
        fm_group([(0, O_GR, 16), (32, O_GR + 16, 16)], 64, ev_copy, [(0, 64, S["GR"])], zero=True)
        for (o0, dst) in ((O_DQ, S["DQT"]), (O_DK, S["DKT"])):
            for h in range(4):
                b0 = o0 + h * 128
                wa = load_w([(0, b0, 128)], 128)
                wsw = load_w([(0, b0 + 32, 32), (32, b0, 32), (64, b0 + 96, 32), (96, b0 + 64, 32)], 128)
                sg = stg()
                for (c0, cn) in chunks:
                    pa = fm_mm(wa, 128, c0, cn)
                    pb = fm_mm(wsw, 128, c0, cn)
                    t1 = tmp()
                    t2 = tmp()
                    fw.op("dve", lambda e, pa=pa, t1=t1, c0=c0, cn=cn: e.tensor_tensor(out=t1[:, 0:cn], in0=pa[:, 0:cn], in1=rc[:, c0:c0 + cn], op=ALU.mult), R=[pa, rc], W=[t1])
                    fw.op("dve", lambda e, pb=pb, t2=t2, c0=c0, cn=cn: e.tensor_tensor(out=t2[:, 0:cn], in0=pb[:, 0:cn], in1=rs[:, c0:c0 + cn], op=ALU.mult), R=[pb, rs], W=[t2])
                    fw.op("dve", lambda e, t1=t1, t2=t2, sg=sg, c0=c0, cn=cn: e.tensor_tensor(out=sg[:, c0:c0 + cn], in0=t1[:, 0:cn], in1=t2[:, 0:cn], op=ALU.add), R=[t1, t2], W=[sg])
                fw.dma("pool", dst[h], sg[:, :], R=[sg])
        for (o0, dst) in ((O_NQ, S["NQT"]), (O_NK, S["NKT"])):
            for g in range(4):
                fm_group([(0, o0 + g * 128, 128)], 128, ev_copy, [(0, 128, dst[g])])
        for n in range(3):
            for g in range(8):
                fm_group([(0, O_GT + n * D + g * 128, 128)], 128, ev_sigm, [(0, 128, S["GT"][n, g * 128:(g + 1) * 128, :])])
        for (o0, ncols, dst) in ((O_GK, 256, S["GK"]), (O_GV, 512, S["GV"]), (O_DV, 512, S["DV"]), (O_NV, 512, S["NV"])):
            for cg in range(ncols // 256):
                wb = load_w([(0, o0 + cg * 256, 256)], 256)
                for i in range(NT):
                    p = pmm()
                    for kc in range(8):
                        fw.op("pe", lambda e, p=p, wb=wb, kc=kc, i=i: e.matmul(p[:, 0:256], lhsT=hT[:, kc, i * 128:(i + 1) * 128], rhs=wb[:, kc, :], start=(kc == 0), stop=(kc == 7)),
                              R=[wb, hT], W=[p])
                    ts = tstg()
                    eng = "act" if i % 2 == 0 else "dve"
                    if eng == "act":
                        fw.op("act", lambda e, p=p, ts=ts: e.activation(out=ts[:], in_=p[:, 0:256], func=AF.Copy), R=[p], W=[ts])
                    else:
                        fw.op("dve", lambda e, p=p, ts=ts: e.tensor_copy(out=ts[:], in_=p[:, 0:256]), R=[p], W=[ts])
                    fw.dma("pool", dst[i * 128:(i + 1) * 128, cg * 256:(cg + 1) * 256], ts[:], R=[ts])
        self.end("proj%d" % l)


def build_program(nlayers=DEPTH, debug=(), stop_after=None):
    kb = KB(nlayers, debug, stop_after)
    kb.build()
    return kb


_CONSTS = None


def make_in_maps(inputs):
    global _CONSTS
    if _CONSTS is None:
        _CONSTS = _consts()
    f = lambda a: np.ascontiguousarray(np.asarray(a, dtype=np.float32))
    shared = {k: f(inputs[k]) for k in INPUT_SHAPES if k not in ("x", "c", "ctx")}
    for k, v in _CONSTS.items():
        shared["k_" + k] = v
    maps = []
    for b in range(8):
        m = dict(shared)
        m["x"] = f(inputs["x"][b])
        m["c"] = f(inputs["c"][b])
        m["ctx"] = f(inputs["ctx"][b])
        maps.append(m)
    return maps


def kernel(**inputs):
    kb = build_program()
    maps = make_in_maps(inputs)
    res = run_bass_kernel_spmd(kb.nc, maps, core_ids=list(range(8)))
    return np.stack([np.asarray(r["out"], dtype=np.float32) for r in res.results], axis=0)


def _ph_gla(self, l):
    fw, I, S, C = self.fw, self.I, self.S, self.C
    self.begin()
    self.warm([AF.Exp, AF.Ln])
    qT = self.sb("qT", [64, 4, T], BF16)
    kT = self.sb("kT", [64, 4, T], BF16)
    kk = self.sb("kk", [128, NT, 256], BF16)
    vv = self.sb("vv", [128, NT, 512], BF16)
    rT = self.sb("rT", [64, T], BF16)
    fw.dma("sp", qT[:], S["GQT"].rearrange("h d t -> d h t"), W=[qT])
    fw.dma("sp", kT[:], S["GKT"].rearrange("h d t -> d h t"), W=[kT])
    fw.dma("sp", kk[:], S["GK"].rearrange("(i p) n -> p i n", p=128), W=[kk])
    fw.dma("sp", vv[:], S["GV"].rearrange("(i p) n -> p i n", p=128), W=[vv])
    fw.dma("sp", rT[:], S["GR"][:, :], W=[rT])
    trif = self.sb("trif", [128, 4, 128], F32)
    trib = self.sb("trib", [128, 4, 128], BF16)
    fw.dma("sp", trif[:], C["tri"].rearrange("a s t -> s a t"), W=[trif])
    fw.op("act", lambda e: e.mul(out=trib[:], in_=trif[:], mul=-1.0 / 16.0), R=[trif], W=[trib])
    maskf = self.sb("maskf", [128, 2, 512], F32)
    fw.dma("sp", maskf[:], C["mask4"].rearrange("a s t -> s a t"), W=[maskf])
    wdf = self.sb("wdf", [64, 256], F32)
    wd = self.sb("wd", [64, 256], BF16)
    fw.op("dve", lambda e: e.memset(wdf[:], 0.0), W=[wdf])
    fw.dma("sp", wdf[0:16, :], I["gla_w_decay"][l, 0], W=[wdf])
    fw.dma("sp", wdf[32:48, :], I["gla_w_decay"][l, 1], W=[wdf])
    fw.op("dve", lambda e: e.tensor_copy(out=wd[:], in_=wdf[:]), R=[wdf], W=[wd])
    bdb = self.sb("bdb", [128, 2, 256], F32)
    fw.dma("sp", bdb[:].rearrange("p a n -> p (a n)"), I["gla_b_decay"][l:l + 1].rearrange("o a n -> o (a n)").partition_broadcast(128), W=[bdb])
    zbp = self.pool("zb", [128, 256], F32, 2)

    Sf = self.sb("Sf", [64, 4, 128], F32)
    Sb = self.sb("Sb", [64, 4, 128], BF16)
    zps = self.pool("zps", [128, 256], F32, 1, psum=True)
    cps = self.pool("cps", [64, 4, 128], F32, 1, psum=True)
    gps = self.pool("gps", [128, 256], F32, 1, psum=True)
    aps = self.pool("aps", [128, 4, 128], F32, 2, psum=True)
    ops_ = self.pool("ops", [128, 4, 128], F32, 2, psum=True)
    kvps = self.pool("kvps", [64, 4, 128], F32, 1, psum=True)
    e1p = self.pool("e1", [128, 256], F32, 2)
    Lbp = self.pool("Lb", [128, 256], BF16, 2)
    Eqp = self.pool("Eq", [64, 4, 128], F32, 2)
    Ekp = self.pool("Ek", [64, 4, 128], F32, 2)
    Estp = self.pool("Est", [128, 256], F32, 2)
    qinp = self.pool("qin", [64, 4, 128], BF16, 2)
    kinp = self.pool("kin", [64, 4, 128], BF16, 2)
    kstp = self.pool("kst", [128, 256], BF16, 2)
    attp = self.pool("att", [128, 4, 128], BF16, 2)
    osbp = self.pool("osb", [128, 4, 128], F32, 2)

    for dr in range(2):
        fw.op("dve", lambda e: e.memset(Sf[:], 0.0), W=[Sf])
        fw.op("dve", lambda e: e.memset(Sb[:], 0.0), W=[Sb])
        order = list(range(NT)) if dr == 0 else [1, 0] + list(range(NT - 1, 1, -1))
        r0 = 0 if dr == 0 else 32
        iu, ig, im = (0, 2, 0) if dr == 0 else (1, 3, 1)
        lastcol = 127 if dr == 0 else 0
        for ci in order:
            c0 = ci * 128
            z = zps()
            fw.op("pe", lambda e: e.matmul(z[:], lhsT=rT[r0:r0 + 16, c0:c0 + 128], rhs=wd[r0:r0 + 16, :], start=True, stop=True), R=[rT, wd], W=[z])
            zb = zbp()
            fw.op("dve", lambda e: e.tensor_tensor(out=zb[:], in0=z[:], in1=bdb[:, dr, :], op=ALU.add), R=[z, bdb], W=[zb])
            e1 = e1p()
            Lb = Lbp()
            fw.op("act", lambda e: e.activation(out=e1[:], in_=zb[:], func=AF.Exp, scale=-1.0), R=[zb], W=[e1])
            fw.op("act", lambda e: e.activation(out=Lb[:], in_=e1[:], func=AF.Ln, bias=1.0), R=[e1], W=[Lb])
            cp = cps()
            for h in range(4):
                fw.op("pe", lambda e, cp=cp, Lb=Lb, h=h: e.matmul(cp[:, h, :], lhsT=Lb[:, h * 64:(h + 1) * 64], rhs=trib[:, iu, :], start=True, stop=True), R=[Lb, trib], W=[cp])
            gp = gps()
            fw.op("pe", lambda e, gp=gp, Lb=Lb: e.matmul(gp[:], lhsT=trib[:, ig, :], rhs=Lb[:], start=True, stop=True), R=[Lb, trib], W=[gp])
            Eq = Eqp(); Ek = Ekp(); Est = Estp()
            fw.op("act", lambda e, cp=cp, Eq=Eq: e.activation(out=Eq[:], in_=cp[:], func=AF.Exp), R=[cp], W=[Eq])
            fw.op("act", lambda e, cp=cp, Ek=Ek: e.activation(out=Ek[:], in_=cp[:], func=AF.Exp, scale=-1.0), R=[cp], W=[Ek])
            fw.op("act", lambda e, gp=gp, Est=Est: e.activation(out=Est[:], in_=gp[:], func=AF.Exp), R=[gp], W=[Est])
            qin = qinp(); kin = kinp(); kst = kstp()
            fw.op("dve", lambda e, qin=qin, Eq=Eq, c0=c0: e.scalar_tensor_tensor(out=qin[:], in0=qT[:, :, c0:c0 + 128], scalar=0.125, in1=Eq[:], op0=ALU.mult, op1=ALU.mult), R=[qT, Eq], W=[qin])
            fw.op("dve", lambda e, kin=kin, Ek=Ek, c0=c0: e.tensor_tensor(out=kin[:], in0=kT[:, :, c0:c0 + 128], in1=Ek[:], op=ALU.mult), R=[kT, Ek], W=[kin])
            fw.op("dve", lambda e, kst=kst, Est=Est, ci=ci: e.tensor_tensor(out=kst[:], in0=kk[:, ci, :], in1=Est[:], op=ALU.mult), R=[kk, Est], W=[kst])
            ap_ = aps()
            for h in range(4):
                fw.op("pe", lambda e, ap_=ap_, kin=kin, qin=qin, h=h: e.matmul(ap_[:, h, :], lhsT=kin[:, h, :], rhs=qin[:, h, :], start=True, stop=True), R=[kin, qin], W=[ap_])
            att = attp()
            fw.op("dve", lambda e, att=att, ap_=ap_: e.tensor_tensor(out=att[:].rearrange("p h t -> p (h t)"), in0=ap_[:].rearrange("p h t -> p (h t)"), in1=maskf[:, im, :], op=ALU.mult), R=[ap_, maskf], W=[att])
            op_ = ops_()
            for h in range(4):
                fw.op("pe", lambda e, op_=op_, att=att, h=h, ci=ci: e.matmul(op_[:, h, :], lhsT=vv[:, ci, h * 128:(h + 1) * 128], rhs=att[:, h, :], start=True, stop=False), R=[vv, att], W=[op_])
                fw.op("pe", lambda e, op_=op_, qin=qin, h=h: e.matmul(op_[:, h, :], lhsT=Sb[:, h, :], rhs=qin[:, h, :], start=False, stop=True), R=[Sb, qin], W=[op_])
            osb = osbp()
            fw.op("act", lambda e, osb=osb, op_=op_: e.activation(out=osb[:], in_=op_[:], func=AF.Copy), R=[op_], W=[osb])
            fw.dma("sp", S["OG"][dr].rearrange("h v t -> v h t")[:, :, c0:c0 + 128], osb[:], R=[osb])
            kv = kvps()
            for h in range(4):
                fw.op("pe", lambda e, kv=kv, kst=kst, h=h, ci=ci: e.matmul(kv[:, h, :], lhsT=kst[:, h * 64:(h + 1) * 64], rhs=vv[:, ci, h * 128:(h + 1) * 128], start=True, stop=True), R=[kst, vv], W=[kv])
            for h in range(4):
                fw.op("dve", lambda e, kv=kv, Eq=Eq, h=h: e.scalar_tensor_tensor(out=Sf[:, h, :], in0=Sf[:, h, :], scalar=Eq[:, h, lastcol:lastcol + 1], in1=kv[:, h, :], op0=ALU.mult, op1=ALU.add), R=[Sf, Eq, kv], W=[Sf])
            fw.op("dve", lambda e: e.tensor_copy(out=Sb[:], in_=Sf[:]), R=[Sf], W=[Sb])
    self.end("gla%d" % l)


def _rms_finish(self, o, n, gcol, extra_scale, pms, onesf, rstdp, tp):
    fw = self.fw
    sq = tp()
    fw.op("dve", lambda e: e.tensor_tensor(out=sq[:, 0:n], in0=o[:, 0:n], in1=o[:, 0:n], op=ALU.mult), R=[o], W=[sq])
    ms = pms()
    fw.op("pe", lambda e: e.matmul(ms[:, 0:n], lhsT=onesf[:], rhs=sq[:, 0:n], start=True, stop=True), R=[onesf, sq], W=[ms])
    rstd = rstdp()
    fw.op("dve", lambda e: e.tensor_scalar_add(out=rstd[:, 0:n], in0=ms[:, 0:n], scalar1=RMS_EPS), R=[ms], W=[rstd])
    fw.op("act", lambda e: e.activation(out=rstd[:, 0:n], in_=rstd[:, 0:n], func=AF.Ln), R=[rstd], W=[rstd])
    fw.op("act", lambda e: e.activation(out=rstd[:, 0:n], in_=rstd[:, 0:n], func=AF.Exp, scale=-0.5), R=[rstd], W=[rstd])
    t = tp()
    fw.op("dve", lambda e: e.tensor_tensor(out=t[:, 0:n], in0=o[:, 0:n], in1=rstd[:, 0:n], op=ALU.mult), R=[o, rstd], W=[t])
    if "DBGC" in self.debug and not getattr(self, "_dbgc_done", False):
        self._dbgc_done = True
        self.scratch("DBGC", [4, 128, 256], F32)
        msb = tp()
        fw.op("dve", lambda e: e.tensor_copy(out=msb[:, 0:n], in_=ms[:, 0:n]), R=[ms], W=[msb])
        for k_, b_ in enumerate((o, sq, msb, rstd)):
            fw.dma("pool", self.S["DBGC"][k_], b_[:, 0:256], R=[b_])
    return t


def _ph_glac(self, l):
    fw, I, S, C = self.fw, self.I, self.S, self.C
    self.begin()
    self.warm([AF.Ln, AF.Exp])
    onesf = self.sb("onesf", [128, 128], F32)
    fw.op("dve", lambda e: e.memset(onesf[:], 1.0 / 128.0), W=[onesf])
    gcol = self.sb("gcol", [128, 1], F32)
    idf, idb = self.load_ident()
    self.diag_cols(gcol, gcol[:], I["gla_norm_g"][l:l + 1, :], 1, idf)
    ofp = self.pool("of", [128, 512], F32, 2)
    obp = self.pool("ob", [128, 512], F32, 2)
    ggp = self.pool("gg", [128, 512], BF16, 2)
    op_ = self.pool("o", [128, 512], F32, 2)
    tp = self.pool("t", [128, 512], F32, 4)
    rstdp = self.pool("rstd", [128, 512], F32, 2)
    pms = self.pool("pms", [128, 512], F32, 2, psum=True)
    outp = self.pool("outb", [128, 512], BF16, 2)
    chunks = [(0, 256)] + [(256 + 512 * j, 512) for j in range(8)]
    for h in range(4):
        for (c0, n) in chunks:
            of = ofp(); ob = obp(); gg = ggp()
            fw.dma("sp", of[:, 0:n], S["OG"][0, h, :, c0:c0 + n], W=[of])
            fw.dma("sp", ob[:, 0:n], S["OG"][1, h, :, c0:c0 + n], W=[ob])
            fw.dma("sp", gg[:, 0:n], S["GG"][h * 128:(h + 1) * 128, c0:c0 + n], W=[gg])
            o = op_()
            fw.op("dve", lambda e, o=o, of=of, ob=ob, n=n: e.tensor_tensor(out=o[:, 0:n], in0=of[:, 0:n], in1=ob[:, 0:n], op=ALU.add), R=[of, ob], W=[o])
            t = _rms_finish(self, o, n, gcol, 1.0, pms, onesf, rstdp, tp)
            ob_ = outp()
            fw.op("dve", lambda e, ob_=ob_, t=t, gg=gg, n=n: e.scalar_tensor_tensor(out=ob_[:, 0:n], in0=t[:, 0:n], scalar=gcol[:, 0:1], in1=gg[:, 0:n], op0=ALU.mult, op1=ALU.mult), R=[t, gcol, gg], W=[ob_])
            fw.dma("pool", S["BR"][0, h * 128:(h + 1) * 128, c0:c0 + n], ob_[:, 0:n], R=[ob_])
    self.end("glac%d" % l)


KB.ph_gla = _ph_gla
KB.ph_glac = _ph_glac


def _ph_diff(self, l):
    fw, I, S, C = self.fw, self.I, self.S, self.C
    lam_init = 0.8 - 0.6 * math.exp(-0.3 * l)
    self.begin()
    self.warm([AF.Exp, AF.Ln])
    KT = self.sb("KT", [128, 4, T], BF16)
    VV = self.sb("VV", [128, NT, 512], BF16)
    fw.dma("sp", KT[:], S["DKT"].rearrange("h d t -> d h t"), W=[KT])
    fw.dma("sp", VV[:], S["DV"].rearrange("(i p) n -> p i n", p=128), W=[VV])
    onesb = self.sb("onesb", [128, 128], BF16)
    fw.op("dve", lambda e: e.memset(onesb[:], 1.0), W=[onesb])
    onesf = self.sb("onesf", [128, 128], F32)
    fw.op("dve", lambda e: e.memset(onesf[:], 1.0 / 128.0), W=[onesf])
    gcol = self.sb("gcol", [128, 1], F32)
    idf, idb = self.load_ident()
    self.diag_cols(gcol, gcol[:], I["diff_norm_g"][l:l + 1, :], 1, idf)
    fw.op("dve", lambda e: e.tensor_scalar_mul(out=gcol[:], in0=gcol[:], scalar1=(1.0 - lam_init)), R=[gcol], W=[gcol])
    lq = self.sb("lq", [128, 128], F32)
    lk = self.sb("lk", [128, 128], F32)
    fw.dma("sp", lq[:], I["diff_lam_q"][l:l + 1].rearrange("o a b -> o (a b)").partition_broadcast(128), W=[lq])
    fw.dma("sp", lk[:], I["diff_lam_k"][l:l + 1].rearrange("o a b -> o (a b)").partition_broadcast(128), W=[lk])
    fw.op("dve", lambda e: e.tensor_tensor(out=lq[:], in0=lq[:], in1=lk[:], op=ALU.mult), R=[lq, lk], W=[lq])
    ls = self.sb("ls", [128, 2], F32)
    fw.op("dve", lambda e: e.reduce_sum(out=ls[:], in_=lq[:].rearrange("p (a b) -> p a b", a=2), axis=AX.X), R=[lq], W=[ls])
    fw.op("act", lambda e: e.activation(out=ls[:], in_=ls[:], func=AF.Exp), R=[ls], W=[ls])
    nlam = self.sb("nlam", [128, 1], F32)
    fw.op("dve", lambda e: e.tensor_tensor(out=nlam[:], in0=ls[:, 1:2], in1=ls[:, 0:1], op=ALU.subtract), R=[ls], W=[nlam])
    fw.op("dve", lambda e: e.tensor_scalar_add(out=nlam[:], in0=nlam[:], scalar1=-lam_init), R=[nlam], W=[nlam])

    qp = self.pool("q", [128, 512], BF16, 3)
    sps = self.pool("sps", [128, 512], F32, 3, psum=True)
    ops_ = self.pool("ops", [128, 512], F32, 2, psum=True)
    dps = self.pool("dps", [128, 512], F32, 2, psum=True)
    pms = self.pool("pms", [128, 512], F32, 1, psum=True)
    ptp = self.pool("pt", [128, 512], BF16, 4)
    w0p = self.pool("w0", [128, 512], F32, 4)
    rcp = self.pool("rc", [128, 512], F32, 2)
    op_ = self.pool("o", [128, 512], F32, 2)
    tp = self.pool("t", [128, 512], F32, 4)
    rstdp = self.pool("rstd", [128, 512], F32, 2)
    outp = self.pool("outb", [128, 512], BF16, 2)
    chunks = [(0, 256, 2)] + [(256 + 512 * j, 512, NT) for j in range(8)]
    items = []
    for h in range(4):
        for (c0, n, nkt) in chunks:
            for comp in range(2):
                for kt in range(nkt):
                    items.append((h, c0, n, nkt, comp, kt))
    LA = 2
    st = {}
    cur = {}
    deferred = []

    def front(i):
        h, c0, n, nkt, comp, kt = items[i]
        key = (h, c0)
        if key not in cur:
            q = qp()
            fw.dma("sp", q[:, 0:n], S["DQT"][h, :, c0:c0 + n], W=[q])
            cur[key] = {"q": q, "ws": []}
        q = cur[key]["q"]
        pb = comp * 64
        sp_ = sps()
        fw.op("pe", lambda e: e.matmul(sp_[:, 0:n], lhsT=KT[pb:pb + 64, h, kt * 128:(kt + 1) * 128], rhs=q[pb:pb + 64, 0:n], start=True, stop=True), R=[KT, q], W=[sp_])
        pt = ptp()
        fw.op("act", lambda e: e.activation(out=pt[:, 0:n], in_=sp_[:, 0:n], func=AF.Exp, scale=0.125), R=[sp_], W=[pt])
        st[i] = pt

    def back(i):
        h, c0, n, nkt, comp, kt = items[i]
        key = (h, c0)
        c = cur[key]
        if kt == 0:
            c["o_ps"] = ops_()
            c["d_ps"] = dps()
        o_ps, d_ps = c["o_ps"], c["d_ps"]
        pt = st.pop(i)
        fw.op("pe", lambda e: e.matmul(o_ps[:, 0:n], lhsT=VV[:, kt, h * 128:(h + 1) * 128], rhs=pt[:, 0:n], start=(kt == 0), stop=(kt == nkt - 1)), R=[VV, pt], W=[o_ps])
        fw.op("pe", lambda e: e.matmul(d_ps[:, 0:n], lhsT=onesb[:], rhs=pt[:, 0:n], start=(kt == 0), stop=(kt == nkt - 1)), R=[onesb, pt], W=[d_ps])
        if kt == nkt - 1:
            rc = rcp()
            fw.op("dve", lambda e: e.reciprocal(out=rc[:, 0:n], in_=d_ps[:, 0:n]), R=[d_ps], W=[rc])
            w = w0p()
            fw.op("dve", lambda e: e.tensor_tensor(out=w[:, 0:n], in0=o_ps[:, 0:n], in1=rc[:, 0:n], op=ALU.mult), R=[o_ps, rc], W=[w])
            c["ws"].append(w)
            if comp == 1:
                ws = c["ws"]
                o = op_()
                fw.op("dve", lambda e: e.scalar_tensor_tensor(out=o[:, 0:n], in0=ws[1][:, 0:n], scalar=nlam[:, 0:1], in1=ws[0][:, 0:n], op0=ALU.mult, op1=ALU.add), R=[ws[0], ws[1], nlam], W=[o])
                sq = tp()
                fw.op("dve", lambda e: e.tensor_tensor(out=sq[:, 0:n], in0=o[:, 0:n], in1=o[:, 0:n], op=ALU.mult), R=[o], W=[sq])

                def tail(o=o, sq=sq, n=n, h=h, c0=c0):
                    ms = pms()
                    fw.op("pe", lambda e: e.matmul(ms[:, 0:n], lhsT=onesf[:], rhs=sq[:, 0:n], start=True, stop=True), R=[onesf, sq], W=[ms])
                    rstd = rstdp()
                    fw.op("dve", lambda e: e.tensor_scalar_add(out=rstd[:, 0:n], in0=ms[:, 0:n], scalar1=RMS_EPS), R=[ms], W=[rstd])
                    fw.op("act", lambda e: e.activation(out=rstd[:, 0:n], in_=rstd[:, 0:n], func=AF.Ln), R=[rstd], W=[rstd])
                    fw.op("act", lambda e: e.activation(out=rstd[:, 0:n], in_=rstd[:, 0:n], func=AF.Exp, scale=-0.5), R=[rstd], W=[rstd])
                    t = tp()
                    fw.op("dve", lambda e: e.tensor_tensor(out=t[:, 0:n], in0=o[:, 0:n], in1=rstd[:, 0:n], op=ALU.mult), R=[o, rstd], W=[t])
                    ob_ = outp()
                    fw.op("act", lambda e: e.activation(out=ob_[:, 0:n], in_=t[:, 0:n], func=AF.Identity, scale=gcol[:, 0:1]), R=[t, gcol], W=[ob_])
                    fw.dma("pool", S["BR"][1, h * 128:(h + 1) * 128, c0:c0 + n], ob_[:, 0:n], R=[ob_])
                deferred.append((i + 4, tail))
                del cur[key]

    nI = len(items)
    for i in range(nI + LA):
        if i < nI:
            front(i)
        if i - LA >= 0:
            back(i - LA)
        while deferred and deferred[0][0] <= i - LA:
            deferred.pop(0)[1]()
    while deferred:
        deferred.pop(0)[1]()
    self.end("diff%d" % l)


KB.ph_diff = _ph_diff


def _ph_na(self, l):
    fw, I, S, C = self.fw, self.I, self.S, self.C
    nc = self.nc
    self.begin()
    self.warm([AF.Exp])
    KT = self.sb("KT", [128, 4, T], BF16)
    VV = self.sb("VV", [128, NT, 512], BF16)
    fw.dma("sp", KT[:], S["NKT"].rearrange("g d t -> d g t"), W=[KT])
    fw.dma("sp", VV[:], S["NV"].rearrange("(i p) n -> p i n", p=128), W=[VV])
    onesb = self.sb("onesb", [128, 64], BF16)
    fw.op("dve", lambda e: e.memset(onesb[:], 1.0), W=[onesb])
    TAB = self.sb("TAB", [128, 16, 512], BF16)
    fw.op("dve", lambda e: e.memset(TAB[:], 0.0), W=[TAB])
    j64 = self.sb("j64", [64, 128], F32)
    fw.dma("sp", j64[:], C["j64"][:, :], W=[j64])
    win = self.sb("win", [128, 512], F32)
    fw.dma("sp", win[:], C["nawin"][:, :], W=[win])
    idf, idb = self.load_ident()
    r120 = self.sb("r120", [120, 31], F32)
    fw.dma("sp", r120[:], I["na_rpb"][l].rearrange("h a j -> (h a) j"), W=[r120])
    r31 = self.sb("r31", [31, 120], F32)
    tps = self.pool("tps", [128, 512], F32, 1, psum=True)
    pt0 = tps()
    fw.op("pe", lambda e: e.transpose(out=pt0[0:31, 0:120], in_=r120[:, :], identity=idf[0:120, 0:120]), R=[r120, idf], W=[pt0])
    fw.op("act", lambda e: e.activation(out=r31[:], in_=pt0[0:31, 0:120], func=AF.Copy), R=[pt0], W=[r31])
    p0 = tps()
    fw.op("pe", lambda e: e.matmul(p0[0:120, 0:31], lhsT=r31[:, :], rhs=j64[0:31, 33:64], start=True, stop=True), R=[r31, j64], W=[p0])
    rev = self.sb("rev", [120, 31], F32)
    fw.op("act", lambda e: e.activation(out=rev[:], in_=p0[0:120, 0:31], func=AF.Copy), R=[p0], W=[rev])
    zt = self.sb("zt", [120, 128], F32)
    fw.op("dve", lambda e: e.memset(zt[:], 0.0), W=[zt])
    nag = Buf("nag")
    NAG = S["NAG"]
    fw.dma("sp", NAG[:, :], zt[:], R=[zt], W=[nag])
    fw.dma("sp", NAG[:, 48:79], rev[:], R=[rev], W=[nag])
    Hk = self.sb("Hk", [64, 120, 64], F32)
    for n0 in range(0, 120, 8):
        hsrc = bass.AP(tensor=NAG.tensor, offset=NAG.offset + n0 * 128, ap=[[1, 64], [128, 8], [1, 64]])
        fw.dma("sp", Hk[:, n0:n0 + 8, :], hsrc, R=[nag], W=[Hk])
    Hk4 = Hk[:].rearrange("m (h a) c -> m h a c", h=8)
    ebp = self.pool("eb", [128, 512], F32, 2)
    for dr in range(15):
        p = tps()
        fw.op("pe", lambda e: e.matmul(p[:].rearrange("p (h c) -> p h c", h=8), lhsT=j64[:, :], rhs=Hk4[:, :, dr, :], start=True, stop=True), R=[j64, Hk], W=[p])
        eb = ebp()
        fw.op("act", lambda e: e.activation(out=eb[:], in_=p[:], func=AF.Exp), R=[p], W=[eb])
        if dr <= 13:
            fw.op("dve", lambda e: e.tensor_tensor(out=TAB[0:64, dr, :], in0=eb[0:64, :], in1=win[0:64, :], op=ALU.mult), R=[eb, win], W=[TAB])
        if dr == 10:
            fw.op("dve", lambda e: e.tensor_tensor(out=TAB[0:64, 15, :], in0=eb[0:64, :], in1=win[0:64, :], op=ALU.mult), R=[eb, win], W=[TAB])
        if dr >= 1:
            fw.op("dve", lambda e: e.tensor_tensor(out=TAB[64:128, dr - 1, :], in0=eb[64:128, :], in1=win[64:128, :], op=ALU.mult), R=[eb, win], W=[TAB])
        if dr == 3:
            fw.op("dve", lambda e: e.tensor_tensor(out=TAB[64:128, 14, :], in0=eb[64:128, :], in1=win[64:128, :], op=ALU.mult), R=[eb, win], W=[TAB])

    if self.stop_after == "natab":
        self.scratch("TAB", [128, 16, 512], BF16)
        fw.dma("pool", S["TAB"], TAB[:], R=[TAB])
        self.end("natab")
        return
    q8p = self.pool("q8", [128, 4, 512], BF16, 2)
    stgp = self.pool("stg", [64, 8, 512], BF16, 2)
    sps = self.pool("sps", [128, 512], F32, 4, psum=True)
    ops_ = self.pool("ops", [64, 512], F32, 2, psum=True)
    dps = self.pool("dps", [64, 512], F32, 1, psum=True)
    ptp = self.pool("pt", [128, 512], BF16, 16)
    rcp = self.pool("rc", [64, 512], F32, 2)
    BRv = S["BR"][2].rearrange("(h d) t -> d h t", h=8)
    NQv = S["NQT"].rearrange("g d t -> d g t")
    ntoggle = 0
    for grp in range(T // 512 + 1):
        if grp == 0:
            tok0, nrows = 0, 4
        else:
            tok0, nrows = 256 + (grp - 1) * 512, 8
        ntok = nrows * 64
        q8 = q8p()
        stg = stgp()
        fw.dma("sp", q8[:, :, 0:ntok], NQv[:, :, tok0:tok0 + ntok], W=[q8])
        for rr in range(nrows):
            qoff = rr * 64
            tiles = [(0, None), (1, None)]
            if grp > 0:
                r = (grp - 1) * 8 + rr
                rs = min(max(r - 4, 0), 56)
                if rs % 2 == 0:
                    for j in range(4):
                        tiles.append((2 + rs // 2 + j, rs + 2 * j - r + 7))
                else:
                    tiles.append((2 + (rs - 1) // 2, 14))
                    for j in range(3):
                        tiles.append((2 + (rs + 1) // 2 + j, rs + 1 + 2 * j - r + 7))
                    tiles.append((2 + (rs + 7) // 2, 15))
            P = []
            for (kt, slot) in tiles:
                spe = sps()
                spo = sps()
                for h in range(8):
                    pb = (h % 2) * 64
                    dstp = spe if h % 2 == 0 else spo
                    fw.op("pe", lambda e: e.matmul(dstp[:, (h // 2) * 64:(h // 2 + 1) * 64], lhsT=KT[pb:pb + 64, h // 2, kt * 128:(kt + 1) * 128], rhs=q8[pb:pb + 64, h // 2, qoff:qoff + 64], start=True, stop=True), R=[KT, q8], W=[dstp])
                pt = ptp()
                ptv = pt[:].rearrange("p (g two q) -> p g two q", two=2, q=64)
                fw.op("act", lambda e: e.activation(out=ptv[:, :, 0, :], in_=spe[:, 0:256].rearrange("p (g q) -> p g q", q=64), func=AF.Exp, scale=0.125), R=[spe], W=[pt])
                fw.op("act", lambda e: e.activation(out=ptv[:, :, 1, :], in_=spo[:, 0:256].rearrange("p (g q) -> p g q", q=64), func=AF.Exp, scale=0.125), R=[spo], W=[pt])
                if slot is not None:
                    assert 0 <= slot <= 15
                    eng = "dve"
                    ntoggle += 1
                    fw.op(eng, lambda e: e.tensor_tensor(out=pt[:], in0=pt[:], in1=TAB[:, slot, :], op=ALU.mult), R=[pt, TAB], W=[pt])
                P.append((kt, pt))
            o_ps = ops_()
            d_ps = dps()
            nk = len(P)
            for h in range(8):
                for i, (kt, pt) in enumerate(P):
                    fw.op("pe", lambda e: e.matmul(o_ps[:, h * 64:(h + 1) * 64], lhsT=VV[:, kt, h * 64:(h + 1) * 64], rhs=pt[:, h * 64:(h + 1) * 64], start=(i == 0), stop=(i == nk - 1)), R=[VV, pt], W=[o_ps])
            for i, (kt, pt) in enumerate(P):
                fw.op("pe", lambda e: e.matmul(d_ps[:], lhsT=onesb[:], rhs=pt[:], start=(i == 0), stop=(i == nk - 1)), R=[onesb, pt], W=[d_ps])
            rc = rcp()
            fw.op("dve", lambda e: e.reciprocal(out=rc[:], in_=d_ps[:]), R=[d_ps], W=[rc])
            fw.op("dve", lambda e: e.tensor_tensor(out=stg[:, :, qoff:qoff + 64], in0=o_ps[:].rearrange("p (h q) -> p h q", h=8), in1=rc[:].rearrange("p (h q) -> p h q", h=8), op=ALU.mult), R=[o_ps, rc], W=[stg])
        fw.dma("pool", BRv[:, :, tok0:tok0 + ntok], stg[:, :, 0:ntok], R=[stg])
    self.end("na%d" % l)


KB.ph_na = _ph_na


def _ln_epilogue(self, ysrc, xt, mbc, gbc, bbc, pools, dst_ap):
    fw = self.fw
    up, stp, mvp, xnp = pools
    u = up()
    for hf in range(2):
        yb, yap = ysrc[hf]
        fw.op("dve", lambda e: e.tensor_tensor(out=u[:, hf * 512:(hf + 1) * 512], in0=yap, in1=mbc[:, hf * 512:(hf + 1) * 512], op=ALU.mult), R=[yb, mbc], W=[u])
    fw.op("dve", lambda e: e.scalar_tensor_tensor(out=u[:], in0=xt[:], scalar=ALPHA, in1=u[:], op0=ALU.mult, op1=ALU.add), R=[xt, u], W=[u])
    st = stp()
    for hf in range(2):
        fw.op("dve", lambda e: e.bn_stats(out=st[:, hf, :], in_=u[:, hf * 512:(hf + 1) * 512]), R=[u], W=[st])
    mv = mvp()
    fw.op("dve", lambda e: e.bn_aggr(out=mv[:, 0:2], in_=st[:].rearrange("p a b -> p (a b)")), R=[st], W=[mv])
    fw.op("dve", lambda e: e.tensor_scalar_add(out=mv[:, 2:3], in0=mv[:, 1:2], scalar1=LN_EPS), R=[mv], W=[mv])
    fw.op("act", lambda e: e.activation(out=mv[:, 2:3], in_=mv[:, 2:3], func=AF.Ln), R=[mv], W=[mv])
    fw.op("act", lambda e: e.activation(out=mv[:, 2:3], in_=mv[:, 2:3], func=AF.Exp, scale=-0.5), R=[mv], W=[mv])
    fw.op("dve", lambda e: e.scalar_tensor_tensor(out=mv[:, 3:4], in0=mv[:, 0:1], scalar=-1.0, in1=mv[:, 2:3], op0=ALU.mult, op1=ALU.mult), R=[mv], W=[mv])
    xn = xnp()
    fw.op("act", lambda e: e.activation(out=xn[:], in_=u[:], func=AF.Identity, bias=mv[:, 3:4], scale=mv[:, 2:3]), R=[u, mv], W=[xn])
    fw.op("dve", lambda e: e.tensor_tensor(out=xn[:], in0=xn[:], in1=gbc[:], op=ALU.mult), R=[xn, gbc], W=[xn])
    fw.op("dve", lambda e: e.tensor_tensor(out=xn[:], in0=xn[:], in1=bbc[:], op=ALU.add), R=[xn, bbc], W=[xn])
    fw.dma("pool", dst_ap, xn[:], R=[xn])


def _bc_tile(self, name, row_ap):
    t = self.sb(name, [128, D], F32)
    self.fw.dma("sp", t[:], row_ap.partition_broadcast(128), W=[t])
    return t


def _ph_merge(self, l):
    fw, I, S, C = self.fw, self.I, self.S, self.C
    self.begin()
    self.warm([AF.Ln, AF.Exp])
    wfp = self.pool("wf", [128, 1024], F32, 2)
    wbr = self.sb("wbr", [128, 3, 4, 1024], BF16)
    wo = self.sb("wo", [128, 8, 1024], BF16)
    k = 0
    for n_ in range(3):
        for ec in range(4):
            wf = wfp()
            fw.dma("sp", wf[:], I["w_branch"][l, n_, ec * 128:(ec + 1) * 128, :], W=[wf])
            if k % 2 == 0:
                fw.op("dve", lambda e: e.tensor_copy(out=wbr[:, n_, ec, :], in_=wf[:]), R=[wf], W=[wbr])
            else:
                fw.op("act", lambda e: e.activation(out=wbr[:, n_, ec, :], in_=wf[:], func=AF.Copy), R=[wf], W=[wbr])
            k += 1
    for dc in range(8):
        wf = wfp()
        fw.dma("sp", wf[:], I["w_o"][l, dc * 128:(dc + 1) * 128, :], W=[wf])
        if dc % 2 == 0:
            fw.op("dve", lambda e: e.tensor_copy(out=wo[:, dc, :], in_=wf[:]), R=[wf], W=[wo])
        else:
            fw.op("act", lambda e: e.activation(out=wo[:, dc, :], in_=wf[:], func=AF.Copy), R=[wf], W=[wo])
    m2 = [_bc_tile(self, "m2L", S["MODR"][l, 0, 0:1, :]), _bc_tile(self, "m2C", S["MODR"][l, 1, 0:1, :])]
    gbc = _bc_tile(self, "gbc", I["ln_g"][l, 0:1, :])
    bbc = _bc_tile(self, "bbc", I["ln_b"][l, 0:1, :])
    brp = self.pool("br", [128, 3, 4, 512], BF16, 2)
    gtp = self.pool("gt", [128, 3, 512], BF16, 2)
    pj = self.pool("pj", [128, 512], F32, 3, psum=True)
    yps = self.pool("yps", [128, 512], F32, 4, psum=True)
    accp = self.pool("acc", [128, 512], F32, 2)
    tmpp = self.pool("tmp", [128, 512], F32, 2)
    mTp = self.pool("mT", [128, 8, 512], BF16, 2)
    xtp = self.pool("xt", [128, D], F32, 2)
    pools = (self.pool("u", [128, D], F32, 2), self.pool("st", [128, 2, 6], F32, 2), self.pool("mv", [128, 4], F32, 2), self.pool("xn", [128, D], F32, 2))
    chunks = [(0, 256)] + [(256 + 512 * j, 512) for j in range(8)]
    BRv = S["BR"].rearrange("n (ec p) t -> p n ec t", p=128)
    GTv = S["GT"].rearrange("n (dc p) t -> p n dc t", p=128)
    for (c0, n) in chunks:
        br = brp()
        for n_ in range(3):
            fw.dma("sp", br[:, n_, :, 0:n], BRv[:, n_, :, c0:c0 + n], W=[br])
        mT = mTp()
        for dc in range(8):
            gt = gtp()
            fw.dma("sp", gt[:, :, 0:n], GTv[:, :, dc, c0:c0 + n], W=[gt])
            acc = accp()
            for n_ in range(3):
                p = pj()
                for ec in range(4):
                    fw.op("pe", lambda e: e.matmul(p[:, 0:n], lhsT=wbr[:, n_, ec, dc * 128:(dc + 1) * 128], rhs=br[:, n_, ec, 0:n], start=(ec == 0), stop=(ec == 3)), R=[wbr, br], W=[p])
                if n_ == 0:
                    fw.op("dve", lambda e: e.tensor_tensor(out=acc[:, 0:n], in0=p[:, 0:n], in1=gt[:, 0, 0:n], op=ALU.mult), R=[p, gt], W=[acc])
                else:
                    tmp = tmpp()
                    fw.op("dve", lambda e: e.tensor_tensor(out=tmp[:, 0:n], in0=p[:, 0:n], in1=gt[:, n_, 0:n], op=ALU.mult), R=[p, gt], W=[tmp])
                    if n_ == 1:
                        fw.op("dve", lambda e: e.tensor_tensor(out=acc[:, 0:n], in0=acc[:, 0:n], in1=tmp[:, 0:n], op=ALU.add), R=[acc, tmp], W=[acc])
                    else:
                        fw.op("dve", lambda e: e.tensor_tensor(out=mT[:, dc, 0:n], in0=acc[:, 0:n], in1=tmp[:, 0:n], op=ALU.add), R=[acc, tmp], W=[mT])
        for tl in range(n // 128):
            ti = c0 // 128 + tl
            ys = []
            for hf in range(2):
                y = yps()
                for dc in range(8):
                    fw.op("pe", lambda e: e.matmul(y[:], lhsT=mT[:, dc, tl * 128:(tl + 1) * 128], rhs=wo[:, dc, hf * 512:(hf + 1) * 512], start=(dc == 0), stop=(dc == 7)), R=[mT, wo], W=[y])
                ys.append((y, y[:]))
            xt = xtp()
            fw.dma("sp", xt[:], self.x_src(l, ti), W=[xt])
            _ln_epilogue(self, ys, xt, m2[1 if ti < 2 else 0], gbc, bbc, pools, S["XS"][ti * 128:(ti + 1) * 128, :])
    self.end("merge%d" % l)


KB.ph_merge = _ph_merge


def _ph_moe(self, l, last):
    fw, I, S, C = self.fw, self.I, self.S, self.C
    self.begin()
    self.warm([AF.Sigmoid, AF.Silu])
    idf, idb = self.load_ident()
    modf = self.sb("modf", [128, 48, 2], F32)
    fw.dma("sp", modf[:], S["MODF"][l], W=[modf])
    m5 = [_bc_tile(self, "m5L", S["MODR"][l, 0, 1:2, :]), _bc_tile(self, "m5C", S["MODR"][l, 1, 1:2, :])]
    gbc = _bc_tile(self, "gbc", I["ln_g"][l, 1:2, :])
    bbc = _bc_tile(self, "bbc", I["ln_b"][l, 1:2, :])
    wr = self.sb("wr", [128, 8, 16], F32)
    fw.dma("sp", wr[:], I["w_router"].rearrange("(kc p) e -> p kc e", p=128), W=[wr])
    brt = self.sb("brt", [128, 16], F32)
    fw.dma("sp", brt[:], I["b_router"].rearrange("(o e) -> o e", o=1).partition_broadcast(128), W=[brt])
    self_f = self.sb("self", [16, 16, 128], F32)
    selb = self.sb("selb", [16, 16, 128], BF16)
    fw.dma("sp", self_f[:], C["sel"], W=[self_f])
    fw.op("dve", lambda e: e.tensor_copy(out=selb[:], in_=self_f[:]), R=[self_f], W=[selb])

    NG = 9
    hT = self.sb("hT", [128, 8, NG * 128], BF16)
    Y = self.sb("Y", [128, NG, D], F32)
    aff = self.sb("aff", [128, NG, 16], F32)
    selv = self.sb("selv", [128, NG, 16], F32)
    comb = self.sb("comb", [128, NG, 16], F32)
    cT = self.sb("cT", [16, NG * 128], BF16)
    r1 = self.sb("r1", [128, NG * 4], F32)
    r2 = self.sb("r2", [128, NG * 4], F32)
    gs = self.sb("gs", [128, NG * 4], F32)
    gmx = self.sb("gmx", [128, NG], F32)
    gmask = self.sb("gmask", [128, NG * 4], F32)
    is1 = self.sb("is1", [128, NG * 4, 4], F32)
    v2 = self.sb("v2", [128, NG * 4, 4], F32)
    oh = self.sb("oh", [128, NG * 4, 4], F32)
    ssum = self.sb("ssum", [128, NG], F32)

    xtp = self.pool("xt", [128, D], F32, 2)
    h32p = self.pool("h32", [128, 8, 128], F32, 2)
    ptr = self.pool("ptr", [128, 4, 128], F32, 2, psum=True)
    sml = self.pool("sml", [128, 512], F32, 1, psum=True)
    gup = self.pool("gu", [128, 512], F32, 3, psum=True)
    yps = self.pool("yps", [128, 512], F32, 2, psum=True)
    wsf = self.pool("wsf", [128, 2048], F32, 2)
    wgp = self.pool("wg", [128, 8, 512], BF16, 2)
    wup = self.pool("wu", [128, 8, 512], BF16, 2)
    wdp = self.pool("wd", [128, 4, 1024], BF16, 2)
    cbp = self.pool("cb", [128, 512], F32, 2)
    sgp = self.pool("sg", [128, 512], F32, 2)
    a1p = self.pool("a1", [128, 512], F32, 2)
    aTp = self.pool("aT", [128, 4, 512], BF16, 2)
    pools = (self.pool("u", [128, D], F32, 1), self.pool("st", [128, 2, 6], F32, 2), self.pool("mv", [128, 4], F32, 2), self.pool("xn", [128, D], F32, 2))
    castk = [0]

    def cast(dst_ap, dstb, src_ap, srcb):
        if castk[0] % 2 == 0:
            fw.op("dve", lambda e: e.tensor_copy(out=dst_ap, in_=src_ap), R=[srcb], W=[dstb])
        else:
            fw.op("act", lambda e: e.activation(out=dst_ap, in_=src_ap, func=AF.Copy), R=[srcb], W=[dstb])
        castk[0] += 1

    for (g0, g1) in ((0, 9), (9, 18), (18, 26), (26, 34)):
        ng = g1 - g0
        for i in range(g0, g1):
            j = i - g0
            st_ = 1 if i < 2 else 0
            xt = xtp()
            fw.dma("sp", xt[:], S["XS"][i * 128:(i + 1) * 128, :], W=[xt])
            h32 = h32p()
            for hf in range(2):
                p = ptr()
                for k4 in range(4):
                    kc = hf * 4 + k4
                    fw.op("pe", lambda e: e.transpose(out=p[:, k4, :], in_=xt[:, kc * 128:(kc + 1) * 128], identity=idf[:]), R=[xt, idf], W=[p])
                for k4 in range(4):
                    kc = hf * 4 + k4
                    fw.op("act", lambda e: e.activation(out=h32[:, kc, :], in_=p[:, k4, :], func=AF.Identity, bias=modf[:, 24 + kc, st_:st_ + 1], scale=modf[:, 32 + kc, st_:st_ + 1]), R=[p, modf], W=[h32])
            fw.op("dve", lambda e: e.tensor_copy(out=hT[:, :, j * 128:(j + 1) * 128], in_=h32[:]), R=[h32], W=[hT])
            lg = sml()
            for kc in range(8):
                fw.op("pe", lambda e: e.matmul(lg[:, 0:16], lhsT=h32[:, kc, :], rhs=wr[:, kc, :], start=(kc == 0), stop=(kc == 7)), R=[h32, wr], W=[lg])
            fw.op("act", lambda e: e.activation(out=aff[:, j, :], in_=lg[:, 0:16], func=AF.Sigmoid), R=[lg], W=[aff])
            fw.op("dve", lambda e: e.tensor_tensor(out=selv[:, j, :], in0=aff[:, j, :], in1=brt[:], op=ALU.add), R=[aff, brt], W=[selv])
        n4 = ng * 4
        s4 = selv[:, 0:ng, :].rearrange("p g (q e) -> p (g q) e", e=4)
        first = True
        for (a, b) in ((0, 1), (0, 2), (0, 3), (1, 2), (1, 3), (2, 3)):
            if first:
                fw.op("dve", lambda e: e.tensor_tensor(out=gs[:, 0:n4], in0=s4[:, :, a], in1=s4[:, :, b], op=ALU.add), R=[selv], W=[gs])
                first = False
            else:
                fw.op("dve", lambda e: e.tensor_tensor(out=r1[:, 0:n4], in0=s4[:, :, a], in1=s4[:, :, b], op=ALU.add), R=[selv], W=[r1])
                fw.op("dve", lambda e: e.tensor_tensor(out=gs[:, 0:n4], in0=gs[:, 0:n4], in1=r1[:, 0:n4], op=ALU.max), R=[gs, r1], W=[gs])
        fw.op("dve", lambda e: e.reduce_max(out=gmx[:, 0:ng], in_=gs[:, 0:n4].rearrange("p (g q) -> p g q", q=4), axis=AX.X), R=[gs], W=[gmx])
        for q in range(4):
            fw.op("dve", lambda e: e.tensor_tensor(out=gmask[:, 0:n4].rearrange("p (g q) -> p g q", q=4)[:, :, q], in0=gs[:, 0:n4].rearrange("p (g q) -> p g q", q=4)[:, :, q], in1=gmx[:, 0:ng], op=ALU.is_ge), R=[gs, gmx], W=[gmask])
        fw.op("dve", lambda e: e.reduce_max(out=r1[:, 0:n4], in_=s4, axis=AX.X), R=[selv], W=[r1])
        for ee in range(4):
            fw.op("dve", lambda e: e.tensor_tensor(out=is1[:, 0:n4, ee], in0=s4[:, :, ee], in1=r1[:, 0:n4], op=ALU.is_ge), R=[selv, r1], W=[is1])
        fw.op("dve", lambda e: e.scalar_tensor_tensor(out=v2[:, 0:n4, :], in0=is1[:, 0:n4, :], scalar=-1.0e9, in1=s4, op0=ALU.mult, op1=ALU.add), R=[is1, selv], W=[v2])
        fw.op("dve", lambda e: e.reduce_max(out=r2[:, 0:n4], in_=v2[:, 0:n4, :], axis=AX.X), R=[v2], W=[r2])
        for ee in range(4):
            fw.op("dve", lambda e: e.tensor_tensor(out=oh[:, 0:n4, ee], in0=v2[:, 0:n4, ee], in1=r2[:, 0:n4], op=ALU.is_ge), R=[v2, r2], W=[oh])
        fw.op("dve", lambda e: e.tensor_tensor(out=oh[:, 0:n4, :], in0=oh[:, 0:n4, :], in1=is1[:, 0:n4, :], op=ALU.add), R=[oh, is1], W=[oh])
        for ee in range(4):
            fw.op("dve", lambda e: e.tensor_tensor(out=oh[:, 0:n4, ee], in0=oh[:, 0:n4, ee], in1=gmask[:, 0:n4], op=ALU.mult), R=[oh, gmask], W=[oh])
        ohv = oh[:, 0:n4, :].rearrange("p (g q) e -> p g (q e)", q=4)
        fw.op("dve", lambda e: e.tensor_tensor(out=comb[:, 0:ng, :], in0=aff[:, 0:ng, :], in1=ohv, op=ALU.mult), R=[aff, oh], W=[comb])
        fw.op("dve", lambda e: e.reduce_sum(out=ssum[:, 0:ng], in_=comb[:, 0:ng, :], axis=AX.X), R=[comb], W=[ssum])
        fw.op("dve", lambda e: e.reciprocal(out=ssum[:, 0:ng], in_=ssum[:, 0:ng]), R=[ssum], W=[ssum])
        for j in range(ng):
            fw.op("dve", lambda e: e.tensor_scalar_mul(out=comb[:, j, :], in0=comb[:, j, :], scalar1=ssum[:, j:j + 1]), R=[comb, ssum], W=[comb])
            pc = sml()
            fw.op("pe", lambda e: e.transpose(out=pc[0:16, 0:128], in_=comb[:, j, :], identity=idf[:]), R=[comb, idf], W=[pc])
            fw.op("act", lambda e: e.activation(out=cT[:, j * 128:(j + 1) * 128], in_=pc[0:16, 0:128], func=AF.Copy), R=[pc], W=[cT])
        gchunks = []
        c = 0
        while c < ng * 128:
            n = min(512, ng * 128 - c)
            gchunks.append((c, n))
            c += n
        mitems = [(ex, c0, n) for ex in range(16) for (c0, n) in gchunks]
        wts = {}
        mst = {}

        def load_w(ex):
            wg = wgp(); wu = wup(); wd = wdp()
            for (wdst, src) in ((wg, I["w_exp_gate"][l, ex]), (wu, I["w_exp_up"][l, ex])):
                for hf in range(2):
                    wf = wsf()
                    fw.dma("sp", wf[:].rearrange("p (k f) -> p k f", k=4), src[hf * 512:(hf + 1) * 512, :].rearrange("(k p) f -> p k f", p=128), W=[wf])
                    cast(wdst[:, hf * 4:(hf + 1) * 4, :], wdst, wf[:].rearrange("p (k f) -> p k f", k=4), wf)
            for hf in range(2):
                wf = wsf()
                fw.dma("sp", wf[:].rearrange("p (k d) -> p k d", k=2), I["w_exp_down"][l, ex, hf * 256:(hf + 1) * 256, :].rearrange("(k p) d -> p k d", p=128), W=[wf])
                cast(wd[:, hf * 2:(hf + 1) * 2, :], wd, wf[:].rearrange("p (k d) -> p k d", k=2), wf)
            wts[ex] = (wg, wu, wd)

        def mfront(i):
            ex, c0, n = mitems[i]
            if ex not in wts:
                load_w(ex)
            wg, wu, wd = wts[ex]
            cbps = sml()
            fw.op("pe", lambda e: e.matmul(cbps[:, 0:n], lhsT=selb[:, ex, :], rhs=cT[:, c0:c0 + n], start=True, stop=True), R=[selb, cT], W=[cbps])
            cb = cbp()
            fw.op("act", lambda e: e.activation(out=cb[:, 0:n], in_=cbps[:, 0:n], func=AF.Copy), R=[cbps], W=[cb])
            aT = aTp()
            for fc in range(4):
                gp = gup(); up_ = gup()
                for kc in range(8):
                    fw.op("pe", lambda e: e.matmul(gp[:, 0:n], lhsT=wg[:, kc, fc * 128:(fc + 1) * 128], rhs=hT[:, kc, c0:c0 + n], start=(kc == 0), stop=(kc == 7)), R=[wg, hT], W=[gp])
                for kc in range(8):
                    fw.op("pe", lambda e: e.matmul(up_[:, 0:n], lhsT=wu[:, kc, fc * 128:(fc + 1) * 128], rhs=hT[:, kc, c0:c0 + n], start=(kc == 0), stop=(kc == 7)), R=[wu, hT], W=[up_])
                sg = sgp()
                fw.op("act", lambda e: e.activation(out=sg[:, 0:n], in_=gp[:, 0:n], func=AF.Silu), R=[gp], W=[sg])
                a1 = a1p()
                fw.op("dve", lambda e: e.tensor_tensor(out=a1[:, 0:n], in0=up_[:, 0:n], in1=sg[:, 0:n], op=ALU.mult), R=[up_, sg], W=[a1])
                fw.op("dve", lambda e: e.tensor_tensor(out=aT[:, fc, 0:n], in0=a1[:, 0:n], in1=cb[:, 0:n], op=ALU.mult), R=[a1, cb], W=[aT])
            mst[i] = aT

        def mback(i):
            ex, c0, n = mitems[i]
            wg, wu, wd = wts[ex]
            aT = mst.pop(i)
            for tl in range(n // 128):
                j = c0 // 128 + tl
                for hf in range(2):
                    y = yps()
                    for fc in range(4):
                        fw.op("pe", lambda e: e.matmul(y[:], lhsT=aT[:, fc, tl * 128:(tl + 1) * 128], rhs=wd[:, fc, hf * 512:(hf + 1) * 512], start=(fc == 0), stop=(fc == 3)), R=[aT, wd], W=[y])
                    if ex == 0:
                        fw.op("act", lambda e: e.activation(out=Y[:, j, hf * 512:(hf + 1) * 512], in_=y[:], func=AF.Copy), R=[y], W=[Y])
                    else:
                        fw.op("dve", lambda e: e.tensor_tensor(out=Y[:, j, hf * 512:(hf + 1) * 512], in0=y[:], in1=Y[:, j, hf * 512:(hf + 1) * 512], op=ALU.add), R=[y, Y], W=[Y])

        nM = len(mitems)
        for i in range(nM + 1):
            if i < nM:
                mfront(i)
            if i >= 1:
                mback(i - 1)
        for i in range(g0, g1):
            j = i - g0
            xt = xtp()
            fw.dma("sp", xt[:], S["XS"][i * 128:(i + 1) * 128, :], W=[xt])
            if last and i >= 2:
                dst = self.out[(i - 2) * 128:(i - 1) * 128, :]
            else:
                dst = S["XS"][i * 128:(i + 1) * 128, :]
            ys = [(Y, Y[:, j, 0:512]), (Y, Y[:, j, 512:1024])]
            _ln_epilogue(self, ys, xt, m5[1 if i < 2 else 0], gbc, bbc, pools, dst)
    self.end("moe%d" % l)


KB.ph_moe = _ph_moe
```

```python
import contextlib
import math
import numpy as np
import ml_dtypes
import concourse.bass as bass
import concourse.mybir as mybir
from concourse.bass_utils import run_bass_kernel_spmd

F32 = mybir.dt.float32
BF16 = mybir.dt.bfloat16
AF = mybir.ActivationFunctionType
ALU = mybir.AluOpType
AX = mybir.AxisListType

D = 1024
LC = 256
LL = 4096
T = LC + LL
NT = T // 128
DEPTH = 4
D_IN = 7712
ALPHA = (2 * DEPTH) ** 0.25
LN_EPS = 1e-5
RMS_EPS = 1e-6
O_GQ, O_GK, O_GV, O_GG, O_GR = 0, 256, 512, 1024, 1536
O_DQ, O_DK, O_DV = 1568, 2080, 2592
O_NQ, O_NK, O_NV = 3104, 3616, 4128
O_GT = 4640


class Buf:
    __slots__ = ("name", "t", "last_write", "reads", "excl")

    def __init__(self, name, t=None, excl=False):
        self.name = name
        self.t = t
        self.last_write = None
        self.reads = {}
        self.excl = excl

    def __getitem__(self, key):
        return self.t[key]


class _Op:
    __slots__ = ("fn", "waits", "tok", "signal", "val")

    def __init__(self, fn):
        self.fn = fn
        self.waits = []
        self.tok = None
        self.signal = False
        self.val = None


class _Rec:
    call = None

    def __getattr__(self, name):
        def f(*a, **k):
            self.call = (name, a, k)
            return self
        return f


class _Eng:
    def __init__(self, name):
        self.name = name
        self.ops = []
        self.seen = {}
        self.is_pe = name == "pe"
        self.sem = None
        self.dsems = []
        self.dcount = []
        self.drr = 0
        self.emitted = 0
        self.sigcount = 0


class FW:
    ENGS = ("pe", "act", "dve", "pool", "sp")

    def __init__(self, nc, st, n_dma_sems=16):
        self.nc = nc
        self.e = {n: _Eng(n) for n in self.ENGS}
        self.n_dma_sems = n_dma_sems
        for en in self.ENGS:
            E = self.e[en]
            E.sem = st.enter_context(nc.semaphore("s_" + en))
            if en in ("sp", "pool", "act"):
                E.dcount = [0] * n_dma_sems
                E.dsems = [st.enter_context(nc.semaphore("d_%s_%d" % (en, i))) for i in range(n_dma_sems)]

    def _deps(self, E, reads, writes):
        deps = {}

        def add(tok):
            if tok is None:
                return
            if tok[0] == "c":
                if E.is_pe and tok[1] == "pe":
                    return
                key = ("c", tok[1])
            else:
                key = ("d", tok[1])
            if key not in deps or deps[key][2] < tok[2]:
                deps[key] = tok

        for b in reads:
            add(b.last_write)
            if b.excl:
                for t in b.reads.values():
                    add(t)
        for b in writes:
            add(b.last_write)
            for t in b.reads.values():
                add(t)
        out = []
        for key, tok in deps.items():
            if E.seen.get(key, -1) >= tok[2]:
                continue
            E.seen[key] = tok[2]
            out.append(tok)
        return out

    def _commit(self, tok, reads, writes):
        for b in writes:
            b.last_write = tok
            b.reads = {}
        for b in reads:
            if b.excl:
                b.last_write = tok
                b.reads = {}
            else:
                b.reads[(tok[0], tok[1])] = tok

    def op(self, eng, fn, R=(), W=()):
        E = self.e[eng]
        rec = _Rec()
        fn(rec)
        call = rec.call
        o = _Op(lambda e, c=call: getattr(e, c[0])(*c[1], **c[2]))
        R = [b for b in R if b is not None]
        W = [b for b in W if b is not None]
        o.waits = self._deps(E, R, W)
        o.tok = ("c", eng, len(E.ops))
        E.ops.append(o)
        self._commit(o.tok, R, W)
        return o

    def dma(self, eng, out, in_, R=(), W=(), **kw):
        E = self.e[eng]
        o = _Op(lambda e: e.dma_start(out=out, in_=in_, **kw))
        R = [b for b in R if b is not None]
        W = [b for b in W if b is not None]
        o.waits = self._deps(E, R, W)
        i = E.drr
        E.drr = (E.drr + 1) % self.n_dma_sems
        key = ("d", (eng, i))
        prev = E.dcount[i]
        if prev > 0 and E.seen.get(key, -1) < prev:
            E.seen[key] = prev
            o.waits.append(("d", (eng, i), prev))
        E.dcount[i] = prev + 16
        o.tok = ("d", (eng, i), prev + 16)
        E.ops.append(o)
        self._commit(o.tok, R, W)
        return o

    def phase_end(self):
        nc = self.nc
        E = self.e["sp"]
        o = _Op(None)
        for en in self.ENGS:
            Q = self.e[en]
            for i, c in enumerate(Q.dcount):
                if c > 0 and E.seen.get(("d", (en, i)), -1) < c:
                    o.waits.append(("d", (en, i), c))
            if en != "sp":
                for q in reversed(Q.ops[Q.emitted:]):
                    if q.tok[0] == "c" and q.fn is not None:
                        o.waits.append(q.tok)
                        break
        o.tok = ("c", "sp", len(E.ops))
        E.ops.append(o)
        for en in self.ENGS:
            Q = self.e[en]
            for q in Q.ops[Q.emitted:]:
                for w in q.waits:
                    if w[0] == "c":
                        P = self.e[w[1]]
                        assert w[2] >= P.emitted, "wait on op of an earlier phase"
                        P.ops[w[2]].signal = True
        for en in self.ENGS:
            Q = self.e[en]
            for q in Q.ops[Q.emitted:]:
                if q.tok[0] == "c" and q.signal:
                    assert q.fn is not None
                    Q.sigcount += 1
                    q.val = Q.sigcount

        with nc.Block() as block:
            def run(en, eng):
                Q = self.e[en]
                for q in Q.ops[Q.emitted:]:
                    for w in q.waits:
                        if w[0] == "c":
                            P = self.e[w[1]]
                            eng.wait_ge(P.sem, P.ops[w[2]].val)
                        else:
                            qn, i = w[1]
                            eng.wait_ge(self.e[qn].dsems[i], w[2])
                    if q.fn is None:
                        continue
                    ins = q.fn(eng)
                    if q.tok[0] == "d":
                        qn, i = q.tok[1]
                        ins.then_inc(self.e[qn].dsems[i], 16)
                    elif q.signal:
                        ins.then_inc(Q.sem, 1)

            @block.tensor
            def _(eng):
                run("pe", eng)

            @block.scalar
            def _(eng):
                run("act", eng)

            @block.vector
            def _(eng):
                run("dve", eng)

            @block.gpsimd
            def _(eng):
                run("pool", eng)

            @block.sync
            def _(eng):
                run("sp", eng)

        nc.all_engine_barrier()
        nops = {en: len(self.e[en].ops) - self.e[en].emitted for en in self.ENGS}
        for en in self.ENGS:
            Q = self.e[en]
            Q.emitted = len(Q.ops)
        for en in self.ENGS:
            Q = self.e[en]
            for pn in self.ENGS:
                P = self.e[pn]
                Q.seen[("c", pn)] = len(P.ops) - 1
                for i, c in enumerate(P.dcount):
                    Q.seen[("d", (pn, i))] = c
        return nops


def _consts():
    c = {}
    c["ident"] = np.eye(128, dtype=np.float32)
    n = np.arange(LL)
    row = (n // 64).astype(np.float32)
    col = (n % 64).astype(np.float32)
    nf = 16
    inv = (10000.0 ** (-np.arange(nf, dtype=np.float32) / nf)).astype(np.float32)
    ang = np.concatenate([row[:, None] * inv, col[:, None] * inv], -1).astype(np.float32)
    cos = np.cos(ang).astype(np.float32)
    sin = np.sin(ang).astype(np.float32)
    ct = np.ones((128, T), np.float32)
    stb = np.zeros((128, T), np.float32)
    for p in range(128):
        j = p % 64
        f = j % 32
        ct[p, LC:] = cos[:, f]
        stb[p, LC:] = (-sin[:, f]) if j < 32 else sin[:, f]
    c["rope_c"] = ct
    c["rope_s"] = stb
    s = np.arange(128)[:, None]
    t = np.arange(128)[None, :]
    le = (s <= t).astype(np.float32)
    gt = (s > t).astype(np.float32)
    tri = np.stack([le, le.T, gt, gt.T]).astype(np.float32)
    c["tri"] = tri
    c["mask4"] = np.stack([np.tile(le, (1, 4)), np.tile(le.T, (1, 4))]).astype(np.float32)
    J = np.zeros((64, 128), np.float32)
    for m in range(64):
        J[m, 63 - m] = 1.0
        J[m, 64 + 63 - m] = 1.0
    c["j64"] = J
    cc = np.arange(64)
    cs = np.clip(cc - 8, 0, 48)
    W = ((cc[:, None] >= cs[None, :]) & (cc[:, None] < cs[None, :] + 16)).astype(np.float32)
    W2 = np.concatenate([W, W], 0)
    c["nawin"] = np.tile(W2[:, None, :], (1, 8, 1)).reshape(128, 512).astype(np.float32)
    sel = np.zeros((16, 16, 128), np.float32)
    for e in range(16):
        sel[e, e, :] = 1.0
    c["sel"] = sel
    return c


CONST_SHAPES = {
    "ident": [128, 128], "rope_c": [128, T], "rope_s": [128, T], "tri": [4, 128, 128],
    "mask4": [2, 128, 512], "j64": [64, 128], "nawin": [128, 512], "sel": [16, 16, 128],
}

INPUT_SHAPES = {
    "x": [LL, D], "c": [D], "ctx": [LC, D], "c_ctx": [D],
    "w_ada": [DEPTH, D, 6 * D], "b_ada": [DEPTH, 6 * D], "w_in": [DEPTH, D, D_IN],
    "gla_w_decay": [DEPTH, 2, 16, 256], "gla_b_decay": [DEPTH, 2, 256], "gla_norm_g": [DEPTH, 128],
    "diff_lam_q": [DEPTH, 2, 64], "diff_lam_k": [DEPTH, 2, 64], "diff_norm_g": [DEPTH, 128],
    "na_rpb": [DEPTH, 8, 15, 31], "w_branch": [DEPTH, 3, 512, D], "w_o": [DEPTH, D, D],
    "ln_g": [DEPTH, 2, D], "ln_b": [DEPTH, 2, D], "w_router": [D, 16], "b_router": [16],
    "w_exp_gate": [DEPTH, 16, D, 512], "w_exp_up": [DEPTH, 16, D, 512], "w_exp_down": [DEPTH, 16, 512, D],
}


class KB:
    def __init__(self, nlayers=DEPTH, debug=(), stop_after=None):
        self.nlayers = nlayers
        self.debug = set(debug)
        self.stop_after = stop_after
        self.nc = bass.Bass("TRN2", target_bir_lowering=False)
        nc = self.nc
        self.I = {k: nc.dram_tensor(k, v, F32, kind="ExternalInput").ap() for k, v in INPUT_SHAPES.items()}
        self.C = {k: nc.dram_tensor("k_" + k, v, F32, kind="ExternalInput").ap() for k, v in CONST_SHAPES.items()}
        self.out = nc.dram_tensor("out", [LL, D], F32, kind="ExternalOutput").ap()
        self.S = {}
        self.uid = 0

    def scratch(self, name, shape, dt):
        kind = "ExternalOutput" if (name in self.debug or name == "TAB") else "Internal"
        self.S[name] = self.nc.dram_tensor("s_" + name, shape, dt, kind=kind).ap()
        return self.S[name]

    def begin(self):
        self.pst = contextlib.ExitStack()
        self.pst.__enter__()
        self._cc = {}

    def warm(self, funcs):
        return
        fw = self.fw
        w = self.sb("warm", [128, 2048], F32)
        fw.op("dve", lambda e: e.memset(w[:], 1.0), W=[w])
        for f in funcs:
            for _ in range(2):
                fw.op("act", lambda e: e.activation(out=w[:], in_=w[:], func=f), R=[w], W=[w])
            fw.op("dve", lambda e: e.memset(w[:], 1.0), W=[w])

    def const_col(self, val):
        if val not in self._cc:
            t = self.sb("cc", [128, 1], F32)
            self.fw.op("dve", lambda e: e.memset(t[:], float(val)), W=[t])
            self._cc[val] = t
        return self._cc[val]

    def end(self, name=""):
        nops = self.fw.phase_end()
        self.pst.__exit__(None, None, None)
        print("phase", name, nops, flush=True)

    def sb(self, name, shape, dt):
        self.uid += 1
        return Buf(name, self.pst.enter_context(self.nc.sbuf_tensor("%s_%d" % (name, self.uid), shape, dt)))

    def ps(self, name, shape, dt=F32):
        self.uid += 1
        return Buf(name, self.pst.enter_context(self.nc.psum_tensor("%s_%d" % (name, self.uid), shape, dt)),
                   excl=True)

    def pool(self, name, shape, dt, n, psum=False):
        bufs = [(self.ps if psum else self.sb)(name + str(i), shape, dt) for i in range(n)]
        state = {"i": 0}

        def nxt():
            b = bufs[state["i"] % n]
            state["i"] += 1
            return b
        return nxt

    def build(self):
        nc = self.nc
        with contextlib.ExitStack() as gst:
            self.fw = FW(nc, gst)
            self.alloc_scratch()
            self.ph_mod()
            if self.stop_after == "mod":
                return nc
            for l in range(self.nlayers):
                self.ph_proj(l)
                if self.stop_after in ("proj", "ht"):
                    break
                self.ph_gla(l)
                self.ph_glac(l)
                if self.stop_after == "gla":
                    break
                self.ph_diff(l)
                if self.stop_after == "diff":
                    break
                self.ph_na(l)
                if self.stop_after in ("na", "natab"):
                    break
                self.ph_merge(l)
                if self.stop_after == "merge":
                    break
                self.ph_moe(l, last=(l == self.nlayers - 1))
        return nc

    def alloc_scratch(self):
        s = self.scratch
        s("XS", [T, D], F32)
        s("MODF", [DEPTH, 128, 48, 2], F32)
        s("MODR", [DEPTH, 2, 2, D], F32)
        s("GQT", [4, 64, T], BF16)
        s("GKT", [4, 64, T], BF16)
        s("GK", [T, 256], BF16)
        s("GV", [T, 512], BF16)
        s("GG", [512, T], BF16)
        s("GR", [64, T], BF16)
        s("DQT", [4, 128, T], BF16)
        s("DKT", [4, 128, T], BF16)
        s("DV", [T, 512], BF16)
        s("NQT", [4, 128, T], BF16)
        s("NKT", [4, 128, T], BF16)
        s("NV", [T, 512], BF16)
        s("GT", [3, D, T], BF16)
        s("OG", [2, 4, 128, T], F32)
        s("BR", [3, 512, T], BF16)
        s("NAG", [120, 128], F32)

    def ph_mod(self):
        fw, I = self.fw, self.I
        self.begin()
        sc = self.sb("sc", [128, 8, 2], F32)
        ones = self.sb("ones", [1, 128], F32)
        idf, idb = self.load_ident()
        sc0 = self.sb("sc0", [128, 8], F32)
        sc1 = self.sb("sc1", [128, 8], F32)
        self.diag_cols(sc0, sc0[:], I["c"].rearrange("(o n) -> o n", o=1), 8, idf)
        self.diag_cols(sc1, sc1[:], I["c_ctx"].rearrange("(o n) -> o n", o=1), 8, idf)
        fw.op("dve", lambda e: e.tensor_copy(out=sc[:, :, 0], in_=sc0[:]), R=[sc0], W=[sc])
        fw.op("dve", lambda e: e.tensor_copy(out=sc[:, :, 1], in_=sc1[:]), R=[sc1], W=[sc])
        fw.op("act", lambda e: e.activation(out=sc[:], in_=sc[:], func=AF.Silu), R=[sc], W=[sc])
        fw.op("dve", lambda e: e.memset(ones[:], 1.0), W=[ones])
        if "DBG1" in self.debug:
            self.scratch("DBG1", [128, 16], F32)
            self.scratch("DBG2", [1, 6 * D], F32)
            fw.dma("pool", self.S["DBG1"], sc[:].rearrange("p k s -> p (k s)"), R=[sc])
        wpool = self.pool("wada", [128, 8, 512], F32, 2)
        psf = self.pool("psf", [128, 4, 2], F32, 2, psum=True)
        psr = self.pool("psr", [2, 512], F32, 2, psum=True)
        for l in range(DEPTH):
            bfm = self.sb("bfm", [128, 48], F32)
            brow = self.sb("brow", [2, 2, D], F32)
            modf = self.sb("modf", [128, 48, 2], F32)
            modr = self.sb("modr", [2, 2, D], F32)
            fw.op("dve", lambda e: e.memset(modf[:], 0.0), W=[modf])
            self.diag_cols(bfm, bfm[:], I["b_ada"][l:l + 1, :], 48, idf)
            for jj, j in enumerate((2, 5)):
                fw.dma("sp", brow[:, jj, :], I["b_ada"][l:l + 1, j * D:(j + 1) * D].partition_broadcast(2), W=[brow])
            for j in (1, 4):
                fw.op("dve", lambda e, j=j: e.tensor_scalar_add(out=bfm[:, j * 8:(j + 1) * 8], in0=bfm[:, j * 8:(j + 1) * 8], scalar1=1.0), R=[bfm], W=[bfm])
            for jn in range(12):
                j = jn // 2
                w = wpool()
                fw.dma("sp", w[:], I["w_ada"][l, :, jn * 512:(jn + 1) * 512].rearrange("(kc p) n -> p kc n", p=128), W=[w])
                if j in (2, 5):
                    p = psr()
                    for kc in range(8):
                        fw.op("pe", lambda e: e.matmul(p[:], lhsT=sc[:, kc, :], rhs=w[:, kc, :], start=(kc == 0), stop=(kc == 7)),
                              R=[sc, w], W=[p])
                    jj = 0 if j == 2 else 1
                    half = jn % 2
                    fw.op("dve", lambda e: e.tensor_tensor(out=modr[:, jj, half * 512:(half + 1) * 512], in0=p[:], in1=brow[:, jj, half * 512:(half + 1) * 512], op=ALU.add),
                          R=[p, brow], W=[modr])
                else:
                    p = psf()
                    for sub in range(4):
                        for kc in range(8):
                            fw.op("pe", lambda e: e.matmul(p[:, sub, :], lhsT=w[:, kc, sub * 128:(sub + 1) * 128], rhs=sc[:, kc, :], start=(kc == 0), stop=(kc == 7)),
                                  R=[sc, w], W=[p])
                    for s_ in range(2):
                        fw.op("dve", lambda e: e.tensor_tensor(out=modf[:, jn * 4:(jn + 1) * 4, s_], in0=p[:, :, s_], in1=bfm[:, jn * 4:(jn + 1) * 4], op=ALU.add),
                              R=[p, bfm], W=[modf])
            fw.dma("pool", self.S["MODF"][l], modf[:], R=[modf])
            fw.dma("pool", self.S["MODR"][l].rearrange("s j d -> s (j d)"), modr[:].rearrange("s j d -> s (j d)"), R=[modr])
        self.end("mod")

    def diag_cols(self, dstbuf, dst, src_row_ap, nch, idf):
        fw = self.fw
        bc = self.sb("dgbc", [128, nch, 128], F32)
        fw.dma("sp", bc[:].rearrange("p k j -> p (k j)"), src_row_ap.partition_broadcast(128), W=[bc])
        for k in range(nch):
            fw.op("dve", lambda e: e.tensor_tensor(out=bc[:, k, :], in0=bc[:, k, :], in1=idf[:], op=ALU.mult), R=[bc, idf], W=[bc])
        fw.op("dve", lambda e: e.reduce_sum(out=dst, in_=bc[:], axis=AX.X), R=[bc], W=[dstbuf])

    def x_src(self, l, i):
        if l == 0:
            if i < 2:
                return self.I["ctx"][i * 128:(i + 1) * 128, :]
            return self.I["x"][(i - 2) * 128:(i - 1) * 128, :]
        return self.S["XS"][i * 128:(i + 1) * 128, :]

    def load_ident(self):
        fw = self.fw
        idf = self.sb("idf", [128, 128], F32)
        idb = self.sb("idb", [128, 128], BF16)
        fw.dma("sp", idf[:], self.C["ident"][:, :], W=[idf])
        fw.op("dve", lambda e: e.tensor_copy(out=idb[:], in_=idf[:]), R=[idf], W=[idb])
        return idf, idb

    def ph_proj(self, l):
        fw, I, S, C = self.fw, self.I, self.S, self.C
        self.begin()
        idf, idb = self.load_ident()
        modf = self.sb("modf", [128, 48, 2], F32)
        fw.dma("sp", modf[:], S["MODF"][l], W=[modf])
        hT = self.sb("hT", [128, 8, T], BF16)
        xp = self.pool("xt", [128, D], F32, 2)
        xbp = self.pool("xb", [128, D], BF16, 2)
        ptr = self.pool("ptr", [128, 8, 128], BF16, 2, psum=True)
        for i in range(NT):
            xt = xp()
            xb = xbp()
            fw.dma("sp", xt[:], self.x_src(l, i), W=[xt])
            fw.op("dve", lambda e, xt=xt, xb=xb: e.tensor_copy(out=xb[:], in_=xt[:]), R=[xt], W=[xb])
            p = ptr()
            for kc in range(8):
                fw.op("pe", lambda e, p=p, xb=xb, kc=kc: e.transpose(out=p[:, kc, :], in_=xb[:, kc * 128:(kc + 1) * 128], identity=idb[:]),
                      R=[xb, idb], W=[p])
            st = 1 if i < 2 else 0
            for kc in range(8):
                fw.op("act", lambda e, p=p, kc=kc, i=i, st=st: e.activation(
                    out=hT[:, kc, i * 128:(i + 1) * 128], in_=p[:, kc, :], func=AF.Identity,
                    bias=modf[:, kc, st:st + 1], scale=modf[:, 8 + kc, st:st + 1]), R=[p, modf], W=[hT])
        if "HT" in self.debug:
            self.scratch("HT", [128, 8, T], BF16)
            fw.dma("pool", S["HT"], hT[:], R=[hT])
        if self.stop_after == "ht":
            self.end("ht")
            return
        rc = self.sb("rc", [128, T], F32)
        rs = self.sb("rs", [128, T], F32)
        fw.dma("sp", rc[:], C["rope_c"][:, :], W=[rc])
        fw.dma("sp", rs[:], C["rope_s"][:, :], W=[rs])

        wfp = self.pool("wf", [128, 8, 256], F32, 2)
        wbp = self.pool("wb", [128, 8, 256], BF16, 3)
        pmm = self.pool("pmm", [128, 512], F32, 4, psum=True)
        stg = self.pool("stg", [128, T], BF16, 2)
        tmp = self.pool("tmp", [128, 512], F32, 3)
        tstg = self.pool("tstg", [128, 256], BF16, 3)
        win = I["w_in"][l]
        chunks = [(0, 256)] + [(256 + 512 * j, 512) for j in range(8)]
        self._cast_i = 0

        def load_w(pieces, ncols, zero=False):
            wf = wfp()
            wb = wbp()
            if zero:
                fw.op("dve", lambda e, wf=wf: e.memset(wf[:], 0.0), W=[wf])
            for (dc, scol, n) in pieces:
                fw.dma("sp", wf[:, :, dc:dc + n], win[:, scol:scol + n].rearrange("(kc p) n -> p kc n", p=128), W=[wf])
            eng = "dve" if (self._cast_i % 2 == 0) else "act"
            self._cast_i += 1
            if eng == "act":
                fw.op("act", lambda e: e.activation(out=wb[:, :, 0:ncols], in_=wf[:, :, 0:ncols], func=AF.Copy), R=[wf], W=[wb])
            else:
                fw.op("dve", lambda e: e.tensor_copy(out=wb[:, :, 0:ncols], in_=wf[:, :, 0:ncols]), R=[wf], W=[wb])
            return wb

        def fm_mm(wb, m, c0, cn, col0=0):
            p = pmm()
            for kc in range(8):
                fw.op("pe", lambda e, p=p, wb=wb, kc=kc: e.matmul(p[0:m, 0:cn], lhsT=wb[:, kc, col0:col0 + m], rhs=hT[:, kc, c0:c0 + cn], start=(kc == 0), stop=(kc == 7)),
                      R=[wb, hT], W=[p])
            return p

        def fm_group(pieces, m, evac, dsts, zero=False):
            wb = load_w(pieces, m, zero)
            sg = stg()
            for (c0, cn) in chunks:
                p = fm_mm(wb, m, c0, cn)
                evac(p, sg, c0, cn, m)
            for (r0, nr, dst) in dsts:
                fw.dma("pool", dst, sg[r0:r0 + nr, :], R=[sg])

        def ev_copy(p, sg, c0, cn, m):
            fw.op("act", lambda e: e.activation(out=sg[0:m, c0:c0 + cn], in_=p[0:m, 0:cn], func=AF.Copy), R=[p], W=[sg])

        def ev_silu(p, sg, c0, cn, m):
            fw.op("act", lambda e: e.activation(out=sg[0:m, c0:c0 + cn], in_=p[0:m, 0:cn], func=AF.Silu), R=[p], W=[sg])

        def ev_sigm(p, sg, c0, cn, m):
            fw.op("act", lambda e: e.activation(out=sg[0:m, c0:c0 + cn], in_=p[0:m, 0:cn], func=AF.Sigmoid), R=[p], W=[sg])

        for (o0, dst) in ((O_GQ, S["GQT"]), (O_GK, S["GKT"])):
            for g in range(2):
                fm_group([(0, o0 + g * 128, 128)], 128, ev_copy,
                         [(0, 64, dst[2 * g]), (64, 64, dst[2 * g + 1])])
        for g in range(4):
            fm_group([(0, O_GG + g * 128, 128)], 128, ev_silu, [(0, 128, S["GG"][g * 128:(g + 1) * 128, :])])
        fm_group([(0, O_GR, 16), (32, O_GR + 16, 16)], 64, ev_copy, [(0, 64, S["GR"])], zero=True)
        for (o0, dst) in ((O_DQ, S["DQT"]), (O_DK, S["DKT"])):
            for h in range(4):
                b0 = o0 + h * 128
                wa = load_w([(0, b0, 128)], 128)
                wsw = load_w([(0, b0 + 32, 32), (32, b0, 32), (64, b0 + 96, 32), (96, b0 + 64, 32)], 128)
                sg = stg()
                for (c0, cn) in chunks:
                    pa = fm_mm(wa, 128, c0, cn)
                    pb = fm_mm(wsw, 128, c0, cn)
                    t1 = tmp()
                    t2 = tmp()
                    fw.op("dve", lambda e, pa=pa, t1=t1, c0=c0, cn=cn: e.tensor_tensor(out=t1[:, 0:cn], in0=pa[:, 0:cn], in1=rc[:, c0:c0 + cn], op=ALU.mult), R=[pa, rc], W=[t1])
                    fw.op("dve", lambda e, pb=pb, t2=t2, c0=c0, cn=cn: e.tensor_tensor(out=t2[:, 0:cn], in0=pb[:, 0:cn], in1=rs[:, c0:c0 + cn], op=ALU.mult), R=[pb, rs], W=[t2])
                    fw.op("dve", lambda e, t1=t1, t2=t2, sg=sg, c0=c0, cn=cn: e.tensor_tensor(out=sg[:, c0:c0 + cn], in0=t1[:, 0:cn], in1=t2[:, 0:cn], op=ALU.add), R=[t1, t2], W=[sg])
                fw.dma("pool", dst[h], sg[:, :], R=[sg])
        for (o0, dst) in ((O_NQ, S["NQT"]), (O_NK, S["NKT"])):
            for g in range(4):
                fm_group([(0, o0 + g * 128, 128)], 128, ev_copy, [(0, 128, dst[g])])
        for n in range(3):
            for g in range(8):
                fm_group([(0, O_GT + n * D + g * 128, 128)], 128, ev_sigm, [(0, 128, S["GT"][n, g * 128:(g + 1) * 128, :])])
        for (o0, ncols, dst) in ((O_GK, 256, S["GK"]), (O_GV, 512, S["GV"]), (O_DV, 512, S["DV"]), (O_NV, 512, S["NV"])):
            for cg in range(ncols // 256):
                wb = load_w([(0, o0 + cg * 256, 256)], 256)
                for i in range(NT):
                    p = pmm()
                    for kc in range(8):
                        fw.op("pe", lambda e, p=p, wb=wb, kc=kc, i=i: e.matmul(p[:, 0:256], lhsT=hT[:, kc, i * 128:(i + 1) * 128], rhs=wb[:, kc, :], start=(kc == 0), stop=(kc == 7)),
                              R=[wb, hT], W=[p])
                    ts = tstg()
                    eng = "act" if i % 2 == 0 else "dve"
                    if eng == "act":
                        fw.op("act", lambda e, p=p, ts=ts: e.activation(out=ts[:], in_=p[:, 0:256], func=AF.Copy), R=[p], W=[ts])
                    else:
                        fw.op("dve", lambda e, p=p, ts=ts: e.tensor_copy(out=ts[:], in_=p[:, 0:256]), R=[p], W=[ts])
                    fw.dma("pool", dst[i * 128:(i + 1) * 128, cg * 256:(cg + 1) * 256], ts[:], R=[ts])
        self.end("proj%d" % l)


def build_program(nlayers=DEPTH, debug=(), stop_after=None):
    kb = KB(nlayers, debug, stop_after)
    kb.build()
    return kb


_CONSTS = None


def make_in_maps(inputs):
    global _CONSTS
    if _CONSTS is None:
        _CONSTS = _consts()
    f = lambda a: np.ascontiguousarray(np.asarray(a, dtype=np.float32))
    shared = {k: f(inputs[k]) for k in INPUT_SHAPES if k not in ("x", "c", "ctx")}
    for k, v in _CONSTS.items():
        shared["k_" + k] = v
    maps = []
    for b in range(8):
        m = dict(shared)
        m["x"] = f(inputs["x"][b])
        m["c"] = f(inputs["c"][b])
        m["ctx"] = f(inputs["ctx"][b])
        maps.append(m)
    return maps


def kernel(**inputs):
    kb = build_program()
    maps = make_in_maps(inputs)
    res = run_bass_kernel_spmd(kb.nc, maps, core_ids=list(range(8)))
    return np.stack([np.asarray(r["out"], dtype=np.float32) for r in res.results], axis=0)


def _ph_gla(self, l):
    fw, I, S, C = self.fw, self.I, self.S, self.C
    self.begin()
    self.warm([AF.Exp, AF.Ln])
    qT = self.sb("qT", [64, 4, T], BF16)
    kT = self.sb("kT", [64, 4, T], BF16)
    kk = self.sb("kk", [128, NT, 256], BF16)
    vv = self.sb("vv", [128, NT, 512], BF16)
    rT = self.sb("rT", [64, T], BF16)
    fw.dma("sp", qT[:], S["GQT"].rearrange("h d t -> d h t"), W=[qT])
    fw.dma("sp", kT[:], S["GKT"].rearrange("h d t -> d h t"), W=[kT])
    fw.dma("sp", kk[:], S["GK"].rearrange("(i p) n -> p i n", p=128), W=[kk])
    fw.dma("sp", vv[:], S["GV"].rearrange("(i p) n -> p i n", p=128), W=[vv])
    fw.dma("sp", rT[:], S["GR"][:, :], W=[rT])
    trif = self.sb("trif", [128, 4, 128], F32)
    trib = self.sb("trib", [128, 4, 128], BF16)
    fw.dma("sp", trif[:], C["tri"].rearrange("a s t -> s a t"), W=[trif])
    fw.op("act", lambda e: e.mul(out=trib[:], in_=trif[:], mul=-1.0 / 16.0), R=[trif], W=[trib])
    maskf = self.sb("maskf", [128, 2, 512], F32)
    fw.dma("sp", maskf[:], C["mask4"].rearrange("a s t -> s a t"), W=[maskf])
    wdf = self.sb("wdf", [64, 256], F32)
    wd = self.sb("wd", [64, 256], BF16)
    fw.op("dve", lambda e: e.memset(wdf[:], 0.0), W=[wdf])
    fw.dma("sp", wdf[0:16, :], I["gla_w_decay"][l, 0], W=[wdf])
    fw.dma("sp", wdf[32:48, :], I["gla_w_decay"][l, 1], W=[wdf])
    fw.op("dve", lambda e: e.tensor_copy(out=wd[:], in_=wdf[:]), R=[wdf], W=[wd])
    bdb = self.sb("bdb", [128, 2, 256], F32)
    fw.dma("sp", bdb[:].rearrange("p a n -> p (a n)"), I["gla_b_decay"][l:l + 1].rearrange("o a n -> o (a n)").partition_broadcast(128), W=[bdb])
    zbp = self.pool("zb", [128, 256], F32, 2)

    Sf = self.sb("Sf", [64, 4, 128], F32)
    Sb = self.sb("Sb", [64, 4, 128], BF16)
    zps = self.pool("zps", [128, 256], F32, 1, psum=True)
    cps = self.pool("cps", [64, 4, 128], F32, 1, psum=True)
    gps = self.pool("gps", [128, 256], F32, 1, psum=True)
    aps = self.pool("aps", [128, 4, 128], F32, 2, psum=True)
    ops_ = self.pool("ops", [128, 4, 128], F32, 2, psum=True)
    kvps = self.pool("kvps", [64, 4, 128], F32, 1, psum=True)
    e1p = self.pool("e1", [128, 256], F32, 2)
    Lbp = self.pool("Lb", [128, 256], BF16, 2)
    Eqp = self.pool("Eq", [64, 4, 128], F32, 2)
    Ekp = self.pool("Ek", [64, 4, 128], F32, 2)
    Estp = self.pool("Est", [128, 256], F32, 2)
    qinp = self.pool("qin", [64, 4, 128], BF16, 2)
    kinp = self.pool("kin", [64, 4, 128], BF16, 2)
    kstp = self.pool("kst", [128, 256], BF16, 2)
    attp = self.pool("att", [128, 4, 128], BF16, 2)
    osbp = self.pool("osb", [128, 4, 128], F32, 2)

    Sf1 = self.sb("Sf1", [64, 4, 128], F32)
    Sb1 = self.sb("Sb1", [64, 4, 128], BF16)
    Sfs, Sbs = [Sf, Sf1], [Sb, Sb1]
    for S_ in Sfs + Sbs:
        fw.op("dve", lambda e: e.memset(S_[:], 0.0), W=[S_])
    orders = [list(range(NT)), [1, 0] + list(range(NT - 1, 1, -1))]
    seq = [(dr_, orders[dr_][step]) for step in range(NT) for dr_ in range(2)]
    for (dr, ci_) in seq:
        Sf, Sb = Sfs[dr], Sbs[dr]
        order = [ci_]
        r0 = 0 if dr == 0 else 32
        iu, ig, im = (0, 2, 0) if dr == 0 else (1, 3, 1)
        lastcol = 127 if dr == 0 else 0
        for ci in order:
            c0 = ci * 128
            z = zps()
            fw.op("pe", lambda e: e.matmul(z[:], lhsT=rT[r0:r0 + 16, c0:c0 + 128], rhs=wd[r0:r0 + 16, :], start=True, stop=True), R=[rT, wd], W=[z])
            zb = zbp()
            fw.op("dve", lambda e: e.tensor_tensor(out=zb[:], in0=z[:], in1=bdb[:, dr, :], op=ALU.add), R=[z, bdb], W=[zb])
            e1 = e1p()
            Lb = Lbp()
            fw.op("act", lambda e: e.activation(out=e1[:], in_=zb[:], func=AF.Exp, scale=-1.0), R=[zb], W=[e1])
            fw.op("act", lambda e: e.activation(out=Lb[:], in_=e1[:], func=AF.Ln, bias=1.0), R=[e1], W=[Lb])
            cp = cps()
            for h in range(4):
                fw.op("pe", lambda e, cp=cp, Lb=Lb, h=h: e.matmul(cp[:, h, :], lhsT=Lb[:, h * 64:(h + 1) * 64], rhs=trib[:, iu, :], start=True, stop=True), R=[Lb, trib], W=[cp])
            gp = gps()
            fw.op("pe", lambda e, gp=gp, Lb=Lb: e.matmul(gp[:], lhsT=trib[:, ig, :], rhs=Lb[:], start=True, stop=True), R=[Lb, trib], W=[gp])
            Eq = Eqp(); Ek = Ekp(); Est = Estp()
            fw.op("act", lambda e, cp=cp, Eq=Eq: e.activation(out=Eq[:], in_=cp[:], func=AF.Exp), R=[cp], W=[Eq])
            fw.op("act", lambda e, cp=cp, Ek=Ek: e.activation(out=Ek[:], in_=cp[:], func=AF.Exp, scale=-1.0), R=[cp], W=[Ek])
            fw.op("act", lambda e, gp=gp, Est=Est: e.activation(out=Est[:], in_=gp[:], func=AF.Exp), R=[gp], W=[Est])
            qin = qinp(); kin = kinp(); kst = kstp()
            fw.op("dve", lambda e, qin=qin, Eq=Eq, c0=c0: e.scalar_tensor_tensor(out=qin[:], in0=qT[:, :, c0:c0 + 128], scalar=0.125, in1=Eq[:], op0=ALU.mult, op1=ALU.mult), R=[qT, Eq], W=[qin])
            fw.op("dve", lambda e, kin=kin, Ek=Ek, c0=c0: e.tensor_tensor(out=kin[:], in0=kT[:, :, c0:c0 + 128], in1=Ek[:], op=ALU.mult), R=[kT, Ek], W=[kin])
            fw.op("dve", lambda e, kst=kst, Est=Est, ci=ci: e.tensor_tensor(out=kst[:], in0=kk[:, ci, :], in1=Est[:], op=ALU.mult), R=[kk, Est], W=[kst])
            ap_ = aps()
            for h in range(4):
                fw.op("pe", lambda e, ap_=ap_, kin=kin, qin=qin, h=h: e.matmul(ap_[:, h, :], lhsT=kin[:, h, :], rhs=qin[:, h, :], start=True, stop=True), R=[kin, qin], W=[ap_])
            att = attp()
            fw.op("dve", lambda e, att=att, ap_=ap_: e.tensor_tensor(out=att[:].rearrange("p h t -> p (h t)"), in0=ap_[:].rearrange("p h t -> p (h t)"), in1=maskf[:, im, :], op=ALU.mult), R=[ap_, maskf], W=[att])
            op_ = ops_()
            for h in range(4):
                fw.op("pe", lambda e, op_=op_, att=att, h=h, ci=ci: e.matmul(op_[:, h, :], lhsT=vv[:, ci, h * 128:(h + 1) * 128], rhs=att[:, h, :], start=True, stop=False), R=[vv, att], W=[op_])
                fw.op("pe", lambda e, op_=op_, qin=qin, h=h: e.matmul(op_[:, h, :], lhsT=Sb[:, h, :], rhs=qin[:, h, :], start=False, stop=True), R=[Sb, qin], W=[op_])
            osb = osbp()
            fw.op("act", lambda e, osb=osb, op_=op_: e.activation(out=osb[:], in_=op_[:], func=AF.Copy), R=[op_], W=[osb])
            fw.dma("sp", S["OG"][dr].rearrange("h v t -> v h t")[:, :, c0:c0 + 128], osb[:], R=[osb])
            kv = kvps()
            for h in range(4):
                fw.op("pe", lambda e, kv=kv, kst=kst, h=h, ci=ci: e.matmul(kv[:, h, :], lhsT=kst[:, h * 64:(h + 1) * 64], rhs=vv[:, ci, h * 128:(h + 1) * 128], start=True, stop=True), R=[kst, vv], W=[kv])
            for h in range(4):
                fw.op("dve", lambda e, kv=kv, Eq=Eq, h=h: e.scalar_tensor_tensor(out=Sf[:, h, :], in0=Sf[:, h, :], scalar=Eq[:, h, lastcol:lastcol + 1], in1=kv[:, h, :], op0=ALU.mult, op1=ALU.add), R=[Sf, Eq, kv], W=[Sf])
            fw.op("dve", lambda e: e.tensor_copy(out=Sb[:], in_=Sf[:]), R=[Sf], W=[Sb])
    self.end("gla%d" % l)


def _rms_finish(self, o, n, gcol, extra_scale, pms, onesf, rstdp, tp):
    fw = self.fw
    sq = tp()
    fw.op("dve", lambda e: e.tensor_tensor(out=sq[:, 0:n], in0=o[:, 0:n], in1=o[:, 0:n], op=ALU.mult), R=[o], W=[sq])
    ms = pms()
    fw.op("pe", lambda e: e.matmul(ms[:, 0:n], lhsT=onesf[:], rhs=sq[:, 0:n], start=True, stop=True), R=[onesf, sq], W=[ms])
    rstd = rstdp()
    fw.op("dve", lambda e: e.tensor_scalar_add(out=rstd[:, 0:n], in0=ms[:, 0:n], scalar1=RMS_EPS), R=[ms], W=[rstd])
    fw.op("act", lambda e: e.activation(out=rstd[:, 0:n], in_=rstd[:, 0:n], func=AF.Ln), R=[rstd], W=[rstd])
    fw.op("act", lambda e: e.activation(out=rstd[:, 0:n], in_=rstd[:, 0:n], func=AF.Exp, scale=-0.5), R=[rstd], W=[rstd])
    t = tp()
    fw.op("dve", lambda e: e.tensor_tensor(out=t[:, 0:n], in0=o[:, 0:n], in1=rstd[:, 0:n], op=ALU.mult), R=[o, rstd], W=[t])
    if "DBGC" in self.debug and not getattr(self, "_dbgc_done", False):
        self._dbgc_done = True
        self.scratch("DBGC", [4, 128, 256], F32)
        msb = tp()
        fw.op("dve", lambda e: e.tensor_copy(out=msb[:, 0:n], in_=ms[:, 0:n]), R=[ms], W=[msb])
        for k_, b_ in enumerate((o, sq, msb, rstd)):
            fw.dma("pool", self.S["DBGC"][k_], b_[:, 0:256], R=[b_])
    return t


def _ph_glac(self, l):
    fw, I, S, C = self.fw, self.I, self.S, self.C
    self.begin()
    self.warm([AF.Ln, AF.Exp])
    onesf = self.sb("onesf", [128, 128], F32)
    fw.op("dve", lambda e: e.memset(onesf[:], 1.0 / 128.0), W=[onesf])
    gcol = self.sb("gcol", [128, 1], F32)
    idf, idb = self.load_ident()
    self.diag_cols(gcol, gcol[:], I["gla_norm_g"][l:l + 1, :], 1, idf)
    ofp = self.pool("of", [128, 512], F32, 2)
    obp = self.pool("ob", [128, 512], F32, 2)
    ggp = self.pool("gg", [128, 512], BF16, 2)
    op_ = self.pool("o", [128, 512], F32, 2)
    tp = self.pool("t", [128, 512], F32, 4)
    rstdp = self.pool("rstd", [128, 512], F32, 2)
    pms = self.pool("pms", [128, 512], F32, 2, psum=True)
    outp = self.pool("outb", [128, 512], BF16, 2)
    chunks = [(0, 256)] + [(256 + 512 * j, 512) for j in range(8)]
    for h in range(4):
        for (c0, n) in chunks:
            of = ofp(); ob = obp(); gg = ggp()
            fw.dma("sp", of[:, 0:n], S["OG"][0, h, :, c0:c0 + n], W=[of])
            fw.dma("sp", ob[:, 0:n], S["OG"][1, h, :, c0:c0 + n], W=[ob])
            fw.dma("sp", gg[:, 0:n], S["GG"][h * 128:(h + 1) * 128, c0:c0 + n], W=[gg])
            o = op_()
            fw.op("dve", lambda e, o=o, of=of, ob=ob, n=n: e.tensor_tensor(out=o[:, 0:n], in0=of[:, 0:n], in1=ob[:, 0:n], op=ALU.add), R=[of, ob], W=[o])
            t = _rms_finish(self, o, n, gcol, 1.0, pms, onesf, rstdp, tp)
            ob_ = outp()
            fw.op("dve", lambda e, ob_=ob_, t=t, gg=gg, n=n: e.scalar_tensor_tensor(out=ob_[:, 0:n], in0=t[:, 0:n], scalar=gcol[:, 0:1], in1=gg[:, 0:n], op0=ALU.mult, op1=ALU.mult), R=[t, gcol, gg], W=[ob_])
            fw.dma("pool", S["BR"][0, h * 128:(h + 1) * 128, c0:c0 + n], ob_[:, 0:n], R=[ob_])
    self.end("glac%d" % l)


KB.ph_gla = _ph_gla
KB.ph_glac = _ph_glac


def _ph_diff(self, l):
    fw, I, S, C = self.fw, self.I, self.S, self.C
    lam_init = 0.8 - 0.6 * math.exp(-0.3 * l)
    self.begin()
    self.warm([AF.Exp, AF.Ln])
    KT = self.sb("KT", [128, 4, T], BF16)
    VV = self.sb("VV", [128, NT, 512], BF16)
    fw.dma("sp", KT[:], S["DKT"].rearrange("h d t -> d h t"), W=[KT])
    fw.dma("sp", VV[:], S["DV"].rearrange("(i p) n -> p i n", p=128), W=[VV])
    onesb = self.sb("onesb", [128, 128], BF16)
    fw.op("dve", lambda e: e.memset(onesb[:], 1.0), W=[onesb])
    onesf = self.sb("onesf", [128, 128], F32)
    fw.op("dve", lambda e: e.memset(onesf[:], 1.0 / 128.0), W=[onesf])
    gcol = self.sb("gcol", [128, 1], F32)
    idf, idb = self.load_ident()
    self.diag_cols(gcol, gcol[:], I["diff_norm_g"][l:l + 1, :], 1, idf)
    fw.op("dve", lambda e: e.tensor_scalar_mul(out=gcol[:], in0=gcol[:], scalar1=(1.0 - lam_init)), R=[gcol], W=[gcol])
    lq = self.sb("lq", [128, 128], F32)
    lk = self.sb("lk", [128, 128], F32)
    fw.dma("sp", lq[:], I["diff_lam_q"][l:l + 1].rearrange("o a b -> o (a b)").partition_broadcast(128), W=[lq])
    fw.dma("sp", lk[:], I["diff_lam_k"][l:l + 1].rearrange("o a b -> o (a b)").partition_broadcast(128), W=[lk])
    fw.op("dve", lambda e: e.tensor_tensor(out=lq[:], in0=lq[:], in1=lk[:], op=ALU.mult), R=[lq, lk], W=[lq])
    ls = self.sb("ls", [128, 2], F32)
    fw.op("dve", lambda e: e.reduce_sum(out=ls[:], in_=lq[:].rearrange("p (a b) -> p a b", a=2), axis=AX.X), R=[lq], W=[ls])
    fw.op("act", lambda e: e.activation(out=ls[:], in_=ls[:], func=AF.Exp), R=[ls], W=[ls])
    nlam = self.sb("nlam", [128, 1], F32)
    fw.op("dve", lambda e: e.tensor_tensor(out=nlam[:], in0=ls[:, 1:2], in1=ls[:, 0:1], op=ALU.subtract), R=[ls], W=[nlam])
    fw.op("dve", lambda e: e.tensor_scalar_add(out=nlam[:], in0=nlam[:], scalar1=-lam_init), R=[nlam], W=[nlam])

    qp = self.pool("q", [128, 512], BF16, 3)
    sps = self.pool("sps", [128, 512], F32, 5, psum=True)
    ops_ = self.pool("ops", [128, 512], F32, 2, psum=True)
    dps = self.pool("dps", [128, 512], F32, 1, psum=True)
    pms = dps
    ptp = self.pool("pt", [128, 512], BF16, 10)
    dacp = self.pool("dacc", [128, 512], F32, 4)
    ones1 = self.sb("ones1", [128, 128], F32)
    fw.op("dve", lambda e: e.memset(ones1[:], 1.0), W=[ones1])
    w0p = self.pool("w0", [128, 512], F32, 4)
    rcp = self.pool("rc", [128, 512], F32, 2)
    op_ = self.pool("o", [128, 512], F32, 2)
    tp = self.pool("t", [128, 512], F32, 4)
    rstdp = self.pool("rstd", [128, 512], F32, 2)
    outp = self.pool("outb", [128, 512], BF16, 2)
    chunks = [(0, 256, 2)] + [(256 + 512 * j, 512, NT) for j in range(8)]
    items = []
    for h in range(4):
        for (c0, n, nkt) in chunks:
            for kt in range(nkt):
                items.append((h, c0, n, nkt, kt))
    LA = 2
    st = {}
    cur = {}
    deferred = []

    def front(i):
        h, c0, n, nkt, kt = items[i]
        key = (h, c0)
        if key not in cur:
            q = qp()
            fw.dma("sp", q[:, 0:n], S["DQT"][h, :, c0:c0 + n], W=[q])
            cur[key] = {"q": q, "ws": []}
        q = cur[key]["q"]
        pts = []
        sp2 = [sps(), sps()]
        for comp in range(2):
            pb = comp * 64
            sp_ = sp2[comp]
            fw.op("pe", lambda e: e.matmul(sp_[:, 0:n], lhsT=KT[pb:pb + 64, h, kt * 128:(kt + 1) * 128], rhs=q[pb:pb + 64, 0:n], start=True, stop=True), R=[KT, q], W=[sp_])
        for comp in range(2):
            sp_ = sp2[comp]
            pt = ptp()
            fw.op("act", lambda e: e.activation(out=pt[:, 0:n], in_=sp_[:, 0:n], func=AF.Exp, scale=0.125), R=[sp_], W=[pt])
            pts.append(pt)
        st[i] = pts

    def back(i):
        h, c0, n, nkt, kt = items[i]
        key = (h, c0)
        c = cur[key]
        if kt == 0:
            c["o_ps"] = [ops_(), ops_()]
            c["dacc"] = [dacp(), dacp()]
        pts = st.pop(i)
        for comp in range(2):
            o_ps, dacc, pt = c["o_ps"][comp], c["dacc"][comp], pts[comp]
            fw.op("pe", lambda e: e.matmul(o_ps[:, 0:n], lhsT=VV[:, kt, h * 128:(h + 1) * 128], rhs=pt[:, 0:n], start=(kt == 0), stop=(kt == nkt - 1)), R=[VV, pt], W=[o_ps])
            if kt == 0:
                fw.op("dve", lambda e: e.tensor_copy(out=dacc[:, 0:n], in_=pt[:, 0:n]), R=[pt], W=[dacc])
            else:
                fw.op("dve", lambda e: e.tensor_tensor(out=dacc[:, 0:n], in0=dacc[:, 0:n], in1=pt[:, 0:n], op=ALU.add), R=[dacc, pt], W=[dacc])
        if kt == nkt - 1:
            for comp in range(2):
                o_ps, dacc = c["o_ps"][comp], c["dacc"][comp]
                d_ps = dps()
                fw.op("pe", lambda e: e.matmul(d_ps[:, 0:n], lhsT=ones1[:], rhs=dacc[:, 0:n], start=True, stop=True), R=[ones1, dacc], W=[d_ps])
                rc = rcp()
                fw.op("dve", lambda e: e.reciprocal(out=rc[:, 0:n], in_=d_ps[:, 0:n]), R=[d_ps], W=[rc])
                w = w0p()
                fw.op("dve", lambda e: e.tensor_tensor(out=w[:, 0:n], in0=o_ps[:, 0:n], in1=rc[:, 0:n], op=ALU.mult), R=[o_ps, rc], W=[w])
                c["ws"].append(w)
            if True:
                comp = 1
                ws = c["ws"]
                o = op_()
                fw.op("dve", lambda e: e.scalar_tensor_tensor(out=o[:, 0:n], in0=ws[1][:, 0:n], scalar=nlam[:, 0:1], in1=ws[0][:, 0:n], op0=ALU.mult, op1=ALU.add), R=[ws[0], ws[1], nlam], W=[o])
                sq = tp()
                fw.op("dve", lambda e: e.tensor_tensor(out=sq[:, 0:n], in0=o[:, 0:n], in1=o[:, 0:n], op=ALU.mult), R=[o], W=[sq])

                def tail(o=o, sq=sq, n=n, h=h, c0=c0):
                    ms = pms()
                    fw.op("pe", lambda e: e.matmul(ms[:, 0:n], lhsT=onesf[:], rhs=sq[:, 0:n], start=True, stop=True), R=[onesf, sq], W=[ms])
                    rstd = rstdp()
                    fw.op("dve", lambda e: e.tensor_scalar_add(out=rstd[:, 0:n], in0=ms[:, 0:n], scalar1=RMS_EPS), R=[ms], W=[rstd])
                    fw.op("act", lambda e: e.activation(out=rstd[:, 0:n], in_=rstd[:, 0:n], func=AF.Ln), R=[rstd], W=[rstd])
                    fw.op("act", lambda e: e.activation(out=rstd[:, 0:n], in_=rstd[:, 0:n], func=AF.Exp, scale=-0.5), R=[rstd], W=[rstd])
                    t = tp()
                    fw.op("dve", lambda e: e.tensor_tensor(out=t[:, 0:n], in0=o[:, 0:n], in1=rstd[:, 0:n], op=ALU.mult), R=[o, rstd], W=[t])
                    ob_ = outp()
                    fw.op("act", lambda e: e.activation(out=ob_[:, 0:n], in_=t[:, 0:n], func=AF.Identity, scale=gcol[:, 0:1]), R=[t, gcol], W=[ob_])
                    fw.dma("pool", S["BR"][1, h * 128:(h + 1) * 128, c0:c0 + n], ob_[:, 0:n], R=[ob_])
                deferred.append((i + 4, tail))
                del cur[key]

    nI = len(items)
    for i in range(nI + LA):
        if i < nI:
            front(i)
        if i - LA >= 0:
            back(i - LA)
        while deferred and deferred[0][0] <= i - LA:
            deferred.pop(0)[1]()
    while deferred:
        deferred.pop(0)[1]()
    self.end("diff%d" % l)


KB.ph_diff = _ph_diff


def _ph_na(self, l):
    fw, I, S, C = self.fw, self.I, self.S, self.C
    nc = self.nc
    self.begin()
    self.warm([AF.Exp])
    KT = self.sb("KT", [128, 4, T], BF16)
    VV = self.sb("VV", [128, NT, 512], BF16)
    fw.dma("sp", KT[:], S["NKT"].rearrange("g d t -> d g t"), W=[KT])
    fw.dma("sp", VV[:], S["NV"].rearrange("(i p) n -> p i n", p=128), W=[VV])
    onesb = self.sb("onesb", [128, 64], BF16)
    fw.op("dve", lambda e: e.memset(onesb[:], 1.0), W=[onesb])
    TAB = self.sb("TAB", [128, 16, 512], BF16)
    fw.op("dve", lambda e: e.memset(TAB[:], 0.0), W=[TAB])
    j64 = self.sb("j64", [64, 128], F32)
    fw.dma("sp", j64[:], C["j64"][:, :], W=[j64])
    win = self.sb("win", [128, 512], F32)
    fw.dma("sp", win[:], C["nawin"][:, :], W=[win])
    idf, idb = self.load_ident()
    r120 = self.sb("r120", [120, 31], F32)
    fw.dma("sp", r120[:], I["na_rpb"][l].rearrange("h a j -> (h a) j"), W=[r120])
    r31 = self.sb("r31", [31, 120], F32)
    tps = self.pool("tps", [128, 512], F32, 1, psum=True)
    pt0 = tps()
    fw.op("pe", lambda e: e.transpose(out=pt0[0:31, 0:120], in_=r120[:, :], identity=idf[0:120, 0:120]), R=[r120, idf], W=[pt0])
    fw.op("act", lambda e: e.activation(out=r31[:], in_=pt0[0:31, 0:120], func=AF.Copy), R=[pt0], W=[r31])
    p0 = tps()
    fw.op("pe", lambda e: e.matmul(p0[0:120, 0:31], lhsT=r31[:, :], rhs=j64[0:31, 33:64], start=True, stop=True), R=[r31, j64], W=[p0])
    rev = self.sb("rev", [120, 31], F32)
    fw.op("act", lambda e: e.activation(out=rev[:], in_=p0[0:120, 0:31], func=AF.Copy), R=[p0], W=[rev])
    zt = self.sb("zt", [120, 128], F32)
    fw.op("dve", lambda e: e.memset(zt[:], 0.0), W=[zt])
    nag = Buf("nag")
    NAG = S["NAG"]
    fw.dma("sp", NAG[:, :], zt[:], R=[zt], W=[nag])
    fw.dma("sp", NAG[:, 48:79], rev[:], R=[rev], W=[nag])
    Hk = self.sb("Hk", [64, 120, 64], F32)
    for n0 in range(0, 120, 8):
        hsrc = bass.AP(tensor=NAG.tensor, offset=NAG.offset + n0 * 128, ap=[[1, 64], [128, 8], [1, 64]])
        fw.dma("sp", Hk[:, n0:n0 + 8, :], hsrc, R=[nag], W=[Hk])
    Hk4 = Hk[:].rearrange("m (h a) c -> m h a c", h=8)
    ebp = self.pool("eb", [128, 512], F32, 2)
    for dr in range(15):
        p = tps()
        fw.op("pe", lambda e: e.matmul(p[:].rearrange("p (h c) -> p h c", h=8), lhsT=j64[:, :], rhs=Hk4[:, :, dr, :], start=True, stop=True), R=[j64, Hk], W=[p])
        eb = ebp()
        fw.op("act", lambda e: e.activation(out=eb[:], in_=p[:], func=AF.Exp), R=[p], W=[eb])
        if dr <= 13:
            fw.op("dve", lambda e: e.tensor_tensor(out=TAB[0:64, dr, :], in0=eb[0:64, :], in1=win[0:64, :], op=ALU.mult), R=[eb, win], W=[TAB])
        if dr == 10:
            fw.op("dve", lambda e: e.tensor_tensor(out=TAB[0:64, 15, :], in0=eb[0:64, :], in1=win[0:64, :], op=ALU.mult), R=[eb, win], W=[TAB])
        if dr >= 1:
            fw.op("dve", lambda e: e.tensor_tensor(out=TAB[64:128, dr - 1, :], in0=eb[64:128, :], in1=win[64:128, :], op=ALU.mult), R=[eb, win], W=[TAB])
        if dr == 3:
            fw.op("dve", lambda e: e.tensor_tensor(out=TAB[64:128, 14, :], in0=eb[64:128, :], in1=win[64:128, :], op=ALU.mult), R=[eb, win], W=[TAB])

    if self.stop_after == "natab":
        self.scratch("TAB", [128, 16, 512], BF16)
        fw.dma("pool", S["TAB"], TAB[:], R=[TAB])
        self.end("natab")
        return
    q8p = self.pool("q8", [128, 4, 512], BF16, 2)
    stgp = self.pool("stg", [64, 8, 512], BF16, 2)
    sps = self.pool("sps", [128, 512], F32, 4, psum=True)
    ops_ = self.pool("ops", [64, 512], F32, 2, psum=True)
    dps = self.pool("dps", [64, 512], F32, 1, psum=True)
    ptp = self.pool("pt", [128, 512], BF16, 16)
    rcp = self.pool("rc", [64, 512], F32, 2)
    BRv = S["BR"][2].rearrange("(h d) t -> d h t", h=8)
    NQv = S["NQT"].rearrange("g d t -> d g t")
    ntoggle = 0
    for grp in range(T // 512 + 1):
        if grp == 0:
            tok0, nrows = 0, 4
        else:
            tok0, nrows = 256 + (grp - 1) * 512, 8
        ntok = nrows * 64
        q8 = q8p()
        stg = stgp()
        fw.dma("sp", q8[:, :, 0:ntok], NQv[:, :, tok0:tok0 + ntok], W=[q8])
        for rr in range(nrows):
            qoff = rr * 64
            tiles = [(0, None), (1, None)]
            if grp > 0:
                r = (grp - 1) * 8 + rr
                rs = min(max(r - 4, 0), 56)
                if rs % 2 == 0:
                    for j in range(4):
                        tiles.append((2 + rs // 2 + j, rs + 2 * j - r + 7))
                else:
                    tiles.append((2 + (rs - 1) // 2, 14))
                    for j in range(3):
                        tiles.append((2 + (rs + 1) // 2 + j, rs + 1 + 2 * j - r + 7))
                    tiles.append((2 + (rs + 7) // 2, 15))
            P = []
            for (kt, slot) in tiles:
                spe = sps()
                spo = sps()
                for h in range(8):
                    pb = (h % 2) * 64
                    dstp = spe if h % 2 == 0 else spo
                    fw.op("pe", lambda e: e.matmul(dstp[:, (h // 2) * 64:(h // 2 + 1) * 64], lhsT=KT[pb:pb + 64, h // 2, kt * 128:(kt + 1) * 128], rhs=q8[pb:pb + 64, h // 2, qoff:qoff + 64], start=True, stop=True), R=[KT, q8], W=[dstp])
                pt = ptp()
                ptv = pt[:].rearrange("p (g two q) -> p g two q", two=2, q=64)
                fw.op("act", lambda e: e.activation(out=ptv[:, :, 0, :], in_=spe[:, 0:256].rearrange("p (g q) -> p g q", q=64), func=AF.Exp, scale=0.125), R=[spe], W=[pt])
                fw.op("act", lambda e: e.activation(out=ptv[:, :, 1, :], in_=spo[:, 0:256].rearrange("p (g q) -> p g q", q=64), func=AF.Exp, scale=0.125), R=[spo], W=[pt])
                if slot is not None:
                    assert 0 <= slot <= 15
                    eng = "dve"
                    ntoggle += 1
                    fw.op(eng, lambda e: e.tensor_tensor(out=pt[:], in0=pt[:], in1=TAB[:, slot, :], op=ALU.mult), R=[pt, TAB], W=[pt])
                P.append((kt, pt))
            o_ps = ops_()
            d_ps = dps()
            nk = len(P)
            for h in range(8):
                for i, (kt, pt) in enumerate(P):
                    fw.op("pe", lambda e: e.matmul(o_ps[:, h * 64:(h + 1) * 64], lhsT=VV[:, kt, h * 64:(h + 1) * 64], rhs=pt[:, h * 64:(h + 1) * 64], start=(i == 0), stop=(i == nk - 1)), R=[VV, pt], W=[o_ps])
            for i, (kt, pt) in enumerate(P):
                fw.op("pe", lambda e: e.matmul(d_ps[:], lhsT=onesb[:], rhs=pt[:], start=(i == 0), stop=(i == nk - 1)), R=[onesb, pt], W=[d_ps])
            rc = rcp()
            fw.op("dve", lambda e: e.reciprocal(out=rc[:], in_=d_ps[:]), R=[d_ps], W=[rc])
            fw.op("dve", lambda e: e.tensor_tensor(out=stg[:, :, qoff:qoff + 64], in0=o_ps[:].rearrange("p (h q) -> p h q", h=8), in1=rc[:].rearrange("p (h q) -> p h q", h=8), op=ALU.mult), R=[o_ps, rc], W=[stg])
        fw.dma("pool", BRv[:, :, tok0:tok0 + ntok], stg[:, :, 0:ntok], R=[stg])
    self.end("na%d" % l)


KB.ph_na = _ph_na


def _ln_epilogue(self, ysrc, xt, mbc, gbc, bbc, pools, dst_ap):
    fw = self.fw
    up, stp, mvp, xnp = pools
    u = up()
    for hf in range(2):
        yb, yap = ysrc[hf]
        fw.op("dve", lambda e: e.tensor_tensor(out=u[:, hf * 512:(hf + 1) * 512], in0=yap, in1=mbc[:, hf * 512:(hf + 1) * 512], op=ALU.mult), R=[yb, mbc], W=[u])
    fw.op("dve", lambda e: e.scalar_tensor_tensor(out=u[:], in0=xt[:], scalar=ALPHA, in1=u[:], op0=ALU.mult, op1=ALU.add), R=[xt, u], W=[u])
    st = stp()
    for hf in range(2):
        fw.op("dve", lambda e: e.bn_stats(out=st[:, hf, :], in_=u[:, hf * 512:(hf + 1) * 512]), R=[u], W=[st])
    mv = mvp()
    fw.op("dve", lambda e: e.bn_aggr(out=mv[:, 0:2], in_=st[:].rearrange("p a b -> p (a b)")), R=[st], W=[mv])
    fw.op("dve", lambda e: e.tensor_scalar_add(out=mv[:, 2:3], in0=mv[:, 1:2], scalar1=LN_EPS), R=[mv], W=[mv])
    fw.op("act", lambda e: e.activation(out=mv[:, 2:3], in_=mv[:, 2:3], func=AF.Ln), R=[mv], W=[mv])
    fw.op("act", lambda e: e.activation(out=mv[:, 2:3], in_=mv[:, 2:3], func=AF.Exp, scale=-0.5), R=[mv], W=[mv])
    fw.op("dve", lambda e: e.scalar_tensor_tensor(out=mv[:, 3:4], in0=mv[:, 0:1], scalar=-1.0, in1=mv[:, 2:3], op0=ALU.mult, op1=ALU.mult), R=[mv], W=[mv])
    xn = xnp()
    fw.op("act", lambda e: e.activation(out=xn[:], in_=u[:], func=AF.Identity, bias=mv[:, 3:4], scale=mv[:, 2:3]), R=[u, mv], W=[xn])
    fw.op("dve", lambda e: e.tensor_tensor(out=xn[:], in0=xn[:], in1=gbc[:], op=ALU.mult), R=[xn, gbc], W=[xn])
    fw.op("dve", lambda e: e.tensor_tensor(out=xn[:], in0=xn[:], in1=bbc[:], op=ALU.add), R=[xn, bbc], W=[xn])
    fw.dma("pool", dst_ap, xn[:], R=[xn])


def _bc_tile(self, name, row_ap):
    t = self.sb(name, [128, D], F32)
    self.fw.dma("sp", t[:], row_ap.partition_broadcast(128), W=[t])
    return t


def _ph_merge(self, l):
    fw, I, S, C = self.fw, self.I, self.S, self.C
    self.begin()
    self.warm([AF.Ln, AF.Exp])
    wfp = self.pool("wf", [128, 1024], F32, 2)
    wbr = self.sb("wbr", [128, 3, 4, 1024], BF16)
    wo = self.sb("wo", [128, 8, 1024], BF16)
    k = 0
    for n_ in range(3):
        for ec in range(4):
            wf = wfp()
            fw.dma("sp", wf[:], I["w_branch"][l, n_, ec * 128:(ec + 1) * 128, :], W=[wf])
            if k % 2 == 0:
                fw.op("dve", lambda e: e.tensor_copy(out=wbr[:, n_, ec, :], in_=wf[:]), R=[wf], W=[wbr])
            else:
                fw.op("act", lambda e: e.activation(out=wbr[:, n_, ec, :], in_=wf[:], func=AF.Copy), R=[wf], W=[wbr])
            k += 1
    for dc in range(8):
        wf = wfp()
        fw.dma("sp", wf[:], I["w_o"][l, dc * 128:(dc + 1) * 128, :], W=[wf])
        if dc % 2 == 0:
            fw.op("dve", lambda e: e.tensor_copy(out=wo[:, dc, :], in_=wf[:]), R=[wf], W=[wo])
        else:
            fw.op("act", lambda e: e.activation(out=wo[:, dc, :], in_=wf[:], func=AF.Copy), R=[wf], W=[wo])
    m2 = [_bc_tile(self, "m2L", S["MODR"][l, 0, 0:1, :]), _bc_tile(self, "m2C", S["MODR"][l, 1, 0:1, :])]
    gbc = _bc_tile(self, "gbc", I["ln_g"][l, 0:1, :])
    bbc = _bc_tile(self, "bbc", I["ln_b"][l, 0:1, :])
    brp = self.pool("br", [128, 3, 4, 512], BF16, 2)
    gtp = self.pool("gt", [128, 3, 512], BF16, 2)
    pj = self.pool("pj", [128, 512], F32, 3, psum=True)
    yps = self.pool("yps", [128, 512], F32, 4, psum=True)
    accp = self.pool("acc", [128, 512], F32, 2)
    tmpp = self.pool("tmp", [128, 512], F32, 2)
    mTp = self.pool("mT", [128, 8, 512], BF16, 2)
    xtp = self.pool("xt", [128, D], F32, 2)
    pools = (self.pool("u", [128, D], F32, 2), self.pool("st", [128, 2, 6], F32, 2), self.pool("mv", [128, 4], F32, 2), self.pool("xn", [128, D], F32, 2))
    chunks = [(0, 256)] + [(256 + 512 * j, 512) for j in range(8)]
    BRv = S["BR"].rearrange("n (ec p) t -> p n ec t", p=128)
    GTv = S["GT"].rearrange("n (dc p) t -> p n dc t", p=128)
    for (c0, n) in chunks:
        br = brp()
        for n_ in range(3):
            fw.dma("sp", br[:, n_, :, 0:n], BRv[:, n_, :, c0:c0 + n], W=[br])
        mT = mTp()
        for dc in range(8):
            gt = gtp()
            fw.dma("sp", gt[:, :, 0:n], GTv[:, :, dc, c0:c0 + n], W=[gt])
            acc = accp()
            for n_ in range(3):
                p = pj()
                for ec in range(4):
                    fw.op("pe", lambda e: e.matmul(p[:, 0:n], lhsT=wbr[:, n_, ec, dc * 128:(dc + 1) * 128], rhs=br[:, n_, ec, 0:n], start=(ec == 0), stop=(ec == 3)), R=[wbr, br], W=[p])
                if n_ == 0:
                    fw.op("dve", lambda e: e.tensor_tensor(out=acc[:, 0:n], in0=p[:, 0:n], in1=gt[:, 0, 0:n], op=ALU.mult), R=[p, gt], W=[acc])
                else:
                    tmp = tmpp()
                    fw.op("dve", lambda e: e.tensor_tensor(out=tmp[:, 0:n], in0=p[:, 0:n], in1=gt[:, n_, 0:n], op=ALU.mult), R=[p, gt], W=[tmp])
                    if n_ == 1:
                        fw.op("dve", lambda e: e.tensor_tensor(out=acc[:, 0:n], in0=acc[:, 0:n], in1=tmp[:, 0:n], op=ALU.add), R=[acc, tmp], W=[acc])
                    else:
                        fw.op("dve", lambda e: e.tensor_tensor(out=mT[:, dc, 0:n], in0=acc[:, 0:n], in1=tmp[:, 0:n], op=ALU.add), R=[acc, tmp], W=[mT])
        for tl in range(n // 128):
            ti = c0 // 128 + tl
            ys = []
            for hf in range(2):
                y = yps()
                for dc in range(8):
                    fw.op("pe", lambda e: e.matmul(y[:], lhsT=mT[:, dc, tl * 128:(tl + 1) * 128], rhs=wo[:, dc, hf * 512:(hf + 1) * 512], start=(dc == 0), stop=(dc == 7)), R=[mT, wo], W=[y])
                ys.append((y, y[:]))
            xt = xtp()
            fw.dma("sp", xt[:], self.x_src(l, ti), W=[xt])
            _ln_epilogue(self, ys, xt, m2[1 if ti < 2 else 0], gbc, bbc, pools, S["XS"][ti * 128:(ti + 1) * 128, :])
    self.end("merge%d" % l)


KB.ph_merge = _ph_merge


def _ph_moe(self, l, last):
    fw, I, S, C = self.fw, self.I, self.S, self.C
    self.begin()
    self.warm([AF.Sigmoid, AF.Silu])
    idf, idb = self.load_ident()
    modf = self.sb("modf", [128, 48, 2], F32)
    fw.dma("sp", modf[:], S["MODF"][l], W=[modf])
    m5 = [_bc_tile(self, "m5L", S["MODR"][l, 0, 1:2, :]), _bc_tile(self, "m5C", S["MODR"][l, 1, 1:2, :])]
    gbc = _bc_tile(self, "gbc", I["ln_g"][l, 1:2, :])
    bbc = _bc_tile(self, "bbc", I["ln_b"][l, 1:2, :])
    wr = self.sb("wr", [128, 8, 16], F32)
    fw.dma("sp", wr[:], I["w_router"].rearrange("(kc p) e -> p kc e", p=128), W=[wr])
    brt = self.sb("brt", [128, 16], F32)
    fw.dma("sp", brt[:], I["b_router"].rearrange("(o e) -> o e", o=1).partition_broadcast(128), W=[brt])
    self_f = self.sb("self", [16, 16, 128], F32)
    selb = self.sb("selb", [16, 16, 128], BF16)
    fw.dma("sp", self_f[:], C["sel"], W=[self_f])
    fw.op("dve", lambda e: e.tensor_copy(out=selb[:], in_=self_f[:]), R=[self_f], W=[selb])

    NG = 9
    hT = self.sb("hT", [128, 8, NG * 128], BF16)
    Y = self.sb("Y", [128, NG, D], F32)
    aff = self.sb("aff", [128, NG, 16], F32)
    selv = self.sb("selv", [128, NG, 16], F32)
    comb = self.sb("comb", [128, NG, 16], F32)
    cT = self.sb("cT", [16, NG * 128], BF16)
    r1 = self.sb("r1", [128, NG * 4], F32)
    r2 = self.sb("r2", [128, NG * 4], F32)
    gs = self.sb("gs", [128, NG * 4], F32)
    gmx = self.sb("gmx", [128, NG], F32)
    gmask = self.sb("gmask", [128, NG * 4], F32)
    is1 = self.sb("is1", [128, NG * 4, 4], F32)
    v2 = self.sb("v2", [128, NG * 4, 4], F32)
    oh = self.sb("oh", [128, NG * 4, 4], F32)
    ssum = self.sb("ssum", [128, NG], F32)

    xtp = self.pool("xt", [128, D], F32, 2)
    h32p = self.pool("h32", [128, 8, 128], F32, 2)
    sml = self.pool("sml", [128, 512], F32, 1, psum=True)
    gen = self.pool("gen", [128, 512], F32, 7, psum=True)
    ptr = gen
    gup = gen
    yps = gen
    wsf = self.pool("wsf", [128, 2048], F32, 2)
    wgp = self.pool("wg", [128, 8, 512], BF16, 2)
    wup = self.pool("wu", [128, 8, 512], BF16, 2)
    wdp = self.pool("wd", [128, 4, 1024], BF16, 2)
    cbp = self.pool("cb", [128, 512], F32, 2)
    sgp = self.pool("sg", [128, 512], F32, 2)
    a1p = self.pool("a1", [128, 512], F32, 2)
    aTp = self.pool("aT", [128, 4, 512], BF16, 2)
    pools = (self.pool("u", [128, D], F32, 1), self.pool("st", [128, 2, 6], F32, 2), self.pool("mv", [128, 4], F32, 2), self.pool("xn", [128, D], F32, 2))
    castk = [0]

    def cast(dst_ap, dstb, src_ap, srcb):
        if castk[0] % 2 == 0:
            fw.op("dve", lambda e: e.tensor_copy(out=dst_ap, in_=src_ap), R=[srcb], W=[dstb])
        else:
            fw.op("act", lambda e: e.activation(out=dst_ap, in_=src_ap, func=AF.Copy), R=[srcb], W=[dstb])
        castk[0] += 1

    for (g0, g1) in ((0, 9), (9, 18), (18, 26), (26, 34)):
        ng = g1 - g0
        for i in range(g0, g1):
            j = i - g0
            st_ = 1 if i < 2 else 0
            xt = xtp()
            fw.dma("sp", xt[:], S["XS"][i * 128:(i + 1) * 128, :], W=[xt])
            h32 = h32p()
            for hf in range(2):
                p = ptr()
                for k4 in range(4):
                    kc = hf * 4 + k4
                    fw.op("pe", lambda e: e.transpose(out=p[:, k4 * 128:(k4 + 1) * 128], in_=xt[:, kc * 128:(kc + 1) * 128], identity=idf[:]), R=[xt, idf], W=[p])
                for k4 in range(4):
                    kc = hf * 4 + k4
                    fw.op("act", lambda e: e.activation(out=h32[:, kc, :], in_=p[:, k4 * 128:(k4 + 1) * 128], func=AF.Identity, bias=modf[:, 24 + kc, st_:st_ + 1], scale=modf[:, 32 + kc, st_:st_ + 1]), R=[p, modf], W=[h32])
            fw.op("dve", lambda e: e.tensor_copy(out=hT[:, :, j * 128:(j + 1) * 128], in_=h32[:]), R=[h32], W=[hT])
            lg = sml()
            for kc in range(8):
                fw.op("pe", lambda e: e.matmul(lg[:, 0:16], lhsT=h32[:, kc, :], rhs=wr[:, kc, :], start=(kc == 0), stop=(kc == 7)), R=[h32, wr], W=[lg])
            fw.op("act", lambda e: e.activation(out=aff[:, j, :], in_=lg[:, 0:16], func=AF.Sigmoid), R=[lg], W=[aff])
            fw.op("dve", lambda e: e.tensor_tensor(out=selv[:, j, :], in0=aff[:, j, :], in1=brt[:], op=ALU.add), R=[aff, brt], W=[selv])
        n4 = ng * 4
        s4 = selv[:, 0:ng, :].rearrange("p g (q e) -> p (g q) e", e=4)
        first = True
        for (a, b) in ((0, 1), (0, 2), (0, 3), (1, 2), (1, 3), (2, 3)):
            if first:
                fw.op("dve", lambda e: e.tensor_tensor(out=gs[:, 0:n4], in0=s4[:, :, a], in1=s4[:, :, b], op=ALU.add), R=[selv], W=[gs])
                first = False
            else:
                fw.op("dve", lambda e: e.tensor_tensor(out=r1[:, 0:n4], in0=s4[:, :, a], in1=s4[:, :, b], op=ALU.add), R=[selv], W=[r1])
                fw.op("dve", lambda e: e.tensor_tensor(out=gs[:, 0:n4], in0=gs[:, 0:n4], in1=r1[:, 0:n4], op=ALU.max), R=[gs, r1], W=[gs])
        fw.op("dve", lambda e: e.reduce_max(out=gmx[:, 0:ng], in_=gs[:, 0:n4].rearrange("p (g q) -> p g q", q=4), axis=AX.X), R=[gs], W=[gmx])
        for q in range(4):
            fw.op("dve", lambda e: e.tensor_tensor(out=gmask[:, 0:n4].rearrange("p (g q) -> p g q", q=4)[:, :, q], in0=gs[:, 0:n4].rearrange("p (g q) -> p g q", q=4)[:, :, q], in1=gmx[:, 0:ng], op=ALU.is_ge), R=[gs, gmx], W=[gmask])
        fw.op("dve", lambda e: e.reduce_max(out=r1[:, 0:n4], in_=s4, axis=AX.X), R=[selv], W=[r1])
        for ee in range(4):
            fw.op("dve", lambda e: e.tensor_tensor(out=is1[:, 0:n4, ee], in0=s4[:, :, ee], in1=r1[:, 0:n4], op=ALU.is_ge), R=[selv, r1], W=[is1])
        fw.op("dve", lambda e: e.scalar_tensor_tensor(out=v2[:, 0:n4, :], in0=is1[:, 0:n4, :], scalar=-1.0e9, in1=s4, op0=ALU.mult, op1=ALU.add), R=[is1, selv], W=[v2])
        fw.op("dve", lambda e: e.reduce_max(out=r2[:, 0:n4], in_=v2[:, 0:n4, :], axis=AX.X), R=[v2], W=[r2])
        for ee in range(4):
            fw.op("dve", lambda e: e.tensor_tensor(out=oh[:, 0:n4, ee], in0=v2[:, 0:n4, ee], in1=r2[:, 0:n4], op=ALU.is_ge), R=[v2, r2], W=[oh])
        fw.op("dve", lambda e: e.tensor_tensor(out=oh[:, 0:n4, :], in0=oh[:, 0:n4, :], in1=is1[:, 0:n4, :], op=ALU.add), R=[oh, is1], W=[oh])
        for ee in range(4):
            fw.op("dve", lambda e: e.tensor_tensor(out=oh[:, 0:n4, ee], in0=oh[:, 0:n4, ee], in1=gmask[:, 0:n4], op=ALU.mult), R=[oh, gmask], W=[oh])
        ohv = oh[:, 0:n4, :].rearrange("p (g q) e -> p g (q e)", q=4)
        fw.op("dve", lambda e: e.tensor_tensor(out=comb[:, 0:ng, :], in0=aff[:, 0:ng, :], in1=ohv, op=ALU.mult), R=[aff, oh], W=[comb])
        fw.op("dve", lambda e: e.reduce_sum(out=ssum[:, 0:ng], in_=comb[:, 0:ng, :], axis=AX.X), R=[comb], W=[ssum])
        fw.op("dve", lambda e: e.reciprocal(out=ssum[:, 0:ng], in_=ssum[:, 0:ng]), R=[ssum], W=[ssum])
        for j in range(ng):
            fw.op("dve", lambda e: e.tensor_scalar_mul(out=comb[:, j, :], in0=comb[:, j, :], scalar1=ssum[:, j:j + 1]), R=[comb, ssum], W=[comb])
            pc = sml()
            fw.op("pe", lambda e: e.transpose(out=pc[0:16, 0:128], in_=comb[:, j, :], identity=idf[:]), R=[comb, idf], W=[pc])
            fw.op("act", lambda e: e.activation(out=cT[:, j * 128:(j + 1) * 128], in_=pc[0:16, 0:128], func=AF.Copy), R=[pc], W=[cT])
        gchunks = []
        c = 0
        while c < ng * 128:
            n = min(512, ng * 128 - c)
            gchunks.append((c, n))
            c += n
        mitems = [(ex, c0, n) for ex in range(16) for (c0, n) in gchunks]
        wts = {}
        mst = {}

        def load_w(ex):
            wg = wgp(); wu = wup(); wd = wdp()
            for (wdst, src) in ((wg, I["w_exp_gate"][l, ex]), (wu, I["w_exp_up"][l, ex])):
                for hf in range(2):
                    wf = wsf()
                    fw.dma("sp", wf[:].rearrange("p (k f) -> p k f", k=4), src[hf * 512:(hf + 1) * 512, :].rearrange("(k p) f -> p k f", p=128), W=[wf])
                    cast(wdst[:, hf * 4:(hf + 1) * 4, :], wdst, wf[:].rearrange("p (k f) -> p k f", k=4), wf)
            for hf in range(2):
                wf = wsf()
                fw.dma("sp", wf[:].rearrange("p (k d) -> p k d", k=2), I["w_exp_down"][l, ex, hf * 256:(hf + 1) * 256, :].rearrange("(k p) d -> p k d", p=128), W=[wf])
                cast(wd[:, hf * 2:(hf + 1) * 2, :], wd, wf[:].rearrange("p (k d) -> p k d", k=2), wf)
            wts[ex] = (wg, wu, wd)

        def mfront(i):
            ex, c0, n = mitems[i]
            if ex not in wts:
                load_w(ex)
            wg, wu, wd = wts[ex]
            cbps = sml()
            fw.op("pe", lambda e: e.matmul(cbps[:, 0:n], lhsT=selb[:, ex, :], rhs=cT[:, c0:c0 + n], start=True, stop=True), R=[selb, cT], W=[cbps])
            cb = cbp()
            fw.op("act", lambda e: e.activation(out=cb[:, 0:n], in_=cbps[:, 0:n], func=AF.Copy), R=[cbps], W=[cb])
            aT = aTp()
            for fc in range(4):
                gp = gup(); up_ = gup()
                for kc in range(8):
                    fw.op("pe", lambda e: e.matmul(gp[:, 0:n], lhsT=wg[:, kc, fc * 128:(fc + 1) * 128], rhs=hT[:, kc, c0:c0 + n], start=(kc == 0), stop=(kc == 7)), R=[wg, hT], W=[gp])
                for kc in range(8):
                    fw.op("pe", lambda e: e.matmul(up_[:, 0:n], lhsT=wu[:, kc, fc * 128:(fc + 1) * 128], rhs=hT[:, kc, c0:c0 + n], start=(kc == 0), stop=(kc == 7)), R=[wu, hT], W=[up_])
                sg = sgp()
                fw.op("act", lambda e: e.activation(out=sg[:, 0:n], in_=gp[:, 0:n], func=AF.Silu), R=[gp], W=[sg])
                a1 = a1p()
                fw.op("dve", lambda e: e.tensor_tensor(out=a1[:, 0:n], in0=up_[:, 0:n], in1=sg[:, 0:n], op=ALU.mult), R=[up_, sg], W=[a1])
                fw.op("dve", lambda e: e.tensor_tensor(out=aT[:, fc, 0:n], in0=a1[:, 0:n], in1=cb[:, 0:n], op=ALU.mult), R=[a1, cb], W=[aT])
            mst[i] = aT

        def mback(i):
            ex, c0, n = mitems[i]
            wg, wu, wd = wts[ex]
            aT = mst.pop(i)
            for tl in range(n // 128):
                j = c0 // 128 + tl
                for hf in range(2):
                    y = yps()
                    for fc in range(4):
                        fw.op("pe", lambda e: e.matmul(y[:], lhsT=aT[:, fc, tl * 128:(tl + 1) * 128], rhs=wd[:, fc, hf * 512:(hf + 1) * 512], start=(fc == 0), stop=(fc == 3)), R=[aT, wd], W=[y])
                    if ex == 0:
                        fw.op("act", lambda e: e.activation(out=Y[:, j, hf * 512:(hf + 1) * 512], in_=y[:], func=AF.Copy), R=[y], W=[Y])
                    else:
                        fw.op("dve", lambda e: e.tensor_tensor(out=Y[:, j, hf * 512:(hf + 1) * 512], in0=y[:], in1=Y[:, j, hf * 512:(hf + 1) * 512], op=ALU.add), R=[y, Y], W=[Y])

        nM = len(mitems)
        nch = len(gchunks)
        load_w(0)
        for i in range(nM + 1):
            if i < nM:
                mfront(i)
                ex_i = mitems[i][0]
                if (i % nch) == min(1, nch - 1) and ex_i + 1 < 16 and i >= 1:
                    mback(i - 1)
                    load_w(ex_i + 1)
                    continue
            if i >= 1:
                mback(i - 1)
        for i in range(g0, g1):
            j = i - g0
            xt = xtp()
            fw.dma("sp", xt[:], S["XS"][i * 128:(i + 1) * 128, :], W=[xt])
            if last and i >= 2:
                dst = self.out[(i - 2) * 128:(i - 1) * 128, :]
            else:
                dst = S["XS"][i * 128:(i + 1) * 128, :]
            ys = [(Y, Y[:, j, 0:512]), (Y, Y[:, j, 512:1024])]
            _ln_epilogue(self, ys, xt, m5[1 if i < 2 else 0], gbc, bbc, pools, dst)
    self.end("moe%d" % l)


KB.ph_moe = _ph_moe
```

```python
import contextlib
import math
import numpy as np
import ml_dtypes
import concourse.bass as bass
import concourse.mybir as mybir
from concourse.bass_utils import run_bass_kernel_spmd

F32 = mybir.dt.float32
BF16 = mybir.dt.bfloat16
AF = mybir.ActivationFunctionType
ALU = mybir.AluOpType
AX = mybir.AxisListType

D = 1024
LC = 256
LL = 4096
T = LC + LL
NT = T // 128
DEPTH = 4
D_IN = 7712
ALPHA = (2 * DEPTH) ** 0.25
LN_EPS = 1e-5
RMS_EPS = 1e-6
O_GQ, O_GK, O_GV, O_GG, O_GR = 0, 256, 512, 1024, 1536
O_DQ, O_DK, O_DV = 1568, 2080, 2592
O_NQ, O_NK, O_NV = 3104, 3616, 4128
O_GT = 4640


class Buf:
    __slots__ = ("name", "t", "last_write", "reads", "excl")

    def __init__(self, name, t=None, excl=False):
        self.name = name
        self.t = t
        self.last_write = None
        self.reads = {}
        self.excl = excl

    def __getitem__(self, key):
        return self.t[key]


class _Op:
    __slots__ = ("fn", "waits", "tok", "signal", "val")

    def __init__(self, fn):
        self.fn = fn
        self.waits = []
        self.tok = None
        self.signal = False
        self.val = None


class _Rec:
    call = None

    def __getattr__(self, name):
        def f(*a, **k):
            self.call = (name, a, k)
            return self
        return f


class _Eng:
    def __init__(self, name):
        self.name = name
        self.ops = []
        self.seen = {}
        self.is_pe = name == "pe"
        self.sem = None
        self.dsems = []
        self.dcount = []
        self.drr = 0
        self.emitted = 0
        self.sigcount = 0


class FW:
    ENGS = ("pe", "act", "dve", "pool", "sp")

    def __init__(self, nc, st, n_dma_sems=16):
        self.nc = nc
        self.e = {n: _Eng(n) for n in self.ENGS}
        self.n_dma_sems = n_dma_sems
        for en in self.ENGS:
            E = self.e[en]
            E.sem = st.enter_context(nc.semaphore("s_" + en))
            if en in ("sp", "pool", "act"):
                E.dcount = [0] * n_dma_sems
                E.dsems = [st.enter_context(nc.semaphore("d_%s_%d" % (en, i))) for i in range(n_dma_sems)]

    def _deps(self, E, reads, writes):
        deps = {}

        def add(tok):
            if tok is None:
                return
            if tok[0] == "c":
                if E.is_pe and tok[1] == "pe":
                    return
                key = ("c", tok[1])
            else:
                key = ("d", tok[1])
            if key not in deps or deps[key][2] < tok[2]:
                deps[key] = tok

        for b in reads:
            add(b.last_write)
            if b.excl:
                for t in b.reads.values():
                    add(t)
        for b in writes:
            add(b.last_write)
            for t in b.reads.values():
                add(t)
        out = []
        for key, tok in deps.items():
            if E.seen.get(key, -1) >= tok[2]:
                continue
            E.seen[key] = tok[2]
            out.append(tok)
        return out

    def _commit(self, tok, reads, writes):
        for b in writes:
            b.last_write = tok
            b.reads = {}
        for b in reads:
            if b.excl:
                b.last_write = tok
                b.reads = {}
            else:
                b.reads[(tok[0], tok[1])] = tok

    def op(self, eng, fn, R=(), W=()):
        E = self.e[eng]
        rec = _Rec()
        fn(rec)
        call = rec.call
        o = _Op(lambda e, c=call: getattr(e, c[0])(*c[1], **c[2]))
        R = [b for b in R if b is not None]
        W = [b for b in W if b is not None]
        o.waits = self._deps(E, R, W)
        o.tok = ("c", eng, len(E.ops))
        E.ops.append(o)
        self._commit(o.tok, R, W)
        return o

    def dma(self, eng, out, in_, R=(), W=(), **kw):
        E = self.e[eng]
        o = _Op(lambda e: e.dma_start(out=out, in_=in_, **kw))
        R = [b for b in R if b is not None]
        W = [b for b in W if b is not None]
        o.waits = self._deps(E, R, W)
        i = E.drr
        E.drr = (E.drr + 1) % self.n_dma_sems
        key = ("d", (eng, i))
        prev = E.dcount[i]
        if prev > 0 and E.seen.get(key, -1) < prev:
            E.seen[key] = prev
            o.waits.append(("d", (eng, i), prev))
        E.dcount[i] = prev + 16
        o.tok = ("d", (eng, i), prev + 16)
        E.ops.append(o)
        self._commit(o.tok, R, W)
        return o

    def phase_end(self):
        nc = self.nc
        E = self.e["sp"]
        o = _Op(None)
        for en in self.ENGS:
            Q = self.e[en]
            for i, c in enumerate(Q.dcount):
                if c > 0 and E.seen.get(("d", (en, i)), -1) < c:
                    o.waits.append(("d", (en, i), c))
            if en != "sp":
                for q in reversed(Q.ops[Q.emitted:]):
                    if q.tok[0] == "c" and q.fn is not None:
                        o.waits.append(q.tok)
                        break
        o.tok = ("c", "sp", len(E.ops))
        E.ops.append(o)
        for en in self.ENGS:
            Q = self.e[en]
            for q in Q.ops[Q.emitted:]:
                for w in q.waits:
                    if w[0] == "c":
                        P = self.e[w[1]]
                        assert w[2] >= P.emitted, "wait on op of an earlier phase"
                        P.ops[w[2]].signal = True
        for en in self.ENGS:
            Q = self.e[en]
            for q in Q.ops[Q.emitted:]:
                if q.tok[0] == "c" and q.signal:
                    assert q.fn is not None
                    Q.sigcount += 1
                    q.val = Q.sigcount

        with nc.Block() as block:
            def run(en, eng):
                Q = self.e[en]
                for q in Q.ops[Q.emitted:]:
                    for w in q.waits:
                        if w[0] == "c":
                            P = self.e[w[1]]
                            eng.wait_ge(P.sem, P.ops[w[2]].val)
                        else:
                            qn, i = w[1]
                            eng.wait_ge(self.e[qn].dsems[i], w[2])
                    if q.fn is None:
                        continue
                    ins = q.fn(eng)
                    if q.tok[0] == "d":
                        qn, i = q.tok[1]
                        ins.then_inc(self.e[qn].dsems[i], 16)
                    elif q.signal:
                        ins.then_inc(Q.sem, 1)

            @block.tensor
            def _(eng):
                run("pe", eng)

            @block.scalar
            def _(eng):
                run("act", eng)

            @block.vector
            def _(eng):
                run("dve", eng)

            @block.gpsimd
            def _(eng):
                run("pool", eng)

            @block.sync
            def _(eng):
                run("sp", eng)

        nc.all_engine_barrier()
        nops = {en: len(self.e[en].ops) - self.e[en].emitted for en in self.ENGS}
        for en in self.ENGS:
            Q = self.e[en]
            Q.emitted = len(Q.ops)
        for en in self.ENGS:
            Q = self.e[en]
            for pn in self.ENGS:
                P = self.e[pn]
                Q.seen[("c", pn)] = len(P.ops) - 1
                for i, c in enumerate(P.dcount):
                    Q.seen[("d", (pn, i))] = c
        return nops


def _consts():
    c = {}
    c["ident"] = np.eye(128, dtype=np.float32)
    n = np.arange(LL)
    row = (n // 64).astype(np.float32)
    col = (n % 64).astype(np.float32)
    nf = 16
    inv = (10000.0 ** (-np.arange(nf, dtype=np.float32) / nf)).astype(np.float32)
    ang = np.concatenate([row[:, None] * inv, col[:, None] * inv], -1).astype(np.float32)
    cos = np.cos(ang).astype(np.float32)
    sin = np.sin(ang).astype(np.float32)
    ct = np.ones((128, T), np.float32)
    stb = np.zeros((128, T), np.float32)
    for p in range(128):
        j = p % 64
        f = j % 32
        ct[p, LC:] = cos[:, f]
        stb[p, LC:] = (-sin[:, f]) if j < 32 else sin[:, f]
    c["rope_c"] = ct
    c["rope_s"] = stb
    s = np.arange(128)[:, None]
    t = np.arange(128)[None, :]
    le = (s <= t).astype(np.float32)
    gt = (s > t).astype(np.float32)
    tri = np.stack([le, le.T, gt, gt.T]).astype(np.float32)
    c["tri"] = tri
    c["mask4"] = np.stack([np.tile(le, (1, 4)), np.tile(le.T, (1, 4))]).astype(np.float32)
    J = np.zeros((64, 128), np.float32)
    for m in range(64):
        J[m, 63 - m] = 1.0
        J[m, 64 + 63 - m] = 1.0
    c["j64"] = J
    cc = np.arange(64)
    cs = np.clip(cc - 8, 0, 48)
    W = ((cc[:, None] >= cs[None, :]) & (cc[:, None] < cs[None, :] + 16)).astype(np.float32)
    W2 = np.concatenate([W, W], 0)
    c["nawin"] = np.tile(W2[:, None, :], (1, 8, 1)).reshape(128, 512).astype(np.float32)
    sel = np.zeros((16, 16, 128), np.float32)
    for e in range(16):
        sel[e, e, :] = 1.0
    c["sel"] = sel
    return c


CONST_SHAPES = {
    "ident": [128, 128], "rope_c": [128, T], "rope_s": [128, T], "tri": [4, 128, 128],
    "mask4": [2, 128, 512], "j64": [64, 128], "nawin": [128, 512], "sel": [16, 16, 128],
}

INPUT_SHAPES = {
    "x": [LL, D], "c": [D], "ctx": [LC, D], "c_ctx": [D],
    "w_ada": [DEPTH, D, 6 * D], "b_ada": [DEPTH, 6 * D], "w_in": [DEPTH, D, D_IN],
    "gla_w_decay": [DEPTH, 2, 16, 256], "gla_b_decay": [DEPTH, 2, 256], "gla_norm_g": [DEPTH, 128],
    "diff_lam_q": [DEPTH, 2, 64], "diff_lam_k": [DEPTH, 2, 64], "diff_norm_g": [DEPTH, 128],
    "na_rpb": [DEPTH, 8, 15, 31], "w_branch": [DEPTH, 3, 512, D], "w_o": [DEPTH, D, D],
    "ln_g": [DEPTH, 2, D], "ln_b": [DEPTH, 2, D], "w_router": [D, 16], "b_router": [16],
    "w_exp_gate": [DEPTH, 16, D, 512], "w_exp_up": [DEPTH, 16, D, 512], "w_exp_down": [DEPTH, 16, 512, D],
}


class KB:
    def __init__(self, nlayers=DEPTH, debug=(), stop_after=None):
        self.nlayers = nlayers
        self.debug = set(debug)
        self.stop_after = stop_after
        self.nc = bass.Bass("TRN2", target_bir_lowering=False)
        nc = self.nc
        self.I = {k: nc.dram_tensor(k, v, F32, kind="ExternalInput").ap() for k, v in INPUT_SHAPES.items()}
        self.C = {k: nc.dram_tensor("k_" + k, v, F32, kind="ExternalInput").ap() for k, v in CONST_SHAPES.items()}
        self.out = nc.dram_tensor("out", [LL, D], F32, kind="ExternalOutput").ap()
        self.S = {}
        self.uid = 0

    def scratch(self, name, shape, dt):
        kind = "ExternalOutput" if (name in self.debug or name == "TAB") else "Internal"
        self.S[name] = self.nc.dram_tensor("s_" + name, shape, dt, kind=kind).ap()
        return self.S[name]

    def begin(self):
        self.pst = contextlib.ExitStack()
        self.pst.__enter__()
        self._cc = {}

    def warm(self, funcs):
        return
        fw = self.fw
        w = self.sb("warm", [128, 2048], F32)
        fw.op("dve", lambda e: e.memset(w[:], 1.0), W=[w])
        for f in funcs:
            for _ in range(2):
                fw.op("act", lambda e: e.activation(out=w[:], in_=w[:], func=f), R=[w], W=[w])
            fw.op("dve", lambda e: e.memset(w[:], 1.0), W=[w])

    def const_col(self, val):
        if val not in self._cc:
            t = self.sb("cc", [128, 1], F32)
            self.fw.op("dve", lambda e: e.memset(t[:], float(val)), W=[t])
            self._cc[val] = t
        return self._cc[val]

    def end(self, name=""):
        nops = self.fw.phase_end()
        self.pst.__exit__(None, None, None)
        print("phase", name, nops, flush=True)

    def sb(self, name, shape, dt):
        self.uid += 1
        return Buf(name, self.pst.enter_context(self.nc.sbuf_tensor("%s_%d" % (name, self.uid), shape, dt)))

    def ps(self, name, shape, dt=F32):
        self.uid += 1
        return Buf(name, self.pst.enter_context(self.nc.psum_tensor("%s_%d" % (name, self.uid), shape, dt)),
                   excl=True)

    def pool(self, name, shape, dt, n, psum=False):
        bufs = [(self.ps if psum else self.sb)(name + str(i), shape, dt) for i in range(n)]
        state = {"i": 0}

        def nxt():
            b = bufs[state["i"] % n]
            state["i"] += 1
            return b
        return nxt

    def build(self):
        nc = self.nc
        with contextlib.ExitStack() as gst:
            self.fw = FW(nc, gst)
            self.alloc_scratch()
            self.ph_mod()
            if self.stop_after == "mod":
                return nc
            for l in range(self.nlayers):
                self.ph_proj(l)
                if self.stop_after in ("proj", "ht"):
                    break
                self.ph_gla(l)
                self.ph_glac(l)
                if self.stop_after == "gla":
                    break
                self.ph_diff(l)
                if self.stop_after == "diff":
                    break
                self.ph_na(l)
                if self.stop_after in ("na", "natab"):
                    break
                self.ph_merge(l)
                if self.stop_after == "merge":
                    break
                self.ph_moe(l, last=(l == self.nlayers - 1))
        return nc

    def alloc_scratch(self):
        s = self.scratch
        s("XS", [T, D], F32)
        s("MODF", [DEPTH, 128, 48, 2], F32)
        s("MODR", [DEPTH, 2, 2, D], F32)
        s("GQT", [4, 64, T], BF16)
        s("GKT", [4, 64, T], BF16)
        s("GK", [T, 256], BF16)
        s("GV", [T, 512], BF16)
        s("GG", [512, T], BF16)
        s("GR", [64, T], BF16)
        s("DQT", [4, 128, T], BF16)
        s("DKT", [4, 128, T], BF16)
        s("DV", [T, 512], BF16)
        s("NQT", [4, 128, T], BF16)
        s("NKT", [4, 128, T], BF16)
        s("NV", [T, 512], BF16)
        s("GT", [3, D, T], BF16)
        s("OG", [2, 4, 128, T], F32)
        s("BR", [3, 512, T], BF16)
        s("NAG", [120, 128], F32)

    def ph_mod(self):
        fw, I = self.fw, self.I
        self.begin()
        sc = self.sb("sc", [128, 8, 2], F32)
        ones = self.sb("ones", [1, 128], F32)
        idf, idb = self.load_ident()
        sc0 = self.sb("sc0", [128, 8], F32)
        sc1 = self.sb("sc1", [128, 8], F32)
        self.diag_cols(sc0, sc0[:], I["c"].rearrange("(o n) -> o n", o=1), 8, idf)
        self.diag_cols(sc1, sc1[:], I["c_ctx"].rearrange("(o n) -> o n", o=1), 8, idf)
        fw.op("dve", lambda e: e.tensor_copy(out=sc[:, :, 0], in_=sc0[:]), R=[sc0], W=[sc])
        fw.op("dve", lambda e: e.tensor_copy(out=sc[:, :, 1], in_=sc1[:]), R=[sc1], W=[sc])
        fw.op("act", lambda e: e.activation(out=sc[:], in_=sc[:], func=AF.Silu), R=[sc], W=[sc])
        fw.op("dve", lambda e: e.memset(ones[:], 1.0), W=[ones])
        if "DBG1" in self.debug:
            self.scratch("DBG1", [128, 16], F32)
            self.scratch("DBG2", [1, 6 * D], F32)
            fw.dma("pool", self.S["DBG1"], sc[:].rearrange("p k s -> p (k s)"), R=[sc])
        wpool = self.pool("wada", [128, 8, 512], F32, 2)
        psf = self.pool("psf", [128, 4, 2], F32, 2, psum=True)
        psr = self.pool("psr", [2, 512], F32, 2, psum=True)
        for l in range(DEPTH):
            bfm = self.sb("bfm", [128, 48], F32)
            brow = self.sb("brow", [2, 2, D], F32)
            modf = self.sb("modf", [128, 48, 2], F32)
            modr = self.sb("modr", [2, 2, D], F32)
            fw.op("dve", lambda e: e.memset(modf[:], 0.0), W=[modf])
            self.diag_cols(bfm, bfm[:], I["b_ada"][l:l + 1, :], 48, idf)
            for jj, j in enumerate((2, 5)):
                fw.dma("sp", brow[:, jj, :], I["b_ada"][l:l + 1, j * D:(j + 1) * D].partition_broadcast(2), W=[brow])
            for j in (1, 4):
                fw.op("dve", lambda e, j=j: e.tensor_scalar_add(out=bfm[:, j * 8:(j + 1) * 8], in0=bfm[:, j * 8:(j + 1) * 8], scalar1=1.0), R=[bfm], W=[bfm])
            for jn in range(12):
                j = jn // 2
                w = wpool()
                fw.dma("sp", w[:], I["w_ada"][l, :, jn * 512:(jn + 1) * 512].rearrange("(kc p) n -> p kc n", p=128), W=[w])
                if j in (2, 5):
                    p = psr()
                    for kc in range(8):
                        fw.op("pe", lambda e: e.matmul(p[:], lhsT=sc[:, kc, :], rhs=w[:, kc, :], start=(kc == 0), stop=(kc == 7)),
                              R=[sc, w], W=[p])
                    jj = 0 if j == 2 else 1
                    half = jn % 2
                    fw.op("dve", lambda e: e.tensor_tensor(out=modr[:, jj, half * 512:(half + 1) * 512], in0=p[:], in1=brow[:, jj, half * 512:(half + 1) * 512], op=ALU.add),
                          R=[p, brow], W=[modr])
                else:
                    p = psf()
                    for sub in range(4):
                        for kc in range(8):
                            fw.op("pe", lambda e: e.matmul(p[:, sub, :], lhsT=w[:, kc, sub * 128:(sub + 1) * 128], rhs=sc[:, kc, :], start=(kc == 0), stop=(kc == 7)),
                                  R=[sc, w], W=[p])
                    for s_ in range(2):
                        fw.op("dve", lambda e: e.tensor_tensor(out=modf[:, jn * 4:(jn + 1) * 4, s_], in0=p[:, :, s_], in1=bfm[:, jn * 4:(jn + 1) * 4], op=ALU.add),
                              R=[p, bfm], W=[modf])
            fw.dma("pool", self.S["MODF"][l], modf[:], R=[modf])
            fw.dma("pool", self.S["MODR"][l].rearrange("s j d -> s (j d)"), modr[:].rearrange("s j d -> s (j d)"), R=[modr])
        self.end("mod")

    def diag_cols(self, dstbuf, dst, src_row_ap, nch, idf):
        fw = self.fw
        bc = self.sb("dgbc", [128, nch, 128], F32)
        fw.dma("sp", bc[:].rearrange("p k j -> p (k j)"), src_row_ap.partition_broadcast(128), W=[bc])
        for k in range(nch):
            fw.op("dve", lambda e: e.tensor_tensor(out=bc[:, k, :], in0=bc[:, k, :], in1=idf[:], op=ALU.mult), R=[bc, idf], W=[bc])
        fw.op("dve", lambda e: e.reduce_sum(out=dst, in_=bc[:], axis=AX.X), R=[bc], W=[dstbuf])

    def x_src(self, l, i):
        if l == 0:
            if i < 2:
                return self.I["ctx"][i * 128:(i + 1) * 128, :]
            return self.I["x"][(i - 2) * 128:(i - 1) * 128, :]
        return self.S["XS"][i * 128:(i + 1) * 128, :]

    def load_ident(self):
        fw = self.fw
        idf = self.sb("idf", [128, 128], F32)
        idb = self.sb("idb", [128, 128], BF16)
        fw.dma("sp", idf[:], self.C["ident"][:, :], W=[idf])
        fw.op("dve", lambda e: e.tensor_copy(out=idb[:], in_=idf[:]), R=[idf], W=[idb])
        return idf, idb

    def ph_proj(self, l):
        fw, I, S, C = self.fw, self.I, self.S, self.C
        self.begin()
        idf, idb = self.load_ident()
        modf = self.sb("modf", [128, 48, 2], F32)
        fw.dma("sp", modf[:], S["MODF"][l], W=[modf])
        hT = self.sb("hT", [128, 8, T], BF16)
        xp = self.pool("xt", [128, D], F32, 2)
        xbp = self.pool("xb", [128, D], BF16, 2)
        ptr = self.pool("ptr", [128, 8, 128], BF16, 2, psum=True)
        for i in range(NT):
            xt = xp()
            xb = xbp()
            fw.dma("sp", xt[:], self.x_src(l, i), W=[xt])
            fw.op("dve", lambda e, xt=xt, xb=xb: e.tensor_copy(out=xb[:], in_=xt[:]), R=[xt], W=[xb])
            p = ptr()
            for kc in range(8):
                fw.op("pe", lambda e, p=p, xb=xb, kc=kc: e.transpose(out=p[:, kc, :], in_=xb[:, kc * 128:(kc + 1) * 128], identity=idb[:]),
                      R=[xb, idb], W=[p])
            st = 1 if i < 2 else 0
            for kc in range(8):
                fw.op("act", lambda e, p=p, kc=kc, i=i, st=st: e.activation(
                    out=hT[:, kc, i * 128:(i + 1) * 128], in_=p[:, kc, :], func=AF.Identity,
                    bias=modf[:, kc, st:st + 1], scale=modf[:, 8 + kc, st:st + 1]), R=[p, modf], W=[hT])
        if "HT" in self.debug:
            self.scratch("HT", [128, 8, T], BF16)
            fw.dma("pool", S["HT"], hT[:], R=[hT])
        if self.stop_after == "ht":
            self.end("ht")
            return
        rc = self.sb("rc", [128, T], F32)
        rs = self.sb("rs", [128, T], F32)
        fw.dma("sp", rc[:], C["rope_c"][:, :], W=[rc])
        fw.dma("sp", rs[:], C["rope_s"][:, :], W=[rs])

        wfp = self.pool("wf", [128, 8, 256], F32, 2)
        wbp = self.pool("wb", [128, 8, 256], BF16, 3)
        pmm = self.pool("pmm", [128, 512], F32, 4, psum=True)
        stg = self.pool("stg", [128, T], BF16, 2)
        tmp = self.pool("tmp", [128, 512], F32, 3)
        tstg = self.pool("tstg", [128, 256], BF16, 3)
        win = I["w_in"][l]
        chunks = [(0, 256)] + [(256 + 512 * j, 512) for j in range(8)]
        self._cast_i = 0

        def load_w(pieces, ncols, zero=False):
            wf = wfp()
            wb = wbp()
            if zero:
                fw.op("dve", lambda e, wf=wf: e.memset(wf[:], 0.0), W=[wf])
            for (dc, scol, n) in pieces:
                fw.dma("sp", wf[:, :, dc:dc + n], win[:, scol:scol + n].rearrange("(kc p) n -> p kc n", p=128), W=[wf])
            eng = "dve" if (self._cast_i % 2 == 0) else "act"
            self._cast_i += 1
            if eng == "act":
                fw.op("act", lambda e: e.activation(out=wb[:, :, 0:ncols], in_=wf[:, :, 0:ncols], func=AF.Copy), R=[wf], W=[wb])
            else:
                fw.op("dve", lambda e: e.tensor_copy(out=wb[:, :, 0:ncols], in_=wf[:, :, 0:ncols]), R=[wf], W=[wb])
            return wb

        def fm_mm(wb, m, c0, cn, col0=0):
            p = pmm()
            for kc in range(8):
                fw.op("pe", lambda e, p=p, wb=wb, kc=kc: e.matmul(p[0:m, 0:cn], lhsT=wb[:, kc, col0:col0 + m], rhs=hT[:, kc, c0:c0 + cn], start=(kc == 0), stop=(kc == 7)),
                      R=[wb, hT], W=[p])
            return p

        def fm_group(pieces, m, evac, dsts, zero=False):
            wb = load_w(pieces, m, zero)
            sg = stg()
            for (c0, cn) in chunks:
                p = fm_mm(wb, m, c0, cn)
                evac(p, sg, c0, cn, m)
            for (r0, nr, dst) in dsts:
                fw.dma("pool", dst, sg[r0:r0 + nr, :], R=[sg])

        def ev_copy(p, sg, c0, cn, m):
            fw.op("act", lambda e: e.activation(out=sg[0:m, c0:c0 + cn], in_=p[0:m, 0:cn], func=AF.Copy), R=[p], W=[sg])

        def ev_silu(p, sg, c0, cn, m):
            fw.op("act", lambda e: e.activation(out=sg[0:m, c0:c0 + cn], in_=p[0:m, 0:cn], func=AF.Silu), R=[p], W=[sg])

        def ev_sigm(p, sg, c0, cn, m):
            fw.op("act", lambda e: e.activation(out=sg[0:m, c0:c0 + cn], in_=p[0:m, 0:cn], func=AF.Sigmoid), R=[p], W=[sg])

        for (o0, dst) in ((O_GQ, S["GQT"]), (O_GK, S["GKT"])):
            for g in range(2):
                fm_group([(0, o0 + g * 128, 128)], 128, ev_copy,
                         [(0, 64, dst[2 * g]), (64, 64, dst[2 * g + 1])])
        for g in range(4):
            fm_group([(0, O_GG + g * 128, 128)], 128, ev_silu, [(0, 128, S["GG"][g * 128:(g + 1) * 128, :])])
        fm_group([(0, O_GR, 16), (32, O_GR + 16, 16)], 64, ev_copy, [(0, 64, S["GR"])], zero=True)
        for (o0, dst) in ((O_DQ, S["DQT"]), (O_DK, S["DKT"])):
            for h in range(4):
                b0 = o0 + h * 128
                wa = load_w([(0, b0, 128)], 128)
                wsw = load_w([(0, b0 + 32, 32), (32, b0, 32), (64, b0 + 96, 32), (96, b0 + 64, 32)], 128)
                sg = stg()
                for (c0, cn) in chunks:
                    pa = fm_mm(wa, 128, c0, cn)
                    pb = fm_mm(wsw, 128, c0, cn)
                    t1 = tmp()
                    t2 = tmp()
                    fw.op("dve", lambda e, pa=pa, t1=t1, c0=c0, cn=cn: e.tensor_tensor(out=t1[:, 0:cn], in0=pa[:, 0:cn], in1=rc[:, c0:c0 + cn], op=ALU.mult), R=[pa, rc], W=[t1])
                    fw.op("dve", lambda e, pb=pb, t2=t2, c0=c0, cn=cn: e.tensor_tensor(out=t2[:, 0:cn], in0=pb[:, 0:cn], in1=rs[:, c0:c0 + cn], op=ALU.mult), R=[pb, rs], W=[t2])
                    fw.op("dve", lambda e, t1=t1, t2=t2, sg=sg, c0=c0, cn=cn: e.tensor_tensor(out=sg[:, c0:c0 + cn], in0=t1[:, 0:cn], in1=t2[:, 0:cn], op=ALU.add), R=[t1, t2], W=[sg])
                fw.dma("pool", dst[h], sg[:, :], R=[sg])
        for (o0, dst) in ((O_NQ, S["NQT"]), (O_NK, S["NKT"])):
            for g in range(4):
                fm_group([(0, o0 + g * 128, 128)], 128, ev_copy, [(0, 128, dst[g])])
        for n in range(3):
            for g in range(8):
                fm_group([(0, O_GT + n * D + g * 128, 128)], 128, ev_sigm, [(0, 128, S["GT"][n, g * 128:(g + 1) * 128, :])])
        for (o0, ncols, dst) in ((O_GK, 256, S["GK"]), (O_GV, 512, S["GV"]), (O_DV, 512, S["DV"]), (O_NV, 512, S["NV"])):
            for cg in range(ncols // 256):
                wb = load_w([(0, o0 + cg * 256, 256)], 256)
                for i in range(NT):
                    p = pmm()
                    for kc in range(8):
                        fw.op("pe", lambda e, p=p, wb=wb, kc=kc, i=i: e.matmul(p[:, 0:256], lhsT=hT[:, kc, i * 128:(i + 1) * 128], rhs=wb[:, kc, :], start=(kc == 0), stop=(kc == 7)),
                              R=[wb, hT], W=[p])
                    ts = tstg()
                    eng = "act" if i % 2 == 0 else "dve"
                    if eng == "act":
                        fw.op("act", lambda e, p=p, ts=ts: e.activation(out=ts[:], in_=p[:, 0:256], func=AF.Copy), R=[p], W=[ts])
                    else:
                        fw.op("dve", lambda e, p=p, ts=ts: e.tensor_copy(out=ts[:], in_=p[:, 0:256]), R=[p], W=[ts])
                    fw.dma("pool", dst[i * 128:(i + 1) * 128, cg * 256:(cg + 1) * 256], ts[:], R=[ts])
        self.end("proj%d" % l)


def build_program(nlayers=DEPTH, debug=(), stop_after=None):
    kb = KB(nlayers, debug, stop_after)
    kb.build()
    return kb


_CONSTS = None


def make_in_maps(inputs):
    global _CONSTS
    if _CONSTS is None:
        _CONSTS = _consts()
    f = lambda a: np.ascontiguousarray(np.asarray(a, dtype=np.float32))
    shared = {k: f(inputs[k]) for k in INPUT_SHAPES if k not in ("x", "c", "ctx")}
    for k, v in _CONSTS.items():
        shared["k_" + k] = v
    maps = []
    for b in range(8):
        m = dict(shared)
        m["x"] = f(inputs["x"][b])
        m["c"] = f(inputs["c"][b])
        m["ctx"] = f(inputs["ctx"][b])
        maps.append(m)
    return maps


def kernel(**inputs):
    kb = build_program()
    maps = make_in_maps(inputs)
    res = run_bass_kernel_spmd(kb.nc, maps, core_ids=list(range(8)))
    return np.stack([np.asarray(r["out"], dtype=np.float32) for r in res.results], axis=0)


def _ph_gla(self, l):
    fw, I, S, C = self.fw, self.I, self.S, self.C
    self.begin()
    self.warm([AF.Exp, AF.Ln])
    qT = self.sb("qT", [64, 4, T], BF16)
    kT = self.sb("kT", [64, 4, T], BF16)
    kk = self.sb("kk", [128, NT, 256], BF16)
    vv = self.sb("vv", [128, NT, 512], BF16)
    rT = self.sb("rT", [64, T], BF16)
    fw.dma("sp", qT[:], S["GQT"].rearrange("h d t -> d h t"), W=[qT])
    fw.dma("sp", kT[:], S["GKT"].rearrange("h d t -> d h t"), W=[kT])
    fw.dma("sp", kk[:], S["GK"].rearrange("(i p) n -> p i n", p=128), W=[kk])
    fw.dma("sp", vv[:], S["GV"].rearrange("(i p) n -> p i n", p=128), W=[vv])
    fw.dma("sp", rT[:], S["GR"][:, :], W=[rT])
    trif = self.sb("trif", [128, 4, 128], F32)
    trib = self.sb("trib", [128, 4, 128], BF16)
    fw.dma("sp", trif[:], C["tri"].rearrange("a s t -> s a t"), W=[trif])
    fw.op("act", lambda e: e.mul(out=trib[:], in_=trif[:], mul=-1.0 / 16.0), R=[trif], W=[trib])
    maskf = self.sb("maskf", [128, 2, 512], F32)
    fw.dma("sp", maskf[:], C["mask4"].rearrange("a s t -> s a t"), W=[maskf])
    wdf = self.sb("wdf", [64, 256], F32)
    wd = self.sb("wd", [64, 256], BF16)
    fw.op("dve", lambda e: e.memset(wdf[:], 0.0), W=[wdf])
    fw.dma("sp", wdf[0:16, :], I["gla_w_decay"][l, 0], W=[wdf])
    fw.dma("sp", wdf[32:48, :], I["gla_w_decay"][l, 1], W=[wdf])
    fw.op("dve", lambda e: e.tensor_copy(out=wd[:], in_=wdf[:]), R=[wdf], W=[wd])
    bdb = self.sb("bdb", [128, 2, 256], F32)
    fw.dma("sp", bdb[:].rearrange("p a n -> p (a n)"), I["gla_b_decay"][l:l + 1].rearrange("o a n -> o (a n)").partition_broadcast(128), W=[bdb])
    zbp = self.pool("zb", [128, 256], F32, 2)

    Sf = self.sb("Sf", [64, 4, 128], F32)
    Sb = self.sb("Sb", [64, 4, 128], BF16)
    zps = self.pool("zps", [128, 256], F32, 1, psum=True)
    cps = self.pool("cps", [64, 4, 128], F32, 1, psum=True)
    gps = self.pool("gps", [128, 256], F32, 1, psum=True)
    aps = self.pool("aps", [128, 4, 128], F32, 2, psum=True)
    ops_ = self.pool("ops", [128, 4, 128], F32, 2, psum=True)
    kvps = self.pool("kvps", [64, 4, 128], F32, 1, psum=True)
    e1p = self.pool("e1", [128, 256], F32, 2)
    Lbp = self.pool("Lb", [128, 256], BF16, 2)
    Eqp = self.pool("Eq", [64, 4, 128], F32, 2)
    Ekp = self.pool("Ek", [64, 4, 128], F32, 2)
    Estp = self.pool("Est", [128, 256], F32, 2)
    qinp = self.pool("qin", [64, 4, 128], BF16, 2)
    kinp = self.pool("kin", [64, 4, 128], BF16, 2)
    kstp = self.pool("kst", [128, 256], BF16, 2)
    attp = self.pool("att", [128, 4, 128], BF16, 2)
    osbp = self.pool("osb", [128, 4, 128], F32, 2)

    Sf1 = self.sb("Sf1", [64, 4, 128], F32)
    Sb1 = self.sb("Sb1", [64, 4, 128], BF16)
    Sfs, Sbs = [Sf, Sf1], [Sb, Sb1]
    for S_ in Sfs + Sbs:
        fw.op("dve", lambda e: e.memset(S_[:], 0.0), W=[S_])
    orders = [list(range(NT)), [1, 0] + list(range(NT - 1, 1, -1))]
    seq = [(dr_, orders[dr_][step]) for step in range(NT) for dr_ in range(2)]
    for (dr, ci_) in seq:
        Sf, Sb = Sfs[dr], Sbs[dr]
        order = [ci_]
        r0 = 0 if dr == 0 else 32
        iu, ig, im = (0, 2, 0) if dr == 0 else (1, 3, 1)
        lastcol = 127 if dr == 0 else 0
        for ci in order:
            c0 = ci * 128
            z = zps()
            fw.op("pe", lambda e: e.matmul(z[:], lhsT=rT[r0:r0 + 16, c0:c0 + 128], rhs=wd[r0:r0 + 16, :], start=True, stop=True), R=[rT, wd], W=[z])
            zb = zbp()
            fw.op("dve", lambda e: e.tensor_tensor(out=zb[:], in0=z[:], in1=bdb[:, dr, :], op=ALU.add), R=[z, bdb], W=[zb])
            e1 = e1p()
            Lb = Lbp()
            fw.op("act", lambda e: e.activation(out=e1[:], in_=zb[:], func=AF.Exp, scale=-1.0), R=[zb], W=[e1])
            fw.op("act", lambda e: e.activation(out=Lb[:], in_=e1[:], func=AF.Ln, bias=1.0), R=[e1], W=[Lb])
            cp = cps()
            for h in range(4):
                fw.op("pe", lambda e, cp=cp, Lb=Lb, h=h: e.matmul(cp[:, h, :], lhsT=Lb[:, h * 64:(h + 1) * 64], rhs=trib[:, iu, :], start=True, stop=True), R=[Lb, trib], W=[cp])
            gp = gps()
            fw.op("pe", lambda e, gp=gp, Lb=Lb: e.matmul(gp[:], lhsT=trib[:, ig, :], rhs=Lb[:], start=True, stop=True), R=[Lb, trib], W=[gp])
            Eq = Eqp(); Ek = Ekp(); Est = Estp()
            fw.op("act", lambda e, cp=cp, Eq=Eq: e.activation(out=Eq[:], in_=cp[:], func=AF.Exp), R=[cp], W=[Eq])
            fw.op("act", lambda e, cp=cp, Ek=Ek: e.activation(out=Ek[:], in_=cp[:], func=AF.Exp, scale=-1.0), R=[cp], W=[Ek])
            fw.op("act", lambda e, gp=gp, Est=Est: e.activation(out=Est[:], in_=gp[:], func=AF.Exp), R=[gp], W=[Est])
            qin = qinp(); kin = kinp(); kst = kstp()
            fw.op("dve", lambda e, qin=qin, Eq=Eq, c0=c0: e.scalar_tensor_tensor(out=qin[:], in0=qT[:, :, c0:c0 + 128], scalar=0.125, in1=Eq[:], op0=ALU.mult, op1=ALU.mult), R=[qT, Eq], W=[qin])
            fw.op("dve", lambda e, kin=kin, Ek=Ek, c0=c0: e.tensor_tensor(out=kin[:], in0=kT[:, :, c0:c0 + 128], in1=Ek[:], op=ALU.mult), R=[kT, Ek], W=[kin])
            fw.op("dve", lambda e, kst=kst, Est=Est, ci=ci: e.tensor_tensor(out=kst[:], in0=kk[:, ci, :], in1=Est[:], op=ALU.mult), R=[kk, Est], W=[kst])
            ap_ = aps()
            for h in range(4):
                fw.op("pe", lambda e, ap_=ap_, kin=kin, qin=qin, h=h: e.matmul(ap_[:, h, :], lhsT=kin[:, h, :], rhs=qin[:, h, :], start=True, stop=True), R=[kin, qin], W=[ap_])
            att = attp()
            fw.op("dve", lambda e, att=att, ap_=ap_: e.tensor_tensor(out=att[:].rearrange("p h t -> p (h t)"), in0=ap_[:].rearrange("p h t -> p (h t)"), in1=maskf[:, im, :], op=ALU.mult), R=[ap_, maskf], W=[att])
            op_ = ops_()
            for h in range(4):
                fw.op("pe", lambda e, op_=op_, att=att, h=h, ci=ci: e.matmul(op_[:, h, :], lhsT=vv[:, ci, h * 128:(h + 1) * 128], rhs=att[:, h, :], start=True, stop=False), R=[vv, att], W=[op_])
                fw.op("pe", lambda e, op_=op_, qin=qin, h=h: e.matmul(op_[:, h, :], lhsT=Sb[:, h, :], rhs=qin[:, h, :], start=False, stop=True), R=[Sb, qin], W=[op_])
            osb = osbp()
            fw.op("act", lambda e, osb=osb, op_=op_: e.activation(out=osb[:], in_=op_[:], func=AF.Copy), R=[op_], W=[osb])
            fw.dma("sp", S["OG"][dr].rearrange("h v t -> v h t")[:, :, c0:c0 + 128], osb[:], R=[osb])
            kv = kvps()
            for h in range(4):
                fw.op("pe", lambda e, kv=kv, kst=kst, h=h, ci=ci: e.matmul(kv[:, h, :], lhsT=kst[:, h * 64:(h + 1) * 64], rhs=vv[:, ci, h * 128:(h + 1) * 128], start=True, stop=True), R=[kst, vv], W=[kv])
            for h in range(4):
                fw.op("dve", lambda e, kv=kv, Eq=Eq, h=h: e.scalar_tensor_tensor(out=Sf[:, h, :], in0=Sf[:, h, :], scalar=Eq[:, h, lastcol:lastcol + 1], in1=kv[:, h, :], op0=ALU.mult, op1=ALU.add), R=[Sf, Eq, kv], W=[Sf])
            fw.op("dve", lambda e: e.tensor_copy(out=Sb[:], in_=Sf[:]), R=[Sf], W=[Sb])
    self.end("gla%d" % l)


def _rms_finish(self, o, n, gcol, extra_scale, pms, onesf, rstdp, tp):
    fw = self.fw
    sq = tp()
    fw.op("dve", lambda e: e.tensor_tensor(out=sq[:, 0:n], in0=o[:, 0:n], in1=o[:, 0:n], op=ALU.mult), R=[o], W=[sq])
    ms = pms()
    fw.op("pe", lambda e: e.matmul(ms[:, 0:n], lhsT=onesf[:], rhs=sq[:, 0:n], start=True, stop=True), R=[onesf, sq], W=[ms])
    rstd = rstdp()
    fw.op("dve", lambda e: e.tensor_scalar_add(out=rstd[:, 0:n], in0=ms[:, 0:n], scalar1=RMS_EPS), R=[ms], W=[rstd])
    fw.op("act", lambda e: e.activation(out=rstd[:, 0:n], in_=rstd[:, 0:n], func=AF.Ln), R=[rstd], W=[rstd])
    fw.op("act", lambda e: e.activation(out=rstd[:, 0:n], in_=rstd[:, 0:n], func=AF.Exp, scale=-0.5), R=[rstd], W=[rstd])
    t = tp()
    fw.op("dve", lambda e: e.tensor_tensor(out=t[:, 0:n], in0=o[:, 0:n], in1=rstd[:, 0:n], op=ALU.mult), R=[o, rstd], W=[t])
    if "DBGC" in self.debug and not getattr(self, "_dbgc_done", False):
        self._dbgc_done = True
        self.scratch("DBGC", [4, 128, 256], F32)
        msb = tp()
        fw.op("dve", lambda e: e.tensor_copy(out=msb[:, 0:n], in_=ms[:, 0:n]), R=[ms], W=[msb])
        for k_, b_ in enumerate((o, sq, msb, rstd)):
            fw.dma("pool", self.S["DBGC"][k_], b_[:, 0:256], R=[b_])
    return t


def _ph_glac(self, l):
    fw, I, S, C = self.fw, self.I, self.S, self.C
    self.begin()
    self.warm([AF.Ln, AF.Exp])
    onesf = self.sb("onesf", [128, 128], F32)
    fw.op("dve", lambda e: e.memset(onesf[:], 1.0 / 128.0), W=[onesf])
    gcol = self.sb("gcol", [128, 1], F32)
    idf, idb = self.load_ident()
    self.diag_cols(gcol, gcol[:], I["gla_norm_g"][l:l + 1, :], 1, idf)
    ofp = self.pool("of", [128, 512], F32, 2)
    obp = self.pool("ob", [128, 512], F32, 2)
    ggp = self.pool("gg", [128, 512], BF16, 2)
    op_ = self.pool("o", [128, 512], F32, 2)
    tp = self.pool("t", [128, 512], F32, 4)
    rstdp = self.pool("rstd", [128, 512], F32, 2)
    pms = self.pool("pms", [128, 512], F32, 2, psum=True)
    outp = self.pool("outb", [128, 512], BF16, 2)
    chunks = [(0, 256)] + [(256 + 512 * j, 512) for j in range(8)]
    for h in range(4):
        for (c0, n) in chunks:
            of = ofp(); ob = obp(); gg = ggp()
            fw.dma("sp", of[:, 0:n], S["OG"][0, h, :, c0:c0 + n], W=[of])
            fw.dma("sp", ob[:, 0:n], S["OG"][1, h, :, c0:c0 + n], W=[ob])
            fw.dma("sp", gg[:, 0:n], S["GG"][h * 128:(h + 1) * 128, c0:c0 + n], W=[gg])
            o = op_()
            fw.op("dve", lambda e, o=o, of=of, ob=ob, n=n: e.tensor_tensor(out=o[:, 0:n], in0=of[:, 0:n], in1=ob[:, 0:n], op=ALU.add), R=[of, ob], W=[o])
            t = _rms_finish(self, o, n, gcol, 1.0, pms, onesf, rstdp, tp)
            ob_ = outp()
            fw.op("dve", lambda e, ob_=ob_, t=t, gg=gg, n=n: e.scalar_tensor_tensor(out=ob_[:, 0:n], in0=t[:, 0:n], scalar=gcol[:, 0:1], in1=gg[:, 0:n], op0=ALU.mult, op1=ALU.mult), R=[t, gcol, gg], W=[ob_])
            fw.dma("pool", S["BR"][0, h * 128:(h + 1) * 128, c0:c0 + n], ob_[:, 0:n], R=[ob_])
    self.end("glac%d" % l)


KB.ph_gla = _ph_gla
KB.ph_glac = _ph_glac


def _ph_diff(self, l):
    fw, I, S, C = self.fw, self.I, self.S, self.C
    lam_init = 0.8 - 0.6 * math.exp(-0.3 * l)
    self.begin()
    self.warm([AF.Exp, AF.Ln])
    KT = self.sb("KT", [128, 4, T], BF16)
    VV = self.sb("VV", [128, NT, 512], BF16)
    fw.dma("sp", KT[:], S["DKT"].rearrange("h d t -> d h t"), W=[KT])
    fw.dma("sp", VV[:], S["DV"].rearrange("(i p) n -> p i n", p=128), W=[VV])
    onesb = self.sb("onesb", [128, 128], BF16)
    fw.op("dve", lambda e: e.memset(onesb[:], 1.0), W=[onesb])
    onesf = self.sb("onesf", [128, 128], F32)
    fw.op("dve", lambda e: e.memset(onesf[:], 1.0 / 128.0), W=[onesf])
    gcol = self.sb("gcol", [128, 1], F32)
    idf, idb = self.load_ident()
    self.diag_cols(gcol, gcol[:], I["diff_norm_g"][l:l + 1, :], 1, idf)
    fw.op("dve", lambda e: e.tensor_scalar_mul(out=gcol[:], in0=gcol[:], scalar1=(1.0 - lam_init)), R=[gcol], W=[gcol])
    lq = self.sb("lq", [128, 128], F32)
    lk = self.sb("lk", [128, 128], F32)
    fw.dma("sp", lq[:], I["diff_lam_q"][l:l + 1].rearrange("o a b -> o (a b)").partition_broadcast(128), W=[lq])
    fw.dma("sp", lk[:], I["diff_lam_k"][l:l + 1].rearrange("o a b -> o (a b)").partition_broadcast(128), W=[lk])
    fw.op("dve", lambda e: e.tensor_tensor(out=lq[:], in0=lq[:], in1=lk[:], op=ALU.mult), R=[lq, lk], W=[lq])
    ls = self.sb("ls", [128, 2], F32)
    fw.op("dve", lambda e: e.reduce_sum(out=ls[:], in_=lq[:].rearrange("p (a b) -> p a b", a=2), axis=AX.X), R=[lq], W=[ls])
    fw.op("act", lambda e: e.activation(out=ls[:], in_=ls[:], func=AF.Exp), R=[ls], W=[ls])
    nlam = self.sb("nlam", [128, 1], F32)
    fw.op("dve", lambda e: e.tensor_tensor(out=nlam[:], in0=ls[:, 1:2], in1=ls[:, 0:1], op=ALU.subtract), R=[ls], W=[nlam])
    fw.op("dve", lambda e: e.tensor_scalar_add(out=nlam[:], in0=nlam[:], scalar1=-lam_init), R=[nlam], W=[nlam])

    qp = self.pool("q", [128, 512], BF16, 3)
    sps = self.pool("sps", [128, 512], F32, 5, psum=True)
    ops_ = self.pool("ops", [128, 512], F32, 2, psum=True)
    dps = self.pool("dps", [128, 512], F32, 1, psum=True)
    pms = dps
    ptp = self.pool("pt", [128, 512], BF16, 10)
    dacp = self.pool("dacc", [128, 512], F32, 4)
    ones1 = self.sb("ones1", [128, 128], F32)
    fw.op("dve", lambda e: e.memset(ones1[:], 1.0), W=[ones1])
    w0p = self.pool("w0", [128, 512], F32, 4)
    rcp = self.pool("rc", [128, 512], F32, 2)
    op_ = self.pool("o", [128, 512], F32, 2)
    tp = self.pool("t", [128, 512], F32, 4)
    rstdp = self.pool("rstd", [128, 512], F32, 2)
    outp = self.pool("outb", [128, 512], BF16, 2)
    chunks = [(0, 256, 2)] + [(256 + 512 * j, 512, NT) for j in range(8)]
    items = []
    for h in range(4):
        for (c0, n, nkt) in chunks:
            for kt in range(nkt):
                items.append((h, c0, n, nkt, kt))
    LA = 2
    st = {}
    cur = {}
    deferred = []

    def front(i):
        h, c0, n, nkt, kt = items[i]
        key = (h, c0)
        if key not in cur:
            q = qp()
            fw.dma("sp", q[:, 0:n], S["DQT"][h, :, c0:c0 + n], W=[q])
            cur[key] = {"q": q, "ws": []}
        q = cur[key]["q"]
        pts = []
        sp2 = [sps(), sps()]
        for comp in range(2):
            pb = comp * 64
            sp_ = sp2[comp]
            fw.op("pe", lambda e: e.matmul(sp_[:, 0:n], lhsT=KT[pb:pb + 64, h, kt * 128:(kt + 1) * 128], rhs=q[pb:pb + 64, 0:n], start=True, stop=True), R=[KT, q], W=[sp_])
        for comp in range(2):
            sp_ = sp2[comp]
            pt = ptp()
            fw.op("act", lambda e: e.activation(out=pt[:, 0:n], in_=sp_[:, 0:n], func=AF.Exp, scale=0.125), R=[sp_], W=[pt])
            pts.append(pt)
        st[i] = pts

    def back(i):
        h, c0, n, nkt, kt = items[i]
        key = (h, c0)
        c = cur[key]
        if kt == 0:
            c["o_ps"] = [ops_(), ops_()]
            c["dacc"] = [dacp(), dacp()]
        pts = st.pop(i)
        for comp in range(2):
            o_ps, dacc, pt = c["o_ps"][comp], c["dacc"][comp], pts[comp]
            fw.op("pe", lambda e: e.matmul(o_ps[:, 0:n], lhsT=VV[:, kt, h * 128:(h + 1) * 128], rhs=pt[:, 0:n], start=(kt == 0), stop=(kt == nkt - 1)), R=[VV, pt], W=[o_ps])
            if kt == 0:
                fw.op("dve", lambda e: e.tensor_copy(out=dacc[:, 0:n], in_=pt[:, 0:n]), R=[pt], W=[dacc])
            else:
                fw.op("dve", lambda e: e.tensor_tensor(out=dacc[:, 0:n], in0=dacc[:, 0:n], in1=pt[:, 0:n], op=ALU.add), R=[dacc, pt], W=[dacc])
        if kt == nkt - 1:
            for comp in range(2):
                o_ps, dacc = c["o_ps"][comp], c["dacc"][comp]
                d_ps = dps()
                fw.op("pe", lambda e: e.matmul(d_ps[:, 0:n], lhsT=ones1[:], rhs=dacc[:, 0:n], start=True, stop=True), R=[ones1, dacc], W=[d_ps])
                rc = rcp()
                fw.op("dve", lambda e: e.reciprocal(out=rc[:, 0:n], in_=d_ps[:, 0:n]), R=[d_ps], W=[rc])
                w = w0p()
                fw.op("dve", lambda e: e.tensor_tensor(out=w[:, 0:n], in0=o_ps[:, 0:n], in1=rc[:, 0:n], op=ALU.mult), R=[o_ps, rc], W=[w])
                c["ws"].append(w)
            if True:
                comp = 1
                ws = c["ws"]
                o = op_()
                fw.op("dve", lambda e: e.scalar_tensor_tensor(out=o[:, 0:n], in0=ws[1][:, 0:n], scalar=nlam[:, 0:1], in1=ws[0][:, 0:n], op0=ALU.mult, op1=ALU.add), R=[ws[0], ws[1], nlam], W=[o])
                sq = tp()
                fw.op("dve", lambda e: e.tensor_tensor(out=sq[:, 0:n], in0=o[:, 0:n], in1=o[:, 0:n], op=ALU.mult), R=[o], W=[sq])

                def tail(o=o, sq=sq, n=n, h=h, c0=c0):
                    ms = pms()
                    fw.op("pe", lambda e: e.matmul(ms[:, 0:n], lhsT=onesf[:], rhs=sq[:, 0:n], start=True, stop=True), R=[onesf, sq], W=[ms])
                    rstd = rstdp()
                    fw.op("dve", lambda e: e.tensor_scalar_add(out=rstd[:, 0:n], in0=ms[:, 0:n], scalar1=RMS_EPS), R=[ms], W=[rstd])
                    fw.op("act", lambda e: e.activation(out=rstd[:, 0:n], in_=rstd[:, 0:n], func=AF.Ln), R=[rstd], W=[rstd])
                    fw.op("act", lambda e: e.activation(out=rstd[:, 0:n], in_=rstd[:, 0:n], func=AF.Exp, scale=-0.5), R=[rstd], W=[rstd])
                    t = tp()
                    fw.op("dve", lambda e: e.tensor_tensor(out=t[:, 0:n], in0=o[:, 0:n], in1=rstd[:, 0:n], op=ALU.mult), R=[o, rstd], W=[t])
                    ob_ = outp()
                    fw.op("act", lambda e: e.activation(out=ob_[:, 0:n], in_=t[:, 0:n], func=AF.Identity, scale=gcol[:, 0:1]), R=[t, gcol], W=[ob_])
                    fw.dma("pool", S["BR"][1, h * 128:(h + 1) * 128, c0:c0 + n], ob_[:, 0:n], R=[ob_])
                deferred.append((i + 4, tail))
                del cur[key]

    nI = len(items)
    for i in range(nI + LA):
        if i < nI:
            front(i)
        if i - LA >= 0:
            back(i - LA)
        while deferred and deferred[0][0] <= i - LA:
            deferred.pop(0)[1]()
    while deferred:
        deferred.pop(0)[1]()
    self.end("diff%d" % l)


KB.ph_diff = _ph_diff


def _ph_na(self, l):
    fw, I, S, C = self.fw, self.I, self.S, self.C
    nc = self.nc
    self.begin()
    self.warm([AF.Exp])
    KT = self.sb("KT", [128, 4, T], BF16)
    VV = self.sb("VV", [128, NT, 512], BF16)
    fw.dma("sp", KT[:], S["NKT"].rearrange("g d t -> d g t"), W=[KT])
    fw.dma("sp", VV[:], S["NV"].rearrange("(i p) n -> p i n", p=128), W=[VV])
    onesb = self.sb("onesb", [128, 64], BF16)
    fw.op("dve", lambda e: e.memset(onesb[:], 1.0), W=[onesb])
    TAB = self.sb("TAB", [128, 16, 512], BF16)
    fw.op("dve", lambda e: e.memset(TAB[:], 0.0), W=[TAB])
    j64 = self.sb("j64", [64, 128], F32)
    fw.dma("sp", j64[:], C["j64"][:, :], W=[j64])
    win = self.sb("win", [128, 512], F32)
    fw.dma("sp", win[:], C["nawin"][:, :], W=[win])
    idf, idb = self.load_ident()
    r120 = self.sb("r120", [120, 31], F32)
    fw.dma("sp", r120[:], I["na_rpb"][l].rearrange("h a j -> (h a) j"), W=[r120])
    r31 = self.sb("r31", [31, 120], F32)
    tps = self.pool("tps", [128, 512], F32, 1, psum=True)
    pt0 = tps()
    fw.op("pe", lambda e: e.transpose(out=pt0[0:31, 0:120], in_=r120[:, :], identity=idf[0:120, 0:120]), R=[r120, idf], W=[pt0])
    fw.op("act", lambda e: e.activation(out=r31[:], in_=pt0[0:31, 0:120], func=AF.Copy), R=[pt0], W=[r31])
    p0 = tps()
    fw.op("pe", lambda e: e.matmul(p0[0:120, 0:31], lhsT=r31[:, :], rhs=j64[0:31, 33:64], start=True, stop=True), R=[r31, j64], W=[p0])
    rev = self.sb("rev", [120, 31], F32)
    fw.op("act", lambda e: e.activation(out=rev[:], in_=p0[0:120, 0:31], func=AF.Copy), R=[p0], W=[rev])
    zt = self.sb("zt", [120, 128], F32)
    fw.op("dve", lambda e: e.memset(zt[:], 0.0), W=[zt])
    nag = Buf("nag")
    NAG = S["NAG"]
    fw.dma("sp", NAG[:, :], zt[:], R=[zt], W=[nag])
    fw.dma("sp", NAG[:, 48:79], rev[:], R=[rev], W=[nag])
    Hk = self.sb("Hk", [64, 120, 64], F32)
    for n0 in range(0, 120, 8):
        hsrc = bass.AP(tensor=NAG.tensor, offset=NAG.offset + n0 * 128, ap=[[1, 64], [128, 8], [1, 64]])
        fw.dma("sp", Hk[:, n0:n0 + 8, :], hsrc, R=[nag], W=[Hk])
    Hk4 = Hk[:].rearrange("m (h a) c -> m h a c", h=8)
    ebp = self.pool("eb", [128, 512], F32, 2)
    for dr in range(15):
        p = tps()
        fw.op("pe", lambda e: e.matmul(p[:].rearrange("p (h c) -> p h c", h=8), lhsT=j64[:, :], rhs=Hk4[:, :, dr, :], start=True, stop=True), R=[j64, Hk], W=[p])
        eb = ebp()
        fw.op("act", lambda e: e.activation(out=eb[:], in_=p[:], func=AF.Exp), R=[p], W=[eb])
        if dr <= 13:
            fw.op("dve", lambda e: e.tensor_tensor(out=TAB[0:64, dr, :], in0=eb[0:64, :], in1=win[0:64, :], op=ALU.mult), R=[eb, win], W=[TAB])
        if dr == 10:
            fw.op("dve", lambda e: e.tensor_tensor(out=TAB[0:64, 15, :], in0=eb[0:64, :], in1=win[0:64, :], op=ALU.mult), R=[eb, win], W=[TAB])
        if dr >= 1:
            fw.op("dve", lambda e: e.tensor_tensor(out=TAB[64:128, dr - 1, :], in0=eb[64:128, :], in1=win[64:128, :], op=ALU.mult), R=[eb, win], W=[TAB])
        if dr == 3:
            fw.op("dve", lambda e: e.tensor_tensor(out=TAB[64:128, 14, :], in0=eb[64:128, :], in1=win[64:128, :], op=ALU.mult), R=[eb, win], W=[TAB])

    if self.stop_after == "natab":
        self.scratch("TAB", [128, 16, 512], BF16)
        fw.dma("pool", S["TAB"], TAB[:], R=[TAB])
        self.end("natab")
        return
    q8p = self.pool("q8", [128, 4, 512], BF16, 2)
    stgp = self.pool("stg", [64, 8, 512], BF16, 2)
    sps = self.pool("sps", [128, 512], F32, 4, psum=True)
    ops_ = self.pool("ops", [64, 512], F32, 2, psum=True)
    dps = self.pool("dps", [64, 512], F32, 1, psum=True)
    ptp = self.pool("pt", [128, 512], BF16, 16)
    rcp = self.pool("rc", [64, 512], F32, 2)
    BRv = S["BR"][2].rearrange("(h d) t -> d h t", h=8)
    NQv = S["NQT"].rearrange("g d t -> d g t")
    ntoggle = 0
    for grp in range(T // 512 + 1):
        if grp == 0:
            tok0, nrows = 0, 4
        else:
            tok0, nrows = 256 + (grp - 1) * 512, 8
        ntok = nrows * 64
        q8 = q8p()
        stg = stgp()
        fw.dma("sp", q8[:, :, 0:ntok], NQv[:, :, tok0:tok0 + ntok], W=[q8])
        for rr in range(nrows):
            qoff = rr * 64
            tiles = [(0, None), (1, None)]
            if grp > 0:
                r = (grp - 1) * 8 + rr
                rs = min(max(r - 4, 0), 56)
                if rs % 2 == 0:
                    for j in range(4):
                        tiles.append((2 + rs // 2 + j, rs + 2 * j - r + 7))
                else:
                    tiles.append((2 + (rs - 1) // 2, 14))
                    for j in range(3):
                        tiles.append((2 + (rs + 1) // 2 + j, rs + 1 + 2 * j - r + 7))
                    tiles.append((2 + (rs + 7) // 2, 15))
            P = []
            for (kt, slot) in tiles:
                spe = sps()
                spo = sps()
                for h in range(8):
                    pb = (h % 2) * 64
                    dstp = spe if h % 2 == 0 else spo
                    fw.op("pe", lambda e: e.matmul(dstp[:, (h // 2) * 64:(h // 2 + 1) * 64], lhsT=KT[pb:pb + 64, h // 2, kt * 128:(kt + 1) * 128], rhs=q8[pb:pb + 64, h // 2, qoff:qoff + 64], start=True, stop=True), R=[KT, q8], W=[dstp])
                pt = ptp()
                ptv = pt[:].rearrange("p (g two q) -> p g two q", two=2, q=64)
                fw.op("act", lambda e: e.activation(out=ptv[:, :, 0, :], in_=spe[:, 0:256].rearrange("p (g q) -> p g q", q=64), func=AF.Exp, scale=0.125), R=[spe], W=[pt])
                fw.op("act", lambda e: e.activation(out=ptv[:, :, 1, :], in_=spo[:, 0:256].rearrange("p (g q) -> p g q", q=64), func=AF.Exp, scale=0.125), R=[spo], W=[pt])
                if slot is not None:
                    assert 0 <= slot <= 15
                    eng = "dve"
                    ntoggle += 1
                    fw.op(eng, lambda e: e.tensor_tensor(out=pt[:], in0=pt[:], in1=TAB[:, slot, :], op=ALU.mult), R=[pt, TAB], W=[pt])
                P.append((kt, pt))
            o_ps = ops_()
            d_ps = dps()
            nk = len(P)
            for h in range(8):
                for i, (kt, pt) in enumerate(P):
                    fw.op("pe", lambda e: e.matmul(o_ps[:, h * 64:(h + 1) * 64], lhsT=VV[:, kt, h * 64:(h + 1) * 64], rhs=pt[:, h * 64:(h + 1) * 64], start=(i == 0), stop=(i == nk - 1)), R=[VV, pt], W=[o_ps])
            for i, (kt, pt) in enumerate(P):
                fw.op("pe", lambda e: e.matmul(d_ps[:], lhsT=onesb[:], rhs=pt[:], start=(i == 0), stop=(i == nk - 1)), R=[onesb, pt], W=[d_ps])
            rc = rcp()
            fw.op("dve", lambda e: e.reciprocal(out=rc[:], in_=d_ps[:]), R=[d_ps], W=[rc])
            fw.op("dve", lambda e: e.tensor_tensor(out=stg[:, :, qoff:qoff + 64], in0=o_ps[:].rearrange("p (h q) -> p h q", h=8), in1=rc[:].rearrange("p (h q) -> p h q", h=8), op=ALU.mult), R=[o_ps, rc], W=[stg])
        fw.dma("pool", BRv[:, :, tok0:tok0 + ntok], stg[:, :, 0:ntok], R=[stg])
    self.end("na%d" % l)


KB.ph_na = _ph_na


def _ln_epilogue(self, ysrc, xt, mbc, gbc, bbc, pools, dst_ap):
    fw = self.fw
    up, stp, mvp, xnp = pools
    u = up()
    for hf in range(2):
        yb, yap = ysrc[hf]
        fw.op("dve", lambda e: e.tensor_tensor(out=u[:, hf * 512:(hf + 1) * 512], in0=yap, in1=mbc[:, hf * 512:(hf + 1) * 512], op=ALU.mult), R=[yb, mbc], W=[u])
    fw.op("dve", lambda e: e.scalar_tensor_tensor(out=u[:], in0=xt[:], scalar=ALPHA, in1=u[:], op0=ALU.mult, op1=ALU.add), R=[xt, u], W=[u])
    st = stp()
    for hf in range(2):
        fw.op("dve", lambda e: e.bn_stats(out=st[:, hf, :], in_=u[:, hf * 512:(hf + 1) * 512]), R=[u], W=[st])
    mv = mvp()
    fw.op("dve", lambda e: e.bn_aggr(out=mv[:, 0:2], in_=st[:].rearrange("p a b -> p (a b)")), R=[st], W=[mv])
    fw.op("dve", lambda e: e.tensor_scalar_add(out=mv[:, 2:3], in0=mv[:, 1:2], scalar1=LN_EPS), R=[mv], W=[mv])
    fw.op("act", lambda e: e.activation(out=mv[:, 2:3], in_=mv[:, 2:3], func=AF.Ln), R=[mv], W=[mv])
    fw.op("act", lambda e: e.activation(out=mv[:, 2:3], in_=mv[:, 2:3], func=AF.Exp, scale=-0.5), R=[mv], W=[mv])
    fw.op("dve", lambda e: e.scalar_tensor_tensor(out=mv[:, 3:4], in0=mv[:, 0:1], scalar=-1.0, in1=mv[:, 2:3], op0=ALU.mult, op1=ALU.mult), R=[mv], W=[mv])
    xn = xnp()
    fw.op("act", lambda e: e.activation(out=xn[:], in_=u[:], func=AF.Identity, bias=mv[:, 3:4], scale=mv[:, 2:3]), R=[u, mv], W=[xn])
    fw.op("dve", lambda e: e.tensor_tensor(out=xn[:], in0=xn[:], in1=gbc[:], op=ALU.mult), R=[xn, gbc], W=[xn])
    fw.op("dve", lambda e: e.tensor_tensor(out=xn[:], in0=xn[:], in1=bbc[:], op=ALU.add), R=[xn, bbc], W=[xn])
    fw.dma("pool", dst_ap, xn[:], R=[xn])


def _bc_tile(self, name, row_ap):
    t = self.sb(name, [128, D], F32)
    self.fw.dma("sp", t[:], row_ap.partition_broadcast(128), W=[t])
    return t


def _ph_merge(self, l):
    fw, I, S, C = self.fw, self.I, self.S, self.C
    self.begin()
    self.warm([AF.Ln, AF.Exp])
    wfp = self.pool("wf", [128, 1024], F32, 2)
    wbr = self.sb("wbr", [128, 3, 4, 1024], BF16)
    wo = self.sb("wo", [128, 8, 1024], BF16)
    k = 0
    for n_ in range(3):
        for ec in range(4):
            wf = wfp()
            fw.dma("sp", wf[:], I["w_branch"][l, n_, ec * 128:(ec + 1) * 128, :], W=[wf])
            if k % 2 == 0:
                fw.op("dve", lambda e: e.tensor_copy(out=wbr[:, n_, ec, :], in_=wf[:]), R=[wf], W=[wbr])
            else:
                fw.op("act", lambda e: e.activation(out=wbr[:, n_, ec, :], in_=wf[:], func=AF.Copy), R=[wf], W=[wbr])
            k += 1
    for dc in range(8):
        wf = wfp()
        fw.dma("sp", wf[:], I["w_o"][l, dc * 128:(dc + 1) * 128, :], W=[wf])
        if dc % 2 == 0:
            fw.op("dve", lambda e: e.tensor_copy(out=wo[:, dc, :], in_=wf[:]), R=[wf], W=[wo])
        else:
            fw.op("act", lambda e: e.activation(out=wo[:, dc, :], in_=wf[:], func=AF.Copy), R=[wf], W=[wo])
    m2 = [_bc_tile(self, "m2L", S["MODR"][l, 0, 0:1, :]), _bc_tile(self, "m2C", S["MODR"][l, 1, 0:1, :])]
    gbc = _bc_tile(self, "gbc", I["ln_g"][l, 0:1, :])
    bbc = _bc_tile(self, "bbc", I["ln_b"][l, 0:1, :])
    brp = self.pool("br", [128, 3, 4, 512], BF16, 2)
    gtp = self.pool("gt", [128, 3, 512], BF16, 2)
    pj = self.pool("pj", [128, 512], F32, 3, psum=True)
    yps = self.pool("yps", [128, 512], F32, 4, psum=True)
    accp = self.pool("acc", [128, 512], F32, 2)
    tmpp = self.pool("tmp", [128, 512], F32, 2)
    mTp = self.pool("mT", [128, 8, 512], BF16, 2)
    xtp = self.pool("xt", [128, D], F32, 2)
    pools = (self.pool("u", [128, D], F32, 2), self.pool("st", [128, 2, 6], F32, 2), self.pool("mv", [128, 4], F32, 2), self.pool("xn", [128, D], F32, 2))
    chunks = [(0, 256)] + [(256 + 512 * j, 512) for j in range(8)]
    BRv = S["BR"].rearrange("n (ec p) t -> p n ec t", p=128)
    GTv = S["GT"].rearrange("n (dc p) t -> p n dc t", p=128)
    for (c0, n) in chunks:
        br = brp()
        for n_ in range(3):
            fw.dma("sp", br[:, n_, :, 0:n], BRv[:, n_, :, c0:c0 + n], W=[br])
        mT = mTp()
        for dc in range(8):
            gt = gtp()
            fw.dma("sp", gt[:, :, 0:n], GTv[:, :, dc, c0:c0 + n], W=[gt])
            acc = accp()
            for n_ in range(3):
                p = pj()
                for ec in range(4):
                    fw.op("pe", lambda e: e.matmul(p[:, 0:n], lhsT=wbr[:, n_, ec, dc * 128:(dc + 1) * 128], rhs=br[:, n_, ec, 0:n], start=(ec == 0), stop=(ec == 3)), R=[wbr, br], W=[p])
                if n_ == 0:
                    fw.op("dve", lambda e: e.tensor_tensor(out=acc[:, 0:n], in0=p[:, 0:n], in1=gt[:, 0, 0:n], op=ALU.mult), R=[p, gt], W=[acc])
                else:
                    tmp = tmpp()
                    fw.op("dve", lambda e: e.tensor_tensor(out=tmp[:, 0:n], in0=p[:, 0:n], in1=gt[:, n_, 0:n], op=ALU.mult), R=[p, gt], W=[tmp])
                    if n_ == 1:
                        fw.op("dve", lambda e: e.tensor_tensor(out=acc[:, 0:n], in0=acc[:, 0:n], in1=tmp[:, 0:n], op=ALU.add), R=[acc, tmp], W=[acc])
                    else:
                        fw.op("dve", lambda e: e.tensor_tensor(out=mT[:, dc, 0:n], in0=acc[:, 0:n], in1=tmp[:, 0:n], op=ALU.add), R=[acc, tmp], W=[mT])
        for tl in range(n // 128):
            ti = c0 // 128 + tl
            ys = []
            for hf in range(2):
                y = yps()
                for dc in range(8):
                    fw.op("pe", lambda e: e.matmul(y[:], lhsT=mT[:, dc, tl * 128:(tl + 1) * 128], rhs=wo[:, dc, hf * 512:(hf + 1) * 512], start=(dc == 0), stop=(dc == 7)), R=[mT, wo], W=[y])
                ys.append((y, y[:]))
            xt = xtp()
            fw.dma("sp", xt[:], self.x_src(l, ti), W=[xt])
            _ln_epilogue(self, ys, xt, m2[1 if ti < 2 else 0], gbc, bbc, pools, S["XS"][ti * 128:(ti + 1) * 128, :])
    self.end("merge%d" % l)


KB.ph_merge = _ph_merge


def _ph_moe(self, l, last):
    fw, I, S, C = self.fw, self.I, self.S, self.C
    self.begin()
    self.warm([AF.Sigmoid, AF.Silu])
    idf, idb = self.load_ident()
    modf = self.sb("modf", [128, 48, 2], F32)
    fw.dma("sp", modf[:], S["MODF"][l], W=[modf])
    m5 = [_bc_tile(self, "m5L", S["MODR"][l, 0, 1:2, :]), _bc_tile(self, "m5C", S["MODR"][l, 1, 1:2, :])]
    gbc = _bc_tile(self, "gbc", I["ln_g"][l, 1:2, :])
    bbc = _bc_tile(self, "bbc", I["ln_b"][l, 1:2, :])
    wr = self.sb("wr", [128, 8, 16], F32)
    fw.dma("sp", wr[:], I["w_router"].rearrange("(kc p) e -> p kc e", p=128), W=[wr])
    brt = self.sb("brt", [128, 16], F32)
    fw.dma("sp", brt[:], I["b_router"].rearrange("(o e) -> o e", o=1).partition_broadcast(128), W=[brt])
    self_f = self.sb("self", [16, 16, 128], F32)
    selb = self.sb("selb", [16, 16, 128], BF16)
    fw.dma("sp", self_f[:], C["sel"], W=[self_f])
    fw.op("dve", lambda e: e.tensor_copy(out=selb[:], in_=self_f[:]), R=[self_f], W=[selb])

    NG = 12
    hT = self.sb("hT", [128, 8, NG * 128], BF16)
    Y = self.sb("Y", [128, NG, D], F32)
    aff = self.sb("aff", [128, NG, 16], F32)
    selv = self.sb("selv", [128, NG, 16], F32)
    comb = self.sb("comb", [128, NG, 16], F32)
    cT = self.sb("cT", [16, NG * 128], BF16)
    r1 = self.sb("r1", [128, NG * 4], F32)
    r2 = self.sb("r2", [128, NG * 4], F32)
    gs = self.sb("gs", [128, NG * 4], F32)
    gmx = self.sb("gmx", [128, NG], F32)
    gmask = self.sb("gmask", [128, NG * 4], F32)
    is1 = self.sb("is1", [128, NG * 4, 4], F32)
    v2 = self.sb("v2", [128, NG * 4, 4], F32)
    oh = self.sb("oh", [128, NG * 4, 4], F32)
    ssum = self.sb("ssum", [128, NG], F32)

    xtp = self.pool("xt", [128, D], F32, 2)
    h32p = self.pool("h32", [128, 8, 128], F32, 2)
    sml = self.pool("sml", [128, 512], F32, 1, psum=True)
    gen = self.pool("gen", [128, 512], F32, 7, psum=True)
    ptr = gen
    gup = gen
    yps = gen
    wgp = self.pool("wg", [128, 8, 512], BF16, 2)
    wup = self.pool("wu", [128, 8, 512], BF16, 2)
    wdp = self.pool("wd", [128, 4, 1024], BF16, 2)
    cbp = self.pool("cb", [128, 512], F32, 2)
    sgp = self.pool("sg", [128, 512], F32, 2)
    a1p = self.pool("a1", [128, 512], F32, 2)
    aTp = self.pool("aT", [128, 4, 512], BF16, 2)
    pools = (self.pool("u", [128, D], F32, 1), self.pool("st", [128, 2, 6], F32, 2), self.pool("mv", [128, 4], F32, 2), self.pool("xn", [128, D], F32, 2))
    castk = [0]

    def cast(dst_ap, dstb, src_ap, srcb):
        if castk[0] % 2 == 0:
            fw.op("dve", lambda e: e.tensor_copy(out=dst_ap, in_=src_ap), R=[srcb], W=[dstb])
        else:
            fw.op("act", lambda e: e.activation(out=dst_ap, in_=src_ap, func=AF.Copy), R=[srcb], W=[dstb])
        castk[0] += 1

    for (g0, g1) in ((0, 12), (12, 24), (24, 34)):
        ng = g1 - g0
        for i in range(g0, g1):
            j = i - g0
            st_ = 1 if i < 2 else 0
            xt = xtp()
            fw.dma("sp", xt[:], S["XS"][i * 128:(i + 1) * 128, :], W=[xt])
            h32 = h32p()
            for hf in range(2):
                p = ptr()
                for k4 in range(4):
                    kc = hf * 4 + k4
                    fw.op("pe", lambda e: e.transpose(out=p[:, k4 * 128:(k4 + 1) * 128], in_=xt[:, kc * 128:(kc + 1) * 128], identity=idf[:]), R=[xt, idf], W=[p])
                for k4 in range(4):
                    kc = hf * 4 + k4
                    fw.op("act", lambda e: e.activation(out=h32[:, kc, :], in_=p[:, k4 * 128:(k4 + 1) * 128], func=AF.Identity, bias=modf[:, 24 + kc, st_:st_ + 1], scale=modf[:, 32 + kc, st_:st_ + 1]), R=[p, modf], W=[h32])
            fw.op("dve", lambda e: e.tensor_copy(out=hT[:, :, j * 128:(j + 1) * 128], in_=h32[:]), R=[h32], W=[hT])
            lg = sml()
            for kc in range(8):
                fw.op("pe", lambda e: e.matmul(lg[:, 0:16], lhsT=h32[:, kc, :], rhs=wr[:, kc, :], start=(kc == 0), stop=(kc == 7)), R=[h32, wr], W=[lg])
            fw.op("act", lambda e: e.activation(out=aff[:, j, :], in_=lg[:, 0:16], func=AF.Sigmoid), R=[lg], W=[aff])
            fw.op("dve", lambda e: e.tensor_tensor(out=selv[:, j, :], in0=aff[:, j, :], in1=brt[:], op=ALU.add), R=[aff, brt], W=[selv])
        n4 = ng * 4
        s4 = selv[:, 0:ng, :].rearrange("p g (q e) -> p (g q) e", e=4)
        first = True
        for (a, b) in ((0, 1), (0, 2), (0, 3), (1, 2), (1, 3), (2, 3)):
            if first:
                fw.op("dve", lambda e: e.tensor_tensor(out=gs[:, 0:n4], in0=s4[:, :, a], in1=s4[:, :, b], op=ALU.add), R=[selv], W=[gs])
                first = False
            else:
                fw.op("dve", lambda e: e.tensor_tensor(out=r1[:, 0:n4], in0=s4[:, :, a], in1=s4[:, :, b], op=ALU.add), R=[selv], W=[r1])
                fw.op("dve", lambda e: e.tensor_tensor(out=gs[:, 0:n4], in0=gs[:, 0:n4], in1=r1[:, 0:n4], op=ALU.max), R=[gs, r1], W=[gs])
        fw.op("dve", lambda e: e.reduce_max(out=gmx[:, 0:ng], in_=gs[:, 0:n4].rearrange("p (g q) -> p g q", q=4), axis=AX.X), R=[gs], W=[gmx])
        for q in range(4):
            fw.op("dve", lambda e: e.tensor_tensor(out=gmask[:, 0:n4].rearrange("p (g q) -> p g q", q=4)[:, :, q], in0=gs[:, 0:n4].rearrange("p (g q) -> p g q", q=4)[:, :, q], in1=gmx[:, 0:ng], op=ALU.is_ge), R=[gs, gmx], W=[gmask])
        fw.op("dve", lambda e: e.reduce_max(out=r1[:, 0:n4], in_=s4, axis=AX.X), R=[selv], W=[r1])
        for ee in range(4):
            fw.op("dve", lambda e: e.tensor_tensor(out=is1[:, 0:n4, ee], in0=s4[:, :, ee], in1=r1[:, 0:n4], op=ALU.is_ge), R=[selv, r1], W=[is1])
        fw.op("dve", lambda e: e.scalar_tensor_tensor(out=v2[:, 0:n4, :], in0=is1[:, 0:n4, :], scalar=-1.0e9, in1=s4, op0=ALU.mult, op1=ALU.add), R=[is1, selv], W=[v2])
        fw.op("dve", lambda e: e.reduce_max(out=r2[:, 0:n4], in_=v2[:, 0:n4, :], axis=AX.X), R=[v2], W=[r2])
        for ee in range(4):
            fw.op("dve", lambda e: e.tensor_tensor(out=oh[:, 0:n4, ee], in0=v2[:, 0:n4, ee], in1=r2[:, 0:n4], op=ALU.is_ge), R=[v2, r2], W=[oh])
        fw.op("dve", lambda e: e.tensor_tensor(out=oh[:, 0:n4, :], in0=oh[:, 0:n4, :], in1=is1[:, 0:n4, :], op=ALU.add), R=[oh, is1], W=[oh])
        for ee in range(4):
            fw.op("dve", lambda e: e.tensor_tensor(out=oh[:, 0:n4, ee], in0=oh[:, 0:n4, ee], in1=gmask[:, 0:n4], op=ALU.mult), R=[oh, gmask], W=[oh])
        ohv = oh[:, 0:n4, :].rearrange("p (g q) e -> p g (q e)", q=4)
        fw.op("dve", lambda e: e.tensor_tensor(out=comb[:, 0:ng, :], in0=aff[:, 0:ng, :], in1=ohv, op=ALU.mult), R=[aff, oh], W=[comb])
        fw.op("dve", lambda e: e.reduce_sum(out=ssum[:, 0:ng], in_=comb[:, 0:ng, :], axis=AX.X), R=[comb], W=[ssum])
        fw.op("dve", lambda e: e.reciprocal(out=ssum[:, 0:ng], in_=ssum[:, 0:ng]), R=[ssum], W=[ssum])
        for j in range(ng):
            fw.op("dve", lambda e: e.tensor_scalar_mul(out=comb[:, j, :], in0=comb[:, j, :], scalar1=ssum[:, j:j + 1]), R=[comb, ssum], W=[comb])
            pc = sml()
            fw.op("pe", lambda e: e.transpose(out=pc[0:16, 0:128], in_=comb[:, j, :], identity=idf[:]), R=[comb, idf], W=[pc])
            fw.op("act", lambda e: e.activation(out=cT[:, j * 128:(j + 1) * 128], in_=pc[0:16, 0:128], func=AF.Copy), R=[pc], W=[cT])
        gchunks = []
        c = 0
        while c < ng * 128:
            n = min(512, ng * 128 - c)
            gchunks.append((c, n))
            c += n
        mitems = [(ex, c0, n) for ex in range(16) for (c0, n) in gchunks]
        wts = {}
        mst = {}

        def load_w(ex):
            wg = wgp(); wu = wup(); wd = wdp()
            for (wdst, src) in ((wg, I["w_exp_gate"][l, ex]), (wu, I["w_exp_up"][l, ex])):
                for hf in range(2):
                    fw.dma("pool", wdst[:, hf * 4:(hf + 1) * 4, :], src[hf * 512:(hf + 1) * 512, :].rearrange("(k p) f -> p k f", p=128), W=[wdst])
            for hf in range(2):
                fw.dma("pool", wd[:, hf * 2:(hf + 1) * 2, :], I["w_exp_down"][l, ex, hf * 256:(hf + 1) * 256, :].rearrange("(k p) d -> p k d", p=128), W=[wd])
            wts[ex] = (wg, wu, wd)

        def mfront(i):
            ex, c0, n = mitems[i]
            if ex not in wts:
                load_w(ex)
            wg, wu, wd = wts[ex]
            cbps = sml()
            fw.op("pe", lambda e: e.matmul(cbps[:, 0:n], lhsT=selb[:, ex, :], rhs=cT[:, c0:c0 + n], start=True, stop=True), R=[selb, cT], W=[cbps])
            cb = cbp()
            fw.op("act", lambda e: e.activation(out=cb[:, 0:n], in_=cbps[:, 0:n], func=AF.Copy), R=[cbps], W=[cb])
            aT = aTp()
            for fc in range(4):
                gp = gup(); up_ = gup()
                for kc in range(8):
                    fw.op("pe", lambda e: e.matmul(gp[:, 0:n], lhsT=wg[:, kc, fc * 128:(fc + 1) * 128], rhs=hT[:, kc, c0:c0 + n], start=(kc == 0), stop=(kc == 7)), R=[wg, hT], W=[gp])
                for kc in range(8):
                    fw.op("pe", lambda e: e.matmul(up_[:, 0:n], lhsT=wu[:, kc, fc * 128:(fc + 1) * 128], rhs=hT[:, kc, c0:c0 + n], start=(kc == 0), stop=(kc == 7)), R=[wu, hT], W=[up_])
                sg = sgp()
                fw.op("act", lambda e: e.activation(out=sg[:, 0:n], in_=gp[:, 0:n], func=AF.Silu), R=[gp], W=[sg])
                a1 = a1p()
                fw.op("dve", lambda e: e.tensor_tensor(out=a1[:, 0:n], in0=up_[:, 0:n], in1=sg[:, 0:n], op=ALU.mult), R=[up_, sg], W=[a1])
                fw.op("dve", lambda e: e.tensor_tensor(out=aT[:, fc, 0:n], in0=a1[:, 0:n], in1=cb[:, 0:n], op=ALU.mult), R=[a1, cb], W=[aT])
            mst[i] = aT

        def mback(i):
            ex, c0, n = mitems[i]
            wg, wu, wd = wts[ex]
            aT = mst.pop(i)
            for tl in range(n // 128):
                j = c0 // 128 + tl
                for hf in range(2):
                    y = yps()
                    for fc in range(4):
                        fw.op("pe", lambda e: e.matmul(y[:], lhsT=aT[:, fc, tl * 128:(tl + 1) * 128], rhs=wd[:, fc, hf * 512:(hf + 1) * 512], start=(fc == 0), stop=(fc == 3)), R=[aT, wd], W=[y])
                    if ex == 0:
                        fw.op("act", lambda e: e.activation(out=Y[:, j, hf * 512:(hf + 1) * 512], in_=y[:], func=AF.Copy), R=[y], W=[Y])
                    else:
                        fw.op("dve", lambda e: e.tensor_tensor(out=Y[:, j, hf * 512:(hf + 1) * 512], in0=y[:], in1=Y[:, j, hf * 512:(hf + 1) * 512], op=ALU.add), R=[y, Y], W=[Y])

        nM = len(mitems)
        nch = len(gchunks)
        load_w(0)
        for i in range(nM + 1):
            if i < nM:
                mfront(i)
                ex_i = mitems[i][0]
                if (i % nch) == min(1, nch - 1) and ex_i + 1 < 16 and i >= 1:
                    mback(i - 1)
                    load_w(ex_i + 1)
                    continue
            if i >= 1:
                mback(i - 1)
        for i in range(g0, g1):
            j = i - g0
            xt = xtp()
            fw.dma("sp", xt[:], S["XS"][i * 128:(i + 1) * 128, :], W=[xt])
            if last and i >= 2:
                dst = self.out[(i - 2) * 128:(i - 1) * 128, :]
            else:
                dst = S["XS"][i * 128:(i + 1) * 128, :]
            ys = [(Y, Y[:, j, 0:512]), (Y, Y[:, j, 512:1024])]
            _ln_epilogue(self, ys, xt, m5[1 if i < 2 else 0], gbc, bbc, pools, dst)
    self.end("moe%d" % l)


KB.ph_moe = _ph_moe
```

```python
import contextlib
import math
import numpy as np
import ml_dtypes
import concourse.bass as bass
import concourse.mybir as mybir
from concourse.bass_utils import run_bass_kernel_spmd

F32 = mybir.dt.float32
BF16 = mybir.dt.bfloat16
AF = mybir.ActivationFunctionType
ALU = mybir.AluOpType
AX = mybir.AxisListType

D = 1024
LC = 256
LL = 4096
T = LC + LL
NT = T // 128
DEPTH = 4
D_IN = 7712
ALPHA = (2 * DEPTH) ** 0.25
LN_EPS = 1e-5
RMS_EPS = 1e-6
O_GQ, O_GK, O_GV, O_GG, O_GR = 0, 256, 512, 1024, 1536
O_DQ, O_DK, O_DV = 1568, 2080, 2592
O_NQ, O_NK, O_NV = 3104, 3616, 4128
O_GT = 4640


class Buf:
    __slots__ = ("name", "t", "last_write", "reads", "excl")

    def __init__(self, name, t=None, excl=False):
        self.name = name
        self.t = t
        self.last_write = None
        self.reads = {}
        self.excl = excl

    def __getitem__(self, key):
        return self.t[key]


class _Op:
    __slots__ = ("fn", "waits", "tok", "signal", "val")

    def __init__(self, fn):
        self.fn = fn
        self.waits = []
        self.tok = None
        self.signal = False
        self.val = None


class _Rec:
    call = None

    def __getattr__(self, name):
        def f(*a, **k):
            self.call = (name, a, k)
            return self
        return f


class _Eng:
    def __init__(self, name):
        self.name = name
        self.ops = []
        self.seen = {}
        self.is_pe = name == "pe"
        self.sem = None
        self.dsems = []
        self.dcount = []
        self.drr = 0
        self.emitted = 0
        self.sigcount = 0


class FW:
    ENGS = ("pe", "act", "dve", "pool", "sp")

    def __init__(self, nc, st, n_dma_sems=16):
        self.nc = nc
        self.e = {n: _Eng(n) for n in self.ENGS}
        self.n_dma_sems = n_dma_sems
        for en in self.ENGS:
            E = self.e[en]
            E.sem = st.enter_context(nc.semaphore("s_" + en))
            if en in ("sp", "pool", "act"):
                E.dcount = [0] * n_dma_sems
                E.dsems = [st.enter_context(nc.semaphore("d_%s_%d" % (en, i))) for i in range(n_dma_sems)]

    def _deps(self, E, reads, writes):
        deps = {}

        def add(tok):
            if tok is None:
                return
            if tok[0] == "c":
                if E.is_pe and tok[1] == "pe":
                    return
                key = ("c", tok[1])
            else:
                key = ("d", tok[1])
            if key not in deps or deps[key][2] < tok[2]:
                deps[key] = tok

        for b in reads:
            add(b.last_write)
            if b.excl:
                for t in b.reads.values():
                    add(t)
        for b in writes:
            add(b.last_write)
            for t in b.reads.values():
                add(t)
        out = []
        for key, tok in deps.items():
            if E.seen.get(key, -1) >= tok[2]:
                continue
            E.seen[key] = tok[2]
            out.append(tok)
        return out

    def _commit(self, tok, reads, writes):
        for b in writes:
            b.last_write = tok
            b.reads = {}
        for b in reads:
            if b.excl:
                b.last_write = tok
                b.reads = {}
            else:
                b.reads[(tok[0], tok[1])] = tok

    def op(self, eng, fn, R=(), W=()):
        E = self.e[eng]
        rec = _Rec()
        fn(rec)
        call = rec.call
        o = _Op(lambda e, c=call: getattr(e, c[0])(*c[1], **c[2]))
        R = [b for b in R if b is not None]
        W = [b for b in W if b is not None]
        o.waits = self._deps(E, R, W)
        o.tok = ("c", eng, len(E.ops))
        E.ops.append(o)
        self._commit(o.tok, R, W)
        return o

    def dma(self, eng, out, in_, R=(), W=(), **kw):
        E = self.e[eng]
        o = _Op(lambda e: e.dma_start(out=out, in_=in_, **kw))
        R = [b for b in R if b is not None]
        W = [b for b in W if b is not None]
        o.waits = self._deps(E, R, W)
        i = E.drr
        E.drr = (E.drr + 1) % self.n_dma_sems
        key = ("d", (eng, i))
        prev = E.dcount[i]
        if prev > 0 and E.seen.get(key, -1) < prev:
            E.seen[key] = prev
            o.waits.append(("d", (eng, i), prev))
        E.dcount[i] = prev + 16
        o.tok = ("d", (eng, i), prev + 16)
        E.ops.append(o)
        self._commit(o.tok, R, W)
        return o

    def phase_end(self):
        nc = self.nc
        E = self.e["sp"]
        o = _Op(None)
        for en in self.ENGS:
            Q = self.e[en]
            for i, c in enumerate(Q.dcount):
                if c > 0 and E.seen.get(("d", (en, i)), -1) < c:
                    o.waits.append(("d", (en, i), c))
            if en != "sp":
                for q in reversed(Q.ops[Q.emitted:]):
                    if q.tok[0] == "c" and q.fn is not None:
                        o.waits.append(q.tok)
                        break
        o.tok = ("c", "sp", len(E.ops))
        E.ops.append(o)
        for en in self.ENGS:
            Q = self.e[en]
            for q in Q.ops[Q.emitted:]:
                for w in q.waits:
                    if w[0] == "c":
                        P = self.e[w[1]]
                        assert w[2] >= P.emitted, "wait on op of an earlier phase"
                        P.ops[w[2]].signal = True
        for en in self.ENGS:
            Q = self.e[en]
            for q in Q.ops[Q.emitted:]:
                if q.tok[0] == "c" and q.signal:
                    assert q.fn is not None
                    Q.sigcount += 1
                    q.val = Q.sigcount

        with nc.Block() as block:
            def run(en, eng):
                Q = self.e[en]
                for q in Q.ops[Q.emitted:]:
                    for w in q.waits:
                        if w[0] == "c":
                            P = self.e[w[1]]
                            eng.wait_ge(P.sem, P.ops[w[2]].val)
                        else:
                            qn, i = w[1]
                            eng.wait_ge(self.e[qn].dsems[i], w[2])
                    if q.fn is None:
                        continue
                    ins = q.fn(eng)
                    if q.tok[0] == "d":
                        qn, i = q.tok[1]
                        ins.then_inc(self.e[qn].dsems[i], 16)
                    elif q.signal:
                        ins.then_inc(Q.sem, 1)

            @block.tensor
            def _(eng):
                run("pe", eng)

            @block.scalar
            def _(eng):
                run("act", eng)

            @block.vector
            def _(eng):
                run("dve", eng)

            @block.gpsimd
            def _(eng):
                run("pool", eng)

            @block.sync
            def _(eng):
                run("sp", eng)

        nc.all_engine_barrier()
        nops = {en: len(self.e[en].ops) - self.e[en].emitted for en in self.ENGS}
        for en in self.ENGS:
            Q = self.e[en]
            Q.emitted = len(Q.ops)
        for en in self.ENGS:
            Q = self.e[en]
            for pn in self.ENGS:
                P = self.e[pn]
                Q.seen[("c", pn)] = len(P.ops) - 1
                for i, c in enumerate(P.dcount):
                    Q.seen[("d", (pn, i))] = c
        return nops


def _consts():
    c = {}
    c["ident"] = np.eye(128, dtype=np.float32)
    n = np.arange(LL)
    row = (n // 64).astype(np.float32)
    col = (n % 64).astype(np.float32)
    nf = 16
    inv = (10000.0 ** (-np.arange(nf, dtype=np.float32) / nf)).astype(np.float32)
    ang = np.concatenate([row[:, None] * inv, col[:, None] * inv], -1).astype(np.float32)
    cos = np.cos(ang).astype(np.float32)
    sin = np.sin(ang).astype(np.float32)
    ct = np.ones((128, T), np.float32)
    stb = np.zeros((128, T), np.float32)
    for p in range(128):
        j = p % 64
        f = j % 32
        ct[p, LC:] = cos[:, f]
        stb[p, LC:] = (-sin[:, f]) if j < 32 else sin[:, f]
    c["rope_c"] = ct
    c["rope_s"] = stb
    s = np.arange(128)[:, None]
    t = np.arange(128)[None, :]
    le = (s <= t).astype(np.float32)
    gt = (s > t).astype(np.float32)
    tri = np.stack([le, le.T, gt, gt.T]).astype(np.float32)
    c["tri"] = tri
    c["mask4"] = np.stack([np.tile(le, (1, 4)), np.tile(le.T, (1, 4))]).astype(np.float32)
    J = np.zeros((64, 128), np.float32)
    for m in range(64):
        J[m, 63 - m] = 1.0
        J[m, 64 + 63 - m] = 1.0
    c["j64"] = J
    cc = np.arange(64)
    cs = np.clip(cc - 8, 0, 48)
    W = ((cc[:, None] >= cs[None, :]) & (cc[:, None] < cs[None, :] + 16)).astype(np.float32)
    W2 = np.concatenate([W, W], 0)
    c["nawin"] = np.tile(W2[:, None, :], (1, 8, 1)).reshape(128, 512).astype(np.float32)
    sel = np.zeros((16, 16, 128), np.float32)
    for e in range(16):
        sel[e, e, :] = 1.0
    c["sel"] = sel
    return c


CONST_SHAPES = {
    "ident": [128, 128], "rope_c": [128, T], "rope_s": [128, T], "tri": [4, 128, 128],
    "mask4": [2, 128, 512], "j64": [64, 128], "nawin": [128, 512], "sel": [16, 16, 128],
}

INPUT_SHAPES = {
    "x": [LL, D], "c": [D], "ctx": [LC, D], "c_ctx": [D],
    "w_ada": [DEPTH, D, 6 * D], "b_ada": [DEPTH, 6 * D], "w_in": [DEPTH, D, D_IN],
    "gla_w_decay": [DEPTH, 2, 16, 256], "gla_b_decay": [DEPTH, 2, 256], "gla_norm_g": [DEPTH, 128],
    "diff_lam_q": [DEPTH, 2, 64], "diff_lam_k": [DEPTH, 2, 64], "diff_norm_g": [DEPTH, 128],
    "na_rpb": [DEPTH, 8, 15, 31], "w_branch": [DEPTH, 3, 512, D], "w_o": [DEPTH, D, D],
    "ln_g": [DEPTH, 2, D], "ln_b": [DEPTH, 2, D], "w_router": [D, 16], "b_router": [16],
    "w_exp_gate": [DEPTH, 16, D, 512], "w_exp_up": [DEPTH, 16, D, 512], "w_exp_down": [DEPTH, 16, 512, D],
}


class KB:
    def __init__(self, nlayers=DEPTH, debug=(), stop_after=None):
        self.nlayers = nlayers
        self.debug = set(debug)
        self.stop_after = stop_after
        self.nc = bass.Bass("TRN2", target_bir_lowering=False)
        nc = self.nc
        self.I = {k: nc.dram_tensor(k, v, F32, kind="ExternalInput").ap() for k, v in INPUT_SHAPES.items()}
        self.C = {k: nc.dram_tensor("k_" + k, v, F32, kind="ExternalInput").ap() for k, v in CONST_SHAPES.items()}
        self.out = nc.dram_tensor("out", [LL, D], F32, kind="ExternalOutput").ap()
        self.S = {}
        self.uid = 0

    def scratch(self, name, shape, dt):
        kind = "ExternalOutput" if (name in self.debug or name == "TAB") else "Internal"
        self.S[name] = self.nc.dram_tensor("s_" + name, shape, dt, kind=kind).ap()
        return self.S[name]

    def begin(self):
        self.pst = contextlib.ExitStack()
        self.pst.__enter__()
        self._cc = {}

    def warm(self, funcs):
        return
        fw = self.fw
        w = self.sb("warm", [128, 2048], F32)
        fw.op("dve", lambda e: e.memset(w[:], 1.0), W=[w])
        for f in funcs:
            for _ in range(2):
                fw.op("act", lambda e: e.activation(out=w[:], in_=w[:], func=f), R=[w], W=[w])
            fw.op("dve", lambda e: e.memset(w[:], 1.0), W=[w])

    def const_col(self, val):
        if val not in self._cc:
            t = self.sb("cc", [128, 1], F32)
            self.fw.op("dve", lambda e: e.memset(t[:], float(val)), W=[t])
            self._cc[val] = t
        return self._cc[val]

    def end(self, name=""):
        nops = self.fw.phase_end()
        self.pst.__exit__(None, None, None)
        print("phase", name, nops, flush=True)

    def sb(self, name, shape, dt):
        self.uid += 1
        return Buf(name, self.pst.enter_context(self.nc.sbuf_tensor("%s_%d" % (name, self.uid), shape, dt)))

    def ps(self, name, shape, dt=F32):
        self.uid += 1
        return Buf(name, self.pst.enter_context(self.nc.psum_tensor("%s_%d" % (name, self.uid), shape, dt)),
                   excl=True)

    def pool(self, name, shape, dt, n, psum=False):
        bufs = [(self.ps if psum else self.sb)(name + str(i), shape, dt) for i in range(n)]
        state = {"i": 0}

        def nxt():
            b = bufs[state["i"] % n]
            state["i"] += 1
            return b
        return nxt

    def build(self):
        nc = self.nc
        with contextlib.ExitStack() as gst:
            self.fw = FW(nc, gst)
            self.alloc_scratch()
            self.ph_mod()
            if self.stop_after == "mod":
                return nc
            for l in range(self.nlayers):
                self.ph_proj(l)
                if self.stop_after in ("proj", "ht"):
                    break
                self.ph_gla(l)
                self.ph_glac(l)
                if self.stop_after == "gla":
                    break
                self.ph_diff(l)
                if self.stop_after == "diff":
                    break
                self.ph_na(l)
                if self.stop_after in ("na", "natab"):
                    break
                self.ph_merge(l)
                if self.stop_after == "merge":
                    break
                self.ph_moe(l, last=(l == self.nlayers - 1))
        return nc

    def alloc_scratch(self):
        s = self.scratch
        s("XS", [T, D], F32)
        s("MODF", [DEPTH, 128, 48, 2], F32)
        s("MODR", [DEPTH, 2, 2, D], F32)
        s("GQT", [4, 64, T], BF16)
        s("GKT", [4, 64, T], BF16)
        s("GK", [T, 256], BF16)
        s("GV", [T, 512], BF16)
        s("GG", [512, T], BF16)
        s("GR", [64, T], BF16)
        s("DQT", [4, 128, T], BF16)
        s("DKT", [4, 128, T], BF16)
        s("DV", [T, 512], BF16)
        s("NQT", [4, 128, T], BF16)
        s("NKT", [4, 128, T], BF16)
        s("NV", [T, 512], BF16)
        s("GT", [3, D, T], BF16)
        s("OG", [2, 4, 128, T], F32)
        s("BR", [3, 512, T], BF16)
        s("NAG", [120, 128], F32)

    def ph_mod(self):
        fw, I = self.fw, self.I
        self.begin()
        sc = self.sb("sc", [128, 8, 2], F32)
        ones = self.sb("ones", [1, 128], F32)
        idf, idb = self.load_ident()
        sc0 = self.sb("sc0", [128, 8], F32)
        sc1 = self.sb("sc1", [128, 8], F32)
        self.diag_cols(sc0, sc0[:], I["c"].rearrange("(o n) -> o n", o=1), 8, idf)
        self.diag_cols(sc1, sc1[:], I["c_ctx"].rearrange("(o n) -> o n", o=1), 8, idf)
        fw.op("dve", lambda e: e.tensor_copy(out=sc[:, :, 0], in_=sc0[:]), R=[sc0], W=[sc])
        fw.op("dve", lambda e: e.tensor_copy(out=sc[:, :, 1], in_=sc1[:]), R=[sc1], W=[sc])
        fw.op("act", lambda e: e.activation(out=sc[:], in_=sc[:], func=AF.Silu), R=[sc], W=[sc])
        fw.op("dve", lambda e: e.memset(ones[:], 1.0), W=[ones])
        if "DBG1" in self.debug:
            self.scratch("DBG1", [128, 16], F32)
            self.scratch("DBG2", [1, 6 * D], F32)
            fw.dma("pool", self.S["DBG1"], sc[:].rearrange("p k s -> p (k s)"), R=[sc])
        wpool = self.pool("wada", [128, 8, 512], F32, 2)
        psf = self.pool("psf", [128, 4, 2], F32, 2, psum=True)
        psr = self.pool("psr", [2, 512], F32, 2, psum=True)
        for l in range(DEPTH):
            bfm = self.sb("bfm", [128, 48], F32)
            brow = self.sb("brow", [2, 2, D], F32)
            modf = self.sb("modf", [128, 48, 2], F32)
            modr = self.sb("modr", [2, 2, D], F32)
            fw.op("dve", lambda e: e.memset(modf[:], 0.0), W=[modf])
            self.diag_cols(bfm, bfm[:], I["b_ada"][l:l + 1, :], 48, idf)
            for jj, j in enumerate((2, 5)):
                fw.dma("sp", brow[:, jj, :], I["b_ada"][l:l + 1, j * D:(j + 1) * D].partition_broadcast(2), W=[brow])
            for j in (1, 4):
                fw.op("dve", lambda e, j=j: e.tensor_scalar_add(out=bfm[:, j * 8:(j + 1) * 8], in0=bfm[:, j * 8:(j + 1) * 8], scalar1=1.0), R=[bfm], W=[bfm])
            for jn in range(12):
                j = jn // 2
                w = wpool()
                fw.dma("sp", w[:], I["w_ada"][l, :, jn * 512:(jn + 1) * 512].rearrange("(kc p) n -> p kc n", p=128), W=[w])
                if j in (2, 5):
                    p = psr()
                    for kc in range(8):
                        fw.op("pe", lambda e: e.matmul(p[:], lhsT=sc[:, kc, :], rhs=w[:, kc, :], start=(kc == 0), stop=(kc == 7)),
                              R=[sc, w], W=[p])
                    jj = 0 if j == 2 else 1
                    half = jn % 2
                    fw.op("dve", lambda e: e.tensor_tensor(out=modr[:, jj, half * 512:(half + 1) * 512], in0=p[:], in1=brow[:, jj, half * 512:(half + 1) * 512], op=ALU.add),
                          R=[p, brow], W=[modr])
                else:
                    p = psf()
                    for sub in range(4):
                        for kc in range(8):
                            fw.op("pe", lambda e: e.matmul(p[:, sub, :], lhsT=w[:, kc, sub * 128:(sub + 1) * 128], rhs=sc[:, kc, :], start=(kc == 0), stop=(kc == 7)),
                                  R=[sc, w], W=[p])
                    for s_ in range(2):
                        fw.op("dve", lambda e: e.tensor_tensor(out=modf[:, jn * 4:(jn + 1) * 4, s_], in0=p[:, :, s_], in1=bfm[:, jn * 4:(jn + 1) * 4], op=ALU.add),
                              R=[p, bfm], W=[modf])
            fw.dma("pool", self.S["MODF"][l], modf[:], R=[modf])
            fw.dma("pool", self.S["MODR"][l].rearrange("s j d -> s (j d)"), modr[:].rearrange("s j d -> s (j d)"), R=[modr])
        self.end("mod")

    def diag_cols(self, dstbuf, dst, src_row_ap, nch, idf):
        fw = self.fw
        bc = self.sb("dgbc", [128, nch, 128], F32)
        fw.dma("sp", bc[:].rearrange("p k j -> p (k j)"), src_row_ap.partition_broadcast(128), W=[bc])
        for k in range(nch):
            fw.op("dve", lambda e: e.tensor_tensor(out=bc[:, k, :], in0=bc[:, k, :], in1=idf[:], op=ALU.mult), R=[bc, idf], W=[bc])
        fw.op("dve", lambda e: e.reduce_sum(out=dst, in_=bc[:], axis=AX.X), R=[bc], W=[dstbuf])

    def x_src(self, l, i):
        if l == 0:
            if i < 2:
                return self.I["ctx"][i * 128:(i + 1) * 128, :]
            return self.I["x"][(i - 2) * 128:(i - 1) * 128, :]
        return self.S["XS"][i * 128:(i + 1) * 128, :]

    def load_ident(self):
        fw = self.fw
        idf = self.sb("idf", [128, 128], F32)
        idb = self.sb("idb", [128, 128], BF16)
        fw.dma("sp", idf[:], self.C["ident"][:, :], W=[idf])
        fw.op("dve", lambda e: e.tensor_copy(out=idb[:], in_=idf[:]), R=[idf], W=[idb])
        return idf, idb

    def ph_proj(self, l):
        fw, I, S, C = self.fw, self.I, self.S, self.C
        self.begin()
        idf, idb = self.load_ident()
        modf = self.sb("modf", [128, 48, 2], F32)
        fw.dma("sp", modf[:], S["MODF"][l], W=[modf])
        hT = self.sb("hT", [128, 8, T], BF16)
        xp = self.pool("xt", [128, D], F32, 2)
        xbp = self.pool("xb", [128, D], BF16, 2)
        ptr = self.pool("ptr", [128, 8, 128], BF16, 2, psum=True)
        for i in range(NT):
            xt = xp()
            xb = xbp()
            fw.dma("sp", xt[:], self.x_src(l, i), W=[xt])
            fw.op("dve", lambda e, xt=xt, xb=xb: e.tensor_copy(out=xb[:], in_=xt[:]), R=[xt], W=[xb])
            p = ptr()
            for kc in range(8):
                fw.op("pe", lambda e, p=p, xb=xb, kc=kc: e.transpose(out=p[:, kc, :], in_=xb[:, kc * 128:(kc + 1) * 128], identity=idb[:]),
                      R=[xb, idb], W=[p])
            st = 1 if i < 2 else 0
            for kc in range(8):
                fw.op("act", lambda e, p=p, kc=kc, i=i, st=st: e.activation(
                    out=hT[:, kc, i * 128:(i + 1) * 128], in_=p[:, kc, :], func=AF.Identity,
                    bias=modf[:, kc, st:st + 1], scale=modf[:, 8 + kc, st:st + 1]), R=[p, modf], W=[hT])
        if "HT" in self.debug:
            self.scratch("HT", [128, 8, T], BF16)
            fw.dma("pool", S["HT"], hT[:], R=[hT])
        if self.stop_after == "ht":
            self.end("ht")
            return
        rc = self.sb("rc", [128, T], F32)
        rs = self.sb("rs", [128, T], F32)
        fw.dma("sp", rc[:], C["rope_c"][:, :], W=[rc])
        fw.dma("sp", rs[:], C["rope_s"][:, :], W=[rs])

        wfp = self.pool("wf", [128, 8, 256], F32, 2)
        wbp = self.pool("wb", [128, 8, 256], BF16, 3)
        pmm = self.pool("pmm", [128, 512], F32, 4, psum=True)
        stg = self.pool("stg", [128, T], BF16, 2)
        tmp = self.pool("tmp", [128, 512], F32, 3)
        tstg = self.pool("tstg", [128, 256], BF16, 3)
        win = I["w_in"][l]
        chunks = [(0, 256)] + [(256 + 512 * j, 512) for j in range(8)]
        self._cast_i = 0

        def load_w(pieces, ncols, zero=False):
            wf = wfp()
            wb = wbp()
            if zero:
                fw.op("dve", lambda e, wf=wf: e.memset(wf[:], 0.0), W=[wf])
            for (dc, scol, n) in pieces:
                fw.dma("sp", wf[:, :, dc:dc + n], win[:, scol:scol + n].rearrange("(kc p) n -> p kc n", p=128), W=[wf])
            eng = "dve" if (self._cast_i % 2 == 0) else "act"
            self._cast_i += 1
            if eng == "act":
                fw.op("act", lambda e: e.activation(out=wb[:, :, 0:ncols], in_=wf[:, :, 0:ncols], func=AF.Copy), R=[wf], W=[wb])
            else:
                fw.op("dve", lambda e: e.tensor_copy(out=wb[:, :, 0:ncols], in_=wf[:, :, 0:ncols]), R=[wf], W=[wb])
            return wb

        def fm_mm(wb, m, c0, cn, col0=0):
            p = pmm()
            for kc in range(8):
                fw.op("pe", lambda e, p=p, wb=wb, kc=kc: e.matmul(p[0:m, 0:cn], lhsT=wb[:, kc, col0:col0 + m], rhs=hT[:, kc, c0:c0 + cn], start=(kc == 0), stop=(kc == 7)),
                      R=[wb, hT], W=[p])
            return p

        def fm_group(pieces, m, evac, dsts, zero=False):
            wb = load_w(pieces, m, zero)
            sg = stg()
            for (c0, cn) in chunks:
                p = fm_mm(wb, m, c0, cn)
                evac(p, sg, c0, cn, m)
            for (r0, nr, dst) in dsts:
                fw.dma("pool", dst, sg[r0:r0 + nr, :], R=[sg])

        def ev_copy(p, sg, c0, cn, m):
            fw.op("act", lambda e: e.activation(out=sg[0:m, c0:c0 + cn], in_=p[0:m, 0:cn], func=AF.Copy), R=[p], W=[sg])

        def ev_silu(p, sg, c0, cn, m):
            fw.op("act", lambda e: e.activation(out=sg[0:m, c0:c0 + cn], in_=p[0:m, 0:cn], func=AF.Silu), R=[p], W=[sg])

        def ev_sigm(p, sg, c0, cn, m):
            fw.op("act", lambda e: e.activation(out=sg[0:m, c0:c0 + cn], in_=p[0:m, 0:cn], func=AF.Sigmoid), R=[p], W=[sg])

        for (o0, dst) in ((O_GQ, S["GQT"]), (O_GK, S["GKT"])):
            for g in range(2):
                fm_group([(0, o0 + g * 128, 128)], 128, ev_copy,
                         [(0, 64, dst[2 * g]), (64, 64, dst[2 * g + 1])])
        for g in range(4):
            fm_group([(0, O_GG + g * 128, 128)], 128, ev_silu, [(0, 128, S["GG"][g * 128:(g + 1) * 128, :])])
        fm_group([(0, O_GR, 16), (32, O_GR + 16, 16)], 64, ev_copy, [(0, 64, S["GR"])], zero=True)
        for (o0, dst) in ((O_DQ, S["DQT"]), (O_DK, S["DKT"])):
            for h in range(4):
                b0 = o0 + h * 128
                wa = load_w([(0, b0, 128)], 128)
                wsw = load_w([(0, b0 + 32, 32), (32, b0, 32), (64, b0 + 96, 32), (96, b0 + 64, 32)], 128)
                sg = stg()
                for (c0, cn) in chunks:
                    pa = fm_mm(wa, 128, c0, cn)
                    pb = fm_mm(wsw, 128, c0, cn)
                    t1 = tmp()
                    t2 = tmp()
                    fw.op("dve", lambda e, pa=pa, t1=t1, c0=c0, cn=cn: e.tensor_tensor(out=t1[:, 0:cn], in0=pa[:, 0:cn], in1=rc[:, c0:c0 + cn], op=ALU.mult), R=[pa, rc], W=[t1])
                    fw.op("dve", lambda e, pb=pb, t2=t2, c0=c0, cn=cn: e.tensor_tensor(out=t2[:, 0:cn], in0=pb[:, 0:cn], in1=rs[:, c0:c0 + cn], op=ALU.mult), R=[pb, rs], W=[t2])
                    fw.op("dve", lambda e, t1=t1, t2=t2, sg=sg, c0=c0, cn=cn: e.tensor_tensor(out=sg[:, c0:c0 + cn], in0=t1[:, 0:cn], in1=t2[:, 0:cn], op=ALU.add), R=[t1, t2], W=[sg])
                fw.dma("pool", dst[h], sg[:, :], R=[sg])
        for (o0, dst) in ((O_NQ, S["NQT"]), (O_NK, S["NKT"])):
            for g in range(4):
                fm_group([(0, o0 + g * 128, 128)], 128, ev_copy, [(0, 128, dst[g])])
        for n in range(3):
            for g in range(8):
                fm_group([(0, O_GT + n * D + g * 128, 128)], 128, ev_sigm, [(0, 128, S["GT"][n, g * 128:(g + 1) * 128, :])])
        for (o0, ncols, dst) in ((O_GK, 256, S["GK"]), (O_GV, 512, S["GV"]), (O_DV, 512, S["DV"]), (O_NV, 512, S["NV"])):
            for cg in range(ncols // 256):
                wb = load_w([(0, o0 + cg * 256, 256)], 256)
                for i in range(NT):
                    p = pmm()
                    for kc in range(8):
                        fw.op("pe", lambda e, p=p, wb=wb, kc=kc, i=i: e.matmul(p[:, 0:256], lhsT=hT[:, kc, i * 128:(i + 1) * 128], rhs=wb[:, kc, :], start=(kc == 0), stop=(kc == 7)),
                              R=[wb, hT], W=[p])
                    ts = tstg()
                    eng = "act" if i % 2 == 0 else "dve"
                    if eng == "act":
                        fw.op("act", lambda e, p=p, ts=ts: e.activation(out=ts[:], in_=p[:, 0:256], func=AF.Copy), R=[p], W=[ts])
                    else:
                        fw.op("dve", lambda e, p=p, ts=ts: e.tensor_copy(out=ts[:], in_=p[:, 0:256]), R=[p], W=[ts])
                    fw.dma("pool", dst[i * 128:(i + 1) * 128, cg * 256:(cg + 1) * 256], ts[:], R=[ts])
        self.end("proj%d" % l)


def build_program(nlayers=DEPTH, debug=(), stop_after=None):
    kb = KB(nlayers, debug, stop_after)
    kb.build()
    return kb


_CONSTS = None


def make_in_maps(inputs):
    global _CONSTS
    if _CONSTS is None:
        _CONSTS = _consts()
    f = lambda a: np.ascontiguousarray(np.asarray(a, dtype=np.float32))
    shared = {k: f(inputs[k]) for k in INPUT_SHAPES if k not in ("x", "c", "ctx")}
    for k, v in _CONSTS.items():
        shared["k_" + k] = v
    maps = []
    for b in range(8):
        m = dict(shared)
        m["x"] = f(inputs["x"][b])
        m["c"] = f(inputs["c"][b])
        m["ctx"] = f(inputs["ctx"][b])
        maps.append(m)
    return maps


def kernel(**inputs):
    kb = build_program()
    maps = make_in_maps(inputs)
    res = run_bass_kernel_spmd(kb.nc, maps, core_ids=list(range(8)))
    return np.stack([np.asarray(r["out"], dtype=np.float32) for r in res.results], axis=0)


def _ph_gla(self, l):
    fw, I, S, C = self.fw, self.I, self.S, self.C
    self.begin()
    self.warm([AF.Exp, AF.Ln])
    qT = self.sb("qT", [64, 4, T], BF16)
    kT = self.sb("kT", [64, 4, T], BF16)
    kk = self.sb("kk", [128, NT, 256], BF16)
    vv = self.sb("vv", [128, NT, 512], BF16)
    rT = self.sb("rT", [64, T], BF16)
    fw.dma("sp", qT[:], S["GQT"].rearrange("h d t -> d h t"), W=[qT])
    fw.dma("sp", kT[:], S["GKT"].rearrange("h d t -> d h t"), W=[kT])
    fw.dma("sp", kk[:], S["GK"].rearrange("(i p) n -> p i n", p=128), W=[kk])
    fw.dma("sp", vv[:], S["GV"].rearrange("(i p) n -> p i n", p=128), W=[vv])
    fw.dma("sp", rT[:], S["GR"][:, :], W=[rT])
    trif = self.sb("trif", [128, 4, 128], F32)
    trib = self.sb("trib", [128, 4, 128], BF16)
    fw.dma("sp", trif[:], C["tri"].rearrange("a s t -> s a t"), W=[trif])
    fw.op("act", lambda e: e.mul(out=trib[:], in_=trif[:], mul=-1.0 / 16.0), R=[trif], W=[trib])
    maskf = self.sb("maskf", [128, 2, 512], F32)
    fw.dma("sp", maskf[:], C["mask4"].rearrange("a s t -> s a t"), W=[maskf])
    wdf = self.sb("wdf", [64, 256], F32)
    wd = self.sb("wd", [64, 256], BF16)
    fw.op("dve", lambda e: e.memset(wdf[:], 0.0), W=[wdf])
    fw.dma("sp", wdf[0:16, :], I["gla_w_decay"][l, 0], W=[wdf])
    fw.dma("sp", wdf[32:48, :], I["gla_w_decay"][l, 1], W=[wdf])
    fw.op("dve", lambda e: e.tensor_copy(out=wd[:], in_=wdf[:]), R=[wdf], W=[wd])
    bdb = self.sb("bdb", [128, 2, 256], F32)
    fw.dma("sp", bdb[:].rearrange("p a n -> p (a n)"), I["gla_b_decay"][l:l + 1].rearrange("o a n -> o (a n)").partition_broadcast(128), W=[bdb])
    zbp = self.pool("zb", [128, 256], F32, 4)

    Sf = self.sb("Sf", [64, 4, 128], F32)
    Sb = self.sb("Sb", [64, 4, 128], BF16)
    zps = self.pool("zps", [128, 256], F32, 1, psum=True)
    cps = self.pool("cps", [64, 4, 128], F32, 1, psum=True)
    gps = self.pool("gps", [128, 256], F32, 1, psum=True)
    aps = self.pool("aps", [128, 4, 128], F32, 2, psum=True)
    ops_ = self.pool("ops", [128, 4, 128], F32, 2, psum=True)
    kvps = self.pool("kvps", [64, 4, 128], F32, 1, psum=True)
    e1p = self.pool("e1", [128, 256], F32, 4)
    Lbp = self.pool("Lb", [128, 256], BF16, 4)
    Eqp = self.pool("Eq", [64, 4, 128], F32, 4)
    Ekp = self.pool("Ek", [64, 4, 128], F32, 4)
    Estp = self.pool("Est", [128, 256], F32, 4)
    qinp = self.pool("qin", [64, 4, 128], BF16, 4)
    kinp = self.pool("kin", [64, 4, 128], BF16, 4)
    kstp = self.pool("kst", [128, 256], BF16, 4)
    attp = self.pool("att", [128, 4, 128], BF16, 4)
    osbp = self.pool("osb", [128, 4, 128], F32, 4)

    Sf1 = self.sb("Sf1", [64, 4, 128], F32)
    Sb1 = self.sb("Sb1", [64, 4, 128], BF16)
    Sfs, Sbs = [Sf, Sf1], [Sb, Sb1]
    for S_ in Sfs + Sbs:
        fw.op("dve", lambda e: e.memset(S_[:], 0.0), W=[S_])
    orders = [list(range(NT)), [1, 0] + list(range(NT - 1, 1, -1))]
    seq = [(dr_, orders[dr_][step]) for step in range(NT) for dr_ in range(2)]
    def chunk(dr, ci):
        Sf, Sb = Sfs[dr], Sbs[dr]
        r0 = 0 if dr == 0 else 32
        iu, ig, im = (0, 2, 0) if dr == 0 else (1, 3, 1)
        lastcol = 127 if dr == 0 else 0
        c0 = ci * 128
        z = zps()
        fw.op("pe", lambda e: e.matmul(z[:], lhsT=rT[r0:r0 + 16, c0:c0 + 128], rhs=wd[r0:r0 + 16, :], start=True, stop=True), R=[rT, wd], W=[z])
        zb = zbp()
        fw.op("dve", lambda e: e.tensor_tensor(out=zb[:], in0=z[:], in1=bdb[:, dr, :], op=ALU.add), R=[z, bdb], W=[zb])
        e1 = e1p()
        Lb = Lbp()
        fw.op("act", lambda e: e.activation(out=e1[:], in_=zb[:], func=AF.Exp, scale=-1.0), R=[zb], W=[e1])
        fw.op("act", lambda e: e.activation(out=Lb[:], in_=e1[:], func=AF.Ln, bias=1.0), R=[e1], W=[Lb])
        yield
        cp = cps()
        for h in range(4):
            fw.op("pe", lambda e, cp=cp, Lb=Lb, h=h: e.matmul(cp[:, h, :], lhsT=Lb[:, h * 64:(h + 1) * 64], rhs=trib[:, iu, :], start=True, stop=True), R=[Lb, trib], W=[cp])
        gp = gps()
        fw.op("pe", lambda e, gp=gp, Lb=Lb: e.matmul(gp[:], lhsT=trib[:, ig, :], rhs=Lb[:], start=True, stop=True), R=[Lb, trib], W=[gp])
        Eq = Eqp(); Ek = Ekp(); Est = Estp()
        fw.op("act", lambda e, cp=cp, Eq=Eq: e.activation(out=Eq[:], in_=cp[:], func=AF.Exp), R=[cp], W=[Eq])
        fw.op("act", lambda e, cp=cp, Ek=Ek: e.activation(out=Ek[:], in_=cp[:], func=AF.Exp, scale=-1.0), R=[cp], W=[Ek])
        fw.op("act", lambda e, gp=gp, Est=Est: e.activation(out=Est[:], in_=gp[:], func=AF.Exp), R=[gp], W=[Est])
        qin = qinp(); kin = kinp(); kst = kstp()
        fw.op("dve", lambda e, qin=qin, Eq=Eq, c0=c0: e.scalar_tensor_tensor(out=qin[:], in0=qT[:, :, c0:c0 + 128], scalar=0.125, in1=Eq[:], op0=ALU.mult, op1=ALU.mult), R=[qT, Eq], W=[qin])
        fw.op("dve", lambda e, kin=kin, Ek=Ek, c0=c0: e.tensor_tensor(out=kin[:], in0=kT[:, :, c0:c0 + 128], in1=Ek[:], op=ALU.mult), R=[kT, Ek], W=[kin])
        fw.op("dve", lambda e, kst=kst, Est=Est, ci=ci: e.tensor_tensor(out=kst[:], in0=kk[:, ci, :], in1=Est[:], op=ALU.mult), R=[kk, Est], W=[kst])
        yield
        ap_ = aps()
        for h in range(4):
            fw.op("pe", lambda e, ap_=ap_, kin=kin, qin=qin, h=h: e.matmul(ap_[:, h, :], lhsT=kin[:, h, :], rhs=qin[:, h, :], start=True, stop=True), R=[kin, qin], W=[ap_])
        att = attp()
        fw.op("dve", lambda e, att=att, ap_=ap_: e.tensor_tensor(out=att[:].rearrange("p h t -> p (h t)"), in0=ap_[:].rearrange("p h t -> p (h t)"), in1=maskf[:, im, :], op=ALU.mult), R=[ap_, maskf], W=[att])
        yield
        op_ = ops_()
        for h in range(4):
            fw.op("pe", lambda e, op_=op_, att=att, h=h, ci=ci: e.matmul(op_[:, h, :], lhsT=vv[:, ci, h * 128:(h + 1) * 128], rhs=att[:, h, :], start=True, stop=False), R=[vv, att], W=[op_])
            fw.op("pe", lambda e, op_=op_, qin=qin, h=h: e.matmul(op_[:, h, :], lhsT=Sb[:, h, :], rhs=qin[:, h, :], start=False, stop=True), R=[Sb, qin], W=[op_])
        osb = osbp()
        fw.op("act", lambda e, osb=osb, op_=op_: e.activation(out=osb[:], in_=op_[:], func=AF.Copy), R=[op_], W=[osb])
        fw.dma("sp", S["OG"][dr].rearrange("h v t -> v h t")[:, :, c0:c0 + 128], osb[:], R=[osb])
        kv = kvps()
        for h in range(4):
            fw.op("pe", lambda e, kv=kv, kst=kst, h=h, ci=ci: e.matmul(kv[:, h, :], lhsT=kst[:, h * 64:(h + 1) * 64], rhs=vv[:, ci, h * 128:(h + 1) * 128], start=True, stop=True), R=[kst, vv], W=[kv])
        for h in range(4):
            fw.op("dve", lambda e, kv=kv, Eq=Eq, h=h: e.scalar_tensor_tensor(out=Sf[:, h, :], in0=Sf[:, h, :], scalar=Eq[:, h, lastcol:lastcol + 1], in1=kv[:, h, :], op0=ALU.mult, op1=ALU.add), R=[Sf, Eq, kv], W=[Sf])
        fw.op("dve", lambda e: e.tensor_copy(out=Sb[:], in_=Sf[:]), R=[Sf], W=[Sb])


    pend = [list(orders[0]), list(orders[1])]
    active = []

    def start(dr_):
        if pend[dr_]:
            g = chunk(dr_, pend[dr_].pop(0))
            active.append([dr_, g])

    start(0); start(1)
    rounds = 0
    while active:
        rounds += 1
        if rounds == 3:
            start(0); start(1)
        for ent in list(active):
            try:
                next(ent[1])
            except StopIteration:
                active.remove(ent)
                start(ent[0])
    self.end("gla%d" % l)


def _rms_finish(self, o, n, gcol, extra_scale, pms, onesf, rstdp, tp):
    fw = self.fw
    sq = tp()
    fw.op("dve", lambda e: e.tensor_tensor(out=sq[:, 0:n], in0=o[:, 0:n], in1=o[:, 0:n], op=ALU.mult), R=[o], W=[sq])
    ms = pms()
    fw.op("pe", lambda e: e.matmul(ms[:, 0:n], lhsT=onesf[:], rhs=sq[:, 0:n], start=True, stop=True), R=[onesf, sq], W=[ms])
    rstd = rstdp()
    fw.op("dve", lambda e: e.tensor_scalar_add(out=rstd[:, 0:n], in0=ms[:, 0:n], scalar1=RMS_EPS), R=[ms], W=[rstd])
    fw.op("act", lambda e: e.activation(out=rstd[:, 0:n], in_=rstd[:, 0:n], func=AF.Ln), R=[rstd], W=[rstd])
    fw.op("act", lambda e: e.activation(out=rstd[:, 0:n], in_=rstd[:, 0:n], func=AF.Exp, scale=-0.5), R=[rstd], W=[rstd])
    t = tp()
    fw.op("dve", lambda e: e.tensor_tensor(out=t[:, 0:n], in0=o[:, 0:n], in1=rstd[:, 0:n], op=ALU.mult), R=[o, rstd], W=[t])
    if "DBGC" in self.debug and not getattr(self, "_dbgc_done", False):
        self._dbgc_done = True
        self.scratch("DBGC", [4, 128, 256], F32)
        msb = tp()
        fw.op("dve", lambda e: e.tensor_copy(out=msb[:, 0:n], in_=ms[:, 0:n]), R=[ms], W=[msb])
        for k_, b_ in enumerate((o, sq, msb, rstd)):
            fw.dma("pool", self.S["DBGC"][k_], b_[:, 0:256], R=[b_])
    return t


def _ph_glac(self, l):
    fw, I, S, C = self.fw, self.I, self.S, self.C
    self.begin()
    self.warm([AF.Ln, AF.Exp])
    onesf = self.sb("onesf", [128, 128], F32)
    fw.op("dve", lambda e: e.memset(onesf[:], 1.0 / 128.0), W=[onesf])
    gcol = self.sb("gcol", [128, 1], F32)
    idf, idb = self.load_ident()
    self.diag_cols(gcol, gcol[:], I["gla_norm_g"][l:l + 1, :], 1, idf)
    ofp = self.pool("of", [128, 512], F32, 2)
    obp = self.pool("ob", [128, 512], F32, 2)
    ggp = self.pool("gg", [128, 512], BF16, 2)
    op_ = self.pool("o", [128, 512], F32, 2)
    tp = self.pool("t", [128, 512], F32, 4)
    rstdp = self.pool("rstd", [128, 512], F32, 2)
    pms = self.pool("pms", [128, 512], F32, 2, psum=True)
    outp = self.pool("outb", [128, 512], BF16, 2)
    chunks = [(0, 256)] + [(256 + 512 * j, 512) for j in range(8)]
    for h in range(4):
        for (c0, n) in chunks:
            of = ofp(); ob = obp(); gg = ggp()
            fw.dma("sp", of[:, 0:n], S["OG"][0, h, :, c0:c0 + n], W=[of])
            fw.dma("sp", ob[:, 0:n], S["OG"][1, h, :, c0:c0 + n], W=[ob])
            fw.dma("sp", gg[:, 0:n], S["GG"][h * 128:(h + 1) * 128, c0:c0 + n], W=[gg])
            o = op_()
            fw.op("dve", lambda e, o=o, of=of, ob=ob, n=n: e.tensor_tensor(out=o[:, 0:n], in0=of[:, 0:n], in1=ob[:, 0:n], op=ALU.add), R=[of, ob], W=[o])
            t = _rms_finish(self, o, n, gcol, 1.0, pms, onesf, rstdp, tp)
            ob_ = outp()
            fw.op("dve", lambda e, ob_=ob_, t=t, gg=gg, n=n: e.scalar_tensor_tensor(out=ob_[:, 0:n], in0=t[:, 0:n], scalar=gcol[:, 0:1], in1=gg[:, 0:n], op0=ALU.mult, op1=ALU.mult), R=[t, gcol, gg], W=[ob_])
            fw.dma("pool", S["BR"][0, h * 128:(h + 1) * 128, c0:c0 + n], ob_[:, 0:n], R=[ob_])
    self.end("glac%d" % l)


KB.ph_gla = _ph_gla
KB.ph_glac = _ph_glac


def _ph_diff(self, l):
    fw, I, S, C = self.fw, self.I, self.S, self.C
    lam_init = 0.8 - 0.6 * math.exp(-0.3 * l)
    self.begin()
    self.warm([AF.Exp, AF.Ln])
    KT = self.sb("KT", [128, 4, T], BF16)
    VV = self.sb("VV", [128, NT, 512], BF16)
    fw.dma("sp", KT[:], S["DKT"].rearrange("h d t -> d h t"), W=[KT])
    fw.dma("sp", VV[:], S["DV"].rearrange("(i p) n -> p i n", p=128), W=[VV])
    onesb = self.sb("onesb", [128, 128], BF16)
    fw.op("dve", lambda e: e.memset(onesb[:], 1.0), W=[onesb])
    onesf = self.sb("onesf", [128, 128], F32)
    fw.op("dve", lambda e: e.memset(onesf[:], 1.0 / 128.0), W=[onesf])
    gcol = self.sb("gcol", [128, 1], F32)
    idf, idb = self.load_ident()
    self.diag_cols(gcol, gcol[:], I["diff_norm_g"][l:l + 1, :], 1, idf)
    fw.op("dve", lambda e: e.tensor_scalar_mul(out=gcol[:], in0=gcol[:], scalar1=(1.0 - lam_init)), R=[gcol], W=[gcol])
    lq = self.sb("lq", [128, 128], F32)
    lk = self.sb("lk", [128, 128], F32)
    fw.dma("sp", lq[:], I["diff_lam_q"][l:l + 1].rearrange("o a b -> o (a b)").partition_broadcast(128), W=[lq])
    fw.dma("sp", lk[:], I["diff_lam_k"][l:l + 1].rearrange("o a b -> o (a b)").partition_broadcast(128), W=[lk])
    fw.op("dve", lambda e: e.tensor_tensor(out=lq[:], in0=lq[:], in1=lk[:], op=ALU.mult), R=[lq, lk], W=[lq])
    ls = self.sb("ls", [128, 2], F32)
    fw.op("dve", lambda e: e.reduce_sum(out=ls[:], in_=lq[:].rearrange("p (a b) -> p a b", a=2), axis=AX.X), R=[lq], W=[ls])
    fw.op("act", lambda e: e.activation(out=ls[:], in_=ls[:], func=AF.Exp), R=[ls], W=[ls])
    nlam = self.sb("nlam", [128, 1], F32)
    fw.op("dve", lambda e: e.tensor_tensor(out=nlam[:], in0=ls[:, 1:2], in1=ls[:, 0:1], op=ALU.subtract), R=[ls], W=[nlam])
    fw.op("dve", lambda e: e.tensor_scalar_add(out=nlam[:], in0=nlam[:], scalar1=-lam_init), R=[nlam], W=[nlam])

    qp = self.pool("q", [128, 512], BF16, 3)
    sps = self.pool("sps", [128, 512], F32, 5, psum=True)
    ops_ = self.pool("ops", [128, 512], F32, 2, psum=True)
    dps = self.pool("dps", [128, 512], F32, 1, psum=True)
    pms = dps
    ptp = self.pool("pt", [128, 512], BF16, 10)
    dacp = self.pool("dacc", [128, 512], F32, 4)
    ones1 = self.sb("ones1", [128, 128], F32)
    fw.op("dve", lambda e: e.memset(ones1[:], 1.0), W=[ones1])
    w0p = self.pool("w0", [128, 512], F32, 4)
    rcp = self.pool("rc", [128, 512], F32, 2)
    op_ = self.pool("o", [128, 512], F32, 2)
    tp = self.pool("t", [128, 512], F32, 4)
    rstdp = self.pool("rstd", [128, 512], F32, 2)
    outp = self.pool("outb", [128, 512], BF16, 2)
    chunks = [(0, 256, 2)] + [(256 + 512 * j, 512, NT) for j in range(8)]
    items = []
    for h in range(4):
        for (c0, n, nkt) in chunks:
            for kt in range(nkt):
                items.append((h, c0, n, nkt, kt))
    LA = 2
    st = {}
    cur = {}
    deferred = []

    def front(i):
        h, c0, n, nkt, kt = items[i]
        key = (h, c0)
        if key not in cur:
            q = qp()
            fw.dma("sp", q[:, 0:n], S["DQT"][h, :, c0:c0 + n], W=[q])
            cur[key] = {"q": q, "ws": []}
        q = cur[key]["q"]
        pts = []
        sp2 = [sps(), sps()]
        for comp in range(2):
            pb = comp * 64
            sp_ = sp2[comp]
            fw.op("pe", lambda e: e.matmul(sp_[:, 0:n], lhsT=KT[pb:pb + 64, h, kt * 128:(kt + 1) * 128], rhs=q[pb:pb + 64, 0:n], start=True, stop=True), R=[KT, q], W=[sp_])
        for comp in range(2):
            sp_ = sp2[comp]
            pt = ptp()
            fw.op("act", lambda e: e.activation(out=pt[:, 0:n], in_=sp_[:, 0:n], func=AF.Exp, scale=0.125), R=[sp_], W=[pt])
            pts.append(pt)
        st[i] = pts

    def back(i):
        h, c0, n, nkt, kt = items[i]
        key = (h, c0)
        c = cur[key]
        if kt == 0:
            c["o_ps"] = [ops_(), ops_()]
            c["dacc"] = [dacp(), dacp()]
        pts = st.pop(i)
        for comp in range(2):
            o_ps, dacc, pt = c["o_ps"][comp], c["dacc"][comp], pts[comp]
            fw.op("pe", lambda e: e.matmul(o_ps[:, 0:n], lhsT=VV[:, kt, h * 128:(h + 1) * 128], rhs=pt[:, 0:n], start=(kt == 0), stop=(kt == nkt - 1)), R=[VV, pt], W=[o_ps])
            if kt == 0:
                fw.op("dve", lambda e: e.tensor_copy(out=dacc[:, 0:n], in_=pt[:, 0:n]), R=[pt], W=[dacc])
            else:
                fw.op("dve", lambda e: e.tensor_tensor(out=dacc[:, 0:n], in0=dacc[:, 0:n], in1=pt[:, 0:n], op=ALU.add), R=[dacc, pt], W=[dacc])
        if kt == nkt - 1:
            for comp in range(2):
                o_ps, dacc = c["o_ps"][comp], c["dacc"][comp]
                d_ps = dps()
                fw.op("pe", lambda e: e.matmul(d_ps[:, 0:n], lhsT=ones1[:], rhs=dacc[:, 0:n], start=True, stop=True), R=[ones1, dacc], W=[d_ps])
                rc = rcp()
                fw.op("dve", lambda e: e.reciprocal(out=rc[:, 0:n], in_=d_ps[:, 0:n]), R=[d_ps], W=[rc])
                w = w0p()
                fw.op("dve", lambda e: e.tensor_tensor(out=w[:, 0:n], in0=o_ps[:, 0:n], in1=rc[:, 0:n], op=ALU.mult), R=[o_ps, rc], W=[w])
                c["ws"].append(w)
            if True:
                comp = 1
                ws = c["ws"]
                o = op_()
                fw.op("dve", lambda e: e.scalar_tensor_tensor(out=o[:, 0:n], in0=ws[1][:, 0:n], scalar=nlam[:, 0:1], in1=ws[0][:, 0:n], op0=ALU.mult, op1=ALU.add), R=[ws[0], ws[1], nlam], W=[o])
                sq = tp()
                fw.op("dve", lambda e: e.tensor_tensor(out=sq[:, 0:n], in0=o[:, 0:n], in1=o[:, 0:n], op=ALU.mult), R=[o], W=[sq])

                def tail(o=o, sq=sq, n=n, h=h, c0=c0):
                    ms = pms()
                    fw.op("pe", lambda e: e.matmul(ms[:, 0:n], lhsT=onesf[:], rhs=sq[:, 0:n], start=True, stop=True), R=[onesf, sq], W=[ms])
                    rstd = rstdp()
                    fw.op("dve", lambda e: e.tensor_scalar_add(out=rstd[:, 0:n], in0=ms[:, 0:n], scalar1=RMS_EPS), R=[ms], W=[rstd])
                    fw.op("act", lambda e: e.activation(out=rstd[:, 0:n], in_=rstd[:, 0:n], func=AF.Ln), R=[rstd], W=[rstd])
                    fw.op("act", lambda e: e.activation(out=rstd[:, 0:n], in_=rstd[:, 0:n], func=AF.Exp, scale=-0.5), R=[rstd], W=[rstd])
                    t = tp()
                    fw.op("dve", lambda e: e.tensor_tensor(out=t[:, 0:n], in0=o[:, 0:n], in1=rstd[:, 0:n], op=ALU.mult), R=[o, rstd], W=[t])
                    ob_ = outp()
                    fw.op("act", lambda e: e.activation(out=ob_[:, 0:n], in_=t[:, 0:n], func=AF.Identity, scale=gcol[:, 0:1]), R=[t, gcol], W=[ob_])
                    fw.dma("pool", S["BR"][1, h * 128:(h + 1) * 128, c0:c0 + n], ob_[:, 0:n], R=[ob_])
                deferred.append((i + 4, tail))
                del cur[key]

    nI = len(items)
    for i in range(nI + LA):
        if i < nI:
            front(i)
        if i - LA >= 0:
            back(i - LA)
        while deferred and deferred[0][0] <= i - LA:
            deferred.pop(0)[1]()
    while deferred:
        deferred.pop(0)[1]()
    self.end("diff%d" % l)


KB.ph_diff = _ph_diff


def _ph_na(self, l):
    fw, I, S, C = self.fw, self.I, self.S, self.C
    nc = self.nc
    self.begin()
    self.warm([AF.Exp])
    KT = self.sb("KT", [128, 4, T], BF16)
    VV = self.sb("VV", [128, NT, 512], BF16)
    fw.dma("sp", KT[:], S["NKT"].rearrange("g d t -> d g t"), W=[KT])
    fw.dma("sp", VV[:], S["NV"].rearrange("(i p) n -> p i n", p=128), W=[VV])
    onesb = self.sb("onesb", [128, 64], BF16)
    fw.op("dve", lambda e: e.memset(onesb[:], 1.0), W=[onesb])
    TAB = self.sb("TAB", [128, 16, 512], BF16)
    fw.op("dve", lambda e: e.memset(TAB[:], 0.0), W=[TAB])
    j64 = self.sb("j64", [64, 128], F32)
    fw.dma("sp", j64[:], C["j64"][:, :], W=[j64])
    win = self.sb("win", [128, 512], F32)
    fw.dma("sp", win[:], C["nawin"][:, :], W=[win])
    idf, idb = self.load_ident()
    r120 = self.sb("r120", [120, 31], F32)
    fw.dma("sp", r120[:], I["na_rpb"][l].rearrange("h a j -> (h a) j"), W=[r120])
    r31 = self.sb("r31", [31, 120], F32)
    tps = self.pool("tps", [128, 512], F32, 1, psum=True)
    pt0 = tps()
    fw.op("pe", lambda e: e.transpose(out=pt0[0:31, 0:120], in_=r120[:, :], identity=idf[0:120, 0:120]), R=[r120, idf], W=[pt0])
    fw.op("act", lambda e: e.activation(out=r31[:], in_=pt0[0:31, 0:120], func=AF.Copy), R=[pt0], W=[r31])
    p0 = tps()
    fw.op("pe", lambda e: e.matmul(p0[0:120, 0:31], lhsT=r31[:, :], rhs=j64[0:31, 33:64], start=True, stop=True), R=[r31, j64], W=[p0])
    rev = self.sb("rev", [120, 31], F32)
    fw.op("act", lambda e: e.activation(out=rev[:], in_=p0[0:120, 0:31], func=AF.Copy), R=[p0], W=[rev])
    zt = self.sb("zt", [120, 128], F32)
    fw.op("dve", lambda e: e.memset(zt[:], 0.0), W=[zt])
    nag = Buf("nag")
    NAG = S["NAG"]
    fw.dma("sp", NAG[:, :], zt[:], R=[zt], W=[nag])
    fw.dma("sp", NAG[:, 48:79], rev[:], R=[rev], W=[nag])
    Hk = self.sb("Hk", [64, 120, 64], F32)
    for n0 in range(0, 120, 8):
        hsrc = bass.AP(tensor=NAG.tensor, offset=NAG.offset + n0 * 128, ap=[[1, 64], [128, 8], [1, 64]])
        fw.dma("sp", Hk[:, n0:n0 + 8, :], hsrc, R=[nag], W=[Hk])
    Hk4 = Hk[:].rearrange("m (h a) c -> m h a c", h=8)
    ebp = self.pool("eb", [128, 512], F32, 2)
    for dr in range(15):
        p = tps()
        fw.op("pe", lambda e: e.matmul(p[:].rearrange("p (h c) -> p h c", h=8), lhsT=j64[:, :], rhs=Hk4[:, :, dr, :], start=True, stop=True), R=[j64, Hk], W=[p])
        eb = ebp()
        fw.op("act", lambda e: e.activation(out=eb[:], in_=p[:], func=AF.Exp), R=[p], W=[eb])
        if dr <= 13:
            fw.op("dve", lambda e: e.tensor_tensor(out=TAB[0:64, dr, :], in0=eb[0:64, :], in1=win[0:64, :], op=ALU.mult), R=[eb, win], W=[TAB])
        if dr == 10:
            fw.op("dve", lambda e: e.tensor_tensor(out=TAB[0:64, 15, :], in0=eb[0:64, :], in1=win[0:64, :], op=ALU.mult), R=[eb, win], W=[TAB])
        if dr >= 1:
            fw.op("dve", lambda e: e.tensor_tensor(out=TAB[64:128, dr - 1, :], in0=eb[64:128, :], in1=win[64:128, :], op=ALU.mult), R=[eb, win], W=[TAB])
        if dr == 3:
            fw.op("dve", lambda e: e.tensor_tensor(out=TAB[64:128, 14, :], in0=eb[64:128, :], in1=win[64:128, :], op=ALU.mult), R=[eb, win], W=[TAB])

    if self.stop_after == "natab":
        self.scratch("TAB", [128, 16, 512], BF16)
        fw.dma("pool", S["TAB"], TAB[:], R=[TAB])
        self.end("natab")
        return
    q8p = self.pool("q8", [128, 4, 512], BF16, 2)
    stgp = self.pool("stg", [64, 8, 512], BF16, 2)
    sps = self.pool("sps", [128, 512], F32, 4, psum=True)
    ops_ = self.pool("ops", [64, 512], F32, 2, psum=True)
    dps = self.pool("dps", [64, 512], F32, 1, psum=True)
    ptp = self.pool("pt", [128, 512], BF16, 16)
    rcp = self.pool("rc", [64, 512], F32, 2)
    q8p = self.pool("q8b", [128, 4, 512], BF16, 3)
    stgp = self.pool("stgb", [64, 8, 512], BF16, 3)
    BRv = S["BR"][2].rearrange("(h d) t -> d h t", h=8)
    NQv = S["NQT"].rearrange("g d t -> d g t")
    rows_ = []
    for grp in range(T // 512 + 1):
        if grp == 0:
            tok0, nrows = 0, 4
        else:
            tok0, nrows = 256 + (grp - 1) * 512, 8
        for rr in range(nrows):
            rows_.append((grp, rr, tok0, nrows))
    gstate = {}
    rstate = {}

    def nfront(i):
        grp, rr, tok0, nrows = rows_[i]
        ntok = nrows * 64
        if grp not in gstate:
            q8 = q8p()
            stg = stgp()
            fw.dma("sp", q8[:, :, 0:ntok], NQv[:, :, tok0:tok0 + ntok], W=[q8])
            gstate[grp] = (q8, stg)
        q8, stg = gstate[grp]
        qoff = rr * 64
        tiles = [(0, None), (1, None)]
        if grp > 0:
            r = (grp - 1) * 8 + rr
            rs = min(max(r - 4, 0), 56)
            if rs % 2 == 0:
                for j in range(4):
                    tiles.append((2 + rs // 2 + j, rs + 2 * j - r + 7))
            else:
                tiles.append((2 + (rs - 1) // 2, 14))
                for j in range(3):
                    tiles.append((2 + (rs + 1) // 2 + j, rs + 1 + 2 * j - r + 7))
                tiles.append((2 + (rs + 7) // 2, 15))
        P = []
        for (kt, slot) in tiles:
            spe = sps()
            spo = sps()
            for h in range(8):
                pb = (h % 2) * 64
                dstp = spe if h % 2 == 0 else spo
                fw.op("pe", lambda e: e.matmul(dstp[:, (h // 2) * 64:(h // 2 + 1) * 64], lhsT=KT[pb:pb + 64, h // 2, kt * 128:(kt + 1) * 128], rhs=q8[pb:pb + 64, h // 2, qoff:qoff + 64], start=True, stop=True), R=[KT, q8], W=[dstp])
            pt = ptp()
            ptv = pt[:].rearrange("p (g two q) -> p g two q", two=2, q=64)
            fw.op("act", lambda e: e.activation(out=ptv[:, :, 0, :], in_=spe[:, 0:256].rearrange("p (g q) -> p g q", q=64), func=AF.Exp, scale=0.125), R=[spe], W=[pt])
            fw.op("act", lambda e: e.activation(out=ptv[:, :, 1, :], in_=spo[:, 0:256].rearrange("p (g q) -> p g q", q=64), func=AF.Exp, scale=0.125), R=[spo], W=[pt])
            if slot is not None:
                assert 0 <= slot <= 15
                fw.op("dve", lambda e: e.tensor_tensor(out=pt[:], in0=pt[:], in1=TAB[:, slot, :], op=ALU.mult), R=[pt, TAB], W=[pt])
            P.append((kt, pt))
        rstate[i] = P

    def nback(i):
        grp, rr, tok0, nrows = rows_[i]
        ntok = nrows * 64
        q8, stg = gstate[grp]
        qoff = rr * 64
        P = rstate.pop(i)
        o_ps = ops_()
        d_ps = dps()
        nk = len(P)
        for h in range(8):
            for ii, (kt, pt) in enumerate(P):
                fw.op("pe", lambda e: e.matmul(o_ps[:, h * 64:(h + 1) * 64], lhsT=VV[:, kt, h * 64:(h + 1) * 64], rhs=pt[:, h * 64:(h + 1) * 64], start=(ii == 0), stop=(ii == nk - 1)), R=[VV, pt], W=[o_ps])
        for ii, (kt, pt) in enumerate(P):
            fw.op("pe", lambda e: e.matmul(d_ps[:], lhsT=onesb[:], rhs=pt[:], start=(ii == 0), stop=(ii == nk - 1)), R=[onesb, pt], W=[d_ps])
        rc = rcp()
        fw.op("dve", lambda e: e.reciprocal(out=rc[:], in_=d_ps[:]), R=[d_ps], W=[rc])
        fw.op("dve", lambda e: e.tensor_tensor(out=stg[:, :, qoff:qoff + 64], in0=o_ps[:].rearrange("p (h q) -> p h q", h=8), in1=rc[:].rearrange("p (h q) -> p h q", h=8), op=ALU.mult), R=[o_ps, rc], W=[stg])
        if rr == nrows - 1:
            fw.dma("pool", BRv[:, :, tok0:tok0 + ntok], stg[:, :, 0:ntok], R=[stg])

    nR = len(rows_)
    for i in range(nR + 1):
        if i < nR:
            nfront(i)
        if i >= 1:
            nback(i - 1)
    self.end("na%d" % l)


KB.ph_na = _ph_na


def _ln_epilogue(self, ysrc, xt, mbc, gbc, bbc, pools, dst_ap):
    fw = self.fw
    up, stp, mvp, xnp = pools
    u = up()
    for hf in range(2):
        yb, yap = ysrc[hf]
        fw.op("dve", lambda e: e.tensor_tensor(out=u[:, hf * 512:(hf + 1) * 512], in0=yap, in1=mbc[:, hf * 512:(hf + 1) * 512], op=ALU.mult), R=[yb, mbc], W=[u])
    fw.op("dve", lambda e: e.scalar_tensor_tensor(out=u[:], in0=xt[:], scalar=ALPHA, in1=u[:], op0=ALU.mult, op1=ALU.add), R=[xt, u], W=[u])
    st = stp()
    for hf in range(2):
        fw.op("dve", lambda e: e.bn_stats(out=st[:, hf, :], in_=u[:, hf * 512:(hf + 1) * 512]), R=[u], W=[st])
    mv = mvp()
    fw.op("dve", lambda e: e.bn_aggr(out=mv[:, 0:2], in_=st[:].rearrange("p a b -> p (a b)")), R=[st], W=[mv])
    fw.op("dve", lambda e: e.tensor_scalar_add(out=mv[:, 2:3], in0=mv[:, 1:2], scalar1=LN_EPS), R=[mv], W=[mv])
    fw.op("act", lambda e: e.activation(out=mv[:, 2:3], in_=mv[:, 2:3], func=AF.Ln), R=[mv], W=[mv])
    fw.op("act", lambda e: e.activation(out=mv[:, 2:3], in_=mv[:, 2:3], func=AF.Exp, scale=-0.5), R=[mv], W=[mv])
    fw.op("dve", lambda e: e.scalar_tensor_tensor(out=mv[:, 3:4], in0=mv[:, 0:1], scalar=-1.0, in1=mv[:, 2:3], op0=ALU.mult, op1=ALU.mult), R=[mv], W=[mv])
    xn = xnp()
    fw.op("act", lambda e: e.activation(out=xn[:], in_=u[:], func=AF.Identity, bias=mv[:, 3:4], scale=mv[:, 2:3]), R=[u, mv], W=[xn])
    fw.op("dve", lambda e: e.tensor_tensor(out=xn[:], in0=xn[:], in1=gbc[:], op=ALU.mult), R=[xn, gbc], W=[xn])
    fw.op("dve", lambda e: e.tensor_tensor(out=xn[:], in0=xn[:], in1=bbc[:], op=ALU.add), R=[xn, bbc], W=[xn])
    fw.dma("pool", dst_ap, xn[:], R=[xn])


def _bc_tile(self, name, row_ap):
    t = self.sb(name, [128, D], F32)
    self.fw.dma("sp", t[:], row_ap.partition_broadcast(128), W=[t])
    return t


def _ph_merge(self, l):
    fw, I, S, C = self.fw, self.I, self.S, self.C
    self.begin()
    self.warm([AF.Ln, AF.Exp])
    wfp = self.pool("wf", [128, 1024], F32, 2)
    wbr = self.sb("wbr", [128, 3, 4, 1024], BF16)
    wo = self.sb("wo", [128, 8, 1024], BF16)
    k = 0
    for n_ in range(3):
        for ec in range(4):
            wf = wfp()
            fw.dma("sp", wf[:], I["w_branch"][l, n_, ec * 128:(ec + 1) * 128, :], W=[wf])
            if k % 2 == 0:
                fw.op("dve", lambda e: e.tensor_copy(out=wbr[:, n_, ec, :], in_=wf[:]), R=[wf], W=[wbr])
            else:
                fw.op("act", lambda e: e.activation(out=wbr[:, n_, ec, :], in_=wf[:], func=AF.Copy), R=[wf], W=[wbr])
            k += 1
    for dc in range(8):
        wf = wfp()
        fw.dma("sp", wf[:], I["w_o"][l, dc * 128:(dc + 1) * 128, :], W=[wf])
        if dc % 2 == 0:
            fw.op("dve", lambda e: e.tensor_copy(out=wo[:, dc, :], in_=wf[:]), R=[wf], W=[wo])
        else:
            fw.op("act", lambda e: e.activation(out=wo[:, dc, :], in_=wf[:], func=AF.Copy), R=[wf], W=[wo])
    m2 = [_bc_tile(self, "m2L", S["MODR"][l, 0, 0:1, :]), _bc_tile(self, "m2C", S["MODR"][l, 1, 0:1, :])]
    gbc = _bc_tile(self, "gbc", I["ln_g"][l, 0:1, :])
    bbc = _bc_tile(self, "bbc", I["ln_b"][l, 0:1, :])
    brp = self.pool("br", [128, 3, 4, 512], BF16, 2)
    gtp = self.pool("gt", [128, 3, 512], BF16, 2)
    pj = self.pool("pj", [128, 512], F32, 3, psum=True)
    yps = self.pool("yps", [128, 512], F32, 4, psum=True)
    accp = self.pool("acc", [128, 512], F32, 2)
    tmpp = self.pool("tmp", [128, 512], F32, 2)
    mTp = self.pool("mT", [128, 8, 512], BF16, 2)
    xtp = self.pool("xt", [128, D], F32, 2)
    pools = (self.pool("u", [128, D], F32, 2), self.pool("st", [128, 2, 6], F32, 2), self.pool("mv", [128, 4], F32, 2), self.pool("xn", [128, D], F32, 2))
    chunks = [(0, 256)] + [(256 + 512 * j, 512) for j in range(8)]
    BRv = S["BR"].rearrange("n (ec p) t -> p n ec t", p=128)
    GTv = S["GT"].rearrange("n (dc p) t -> p n dc t", p=128)
    for (c0, n) in chunks:
        br = brp()
        for n_ in range(3):
            fw.dma("sp", br[:, n_, :, 0:n], BRv[:, n_, :, c0:c0 + n], W=[br])
        mT = mTp()
        for dc in range(8):
            gt = gtp()
            fw.dma("sp", gt[:, :, 0:n], GTv[:, :, dc, c0:c0 + n], W=[gt])
            acc = accp()
            for n_ in range(3):
                p = pj()
                for ec in range(4):
                    fw.op("pe", lambda e: e.matmul(p[:, 0:n], lhsT=wbr[:, n_, ec, dc * 128:(dc + 1) * 128], rhs=br[:, n_, ec, 0:n], start=(ec == 0), stop=(ec == 3)), R=[wbr, br], W=[p])
                if n_ == 0:
                    fw.op("dve", lambda e: e.tensor_tensor(out=acc[:, 0:n], in0=p[:, 0:n], in1=gt[:, 0, 0:n], op=ALU.mult), R=[p, gt], W=[acc])
                else:
                    tmp = tmpp()
                    fw.op("dve", lambda e: e.tensor_tensor(out=tmp[:, 0:n], in0=p[:, 0:n], in1=gt[:, n_, 0:n], op=ALU.mult), R=[p, gt], W=[tmp])
                    if n_ == 1:
                        fw.op("dve", lambda e: e.tensor_tensor(out=acc[:, 0:n], in0=acc[:, 0:n], in1=tmp[:, 0:n], op=ALU.add), R=[acc, tmp], W=[acc])
                    else:
                        fw.op("dve", lambda e: e.tensor_tensor(out=mT[:, dc, 0:n], in0=acc[:, 0:n], in1=tmp[:, 0:n], op=ALU.add), R=[acc, tmp], W=[mT])
        for tl in range(n // 128):
            ti = c0 // 128 + tl
            ys = []
            for hf in range(2):
                y = yps()
                for dc in range(8):
                    fw.op("pe", lambda e: e.matmul(y[:], lhsT=mT[:, dc, tl * 128:(tl + 1) * 128], rhs=wo[:, dc, hf * 512:(hf + 1) * 512], start=(dc == 0), stop=(dc == 7)), R=[mT, wo], W=[y])
                ys.append((y, y[:]))
            xt = xtp()
            fw.dma("sp", xt[:], self.x_src(l, ti), W=[xt])
            _ln_epilogue(self, ys, xt, m2[1 if ti < 2 else 0], gbc, bbc, pools, S["XS"][ti * 128:(ti + 1) * 128, :])
    self.end("merge%d" % l)


KB.ph_merge = _ph_merge


def _ph_moe(self, l, last):
    fw, I, S, C = self.fw, self.I, self.S, self.C
    self.begin()
    self.warm([AF.Sigmoid, AF.Silu])
    idf, idb = self.load_ident()
    modf = self.sb("modf", [128, 48, 2], F32)
    fw.dma("sp", modf[:], S["MODF"][l], W=[modf])
    m5 = [_bc_tile(self, "m5L", S["MODR"][l, 0, 1:2, :]), _bc_tile(self, "m5C", S["MODR"][l, 1, 1:2, :])]
    gbc = _bc_tile(self, "gbc", I["ln_g"][l, 1:2, :])
    bbc = _bc_tile(self, "bbc", I["ln_b"][l, 1:2, :])
    wr = self.sb("wr", [128, 8, 16], F32)
    fw.dma("sp", wr[:], I["w_router"].rearrange("(kc p) e -> p kc e", p=128), W=[wr])
    brt = self.sb("brt", [128, 16], F32)
    fw.dma("sp", brt[:], I["b_router"].rearrange("(o e) -> o e", o=1).partition_broadcast(128), W=[brt])
    self_f = self.sb("self", [16, 16, 128], F32)
    selb = self.sb("selb", [16, 16, 128], BF16)
    fw.dma("sp", self_f[:], C["sel"], W=[self_f])
    fw.op("dve", lambda e: e.tensor_copy(out=selb[:], in_=self_f[:]), R=[self_f], W=[selb])

    NG = 12
    hT = self.sb("hT", [128, 8, NG * 128], BF16)
    Y = self.sb("Y", [128, NG, D], F32)
    aff = self.sb("aff", [128, NG, 16], F32)
    selv = self.sb("selv", [128, NG, 16], F32)
    comb = self.sb("comb", [128, NG, 16], F32)
    cT = self.sb("cT", [16, NG * 128], BF16)
    r1 = self.sb("r1", [128, NG * 4], F32)
    r2 = self.sb("r2", [128, NG * 4], F32)
    gs = self.sb("gs", [128, NG * 4], F32)
    gmx = self.sb("gmx", [128, NG], F32)
    gmask = self.sb("gmask", [128, NG * 4], F32)
    is1 = self.sb("is1", [128, NG * 4, 4], F32)
    v2 = self.sb("v2", [128, NG * 4, 4], F32)
    oh = self.sb("oh", [128, NG * 4, 4], F32)
    ssum = self.sb("ssum", [128, NG], F32)

    xtp = self.pool("xt", [128, D], F32, 2)
    h32p = self.pool("h32", [128, 8, 128], F32, 2)
    sml = self.pool("sml", [128, 512], F32, 1, psum=True)
    gen = self.pool("gen", [128, 512], F32, 7, psum=True)
    ptr = gen
    gup = gen
    yps = gen
    wgp = self.pool("wg", [128, 8, 512], BF16, 2)
    wup = self.pool("wu", [128, 8, 512], BF16, 2)
    wdp = self.pool("wd", [128, 4, 1024], BF16, 2)
    cbp = self.pool("cb", [128, 512], F32, 2)
    sgp = self.pool("sg", [128, 512], F32, 2)
    a1p = self.pool("a1", [128, 512], F32, 2)
    aTp = self.pool("aT", [128, 4, 512], BF16, 2)
    pools = (self.pool("u", [128, D], F32, 1), self.pool("st", [128, 2, 6], F32, 2), self.pool("mv", [128, 4], F32, 2), self.pool("xn", [128, D], F32, 2))
    castk = [0]

    def cast(dst_ap, dstb, src_ap, srcb):
        if castk[0] % 2 == 0:
            fw.op("dve", lambda e: e.tensor_copy(out=dst_ap, in_=src_ap), R=[srcb], W=[dstb])
        else:
            fw.op("act", lambda e: e.activation(out=dst_ap, in_=src_ap, func=AF.Copy), R=[srcb], W=[dstb])
        castk[0] += 1

    for (g0, g1) in ((0, 12), (12, 24), (24, 34)):
        ng = g1 - g0
        for i in range(g0, g1):
            j = i - g0
            st_ = 1 if i < 2 else 0
            xt = xtp()
            fw.dma("sp", xt[:], S["XS"][i * 128:(i + 1) * 128, :], W=[xt])
            h32 = h32p()
            for hf in range(2):
                p = ptr()
                for k4 in range(4):
                    kc = hf * 4 + k4
                    fw.op("pe", lambda e: e.transpose(out=p[:, k4 * 128:(k4 + 1) * 128], in_=xt[:, kc * 128:(kc + 1) * 128], identity=idf[:]), R=[xt, idf], W=[p])
                for k4 in range(4):
                    kc = hf * 4 + k4
                    fw.op("act", lambda e: e.activation(out=h32[:, kc, :], in_=p[:, k4 * 128:(k4 + 1) * 128], func=AF.Identity, bias=modf[:, 24 + kc, st_:st_ + 1], scale=modf[:, 32 + kc, st_:st_ + 1]), R=[p, modf], W=[h32])
            fw.op("dve", lambda e: e.tensor_copy(out=hT[:, :, j * 128:(j + 1) * 128], in_=h32[:]), R=[h32], W=[hT])
            lg = sml()
            for kc in range(8):
                fw.op("pe", lambda e: e.matmul(lg[:, 0:16], lhsT=h32[:, kc, :], rhs=wr[:, kc, :], start=(kc == 0), stop=(kc == 7)), R=[h32, wr], W=[lg])
            fw.op("act", lambda e: e.activation(out=aff[:, j, :], in_=lg[:, 0:16], func=AF.Sigmoid), R=[lg], W=[aff])
            fw.op("dve", lambda e: e.tensor_tensor(out=selv[:, j, :], in0=aff[:, j, :], in1=brt[:], op=ALU.add), R=[aff, brt], W=[selv])
        n4 = ng * 4
        s4 = selv[:, 0:ng, :].rearrange("p g (q e) -> p (g q) e", e=4)
        first = True
        for (a, b) in ((0, 1), (0, 2), (0, 3), (1, 2), (1, 3), (2, 3)):
            if first:
                fw.op("dve", lambda e: e.tensor_tensor(out=gs[:, 0:n4], in0=s4[:, :, a], in1=s4[:, :, b], op=ALU.add), R=[selv], W=[gs])
                first = False
            else:
                fw.op("dve", lambda e: e.tensor_tensor(out=r1[:, 0:n4], in0=s4[:, :, a], in1=s4[:, :, b], op=ALU.add), R=[selv], W=[r1])
                fw.op("dve", lambda e: e.tensor_tensor(out=gs[:, 0:n4], in0=gs[:, 0:n4], in1=r1[:, 0:n4], op=ALU.max), R=[gs, r1], W=[gs])
        fw.op("dve", lambda e: e.reduce_max(out=gmx[:, 0:ng], in_=gs[:, 0:n4].rearrange("p (g q) -> p g q", q=4), axis=AX.X), R=[gs], W=[gmx])
        for q in range(4):
            fw.op("dve", lambda e: e.tensor_tensor(out=gmask[:, 0:n4].rearrange("p (g q) -> p g q", q=4)[:, :, q], in0=gs[:, 0:n4].rearrange("p (g q) -> p g q", q=4)[:, :, q], in1=gmx[:, 0:ng], op=ALU.is_ge), R=[gs, gmx], W=[gmask])
        fw.op("dve", lambda e: e.reduce_max(out=r1[:, 0:n4], in_=s4, axis=AX.X), R=[selv], W=[r1])
        for ee in range(4):
            fw.op("dve", lambda e: e.tensor_tensor(out=is1[:, 0:n4, ee], in0=s4[:, :, ee], in1=r1[:, 0:n4], op=ALU.is_ge), R=[selv, r1], W=[is1])
        fw.op("dve", lambda e: e.scalar_tensor_tensor(out=v2[:, 0:n4, :], in0=is1[:, 0:n4, :], scalar=-1.0e9, in1=s4, op0=ALU.mult, op1=ALU.add), R=[is1, selv], W=[v2])
        fw.op("dve", lambda e: e.reduce_max(out=r2[:, 0:n4], in_=v2[:, 0:n4, :], axis=AX.X), R=[v2], W=[r2])
        for ee in range(4):
            fw.op("dve", lambda e: e.tensor_tensor(out=oh[:, 0:n4, ee], in0=v2[:, 0:n4, ee], in1=r2[:, 0:n4], op=ALU.is_ge), R=[v2, r2], W=[oh])
        fw.op("dve", lambda e: e.tensor_tensor(out=oh[:, 0:n4, :], in0=oh[:, 0:n4, :], in1=is1[:, 0:n4, :], op=ALU.add), R=[oh, is1], W=[oh])
        for ee in range(4):
            fw.op("dve", lambda e: e.tensor_tensor(out=oh[:, 0:n4, ee], in0=oh[:, 0:n4, ee], in1=gmask[:, 0:n4], op=ALU.mult), R=[oh, gmask], W=[oh])
        ohv = oh[:, 0:n4, :].rearrange("p (g q) e -> p g (q e)", q=4)
        fw.op("dve", lambda e: e.tensor_tensor(out=comb[:, 0:ng, :], in0=aff[:, 0:ng, :], in1=ohv, op=ALU.mult), R=[aff, oh], W=[comb])
        fw.op("dve", lambda e: e.reduce_sum(out=ssum[:, 0:ng], in_=comb[:, 0:ng, :], axis=AX.X), R=[comb], W=[ssum])
        fw.op("dve", lambda e: e.reciprocal(out=ssum[:, 0:ng], in_=ssum[:, 0:ng]), R=[ssum], W=[ssum])
        for j in range(ng):
            fw.op("dve", lambda e: e.tensor_scalar_mul(out=comb[:, j, :], in0=comb[:, j, :], scalar1=ssum[:, j:j + 1]), R=[comb, ssum], W=[comb])
            pc = sml()
            fw.op("pe", lambda e: e.transpose(out=pc[0:16, 0:128], in_=comb[:, j, :], identity=idf[:]), R=[comb, idf], W=[pc])
            fw.op("act", lambda e: e.activation(out=cT[:, j * 128:(j + 1) * 128], in_=pc[0:16, 0:128], func=AF.Copy), R=[pc], W=[cT])
        gchunks = []
        c = 0
        while c < ng * 128:
            n = min(512, ng * 128 - c)
            gchunks.append((c, n))
            c += n
        mitems = [(ex, c0, n) for ex in range(16) for (c0, n) in gchunks]
        wts = {}
        mst = {}

        def load_w(ex):
            wg = wgp(); wu = wup(); wd = wdp()
            for (wdst, src) in ((wg, I["w_exp_gate"][l, ex]), (wu, I["w_exp_up"][l, ex])):
                for hf in range(2):
                    fw.dma("pool", wdst[:, hf * 4:(hf + 1) * 4, :], src[hf * 512:(hf + 1) * 512, :].rearrange("(k p) f -> p k f", p=128), W=[wdst])
            for hf in range(2):
                fw.dma("pool", wd[:, hf * 2:(hf + 1) * 2, :], I["w_exp_down"][l, ex, hf * 256:(hf + 1) * 256, :].rearrange("(k p) d -> p k d", p=128), W=[wd])
            wts[ex] = (wg, wu, wd)

        def mfront(i):
            ex, c0, n = mitems[i]
            if ex not in wts:
                load_w(ex)
            wg, wu, wd = wts[ex]
            cbps = sml()
            fw.op("pe", lambda e: e.matmul(cbps[:, 0:n], lhsT=selb[:, ex, :], rhs=cT[:, c0:c0 + n], start=True, stop=True), R=[selb, cT], W=[cbps])
            cb = cbp()
            fw.op("act", lambda e: e.activation(out=cb[:, 0:n], in_=cbps[:, 0:n], func=AF.Copy), R=[cbps], W=[cb])
            aT = aTp()
            for fc in range(4):
                gp = gup(); up_ = gup()
                for kc in range(8):
                    fw.op("pe", lambda e: e.matmul(gp[:, 0:n], lhsT=wg[:, kc, fc * 128:(fc + 1) * 128], rhs=hT[:, kc, c0:c0 + n], start=(kc == 0), stop=(kc == 7)), R=[wg, hT], W=[gp])
                for kc in range(8):
                    fw.op("pe", lambda e: e.matmul(up_[:, 0:n], lhsT=wu[:, kc, fc * 128:(fc + 1) * 128], rhs=hT[:, kc, c0:c0 + n], start=(kc == 0), stop=(kc == 7)), R=[wu, hT], W=[up_])
                sg = sgp()
                fw.op("act", lambda e: e.activation(out=sg[:, 0:n], in_=gp[:, 0:n], func=AF.Silu), R=[gp], W=[sg])
                a1 = a1p()
                fw.op("dve", lambda e: e.tensor_tensor(out=a1[:, 0:n], in0=up_[:, 0:n], in1=sg[:, 0:n], op=ALU.mult), R=[up_, sg], W=[a1])
                fw.op("dve", lambda e: e.tensor_tensor(out=aT[:, fc, 0:n], in0=a1[:, 0:n], in1=cb[:, 0:n], op=ALU.mult), R=[a1, cb], W=[aT])
            mst[i] = aT

        def mback(i):
            ex, c0, n = mitems[i]
            wg, wu, wd = wts[ex]
            aT = mst.pop(i)
            for tl in range(n // 128):
                j = c0 // 128 + tl
                for hf in range(2):
                    y = yps()
                    for fc in range(4):
                        fw.op("pe", lambda e: e.matmul(y[:], lhsT=aT[:, fc, tl * 128:(tl + 1) * 128], rhs=wd[:, fc, hf * 512:(hf + 1) * 512], start=(fc == 0), stop=(fc == 3)), R=[aT, wd], W=[y])
                    if ex == 0:
                        fw.op("act", lambda e: e.activation(out=Y[:, j, hf * 512:(hf + 1) * 512], in_=y[:], func=AF.Copy), R=[y], W=[Y])
                    else:
                        fw.op("dve", lambda e: e.tensor_tensor(out=Y[:, j, hf * 512:(hf + 1) * 512], in0=y[:], in1=Y[:, j, hf * 512:(hf + 1) * 512], op=ALU.add), R=[y, Y], W=[Y])

        nM = len(mitems)
        nch = len(gchunks)
        load_w(0)
        for i in range(nM + 1):
            if i < nM:
                mfront(i)
                ex_i = mitems[i][0]
                if (i % nch) == min(1, nch - 1) and ex_i + 1 < 16 and i >= 1:
                    mback(i - 1)
                    load_w(ex_i + 1)
                    continue
            if i >= 1:
                mback(i - 1)
        for i in range(g0, g1):
            j = i - g0
            xt = xtp()
            fw.dma("sp", xt[:], S["XS"][i * 128:(i + 1) * 128, :], W=[xt])
            if last and i >= 2:
                dst = self.out[(i - 2) * 128:(i - 1) * 128, :]
            else:
                dst = S["XS"][i * 128:(i + 1) * 128, :]
            ys = [(Y, Y[:, j, 0:512]), (Y, Y[:, j, 512:1024])]
            _ln_epilogue(self, ys, xt, m5[1 if i < 2 else 0], gbc, bbc, pools, dst)
    self.end("moe%d" % l)


KB.ph_moe = _ph_moe
```

```python
import contextlib
import math
import numpy as np
import ml_dtypes
import concourse.bass as bass
import concourse.mybir as mybir
from concourse.bass_utils import run_bass_kernel_spmd

F32 = mybir.dt.float32
BF16 = mybir.dt.bfloat16
AF = mybir.ActivationFunctionType
ALU = mybir.AluOpType
AX = mybir.AxisListType

D = 1024
LC = 256
LL = 4096
T = LC + LL
NT = T // 128
DEPTH = 4
D_IN = 7712
ALPHA = (2 * DEPTH) ** 0.25
LN_EPS = 1e-5
RMS_EPS = 1e-6
O_GQ, O_GK, O_GV, O_GG, O_GR = 0, 256, 512, 1024, 1536
O_DQ, O_DK, O_DV = 1568, 2080, 2592
O_NQ, O_NK, O_NV = 3104, 3616, 4128
O_GT = 4640


class Buf:
    __slots__ = ("name", "t", "last_write", "reads", "excl")

    def __init__(self, name, t=None, excl=False):
        self.name = name
        self.t = t
        self.last_write = None
        self.reads = {}
        self.excl = excl

    def __getitem__(self, key):
        return self.t[key]


class _Op:
    __slots__ = ("fn", "waits", "tok", "signal", "val")

    def __init__(self, fn):
        self.fn = fn
        self.waits = []
        self.tok = None
        self.signal = False
        self.val = None


class _Rec:
    call = None

    def __getattr__(self, name):
        def f(*a, **k):
            self.call = (name, a, k)
            return self
        return f


class _Eng:
    def __init__(self, name):
        self.name = name
        self.ops = []
        self.seen = {}
        self.is_pe = name == "pe"
        self.sem = None
        self.dsems = []
        self.dcount = []
        self.drr = 0
        self.emitted = 0
        self.sigcount = 0


class FW:
    ENGS = ("pe", "act", "dve", "pool", "sp")

    def __init__(self, nc, st, n_dma_sems=16):
        self.nc = nc
        self.e = {n: _Eng(n) for n in self.ENGS}
        self.n_dma_sems = n_dma_sems
        for en in self.ENGS:
            E = self.e[en]
            E.sem = st.enter_context(nc.semaphore("s_" + en))
            if en in ("sp", "pool", "act"):
                E.dcount = [0] * n_dma_sems
                E.dsems = [st.enter_context(nc.semaphore("d_%s_%d" % (en, i))) for i in range(n_dma_sems)]

    def _deps(self, E, reads, writes):
        deps = {}

        def add(tok):
            if tok is None:
                return
            if tok[0] == "c":
                if E.is_pe and tok[1] == "pe":
                    return
                key = ("c", tok[1])
            else:
                key = ("d", tok[1])
            if key not in deps or deps[key][2] < tok[2]:
                deps[key] = tok

        for b in reads:
            add(b.last_write)
            if b.excl:
                for t in b.reads.values():
                    add(t)
        for b in writes:
            add(b.last_write)
            for t in b.reads.values():
                add(t)
        out = []
        for key, tok in deps.items():
            if E.seen.get(key, -1) >= tok[2]:
                continue
            E.seen[key] = tok[2]
            out.append(tok)
        return out

    def _commit(self, tok, reads, writes):
        for b in writes:
            b.last_write = tok
            b.reads = {}
        for b in reads:
            if b.excl:
                b.last_write = tok
                b.reads = {}
            else:
                b.reads[(tok[0], tok[1])] = tok

    def op(self, eng, fn, R=(), W=()):
        E = self.e[eng]
        rec = _Rec()
        fn(rec)
        call = rec.call
        o = _Op(lambda e, c=call: getattr(e, c[0])(*c[1], **c[2]))
        R = [b for b in R if b is not None]
        W = [b for b in W if b is not None]
        o.waits = self._deps(E, R, W)
        o.tok = ("c", eng, len(E.ops))
        E.ops.append(o)
        self._commit(o.tok, R, W)
        return o

    def dma(self, eng, out, in_, R=(), W=(), **kw):
        E = self.e[eng]
        o = _Op(lambda e: e.dma_start(out=out, in_=in_, **kw))
        R = [b for b in R if b is not None]
        W = [b for b in W if b is not None]
        o.waits = self._deps(E, R, W)
        i = E.drr
        E.drr = (E.drr + 1) % self.n_dma_sems
        key = ("d", (eng, i))
        prev = E.dcount[i]
        if prev > 0 and E.seen.get(key, -1) < prev:
            E.seen[key] = prev
            o.waits.append(("d", (eng, i), prev))
        E.dcount[i] = prev + 16
        o.tok = ("d", (eng, i), prev + 16)
        E.ops.append(o)
        self._commit(o.tok, R, W)
        return o

    def phase_end(self):
        nc = self.nc
        E = self.e["sp"]
        o = _Op(None)
        for en in self.ENGS:
            Q = self.e[en]
            for i, c in enumerate(Q.dcount):
                if c > 0 and E.seen.get(("d", (en, i)), -1) < c:
                    o.waits.append(("d", (en, i), c))
            if en != "sp":
                for q in reversed(Q.ops[Q.emitted:]):
                    if q.tok[0] == "c" and q.fn is not None:
                        o.waits.append(q.tok)
                        break
        o.tok = ("c", "sp", len(E.ops))
        E.ops.append(o)
        for en in self.ENGS:
            Q = self.e[en]
            for q in Q.ops[Q.emitted:]:
                for w in q.waits:
                    if w[0] == "c":
                        P = self.e[w[1]]
                        assert w[2] >= P.emitted, "wait on op of an earlier phase"
                        P.ops[w[2]].signal = True
        for en in self.ENGS:
            Q = self.e[en]
            for q in Q.ops[Q.emitted:]:
                if q.tok[0] == "c" and q.signal:
                    assert q.fn is not None
                    Q.sigcount += 1
                    q.val = Q.sigcount

        with nc.Block() as block:
            def run(en, eng):
                Q = self.e[en]
                for q in Q.ops[Q.emitted:]:
                    for w in q.waits:
                        if w[0] == "c":
                            P = self.e[w[1]]
                            eng.wait_ge(P.sem, P.ops[w[2]].val)
                        else:
                            qn, i = w[1]
                            eng.wait_ge(self.e[qn].dsems[i], w[2])
                    if q.fn is None:
                        continue
                    ins = q.fn(eng)
                    if q.tok[0] == "d":
                        qn, i = q.tok[1]
                        ins.then_inc(self.e[qn].dsems[i], 16)
                    elif q.signal:
                        ins.then_inc(Q.sem, 1)

            @block.tensor
            def _(eng):
                run("pe", eng)

            @block.scalar
            def _(eng):
                run("act", eng)

            @block.vector
            def _(eng):
                run("dve", eng)

            @block.gpsimd
            def _(eng):
                run("pool", eng)

            @block.sync
            def _(eng):
                run("sp", eng)

        nc.all_engine_barrier()
        nops = {en: (len(self.e[en].ops) - self.e[en].emitted, sum(len(q.waits) for q in self.e[en].ops[self.e[en].emitted:])) for en in self.ENGS}
        for en in self.ENGS:
            Q = self.e[en]
            Q.emitted = len(Q.ops)
        for en in self.ENGS:
            Q = self.e[en]
            for pn in self.ENGS:
                P = self.e[pn]
                Q.seen[("c", pn)] = len(P.ops) - 1
                for i, c in enumerate(P.dcount):
                    Q.seen[("d", (pn, i))] = c
        return nops


def _consts():
    c = {}
    c["ident"] = np.eye(128, dtype=np.float32)
    n = np.arange(LL)
    row = (n // 64).astype(np.float32)
    col = (n % 64).astype(np.float32)
    nf = 16
    inv = (10000.0 ** (-np.arange(nf, dtype=np.float32) / nf)).astype(np.float32)
    ang = np.concatenate([row[:, None] * inv, col[:, None] * inv], -1).astype(np.float32)
    cos = np.cos(ang).astype(np.float32)
    sin = np.sin(ang).astype(np.float32)
    ct = np.ones((128, T), np.float32)
    stb = np.zeros((128, T), np.float32)
    for p in range(128):
        j = p % 64
        f = j % 32
        ct[p, LC:] = cos[:, f]
        stb[p, LC:] = (-sin[:, f]) if j < 32 else sin[:, f]
    c["rope_c"] = ct
    c["rope_s"] = stb
    s = np.arange(128)[:, None]
    t = np.arange(128)[None, :]
    le = (s <= t).astype(np.float32)
    gt = (s > t).astype(np.float32)
    tri = np.stack([le, le.T, gt, gt.T]).astype(np.float32)
    c["tri"] = tri
    c["mask4"] = np.stack([np.tile(le, (1, 4)), np.tile(le.T, (1, 4))]).astype(np.float32)
    J = np.zeros((64, 128), np.float32)
    for m in range(64):
        J[m, 63 - m] = 1.0
        J[m, 64 + 63 - m] = 1.0
    c["j64"] = J
    cc = np.arange(64)
    cs = np.clip(cc - 8, 0, 48)
    W = ((cc[:, None] >= cs[None, :]) & (cc[:, None] < cs[None, :] + 16)).astype(np.float32)
    W2 = np.concatenate([W, W], 0)
    c["nawin"] = np.tile(W2[:, None, :], (1, 8, 1)).reshape(128, 512).astype(np.float32)
    sel = np.zeros((16, 16, 128), np.float32)
    for e in range(16):
        sel[e, e, :] = 1.0
    c["sel"] = sel
    return c


CONST_SHAPES = {
    "ident": [128, 128], "rope_c": [128, T], "rope_s": [128, T], "tri": [4, 128, 128],
    "mask4": [2, 128, 512], "j64": [64, 128], "nawin": [128, 512], "sel": [16, 16, 128],
}

INPUT_SHAPES = {
    "x": [LL, D], "c": [D], "ctx": [LC, D], "c_ctx": [D],
    "w_ada": [DEPTH, D, 6 * D], "b_ada": [DEPTH, 6 * D], "w_in": [DEPTH, D, D_IN],
    "gla_w_decay": [DEPTH, 2, 16, 256], "gla_b_decay": [DEPTH, 2, 256], "gla_norm_g": [DEPTH, 128],
    "diff_lam_q": [DEPTH, 2, 64], "diff_lam_k": [DEPTH, 2, 64], "diff_norm_g": [DEPTH, 128],
    "na_rpb": [DEPTH, 8, 15, 31], "w_branch": [DEPTH, 3, 512, D], "w_o": [DEPTH, D, D],
    "ln_g": [DEPTH, 2, D], "ln_b": [DEPTH, 2, D], "w_router": [D, 16], "b_router": [16],
    "w_exp_gate": [DEPTH, 16, D, 512], "w_exp_up": [DEPTH, 16, D, 512], "w_exp_down": [DEPTH, 16, 512, D],
}


class KB:
    def __init__(self, nlayers=DEPTH, debug=(), stop_after=None):
        self.nlayers = nlayers
        self.debug = set(debug)
        self.stop_after = stop_after
        self.nc = bass.Bass("TRN2", target_bir_lowering=False)
        nc = self.nc
        self.I = {k: nc.dram_tensor(k, v, F32, kind="ExternalInput").ap() for k, v in INPUT_SHAPES.items()}
        self.C = {k: nc.dram_tensor("k_" + k, v, F32, kind="ExternalInput").ap() for k, v in CONST_SHAPES.items()}
        self.out = nc.dram_tensor("out", [LL, D], F32, kind="ExternalOutput").ap()
        self.S = {}
        self.uid = 0

    def scratch(self, name, shape, dt):
        kind = "ExternalOutput" if (name in self.debug or name == "TAB") else "Internal"
        self.S[name] = self.nc.dram_tensor("s_" + name, shape, dt, kind=kind).ap()
        return self.S[name]

    def begin(self):
        self.pst = contextlib.ExitStack()
        self.pst.__enter__()
        self._cc = {}

    def warm(self, funcs):
        return
        fw = self.fw
        w = self.sb("warm", [128, 2048], F32)
        fw.op("dve", lambda e: e.memset(w[:], 1.0), W=[w])
        for f in funcs:
            for _ in range(2):
                fw.op("act", lambda e: e.activation(out=w[:], in_=w[:], func=f), R=[w], W=[w])
            fw.op("dve", lambda e: e.memset(w[:], 1.0), W=[w])

    def const_col(self, val):
        if val not in self._cc:
            t = self.sb("cc", [128, 1], F32)
            self.fw.op("dve", lambda e: e.memset(t[:], float(val)), W=[t])
            self._cc[val] = t
        return self._cc[val]

    def end(self, name=""):
        nops = self.fw.phase_end()
        self.pst.__exit__(None, None, None)
        print("phase", name, nops, flush=True)

    def sb(self, name, shape, dt):
        self.uid += 1
        return Buf(name, self.pst.enter_context(self.nc.sbuf_tensor("%s_%d" % (name, self.uid), shape, dt)))

    def ps(self, name, shape, dt=F32):
        self.uid += 1
        return Buf(name, self.pst.enter_context(self.nc.psum_tensor("%s_%d" % (name, self.uid), shape, dt)),
                   excl=True)

    def pool(self, name, shape, dt, n, psum=False):
        bufs = [(self.ps if psum else self.sb)(name + str(i), shape, dt) for i in range(n)]
        state = {"i": 0}

        def nxt():
            b = bufs[state["i"] % n]
            state["i"] += 1
            return b
        return nxt

    def build(self):
        nc = self.nc
        with contextlib.ExitStack() as gst:
            self.fw = FW(nc, gst)
            self.alloc_scratch()
            self.ph_mod()
            if self.stop_after == "mod":
                return nc
            for l in range(self.nlayers):
                self.ph_proj(l)
                if self.stop_after in ("proj", "ht"):
                    break
                self.ph_gla(l)
                self.ph_glac(l)
                if self.stop_after == "gla":
                    break
                self.ph_diff(l)
                if self.stop_after == "diff":
                    break
                self.ph_na(l)
                if self.stop_after in ("na", "natab"):
                    break
                self.ph_merge(l)
                if self.stop_after == "merge":
                    break
                self.ph_moe(l, last=(l == self.nlayers - 1))
        return nc

    def alloc_scratch(self):
        s = self.scratch
        s("XS", [T, D], F32)
        s("MODF", [DEPTH, 128, 48, 2], F32)
        s("MODR", [DEPTH, 2, 2, D], F32)
        s("GQT", [4, 64, T], BF16)
        s("GKT", [4, 64, T], BF16)
        s("GK", [T, 256], BF16)
        s("GV", [T, 512], BF16)
        s("GG", [512, T], BF16)
        s("GR", [64, T], BF16)
        s("DQT", [4, 128, T], BF16)
        s("DKT", [4, 128, T], BF16)
        s("DV", [T, 512], BF16)
        s("NQT", [4, 128, T], BF16)
        s("NKT", [4, 128, T], BF16)
        s("NV", [T, 512], BF16)
        s("GT", [3, D, T], BF16)
        s("OG", [2, 4, 128, T], F32)
        s("BR", [3, 512, T], BF16)
        s("NAG", [120, 128], F32)

    def ph_mod(self):
        fw, I = self.fw, self.I
        self.begin()
        sc = self.sb("sc", [128, 8, 2], F32)
        ones = self.sb("ones", [1, 128], F32)
        idf, idb = self.load_ident()
        sc0 = self.sb("sc0", [128, 8], F32)
        sc1 = self.sb("sc1", [128, 8], F32)
        self.diag_cols(sc0, sc0[:], I["c"].rearrange("(o n) -> o n", o=1), 8, idf)
        self.diag_cols(sc1, sc1[:], I["c_ctx"].rearrange("(o n) -> o n", o=1), 8, idf)
        fw.op("dve", lambda e: e.tensor_copy(out=sc[:, :, 0], in_=sc0[:]), R=[sc0], W=[sc])
        fw.op("dve", lambda e: e.tensor_copy(out=sc[:, :, 1], in_=sc1[:]), R=[sc1], W=[sc])
        fw.op("act", lambda e: e.activation(out=sc[:], in_=sc[:], func=AF.Silu), R=[sc], W=[sc])
        fw.op("dve", lambda e: e.memset(ones[:], 1.0), W=[ones])
        if "DBG1" in self.debug:
            self.scratch("DBG1", [128, 16], F32)
            self.scratch("DBG2", [1, 6 * D], F32)
            fw.dma("pool", self.S["DBG1"], sc[:].rearrange("p k s -> p (k s)"), R=[sc])
        wpool = self.pool("wada", [128, 8, 512], F32, 2)
        psf = self.pool("psf", [128, 4, 2], F32, 2, psum=True)
        psr = self.pool("psr", [2, 512], F32, 2, psum=True)
        for l in range(DEPTH):
            bfm = self.sb("bfm", [128, 48], F32)
            brow = self.sb("brow", [2, 2, D], F32)
            modf = self.sb("modf", [128, 48, 2], F32)
            modr = self.sb("modr", [2, 2, D], F32)
            fw.op("dve", lambda e: e.memset(modf[:], 0.0), W=[modf])
            self.diag_cols(bfm, bfm[:], I["b_ada"][l:l + 1, :], 48, idf)
            for jj, j in enumerate((2, 5)):
                fw.dma("sp", brow[:, jj, :], I["b_ada"][l:l + 1, j * D:(j + 1) * D].partition_broadcast(2), W=[brow])
            for j in (1, 4):
                fw.op("dve", lambda e, j=j: e.tensor_scalar_add(out=bfm[:, j * 8:(j + 1) * 8], in0=bfm[:, j * 8:(j + 1) * 8], scalar1=1.0), R=[bfm], W=[bfm])
            for jn in range(12):
                j = jn // 2
                w = wpool()
                fw.dma("sp", w[:], I["w_ada"][l, :, jn * 512:(jn + 1) * 512].rearrange("(kc p) n -> p kc n", p=128), W=[w])
                if j in (2, 5):
                    p = psr()
                    for kc in range(8):
                        fw.op("pe", lambda e: e.matmul(p[:], lhsT=sc[:, kc, :], rhs=w[:, kc, :], start=(kc == 0), stop=(kc == 7)),
                              R=[sc, w], W=[p])
                    jj = 0 if j == 2 else 1
                    half = jn % 2
                    fw.op("dve", lambda e: e.tensor_tensor(out=modr[:, jj, half * 512:(half + 1) * 512], in0=p[:], in1=brow[:, jj, half * 512:(half + 1) * 512], op=ALU.add),
                          R=[p, brow], W=[modr])
                else:
                    p = psf()
                    for sub in range(4):
                        for kc in range(8):
                            fw.op("pe", lambda e: e.matmul(p[:, sub, :], lhsT=w[:, kc, sub * 128:(sub + 1) * 128], rhs=sc[:, kc, :], start=(kc == 0), stop=(kc == 7)),
                                  R=[sc, w], W=[p])
                    for s_ in range(2):
                        fw.op("dve", lambda e: e.tensor_tensor(out=modf[:, jn * 4:(jn + 1) * 4, s_], in0=p[:, :, s_], in1=bfm[:, jn * 4:(jn + 1) * 4], op=ALU.add),
                              R=[p, bfm], W=[modf])
            fw.dma("pool", self.S["MODF"][l], modf[:], R=[modf])
            fw.dma("pool", self.S["MODR"][l].rearrange("s j d -> s (j d)"), modr[:].rearrange("s j d -> s (j d)"), R=[modr])
        self.end("mod")

    def diag_cols(self, dstbuf, dst, src_row_ap, nch, idf):
        fw = self.fw
        bc = self.sb("dgbc", [128, nch, 128], F32)
        fw.dma("sp", bc[:].rearrange("p k j -> p (k j)"), src_row_ap.partition_broadcast(128), W=[bc])
        for k in range(nch):
            fw.op("dve", lambda e: e.tensor_tensor(out=bc[:, k, :], in0=bc[:, k, :], in1=idf[:], op=ALU.mult), R=[bc, idf], W=[bc])
        fw.op("dve", lambda e: e.reduce_sum(out=dst, in_=bc[:], axis=AX.X), R=[bc], W=[dstbuf])

    def x_src(self, l, i):
        if l == 0:
            if i < 2:
                return self.I["ctx"][i * 128:(i + 1) * 128, :]
            return self.I["x"][(i - 2) * 128:(i - 1) * 128, :]
        return self.S["XS"][i * 128:(i + 1) * 128, :]

    def load_ident(self):
        fw = self.fw
        idf = self.sb("idf", [128, 128], F32)
        idb = self.sb("idb", [128, 128], BF16)
        fw.dma("sp", idf[:], self.C["ident"][:, :], W=[idf])
        fw.op("dve", lambda e: e.tensor_copy(out=idb[:], in_=idf[:]), R=[idf], W=[idb])
        return idf, idb

    def ph_proj(self, l):
        fw, I, S, C = self.fw, self.I, self.S, self.C
        self.begin()
        idf, idb = self.load_ident()
        modf = self.sb("modf", [128, 48, 2], F32)
        fw.dma("sp", modf[:], S["MODF"][l], W=[modf])
        hT = self.sb("hT", [128, 8, T], BF16)
        xp = self.pool("xt", [128, D], F32, 2)
        xbp = self.pool("xb", [128, D], BF16, 2)
        ptr = self.pool("ptr", [128, 8, 128], BF16, 2, psum=True)
        for i in range(NT):
            xt = xp()
            xb = xbp()
            fw.dma("sp", xt[:], self.x_src(l, i), W=[xt])
            fw.op("dve", lambda e, xt=xt, xb=xb: e.tensor_copy(out=xb[:], in_=xt[:]), R=[xt], W=[xb])
            p = ptr()
            for kc in range(8):
                fw.op("pe", lambda e, p=p, xb=xb, kc=kc: e.transpose(out=p[:, kc, :], in_=xb[:, kc * 128:(kc + 1) * 128], identity=idb[:]),
                      R=[xb, idb], W=[p])
            st = 1 if i < 2 else 0
            for kc in range(8):
                fw.op("act", lambda e, p=p, kc=kc, i=i, st=st: e.activation(
                    out=hT[:, kc, i * 128:(i + 1) * 128], in_=p[:, kc, :], func=AF.Identity,
                    bias=modf[:, kc, st:st + 1], scale=modf[:, 8 + kc, st:st + 1]), R=[p, modf], W=[hT])
        if "HT" in self.debug:
            self.scratch("HT", [128, 8, T], BF16)
            fw.dma("pool", S["HT"], hT[:], R=[hT])
        if self.stop_after == "ht":
            self.end("ht")
            return
        rc = self.sb("rc", [128, T], F32)
        rs = self.sb("rs", [128, T], F32)
        fw.dma("sp", rc[:], C["rope_c"][:, :], W=[rc])
        fw.dma("sp", rs[:], C["rope_s"][:, :], W=[rs])

        wfp = self.pool("wf", [128, 8, 256], F32, 2)
        wbp = self.pool("wb", [128, 8, 256], BF16, 3)
        pmm = self.pool("pmm", [128, 512], F32, 4, psum=True)
        stg = self.pool("stg", [128, T], BF16, 2)
        tmp = self.pool("tmp", [128, 512], F32, 3)
        tstg = self.pool("tstg", [128, 256], BF16, 3)
        win = I["w_in"][l]
        chunks = [(0, 256)] + [(256 + 512 * j, 512) for j in range(8)]
        self._cast_i = 0

        def load_w(pieces, ncols, zero=False):
            wf = wfp()
            wb = wbp()
            if zero:
                fw.op("dve", lambda e, wf=wf: e.memset(wf[:], 0.0), W=[wf])
            for (dc, scol, n) in pieces:
                fw.dma("sp", wf[:, :, dc:dc + n], win[:, scol:scol + n].rearrange("(kc p) n -> p kc n", p=128), W=[wf])
            eng = "dve" if (self._cast_i % 2 == 0) else "act"
            self._cast_i += 1
            if eng == "act":
                fw.op("act", lambda e: e.activation(out=wb[:, :, 0:ncols], in_=wf[:, :, 0:ncols], func=AF.Copy), R=[wf], W=[wb])
            else:
                fw.op("dve", lambda e: e.tensor_copy(out=wb[:, :, 0:ncols], in_=wf[:, :, 0:ncols]), R=[wf], W=[wb])
            return wb

        def fm_mm(wb, m, c0, cn, col0=0):
            p = pmm()
            for kc in range(8):
                fw.op("pe", lambda e, p=p, wb=wb, kc=kc: e.matmul(p[0:m, 0:cn], lhsT=wb[:, kc, col0:col0 + m], rhs=hT[:, kc, c0:c0 + cn], start=(kc == 0), stop=(kc == 7)),
                      R=[wb, hT], W=[p])
            return p

        def fm_group(pieces, m, evac, dsts, zero=False):
            wb = load_w(pieces, m, zero)
            sg = stg()
            for (c0, cn) in chunks:
                p = fm_mm(wb, m, c0, cn)
                evac(p, sg, c0, cn, m)
            for (r0, nr, dst) in dsts:
                fw.dma("pool", dst, sg[r0:r0 + nr, :], R=[sg])

        def ev_copy(p, sg, c0, cn, m):
            fw.op("act", lambda e: e.activation(out=sg[0:m, c0:c0 + cn], in_=p[0:m, 0:cn], func=AF.Copy), R=[p], W=[sg])

        def ev_silu(p, sg, c0, cn, m):
            fw.op("act", lambda e: e.activation(out=sg[0:m, c0:c0 + cn], in_=p[0:m, 0:cn], func=AF.Silu), R=[p], W=[sg])

        def ev_sigm(p, sg, c0, cn, m):
            fw.op("act", lambda e: e.activation(out=sg[0:m, c0:c0 + cn], in_=p[0:m, 0:cn], func=AF.Sigmoid), R=[p], W=[sg])

        for (o0, dst) in ((O_GQ, S["GQT"]), (O_GK, S["GKT"])):
            for g in range(2):
                fm_group([(0, o0 + g * 128, 128)], 128, ev_copy,
                         [(0, 64, dst[2 * g]), (64, 64, dst[2 * g + 1])])
        for g in range(4):
            fm_group([(0, O_GG + g * 128, 128)], 128, ev_silu, [(0, 128, S["GG"][g * 128:(g + 1) * 128, :])])
        fm_group([(0, O_GR, 16), (32, O_GR + 16, 16)], 64, ev_copy, [(0, 64, S["GR"])], zero=True)
        for (o0, dst) in ((O_DQ, S["DQT"]), (O_DK, S["DKT"])):
            for h in range(4):
                b0 = o0 + h * 128
                wa = load_w([(0, b0, 128)], 128)
                wsw = load_w([(0, b0 + 32, 32), (32, b0, 32), (64, b0 + 96, 32), (96, b0 + 64, 32)], 128)
                sg = stg()
                for (c0, cn) in chunks:
                    pa = fm_mm(wa, 128, c0, cn)
                    pb = fm_mm(wsw, 128, c0, cn)
                    t1 = tmp()
                    t2 = tmp()
                    fw.op("dve", lambda e, pa=pa, t1=t1, c0=c0, cn=cn: e.tensor_tensor(out=t1[:, 0:cn], in0=pa[:, 0:cn], in1=rc[:, c0:c0 + cn], op=ALU.mult), R=[pa, rc], W=[t1])
                    fw.op("dve", lambda e, pb=pb, t2=t2, c0=c0, cn=cn: e.tensor_tensor(out=t2[:, 0:cn], in0=pb[:, 0:cn], in1=rs[:, c0:c0 + cn], op=ALU.mult), R=[pb, rs], W=[t2])
                    fw.op("dve", lambda e, t1=t1, t2=t2, sg=sg, c0=c0, cn=cn: e.tensor_tensor(out=sg[:, c0:c0 + cn], in0=t1[:, 0:cn], in1=t2[:, 0:cn], op=ALU.add), R=[t1, t2], W=[sg])
                fw.dma("pool", dst[h], sg[:, :], R=[sg])
        for (o0, dst) in ((O_NQ, S["NQT"]), (O_NK, S["NKT"])):
            for g in range(4):
                fm_group([(0, o0 + g * 128, 128)], 128, ev_copy, [(0, 128, dst[g])])
        for n in range(3):
            for g in range(8):
                fm_group([(0, O_GT + n * D + g * 128, 128)], 128, ev_sigm, [(0, 128, S["GT"][n, g * 128:(g + 1) * 128, :])])
        for (o0, ncols, dst) in ((O_GK, 256, S["GK"]), (O_GV, 512, S["GV"]), (O_DV, 512, S["DV"]), (O_NV, 512, S["NV"])):
            for cg in range(ncols // 256):
                wb = load_w([(0, o0 + cg * 256, 256)], 256)
                for i in range(NT):
                    p = pmm()
                    for kc in range(8):
                        fw.op("pe", lambda e, p=p, wb=wb, kc=kc, i=i: e.matmul(p[:, 0:256], lhsT=hT[:, kc, i * 128:(i + 1) * 128], rhs=wb[:, kc, :], start=(kc == 0), stop=(kc == 7)),
                              R=[wb, hT], W=[p])
                    ts = tstg()
                    eng = "act" if i % 2 == 0 else "dve"
                    if eng == "act":
                        fw.op("act", lambda e, p=p, ts=ts: e.activation(out=ts[:], in_=p[:, 0:256], func=AF.Copy), R=[p], W=[ts])
                    else:
                        fw.op("dve", lambda e, p=p, ts=ts: e.tensor_copy(out=ts[:], in_=p[:, 0:256]), R=[p], W=[ts])
                    fw.dma("pool", dst[i * 128:(i + 1) * 128, cg * 256:(cg + 1) * 256], ts[:], R=[ts])
        self.end("proj%d" % l)


def build_program(nlayers=DEPTH, debug=(), stop_after=None):
    kb = KB(nlayers, debug, stop_after)
    kb.build()
    return kb


_CONSTS = None


def make_in_maps(inputs):
    global _CONSTS
    if _CONSTS is None:
        _CONSTS = _consts()
    f = lambda a: np.ascontiguousarray(np.asarray(a, dtype=np.float32))
    shared = {k: f(inputs[k]) for k in INPUT_SHAPES if k not in ("x", "c", "ctx")}
    for k, v in _CONSTS.items():
        shared["k_" + k] = v
    maps = []
    for b in range(8):
        m = dict(shared)
        m["x"] = f(inputs["x"][b])
        m["c"] = f(inputs["c"][b])
        m["ctx"] = f(inputs["ctx"][b])
        maps.append(m)
    return maps


def kernel(**inputs):
    kb = build_program()
    maps = make_in_maps(inputs)
    res = run_bass_kernel_spmd(kb.nc, maps, core_ids=list(range(8)))
    return np.stack([np.asarray(r["out"], dtype=np.float32) for r in res.results], axis=0)


def _ph_gla(self, l):
    fw, I, S, C = self.fw, self.I, self.S, self.C
    self.begin()
    self.warm([AF.Exp, AF.Ln])
    qT = self.sb("qT", [64, 4, T], BF16)
    kT = self.sb("kT", [64, 4, T], BF16)
    kk = self.sb("kk", [128, NT, 256], BF16)
    vv = self.sb("vv", [128, NT, 512], BF16)
    rT = self.sb("rT", [64, T], BF16)
    fw.dma("sp", qT[:], S["GQT"].rearrange("h d t -> d h t"), W=[qT])
    fw.dma("sp", kT[:], S["GKT"].rearrange("h d t -> d h t"), W=[kT])
    fw.dma("sp", kk[:], S["GK"].rearrange("(i p) n -> p i n", p=128), W=[kk])
    fw.dma("sp", vv[:], S["GV"].rearrange("(i p) n -> p i n", p=128), W=[vv])
    fw.dma("sp", rT[:], S["GR"][:, :], W=[rT])
    trif = self.sb("trif", [128, 4, 128], F32)
    trib = self.sb("trib", [128, 4, 128], BF16)
    fw.dma("sp", trif[:], C["tri"].rearrange("a s t -> s a t"), W=[trif])
    fw.op("act", lambda e: e.mul(out=trib[:], in_=trif[:], mul=-1.0 / 16.0), R=[trif], W=[trib])
    maskf = self.sb("maskf", [128, 2, 512], F32)
    fw.dma("sp", maskf[:], C["mask4"].rearrange("a s t -> s a t"), W=[maskf])
    wdf = self.sb("wdf", [64, 256], F32)
    wd = self.sb("wd", [64, 256], BF16)
    fw.op("dve", lambda e: e.memset(wdf[:], 0.0), W=[wdf])
    fw.dma("sp", wdf[0:16, :], I["gla_w_decay"][l, 0], W=[wdf])
    fw.dma("sp", wdf[32:48, :], I["gla_w_decay"][l, 1], W=[wdf])
    fw.op("dve", lambda e: e.tensor_copy(out=wd[:], in_=wdf[:]), R=[wdf], W=[wd])
    bdb = self.sb("bdb", [128, 2, 256], F32)
    fw.dma("sp", bdb[:].rearrange("p a n -> p (a n)"), I["gla_b_decay"][l:l + 1].rearrange("o a n -> o (a n)").partition_broadcast(128), W=[bdb])
    zbp = self.pool("zb", [128, 256], F32, 4)

    Sf = self.sb("Sf", [64, 4, 128], F32)
    Sb = self.sb("Sb", [64, 4, 128], BF16)
    zps = self.pool("zps", [128, 256], F32, 1, psum=True)
    cps = self.pool("cps", [64, 4, 128], F32, 1, psum=True)
    gps = self.pool("gps", [128, 256], F32, 1, psum=True)
    aps = self.pool("aps", [128, 4, 128], F32, 2, psum=True)
    ops_ = self.pool("ops", [128, 4, 128], F32, 2, psum=True)
    kvps = self.pool("kvps", [64, 4, 128], F32, 1, psum=True)
    e1p = self.pool("e1", [128, 256], F32, 4)
    Lbp = self.pool("Lb", [128, 256], BF16, 4)
    Eqp = self.pool("Eq", [64, 4, 128], F32, 4)
    Ekp = self.pool("Ek", [64, 4, 128], F32, 4)
    Estp = self.pool("Est", [128, 256], F32, 4)
    qinp = self.pool("qin", [64, 4, 128], BF16, 4)
    kinp = self.pool("kin", [64, 4, 128], BF16, 4)
    kstp = self.pool("kst", [128, 256], BF16, 4)
    attp = self.pool("att", [128, 4, 128], BF16, 4)
    osbp = self.pool("osb", [128, 4, 128], F32, 4)

    Sf1 = self.sb("Sf1", [64, 4, 128], F32)
    Sb1 = self.sb("Sb1", [64, 4, 128], BF16)
    Sfs, Sbs = [Sf, Sf1], [Sb, Sb1]
    for S_ in Sfs + Sbs:
        fw.op("dve", lambda e: e.memset(S_[:], 0.0), W=[S_])
    orders = [list(range(NT)), [1, 0] + list(range(NT - 1, 1, -1))]
    seq = [(dr_, orders[dr_][step]) for step in range(NT) for dr_ in range(2)]
    def chunk(dr, ci):
        Sf, Sb = Sfs[dr], Sbs[dr]
        r0 = 0 if dr == 0 else 32
        iu, ig, im = (0, 2, 0) if dr == 0 else (1, 3, 1)
        lastcol = 127 if dr == 0 else 0
        c0 = ci * 128
        z = zps()
        fw.op("pe", lambda e: e.matmul(z[:], lhsT=rT[r0:r0 + 16, c0:c0 + 128], rhs=wd[r0:r0 + 16, :], start=True, stop=True), R=[rT, wd], W=[z])
        zb = zbp()
        fw.op("dve", lambda e: e.tensor_tensor(out=zb[:], in0=z[:], in1=bdb[:, dr, :], op=ALU.add), R=[z, bdb], W=[zb])
        e1 = e1p()
        Lb = Lbp()
        fw.op("act", lambda e: e.activation(out=e1[:], in_=zb[:], func=AF.Exp, scale=-1.0), R=[zb], W=[e1])
        fw.op("act", lambda e: e.activation(out=Lb[:], in_=e1[:], func=AF.Ln, bias=1.0), R=[e1], W=[Lb])
        yield
        cp = cps()
        for h in range(4):
            fw.op("pe", lambda e, cp=cp, Lb=Lb, h=h: e.matmul(cp[:, h, :], lhsT=Lb[:, h * 64:(h + 1) * 64], rhs=trib[:, iu, :], start=True, stop=True), R=[Lb, trib], W=[cp])
        gp = gps()
        fw.op("pe", lambda e, gp=gp, Lb=Lb: e.matmul(gp[:], lhsT=trib[:, ig, :], rhs=Lb[:], start=True, stop=True), R=[Lb, trib], W=[gp])
        Eq = Eqp(); Ek = Ekp(); Est = Estp()
        fw.op("act", lambda e, cp=cp, Eq=Eq: e.activation(out=Eq[:], in_=cp[:], func=AF.Exp), R=[cp], W=[Eq])
        fw.op("act", lambda e, cp=cp, Ek=Ek: e.activation(out=Ek[:], in_=cp[:], func=AF.Exp, scale=-1.0), R=[cp], W=[Ek])
        fw.op("act", lambda e, gp=gp, Est=Est: e.activation(out=Est[:], in_=gp[:], func=AF.Exp), R=[gp], W=[Est])
        qin = qinp(); kin = kinp(); kst = kstp()
        fw.op("dve", lambda e, qin=qin, Eq=Eq, c0=c0: e.scalar_tensor_tensor(out=qin[:], in0=qT[:, :, c0:c0 + 128], scalar=0.125, in1=Eq[:], op0=ALU.mult, op1=ALU.mult), R=[qT, Eq], W=[qin])
        fw.op("dve", lambda e, kin=kin, Ek=Ek, c0=c0: e.tensor_tensor(out=kin[:], in0=kT[:, :, c0:c0 + 128], in1=Ek[:], op=ALU.mult), R=[kT, Ek], W=[kin])
        fw.op("dve", lambda e, kst=kst, Est=Est, ci=ci: e.tensor_tensor(out=kst[:], in0=kk[:, ci, :], in1=Est[:], op=ALU.mult), R=[kk, Est], W=[kst])
        yield
        ap_ = aps()
        for h in range(4):
            fw.op("pe", lambda e, ap_=ap_, kin=kin, qin=qin, h=h: e.matmul(ap_[:, h, :], lhsT=kin[:, h, :], rhs=qin[:, h, :], start=True, stop=True), R=[kin, qin], W=[ap_])
        att = attp()
        fw.op("dve", lambda e, att=att, ap_=ap_: e.tensor_tensor(out=att[:].rearrange("p h t -> p (h t)"), in0=ap_[:].rearrange("p h t -> p (h t)"), in1=maskf[:, im, :], op=ALU.mult), R=[ap_, maskf], W=[att])
        yield
        op_ = ops_()
        for h in range(4):
            fw.op("pe", lambda e, op_=op_, att=att, h=h, ci=ci: e.matmul(op_[:, h, :], lhsT=vv[:, ci, h * 128:(h + 1) * 128], rhs=att[:, h, :], start=True, stop=False), R=[vv, att], W=[op_])
            fw.op("pe", lambda e, op_=op_, qin=qin, h=h: e.matmul(op_[:, h, :], lhsT=Sb[:, h, :], rhs=qin[:, h, :], start=False, stop=True), R=[Sb, qin], W=[op_])
        osb = osbp()
        fw.op("act", lambda e, osb=osb, op_=op_: e.activation(out=osb[:], in_=op_[:], func=AF.Copy), R=[op_], W=[osb])
        fw.dma("sp", S["OG"][dr].rearrange("h v t -> v h t")[:, :, c0:c0 + 128], osb[:], R=[osb])
        kv = kvps()
        for h in range(4):
            fw.op("pe", lambda e, kv=kv, kst=kst, h=h, ci=ci: e.matmul(kv[:, h, :], lhsT=kst[:, h * 64:(h + 1) * 64], rhs=vv[:, ci, h * 128:(h + 1) * 128], start=True, stop=True), R=[kst, vv], W=[kv])
        for h in range(4):
            fw.op("dve", lambda e, kv=kv, Eq=Eq, h=h: e.scalar_tensor_tensor(out=Sf[:, h, :], in0=Sf[:, h, :], scalar=Eq[:, h, lastcol:lastcol + 1], in1=kv[:, h, :], op0=ALU.mult, op1=ALU.add), R=[Sf, Eq, kv], W=[Sf])
        fw.op("dve", lambda e: e.tensor_copy(out=Sb[:], in_=Sf[:]), R=[Sf], W=[Sb])


    pend = [list(orders[0]), list(orders[1])]
    active = []

    def start(dr_):
        if pend[dr_]:
            g = chunk(dr_, pend[dr_].pop(0))
            active.append([dr_, g])

    start(0); start(1)
    rounds = 0
    while active:
        rounds += 1
        if rounds == 3:
            start(0); start(1)
        for ent in list(active):
            try:
                next(ent[1])
            except StopIteration:
                active.remove(ent)
                start(ent[0])
    self.end("gla%d" % l)


def _rms_finish(self, o, n, gcol, extra_scale, pms, onesf, rstdp, tp):
    fw = self.fw
    sq = tp()
    fw.op("dve", lambda e: e.tensor_tensor(out=sq[:, 0:n], in0=o[:, 0:n], in1=o[:, 0:n], op=ALU.mult), R=[o], W=[sq])
    ms = pms()
    fw.op("pe", lambda e: e.matmul(ms[:, 0:n], lhsT=onesf[:], rhs=sq[:, 0:n], start=True, stop=True), R=[onesf, sq], W=[ms])
    rstd = rstdp()
    fw.op("dve", lambda e: e.tensor_scalar_add(out=rstd[:, 0:n], in0=ms[:, 0:n], scalar1=RMS_EPS), R=[ms], W=[rstd])
    fw.op("act", lambda e: e.activation(out=rstd[:, 0:n], in_=rstd[:, 0:n], func=AF.Ln), R=[rstd], W=[rstd])
    fw.op("act", lambda e: e.activation(out=rstd[:, 0:n], in_=rstd[:, 0:n], func=AF.Exp, scale=-0.5), R=[rstd], W=[rstd])
    t = tp()
    fw.op("dve", lambda e: e.tensor_tensor(out=t[:, 0:n], in0=o[:, 0:n], in1=rstd[:, 0:n], op=ALU.mult), R=[o, rstd], W=[t])
    if "DBGC" in self.debug and not getattr(self, "_dbgc_done", False):
        self._dbgc_done = True
        self.scratch("DBGC", [4, 128, 256], F32)
        msb = tp()
        fw.op("dve", lambda e: e.tensor_copy(out=msb[:, 0:n], in_=ms[:, 0:n]), R=[ms], W=[msb])
        for k_, b_ in enumerate((o, sq, msb, rstd)):
            fw.dma("pool", self.S["DBGC"][k_], b_[:, 0:256], R=[b_])
    return t


def _ph_glac(self, l):
    fw, I, S, C = self.fw, self.I, self.S, self.C
    self.begin()
    self.warm([AF.Ln, AF.Exp])
    onesf = self.sb("onesf", [128, 128], F32)
    fw.op("dve", lambda e: e.memset(onesf[:], 1.0 / 128.0), W=[onesf])
    gcol = self.sb("gcol", [128, 1], F32)
    idf, idb = self.load_ident()
    self.diag_cols(gcol, gcol[:], I["gla_norm_g"][l:l + 1, :], 1, idf)
    ofp = self.pool("of", [128, 512], F32, 2)
    obp = self.pool("ob", [128, 512], F32, 2)
    ggp = self.pool("gg", [128, 512], BF16, 2)
    op_ = self.pool("o", [128, 512], F32, 2)
    tp = self.pool("t", [128, 512], F32, 4)
    rstdp = self.pool("rstd", [128, 512], F32, 2)
    pms = self.pool("pms", [128, 512], F32, 2, psum=True)
    outp = self.pool("outb", [128, 512], BF16, 2)
    chunks = [(0, 256)] + [(256 + 512 * j, 512) for j in range(8)]
    for h in range(4):
        for (c0, n) in chunks:
            of = ofp(); ob = obp(); gg = ggp()
            fw.dma("sp", of[:, 0:n], S["OG"][0, h, :, c0:c0 + n], W=[of])
            fw.dma("sp", ob[:, 0:n], S["OG"][1, h, :, c0:c0 + n], W=[ob])
            fw.dma("sp", gg[:, 0:n], S["GG"][h * 128:(h + 1) * 128, c0:c0 + n], W=[gg])
            o = op_()
            fw.op("dve", lambda e, o=o, of=of, ob=ob, n=n: e.tensor_tensor(out=o[:, 0:n], in0=of[:, 0:n], in1=ob[:, 0:n], op=ALU.add), R=[of, ob], W=[o])
            t = _rms_finish(self, o, n, gcol, 1.0, pms, onesf, rstdp, tp)
            ob_ = outp()
            fw.op("dve", lambda e, ob_=ob_, t=t, gg=gg, n=n: e.scalar_tensor_tensor(out=ob_[:, 0:n], in0=t[:, 0:n], scalar=gcol[:, 0:1], in1=gg[:, 0:n], op0=ALU.mult, op1=ALU.mult), R=[t, gcol, gg], W=[ob_])
            fw.dma("pool", S["BR"][0, h * 128:(h + 1) * 128, c0:c0 + n], ob_[:, 0:n], R=[ob_])
    self.end("glac%d" % l)


KB.ph_gla = _ph_gla
KB.ph_glac = _ph_glac


def _ph_diff(self, l):
    fw, I, S, C = self.fw, self.I, self.S, self.C
    lam_init = 0.8 - 0.6 * math.exp(-0.3 * l)
    self.begin()
    self.warm([AF.Exp, AF.Ln])
    KT = self.sb("KT", [128, 4, T], BF16)
    VV = self.sb("VV", [128, NT, 512], BF16)
    fw.dma("sp", KT[:], S["DKT"].rearrange("h d t -> d h t"), W=[KT])
    fw.dma("sp", VV[:], S["DV"].rearrange("(i p) n -> p i n", p=128), W=[VV])
    onesb = self.sb("onesb", [128, 128], BF16)
    fw.op("dve", lambda e: e.memset(onesb[:], 1.0), W=[onesb])
    onesf = self.sb("onesf", [128, 128], F32)
    fw.op("dve", lambda e: e.memset(onesf[:], 1.0 / 128.0), W=[onesf])
    gcol = self.sb("gcol", [128, 1], F32)
    idf, idb = self.load_ident()
    self.diag_cols(gcol, gcol[:], I["diff_norm_g"][l:l + 1, :], 1, idf)
    fw.op("dve", lambda e: e.tensor_scalar_mul(out=gcol[:], in0=gcol[:], scalar1=(1.0 - lam_init)), R=[gcol], W=[gcol])
    lq = self.sb("lq", [128, 128], F32)
    lk = self.sb("lk", [128, 128], F32)
    fw.dma("sp", lq[:], I["diff_lam_q"][l:l + 1].rearrange("o a b -> o (a b)").partition_broadcast(128), W=[lq])
    fw.dma("sp", lk[:], I["diff_lam_k"][l:l + 1].rearrange("o a b -> o (a b)").partition_broadcast(128), W=[lk])
    fw.op("dve", lambda e: e.tensor_tensor(out=lq[:], in0=lq[:], in1=lk[:], op=ALU.mult), R=[lq, lk], W=[lq])
    ls = self.sb("ls", [128, 2], F32)
    fw.op("dve", lambda e: e.reduce_sum(out=ls[:], in_=lq[:].rearrange("p (a b) -> p a b", a=2), axis=AX.X), R=[lq], W=[ls])
    fw.op("act", lambda e: e.activation(out=ls[:], in_=ls[:], func=AF.Exp), R=[ls], W=[ls])
    nlam = self.sb("nlam", [128, 1], F32)
    fw.op("dve", lambda e: e.tensor_tensor(out=nlam[:], in0=ls[:, 1:2], in1=ls[:, 0:1], op=ALU.subtract), R=[ls], W=[nlam])
    fw.op("dve", lambda e: e.tensor_scalar_add(out=nlam[:], in0=nlam[:], scalar1=-lam_init), R=[nlam], W=[nlam])

    qp = self.pool("q", [128, 512], BF16, 3)
    sps = self.pool("sps", [128, 512], F32, 5, psum=True)
    ops_ = self.pool("ops", [128, 512], F32, 2, psum=True)
    dps = self.pool("dps", [128, 512], F32, 1, psum=True)
    pms = dps
    ptp = self.pool("pt", [128, 512], BF16, 10)
    dacp = self.pool("dacc", [128, 512], F32, 4)
    ones1 = self.sb("ones1", [128, 128], F32)
    fw.op("dve", lambda e: e.memset(ones1[:], 1.0), W=[ones1])
    w0p = self.pool("w0", [128, 512], F32, 4)
    rcp = self.pool("rc", [128, 512], F32, 2)
    op_ = self.pool("o", [128, 512], F32, 2)
    tp = self.pool("t", [128, 512], F32, 4)
    rstdp = self.pool("rstd", [128, 512], F32, 2)
    outp = self.pool("outb", [128, 512], BF16, 2)
    chunks = [(0, 256, 2)] + [(256 + 512 * j, 512, NT) for j in range(8)]
    items = []
    for h in range(4):
        for (c0, n, nkt) in chunks:
            for kt in range(nkt):
                items.append((h, c0, n, nkt, kt))
    LA = 2
    st = {}
    cur = {}
    deferred = []

    def front(i):
        h, c0, n, nkt, kt = items[i]
        key = (h, c0)
        if key not in cur:
            q = qp()
            fw.dma("sp", q[:, 0:n], S["DQT"][h, :, c0:c0 + n], W=[q])
            cur[key] = {"q": q, "ws": []}
        q = cur[key]["q"]
        pts = []
        sp2 = [sps(), sps()]
        for comp in (1, 0):
            pb = comp * 64
            sp_ = sp2[comp]
            fw.op("pe", lambda e: e.matmul(sp_[:, 0:n], lhsT=KT[pb:pb + 64, h, kt * 128:(kt + 1) * 128], rhs=q[pb:pb + 64, 0:n], start=True, stop=True), R=[KT, q], W=[sp_])
        for comp in range(2):
            sp_ = sp2[comp]
            pt = ptp()
            fw.op("act", lambda e: e.activation(out=pt[:, 0:n], in_=sp_[:, 0:n], func=AF.Exp, scale=0.125), R=[sp_], W=[pt])
            pts.append(pt)
        st[i] = pts

    def back(i):
        h, c0, n, nkt, kt = items[i]
        key = (h, c0)
        c = cur[key]
        if kt == 0:
            c["o_ps"] = [ops_(), ops_()]
            c["dacc"] = [dacp(), dacp()]
        pts = st.pop(i)
        for comp in (1, 0):
            o_ps, dacc, pt = c["o_ps"][comp], c["dacc"][comp], pts[comp]
            fw.op("pe", lambda e: e.matmul(o_ps[:, 0:n], lhsT=VV[:, kt, h * 128:(h + 1) * 128], rhs=pt[:, 0:n], start=(kt == 0), stop=(kt == nkt - 1)), R=[VV, pt], W=[o_ps])
            if kt == 0:
                fw.op("dve", lambda e: e.tensor_copy(out=dacc[:, 0:n], in_=pt[:, 0:n]), R=[pt], W=[dacc])
            else:
                fw.op("dve", lambda e: e.tensor_tensor(out=dacc[:, 0:n], in0=dacc[:, 0:n], in1=pt[:, 0:n], op=ALU.add), R=[dacc, pt], W=[dacc])
        if kt == nkt - 1:
            for comp in range(2):
                o_ps, dacc = c["o_ps"][comp], c["dacc"][comp]
                d_ps = dps()
                fw.op("pe", lambda e: e.matmul(d_ps[:, 0:n], lhsT=ones1[:], rhs=dacc[:, 0:n], start=True, stop=True), R=[ones1, dacc], W=[d_ps])
                rc = rcp()
                fw.op("dve", lambda e: e.reciprocal(out=rc[:, 0:n], in_=d_ps[:, 0:n]), R=[d_ps], W=[rc])
                w = w0p()
                fw.op("dve", lambda e: e.tensor_tensor(out=w[:, 0:n], in0=o_ps[:, 0:n], in1=rc[:, 0:n], op=ALU.mult), R=[o_ps, rc], W=[w])
                c["ws"].append(w)
            if True:
                comp = 1
                ws = c["ws"]
                o = op_()
                fw.op("dve", lambda e: e.scalar_tensor_tensor(out=o[:, 0:n], in0=ws[1][:, 0:n], scalar=nlam[:, 0:1], in1=ws[0][:, 0:n], op0=ALU.mult, op1=ALU.add), R=[ws[0], ws[1], nlam], W=[o])
                sq = tp()
                fw.op("dve", lambda e: e.tensor_tensor(out=sq[:, 0:n], in0=o[:, 0:n], in1=o[:, 0:n], op=ALU.mult), R=[o], W=[sq])

                def tail(o=o, sq=sq, n=n, h=h, c0=c0):
                    ms = pms()
                    fw.op("pe", lambda e: e.matmul(ms[:, 0:n], lhsT=onesf[:], rhs=sq[:, 0:n], start=True, stop=True), R=[onesf, sq], W=[ms])
                    rstd = rstdp()
                    fw.op("dve", lambda e: e.tensor_scalar_add(out=rstd[:, 0:n], in0=ms[:, 0:n], scalar1=RMS_EPS), R=[ms], W=[rstd])
                    fw.op("act", lambda e: e.activation(out=rstd[:, 0:n], in_=rstd[:, 0:n], func=AF.Ln), R=[rstd], W=[rstd])
                    fw.op("act", lambda e: e.activation(out=rstd[:, 0:n], in_=rstd[:, 0:n], func=AF.Exp, scale=-0.5), R=[rstd], W=[rstd])
                    t = tp()
                    fw.op("dve", lambda e: e.tensor_tensor(out=t[:, 0:n], in0=o[:, 0:n], in1=rstd[:, 0:n], op=ALU.mult), R=[o, rstd], W=[t])
                    ob_ = outp()
                    fw.op("act", lambda e: e.activation(out=ob_[:, 0:n], in_=t[:, 0:n], func=AF.Identity, scale=gcol[:, 0:1]), R=[t, gcol], W=[ob_])
                    fw.dma("pool", S["BR"][1, h * 128:(h + 1) * 128, c0:c0 + n], ob_[:, 0:n], R=[ob_])
                deferred.append((i + 4, tail))
                del cur[key]

    nI = len(items)
    for i in range(nI + LA):
        if i < nI:
            front(i)
        if i - LA >= 0:
            back(i - LA)
        while deferred and deferred[0][0] <= i - LA:
            deferred.pop(0)[1]()
    while deferred:
        deferred.pop(0)[1]()
    self.end("diff%d" % l)


KB.ph_diff = _ph_diff


def _ph_na(self, l):
    fw, I, S, C = self.fw, self.I, self.S, self.C
    nc = self.nc
    self.begin()
    self.warm([AF.Exp])
    KT = self.sb("KT", [128, 4, T], BF16)
    VV = self.sb("VV", [128, NT, 512], BF16)
    fw.dma("sp", KT[:], S["NKT"].rearrange("g d t -> d g t"), W=[KT])
    fw.dma("sp", VV[:], S["NV"].rearrange("(i p) n -> p i n", p=128), W=[VV])
    onesb = self.sb("onesb", [128, 64], BF16)
    fw.op("dve", lambda e: e.memset(onesb[:], 1.0), W=[onesb])
    TAB = self.sb("TAB", [128, 16, 512], BF16)
    fw.op("dve", lambda e: e.memset(TAB[:], 0.0), W=[TAB])
    j64 = self.sb("j64", [64, 128], F32)
    fw.dma("sp", j64[:], C["j64"][:, :], W=[j64])
    win = self.sb("win", [128, 512], F32)
    fw.dma("sp", win[:], C["nawin"][:, :], W=[win])
    idf, idb = self.load_ident()
    r120 = self.sb("r120", [120, 31], F32)
    fw.dma("sp", r120[:], I["na_rpb"][l].rearrange("h a j -> (h a) j"), W=[r120])
    r31 = self.sb("r31", [31, 120], F32)
    tps = self.pool("tps", [128, 512], F32, 1, psum=True)
    pt0 = tps()
    fw.op("pe", lambda e: e.transpose(out=pt0[0:31, 0:120], in_=r120[:, :], identity=idf[0:120, 0:120]), R=[r120, idf], W=[pt0])
    fw.op("act", lambda e: e.activation(out=r31[:], in_=pt0[0:31, 0:120], func=AF.Copy), R=[pt0], W=[r31])
    p0 = tps()
    fw.op("pe", lambda e: e.matmul(p0[0:120, 0:31], lhsT=r31[:, :], rhs=j64[0:31, 33:64], start=True, stop=True), R=[r31, j64], W=[p0])
    rev = self.sb("rev", [120, 31], F32)
    fw.op("act", lambda e: e.activation(out=rev[:], in_=p0[0:120, 0:31], func=AF.Copy), R=[p0], W=[rev])
    zt = self.sb("zt", [120, 128], F32)
    fw.op("dve", lambda e: e.memset(zt[:], 0.0), W=[zt])
    nag = Buf("nag")
    NAG = S["NAG"]
    fw.dma("sp", NAG[:, :], zt[:], R=[zt], W=[nag])
    fw.dma("sp", NAG[:, 48:79], rev[:], R=[rev], W=[nag])
    Hk = self.sb("Hk", [64, 120, 64], F32)
    for n0 in range(0, 120, 8):
        hsrc = bass.AP(tensor=NAG.tensor, offset=NAG.offset + n0 * 128, ap=[[1, 64], [128, 8], [1, 64]])
        fw.dma("sp", Hk[:, n0:n0 + 8, :], hsrc, R=[nag], W=[Hk])
    Hk4 = Hk[:].rearrange("m (h a) c -> m h a c", h=8)
    ebp = self.pool("eb", [128, 512], F32, 2)
    for dr in range(15):
        p = tps()
        fw.op("pe", lambda e: e.matmul(p[:].rearrange("p (h c) -> p h c", h=8), lhsT=j64[:, :], rhs=Hk4[:, :, dr, :], start=True, stop=True), R=[j64, Hk], W=[p])
        eb = ebp()
        fw.op("act", lambda e: e.activation(out=eb[:], in_=p[:], func=AF.Exp), R=[p], W=[eb])
        if dr <= 13:
            fw.op("dve", lambda e: e.tensor_tensor(out=TAB[0:64, dr, :], in0=eb[0:64, :], in1=win[0:64, :], op=ALU.mult), R=[eb, win], W=[TAB])
        if dr == 10:
            fw.op("dve", lambda e: e.tensor_tensor(out=TAB[0:64, 15, :], in0=eb[0:64, :], in1=win[0:64, :], op=ALU.mult), R=[eb, win], W=[TAB])
        if dr >= 1:
            fw.op("dve", lambda e: e.tensor_tensor(out=TAB[64:128, dr - 1, :], in0=eb[64:128, :], in1=win[64:128, :], op=ALU.mult), R=[eb, win], W=[TAB])
        if dr == 3:
            fw.op("dve", lambda e: e.tensor_tensor(out=TAB[64:128, 14, :], in0=eb[64:128, :], in1=win[64:128, :], op=ALU.mult), R=[eb, win], W=[TAB])

    if self.stop_after == "natab":
        self.scratch("TAB", [128, 16, 512], BF16)
        fw.dma("pool", S["TAB"], TAB[:], R=[TAB])
        self.end("natab")
        return
    q8p = self.pool("q8", [128, 4, 512], BF16, 2)
    stgp = self.pool("stg", [64, 8, 512], BF16, 2)
    sps = self.pool("sps", [128, 512], F32, 4, psum=True)
    ops_ = self.pool("ops", [64, 512], F32, 2, psum=True)
    dps = self.pool("dps", [64, 512], F32, 1, psum=True)
    ptp = self.pool("pt", [128, 512], BF16, 16)
    rcp = self.pool("rc", [64, 512], F32, 2)
    q8p = self.pool("q8b", [128, 4, 512], BF16, 3)
    stgp = self.pool("stgb", [64, 8, 512], BF16, 3)
    BRv = S["BR"][2].rearrange("(h d) t -> d h t", h=8)
    NQv = S["NQT"].rearrange("g d t -> d g t")
    rows_ = []
    for grp in range(T // 512 + 1):
        if grp == 0:
            tok0, nrows = 0, 4
        else:
            tok0, nrows = 256 + (grp - 1) * 512, 8
        for rr in range(nrows):
            rows_.append((grp, rr, tok0, nrows))
    gstate = {}
    rstate = {}

    def nfront(i):
        grp, rr, tok0, nrows = rows_[i]
        ntok = nrows * 64
        if grp not in gstate:
            q8 = q8p()
            stg = stgp()
            fw.dma("sp", q8[:, :, 0:ntok], NQv[:, :, tok0:tok0 + ntok], W=[q8])
            gstate[grp] = (q8, stg)
        q8, stg = gstate[grp]
        qoff = rr * 64
        tiles = [(0, None), (1, None)]
        if grp > 0:
            r = (grp - 1) * 8 + rr
            rs = min(max(r - 4, 0), 56)
            if rs % 2 == 0:
                for j in range(4):
                    tiles.append((2 + rs // 2 + j, rs + 2 * j - r + 7))
            else:
                tiles.append((2 + (rs - 1) // 2, 14))
                for j in range(3):
                    tiles.append((2 + (rs + 1) // 2 + j, rs + 1 + 2 * j - r + 7))
                tiles.append((2 + (rs + 7) // 2, 15))
        P = []
        for (kt, slot) in tiles:
            spe = sps()
            spo = sps()
            for h in range(8):
                pb = (h % 2) * 64
                dstp = spe if h % 2 == 0 else spo
                fw.op("pe", lambda e: e.matmul(dstp[:, (h // 2) * 64:(h // 2 + 1) * 64], lhsT=KT[pb:pb + 64, h // 2, kt * 128:(kt + 1) * 128], rhs=q8[pb:pb + 64, h // 2, qoff:qoff + 64], start=True, stop=True), R=[KT, q8], W=[dstp])
            pt = ptp()
            ptv = pt[:].rearrange("p (g two q) -> p g two q", two=2, q=64)
            fw.op("act", lambda e: e.activation(out=ptv[:, :, 0, :], in_=spe[:, 0:256].rearrange("p (g q) -> p g q", q=64), func=AF.Exp, scale=0.125), R=[spe], W=[pt])
            fw.op("act", lambda e: e.activation(out=ptv[:, :, 1, :], in_=spo[:, 0:256].rearrange("p (g q) -> p g q", q=64), func=AF.Exp, scale=0.125), R=[spo], W=[pt])
            if slot is not None:
                assert 0 <= slot <= 15
                fw.op("dve", lambda e: e.tensor_tensor(out=pt[:], in0=pt[:], in1=TAB[:, slot, :], op=ALU.mult), R=[pt, TAB], W=[pt])
            P.append((kt, pt))
        rstate[i] = P

    def nback(i):
        grp, rr, tok0, nrows = rows_[i]
        ntok = nrows * 64
        q8, stg = gstate[grp]
        qoff = rr * 64
        P = rstate.pop(i)
        o_ps = ops_()
        d_ps = dps()
        nk = len(P)
        for h in range(8):
            for ii, (kt, pt) in enumerate(P):
                fw.op("pe", lambda e: e.matmul(o_ps[:, h * 64:(h + 1) * 64], lhsT=VV[:, kt, h * 64:(h + 1) * 64], rhs=pt[:, h * 64:(h + 1) * 64], start=(ii == 0), stop=(ii == nk - 1)), R=[VV, pt], W=[o_ps])
        for ii, (kt, pt) in enumerate(P):
            fw.op("pe", lambda e: e.matmul(d_ps[:], lhsT=onesb[:], rhs=pt[:], start=(ii == 0), stop=(ii == nk - 1)), R=[onesb, pt], W=[d_ps])
        rc = rcp()
        fw.op("dve", lambda e: e.reciprocal(out=rc[:], in_=d_ps[:]), R=[d_ps], W=[rc])
        fw.op("dve", lambda e: e.tensor_tensor(out=stg[:, :, qoff:qoff + 64], in0=o_ps[:].rearrange("p (h q) -> p h q", h=8), in1=rc[:].rearrange("p (h q) -> p h q", h=8), op=ALU.mult), R=[o_ps, rc], W=[stg])
        if rr == nrows - 1:
            fw.dma("pool", BRv[:, :, tok0:tok0 + ntok], stg[:, :, 0:ntok], R=[stg])

    nR = len(rows_)
    for i in range(nR + 1):
        if i < nR:
            nfront(i)
        if i >= 1:
            nback(i - 1)
    self.end("na%d" % l)


KB.ph_na = _ph_na


def _ln_epilogue(self, ysrc, xt, mbc, gbc, bbc, pools, dst_ap):
    fw = self.fw
    up, stp, mvp, xnp = pools
    u = up()
    for hf in range(2):
        yb, yap = ysrc[hf]
        fw.op("dve", lambda e: e.tensor_tensor(out=u[:, hf * 512:(hf + 1) * 512], in0=yap, in1=mbc[:, hf * 512:(hf + 1) * 512], op=ALU.mult), R=[yb, mbc], W=[u])
    fw.op("dve", lambda e: e.scalar_tensor_tensor(out=u[:], in0=xt[:], scalar=ALPHA, in1=u[:], op0=ALU.mult, op1=ALU.add), R=[xt, u], W=[u])
    st = stp()
    for hf in range(2):
        fw.op("dve", lambda e: e.bn_stats(out=st[:, hf, :], in_=u[:, hf * 512:(hf + 1) * 512]), R=[u], W=[st])
    mv = mvp()
    fw.op("dve", lambda e: e.bn_aggr(out=mv[:, 0:2], in_=st[:].rearrange("p a b -> p (a b)")), R=[st], W=[mv])
    fw.op("dve", lambda e: e.tensor_scalar_add(out=mv[:, 2:3], in0=mv[:, 1:2], scalar1=LN_EPS), R=[mv], W=[mv])
    fw.op("act", lambda e: e.activation(out=mv[:, 2:3], in_=mv[:, 2:3], func=AF.Ln), R=[mv], W=[mv])
    fw.op("act", lambda e: e.activation(out=mv[:, 2:3], in_=mv[:, 2:3], func=AF.Exp, scale=-0.5), R=[mv], W=[mv])
    fw.op("dve", lambda e: e.scalar_tensor_tensor(out=mv[:, 3:4], in0=mv[:, 0:1], scalar=-1.0, in1=mv[:, 2:3], op0=ALU.mult, op1=ALU.mult), R=[mv], W=[mv])
    xn = xnp()
    fw.op("act", lambda e: e.activation(out=xn[:], in_=u[:], func=AF.Identity, bias=mv[:, 3:4], scale=mv[:, 2:3]), R=[u, mv], W=[xn])
    fw.op("dve", lambda e: e.tensor_tensor(out=xn[:], in0=xn[:], in1=gbc[:], op=ALU.mult), R=[xn, gbc], W=[xn])
    fw.op("dve", lambda e: e.tensor_tensor(out=xn[:], in0=xn[:], in1=bbc[:], op=ALU.add), R=[xn, bbc], W=[xn])
    fw.dma("pool", dst_ap, xn[:], R=[xn])


def _bc_tile(self, name, row_ap):
    t = self.sb(name, [128, D], F32)
    self.fw.dma("sp", t[:], row_ap.partition_broadcast(128), W=[t])
    return t


def _ph_merge(self, l):
    fw, I, S, C = self.fw, self.I, self.S, self.C
    self.begin()
    self.warm([AF.Ln, AF.Exp])
    wfp = self.pool("wf", [128, 1024], F32, 2)
    wbr = self.sb("wbr", [128, 3, 4, 1024], BF16)
    wo = self.sb("wo", [128, 8, 1024], BF16)
    k = 0
    for n_ in range(3):
        for ec in range(4):
            wf = wfp()
            fw.dma("sp", wf[:], I["w_branch"][l, n_, ec * 128:(ec + 1) * 128, :], W=[wf])
            if k % 2 == 0:
                fw.op("dve", lambda e: e.tensor_copy(out=wbr[:, n_, ec, :], in_=wf[:]), R=[wf], W=[wbr])
            else:
                fw.op("act", lambda e: e.activation(out=wbr[:, n_, ec, :], in_=wf[:], func=AF.Copy), R=[wf], W=[wbr])
            k += 1
    for dc in range(8):
        wf = wfp()
        fw.dma("sp", wf[:], I["w_o"][l, dc * 128:(dc + 1) * 128, :], W=[wf])
        if dc % 2 == 0:
            fw.op("dve", lambda e: e.tensor_copy(out=wo[:, dc, :], in_=wf[:]), R=[wf], W=[wo])
        else:
            fw.op("act", lambda e: e.activation(out=wo[:, dc, :], in_=wf[:], func=AF.Copy), R=[wf], W=[wo])
    m2 = [_bc_tile(self, "m2L", S["MODR"][l, 0, 0:1, :]), _bc_tile(self, "m2C", S["MODR"][l, 1, 0:1, :])]
    gbc = _bc_tile(self, "gbc", I["ln_g"][l, 0:1, :])
    bbc = _bc_tile(self, "bbc", I["ln_b"][l, 0:1, :])
    brp = self.pool("br", [128, 3, 4, 512], BF16, 2)
    gtp = self.pool("gt", [128, 3, 512], BF16, 2)
    pj = self.pool("pj", [128, 512], F32, 3, psum=True)
    yps = self.pool("yps", [128, 512], F32, 4, psum=True)
    accp = self.pool("acc", [128, 512], F32, 2)
    tmpp = self.pool("tmp", [128, 512], F32, 2)
    mTp = self.pool("mT", [128, 8, 512], BF16, 2)
    xtp = self.pool("xt", [128, D], F32, 2)
    pools = (self.pool("u", [128, D], F32, 2), self.pool("st", [128, 2, 6], F32, 2), self.pool("mv", [128, 4], F32, 2), self.pool("xn", [128, D], F32, 2))
    chunks = [(0, 256)] + [(256 + 512 * j, 512) for j in range(8)]
    BRv = S["BR"].rearrange("n (ec p) t -> p n ec t", p=128)
    GTv = S["GT"].rearrange("n (dc p) t -> p n dc t", p=128)
    for (c0, n) in chunks:
        br = brp()
        for n_ in range(3):
            fw.dma("sp", br[:, n_, :, 0:n], BRv[:, n_, :, c0:c0 + n], W=[br])
        mT = mTp()
        for dc in range(8):
            gt = gtp()
            fw.dma("sp", gt[:, :, 0:n], GTv[:, :, dc, c0:c0 + n], W=[gt])
            acc = accp()
            for n_ in range(3):
                p = pj()
                for ec in range(4):
                    fw.op("pe", lambda e: e.matmul(p[:, 0:n], lhsT=wbr[:, n_, ec, dc * 128:(dc + 1) * 128], rhs=br[:, n_, ec, 0:n], start=(ec == 0), stop=(ec == 3)), R=[wbr, br], W=[p])
                if n_ == 0:
                    fw.op("dve", lambda e: e.tensor_tensor(out=acc[:, 0:n], in0=p[:, 0:n], in1=gt[:, 0, 0:n], op=ALU.mult), R=[p, gt], W=[acc])
                else:
                    tmp = tmpp()
                    fw.op("dve", lambda e: e.tensor_tensor(out=tmp[:, 0:n], in0=p[:, 0:n], in1=gt[:, n_, 0:n], op=ALU.mult), R=[p, gt], W=[tmp])
                    if n_ == 1:
                        fw.op("dve", lambda e: e.tensor_tensor(out=acc[:, 0:n], in0=acc[:, 0:n], in1=tmp[:, 0:n], op=ALU.add), R=[acc, tmp], W=[acc])
                    else:
                        fw.op("dve", lambda e: e.tensor_tensor(out=mT[:, dc, 0:n], in0=acc[:, 0:n], in1=tmp[:, 0:n], op=ALU.add), R=[acc, tmp], W=[mT])
        for tl in range(n // 128):
            ti = c0 // 128 + tl
            ys = []
            for hf in range(2):
                y = yps()
                for dc in range(8):
                    fw.op("pe", lambda e: e.matmul(y[:], lhsT=mT[:, dc, tl * 128:(tl + 1) * 128], rhs=wo[:, dc, hf * 512:(hf + 1) * 512], start=(dc == 0), stop=(dc == 7)), R=[mT, wo], W=[y])
                ys.append((y, y[:]))
            xt = xtp()
            fw.dma("sp", xt[:], self.x_src(l, ti), W=[xt])
            _ln_epilogue(self, ys, xt, m2[1 if ti < 2 else 0], gbc, bbc, pools, S["XS"][ti * 128:(ti + 1) * 128, :])
    self.end("merge%d" % l)


KB.ph_merge = _ph_merge


def _ph_moe(self, l, last):
    fw, I, S, C = self.fw, self.I, self.S, self.C
    self.begin()
    self.warm([AF.Sigmoid, AF.Silu])
    idf, idb = self.load_ident()
    modf = self.sb("modf", [128, 48, 2], F32)
    fw.dma("sp", modf[:], S["MODF"][l], W=[modf])
    m5 = [_bc_tile(self, "m5L", S["MODR"][l, 0, 1:2, :]), _bc_tile(self, "m5C", S["MODR"][l, 1, 1:2, :])]
    gbc = _bc_tile(self, "gbc", I["ln_g"][l, 1:2, :])
    bbc = _bc_tile(self, "bbc", I["ln_b"][l, 1:2, :])
    wr = self.sb("wr", [128, 8, 16], F32)
    fw.dma("sp", wr[:], I["w_router"].rearrange("(kc p) e -> p kc e", p=128), W=[wr])
    brt = self.sb("brt", [128, 16], F32)
    fw.dma("sp", brt[:], I["b_router"].rearrange("(o e) -> o e", o=1).partition_broadcast(128), W=[brt])
    self_f = self.sb("self", [16, 16, 128], F32)
    selb = self.sb("selb", [16, 16, 128], BF16)
    fw.dma("sp", self_f[:], C["sel"], W=[self_f])
    fw.op("dve", lambda e: e.tensor_copy(out=selb[:], in_=self_f[:]), R=[self_f], W=[selb])

    NG = 12
    hT = self.sb("hT", [128, 8, NG * 128], BF16)
    Y = self.sb("Y", [128, NG, D], F32)
    aff = self.sb("aff", [128, NG, 16], F32)
    selv = self.sb("selv", [128, NG, 16], F32)
    comb = self.sb("comb", [128, NG, 16], F32)
    cT = self.sb("cT", [16, NG * 128], BF16)
    r1 = self.sb("r1", [128, NG * 4], F32)
    r2 = self.sb("r2", [128, NG * 4], F32)
    gs = self.sb("gs", [128, NG * 4], F32)
    gmx = self.sb("gmx", [128, NG], F32)
    gmask = self.sb("gmask", [128, NG * 4], F32)
    is1 = self.sb("is1", [128, NG * 4, 4], F32)
    v2 = self.sb("v2", [128, NG * 4, 4], F32)
    oh = self.sb("oh", [128, NG * 4, 4], F32)
    ssum = self.sb("ssum", [128, NG], F32)

    xtp = self.pool("xt", [128, D], F32, 2)
    h32p = self.pool("h32", [128, 8, 128], F32, 2)
    sml = self.pool("sml", [128, 512], F32, 1, psum=True)
    gen = self.pool("gen", [128, 512], F32, 7, psum=True)
    ptr = gen
    gup = gen
    yps = gen
    wgp = self.pool("wg", [128, 8, 512], BF16, 2)
    wup = self.pool("wu", [128, 8, 512], BF16, 2)
    wdp = self.pool("wd", [128, 4, 1024], BF16, 2)
    cbp = self.pool("cb", [128, 512], F32, 2)
    sgp = self.pool("sg", [128, 512], F32, 2)
    a1p = self.pool("a1", [128, 512], F32, 2)
    aTp = self.pool("aT", [128, 4, 512], BF16, 2)
    pools = (self.pool("u", [128, D], F32, 1), self.pool("st", [128, 2, 6], F32, 2), self.pool("mv", [128, 4], F32, 2), self.pool("xn", [128, D], F32, 2))
    castk = [0]

    def cast(dst_ap, dstb, src_ap, srcb):
        if castk[0] % 2 == 0:
            fw.op("dve", lambda e: e.tensor_copy(out=dst_ap, in_=src_ap), R=[srcb], W=[dstb])
        else:
            fw.op("act", lambda e: e.activation(out=dst_ap, in_=src_ap, func=AF.Copy), R=[srcb], W=[dstb])
        castk[0] += 1

    for (g0, g1) in ((0, 12), (12, 24), (24, 34)):
        ng = g1 - g0
        for i in range(g0, g1):
            j = i - g0
            st_ = 1 if i < 2 else 0
            xt = xtp()
            fw.dma("sp", xt[:], S["XS"][i * 128:(i + 1) * 128, :], W=[xt])
            h32 = h32p()
            for hf in range(2):
                p = ptr()
                for k4 in range(4):
                    kc = hf * 4 + k4
                    fw.op("pe", lambda e: e.transpose(out=p[:, k4 * 128:(k4 + 1) * 128], in_=xt[:, kc * 128:(kc + 1) * 128], identity=idf[:]), R=[xt, idf], W=[p])
                for k4 in range(4):
                    kc = hf * 4 + k4
                    fw.op("act", lambda e: e.activation(out=h32[:, kc, :], in_=p[:, k4 * 128:(k4 + 1) * 128], func=AF.Identity, bias=modf[:, 24 + kc, st_:st_ + 1], scale=modf[:, 32 + kc, st_:st_ + 1]), R=[p, modf], W=[h32])
            fw.op("dve", lambda e: e.tensor_copy(out=hT[:, :, j * 128:(j + 1) * 128], in_=h32[:]), R=[h32], W=[hT])
            lg = sml()
            for kc in range(8):
                fw.op("pe", lambda e: e.matmul(lg[:, 0:16], lhsT=h32[:, kc, :], rhs=wr[:, kc, :], start=(kc == 0), stop=(kc == 7)), R=[h32, wr], W=[lg])
            fw.op("act", lambda e: e.activation(out=aff[:, j, :], in_=lg[:, 0:16], func=AF.Sigmoid), R=[lg], W=[aff])
            fw.op("dve", lambda e: e.tensor_tensor(out=selv[:, j, :], in0=aff[:, j, :], in1=brt[:], op=ALU.add), R=[aff, brt], W=[selv])
        n4 = ng * 4
        s4 = selv[:, 0:ng, :].rearrange("p g (q e) -> p (g q) e", e=4)
        first = True
        for (a, b) in ((0, 1), (0, 2), (0, 3), (1, 2), (1, 3), (2, 3)):
            if first:
                fw.op("dve", lambda e: e.tensor_tensor(out=gs[:, 0:n4], in0=s4[:, :, a], in1=s4[:, :, b], op=ALU.add), R=[selv], W=[gs])
                first = False
            else:
                fw.op("dve", lambda e: e.tensor_tensor(out=r1[:, 0:n4], in0=s4[:, :, a], in1=s4[:, :, b], op=ALU.add), R=[selv], W=[r1])
                fw.op("dve", lambda e: e.tensor_tensor(out=gs[:, 0:n4], in0=gs[:, 0:n4], in1=r1[:, 0:n4], op=ALU.max), R=[gs, r1], W=[gs])
        fw.op("dve", lambda e: e.reduce_max(out=gmx[:, 0:ng], in_=gs[:, 0:n4].rearrange("p (g q) -> p g q", q=4), axis=AX.X), R=[gs], W=[gmx])
        for q in range(4):
            fw.op("dve", lambda e: e.tensor_tensor(out=gmask[:, 0:n4].rearrange("p (g q) -> p g q", q=4)[:, :, q], in0=gs[:, 0:n4].rearrange("p (g q) -> p g q", q=4)[:, :, q], in1=gmx[:, 0:ng], op=ALU.is_ge), R=[gs, gmx], W=[gmask])
        fw.op("dve", lambda e: e.reduce_max(out=r1[:, 0:n4], in_=s4, axis=AX.X), R=[selv], W=[r1])
        for ee in range(4):
            fw.op("dve", lambda e: e.tensor_tensor(out=is1[:, 0:n4, ee], in0=s4[:, :, ee], in1=r1[:, 0:n4], op=ALU.is_ge), R=[selv, r1], W=[is1])
        fw.op("dve", lambda e: e.scalar_tensor_tensor(out=v2[:, 0:n4, :], in0=is1[:, 0:n4, :], scalar=-1.0e9, in1=s4, op0=ALU.mult, op1=ALU.add), R=[is1, selv], W=[v2])
        fw.op("dve", lambda e: e.reduce_max(out=r2[:, 0:n4], in_=v2[:, 0:n4, :], axis=AX.X), R=[v2], W=[r2])
        for ee in range(4):
            fw.op("dve", lambda e: e.tensor_tensor(out=oh[:, 0:n4, ee], in0=v2[:, 0:n4, ee], in1=r2[:, 0:n4], op=ALU.is_ge), R=[v2, r2], W=[oh])
        fw.op("dve", lambda e: e.tensor_tensor(out=oh[:, 0:n4, :], in0=oh[:, 0:n4, :], in1=is1[:, 0:n4, :], op=ALU.add), R=[oh, is1], W=[oh])
        for ee in range(4):
            fw.op("dve", lambda e: e.tensor_tensor(out=oh[:, 0:n4, ee], in0=oh[:, 0:n4, ee], in1=gmask[:, 0:n4], op=ALU.mult), R=[oh, gmask], W=[oh])
        ohv = oh[:, 0:n4, :].rearrange("p (g q) e -> p g (q e)", q=4)
        fw.op("dve", lambda e: e.tensor_tensor(out=comb[:, 0:ng, :], in0=aff[:, 0:ng, :], in1=ohv, op=ALU.mult), R=[aff, oh], W=[comb])
        fw.op("dve", lambda e: e.reduce_sum(out=ssum[:, 0:ng], in_=comb[:, 0:ng, :], axis=AX.X), R=[comb], W=[ssum])
        fw.op("dve", lambda e: e.reciprocal(out=ssum[:, 0:ng], in_=ssum[:, 0:ng]), R=[ssum], W=[ssum])
        for j in range(ng):
            fw.op("dve", lambda e: e.tensor_scalar_mul(out=comb[:, j, :], in0=comb[:, j, :], scalar1=ssum[:, j:j + 1]), R=[comb, ssum], W=[comb])
            pc = sml()
            fw.op("pe", lambda e: e.transpose(out=pc[0:16, 0:128], in_=comb[:, j, :], identity=idf[:]), R=[comb, idf], W=[pc])
            fw.op("act", lambda e: e.activation(out=cT[:, j * 128:(j + 1) * 128], in_=pc[0:16, 0:128], func=AF.Copy), R=[pc], W=[cT])
        gchunks = []
        c = 0
        while c < ng * 128:
            n = min(512, ng * 128 - c)
            gchunks.append((c, n))
            c += n
        mitems = [(ex, c0, n) for ex in range(16) for (c0, n) in gchunks]
        wts = {}
        mst = {}

        def load_w(ex):
            wg = wgp(); wu = wup(); wd = wdp()
            for (wdst, src) in ((wg, I["w_exp_gate"][l, ex]), (wu, I["w_exp_up"][l, ex])):
                for hf in range(2):
                    fw.dma("pool", wdst[:, hf * 4:(hf + 1) * 4, :], src[hf * 512:(hf + 1) * 512, :].rearrange("(k p) f -> p k f", p=128), W=[wdst])
            for hf in range(2):
                fw.dma("pool", wd[:, hf * 2:(hf + 1) * 2, :], I["w_exp_down"][l, ex, hf * 256:(hf + 1) * 256, :].rearrange("(k p) d -> p k d", p=128), W=[wd])
            wts[ex] = (wg, wu, wd)

        def mfront(i):
            ex, c0, n = mitems[i]
            if ex not in wts:
                load_w(ex)
            wg, wu, wd = wts[ex]
            cbps = sml()
            fw.op("pe", lambda e: e.matmul(cbps[:, 0:n], lhsT=selb[:, ex, :], rhs=cT[:, c0:c0 + n], start=True, stop=True), R=[selb, cT], W=[cbps])
            cb = cbp()
            fw.op("act", lambda e: e.activation(out=cb[:, 0:n], in_=cbps[:, 0:n], func=AF.Copy), R=[cbps], W=[cb])
            aT = aTp()
            for fc in range(4):
                gp = gup(); up_ = gup()
                for kc in range(8):
                    fw.op("pe", lambda e: e.matmul(gp[:, 0:n], lhsT=wg[:, kc, fc * 128:(fc + 1) * 128], rhs=hT[:, kc, c0:c0 + n], start=(kc == 0), stop=(kc == 7)), R=[wg, hT], W=[gp])
                for kc in range(8):
                    fw.op("pe", lambda e: e.matmul(up_[:, 0:n], lhsT=wu[:, kc, fc * 128:(fc + 1) * 128], rhs=hT[:, kc, c0:c0 + n], start=(kc == 0), stop=(kc == 7)), R=[wu, hT], W=[up_])
                sg = sgp()
                fw.op("act", lambda e: e.activation(out=sg[:, 0:n], in_=gp[:, 0:n], func=AF.Silu), R=[gp], W=[sg])
                a1 = a1p()
                fw.op("dve", lambda e: e.tensor_tensor(out=a1[:, 0:n], in0=up_[:, 0:n], in1=sg[:, 0:n], op=ALU.mult), R=[up_, sg], W=[a1])
                fw.op("dve", lambda e: e.tensor_tensor(out=aT[:, fc, 0:n], in0=a1[:, 0:n], in1=cb[:, 0:n], op=ALU.mult), R=[a1, cb], W=[aT])
            mst[i] = aT

        def mback(i):
            ex, c0, n = mitems[i]
            wg, wu, wd = wts[ex]
            aT = mst.pop(i)
            for tl in range(n // 128):
                j = c0 // 128 + tl
                for hf in range(2):
                    y = yps()
                    for fc in range(4):
                        fw.op("pe", lambda e: e.matmul(y[:], lhsT=aT[:, fc, tl * 128:(tl + 1) * 128], rhs=wd[:, fc, hf * 512:(hf + 1) * 512], start=(fc == 0), stop=(fc == 3)), R=[aT, wd], W=[y])
                    if ex == 0:
                        fw.op("act", lambda e: e.activation(out=Y[:, j, hf * 512:(hf + 1) * 512], in_=y[:], func=AF.Copy), R=[y], W=[Y])
                    else:
                        fw.op("dve", lambda e: e.tensor_tensor(out=Y[:, j, hf * 512:(hf + 1) * 512], in0=y[:], in1=Y[:, j, hf * 512:(hf + 1) * 512], op=ALU.add), R=[y, Y], W=[Y])

        nM = len(mitems)
        nch = len(gchunks)
        load_w(0)
        for i in range(nM + 1):
            if i < nM:
                mfront(i)
                ex_i = mitems[i][0]
                if (i % nch) == min(1, nch - 1) and ex_i + 1 < 16 and i >= 1:
                    mback(i - 1)
                    load_w(ex_i + 1)
                    continue
            if i >= 1:
                mback(i - 1)
        for i in range(g0, g1):
            j = i - g0
            xt = xtp()
            fw.dma("sp", xt[:], S["XS"][i * 128:(i + 1) * 128, :], W=[xt])
            if last and i >= 2:
                dst = self.out[(i - 2) * 128:(i - 1) * 128, :]
            else:
                dst = S["XS"][i * 128:(i + 1) * 128, :]
            ys = [(Y, Y[:, j, 0:512]), (Y, Y[:, j, 512:1024])]
            _ln_epilogue(self, ys, xt, m5[1 if i < 2 else 0], gbc, bbc, pools, dst)
    self.end("moe%d" % l)


KB.ph_moe = _ph_moe
```
